# Optimizing a Trainium2 kernel written in Bass

```python
import math
import jax, jax.numpy as jnp
from jax import lax
import numpy as np

D_MODEL = 1024
BATCH = 8
SEQ = 4096
DEPTH = 1

HEAD_DIM = 64
N_SB_HEADS = 8
N_FOX_HEADS = 8
SB_WIDTH = N_SB_HEADS * HEAD_DIM
FOX_WIDTH = N_FOX_HEADS * HEAD_DIM
Q_BLOCK = 128
N_GROUPS = 4
EXPERTS_PER_GROUP = 8
N_EXPERTS = N_GROUPS * EXPERTS_PER_GROUP
TOP_K_IN_GROUP = 2
D_EXPERT = D_MODEL // 4
RMS_EPS = 1e-6

_COLS = [SB_WIDTH, SB_WIDTH, SB_WIDTH,
         FOX_WIDTH, FOX_WIDTH, FOX_WIDTH,
         N_FOX_HEADS,
         D_MODEL, D_MODEL]
IN_COLS = sum(_COLS)
SPLIT_POINTS = [int(v) for v in np.cumsum(_COLS)[:-1]]

kernel_name = "hybrid_stickbreak_fox_hiermoe"


def rmsnorm(x, g):
    xf = x.astype(jnp.float32)
    y = xf * lax.rsqrt(jnp.mean(xf * xf, axis=-1, keepdims=True) + RMS_EPS)
    return (y * g.astype(jnp.float32)).astype(x.dtype)


def split_heads(t, n_heads):
    b, s, _ = t.shape
    return t.reshape(b, s, n_heads, HEAD_DIM).transpose(0, 2, 1, 3)


def merge_heads(t):
    b, h, s, d = t.shape
    return t.transpose(0, 2, 1, 3).reshape(b, s, h * d)


def to_blocks(t):
    b, h, s = t.shape[:3]
    rest = t.shape[3:]
    nb = s // Q_BLOCK
    t = t.reshape((b, h, nb, Q_BLOCK) + rest)
    return jnp.moveaxis(t, 2, 0)


def from_blocks(t):
    nb, b, h, qb, d = t.shape
    return jnp.moveaxis(t, 0, 2).reshape(b, h, nb * qb, d)


def stick_breaking_attention(q, k, v):
    s_len = q.shape[2]
    nb = s_len // Q_BLOCK
    scale = 1.0 / math.sqrt(HEAD_DIM)
    kf = k.astype(jnp.float32)
    vf = v.astype(jnp.float32)
    kpos = jnp.arange(s_len)

    def one_block(args):
        qb, bi = args
        qpos = bi * Q_BLOCK + jnp.arange(Q_BLOCK)
        z = jnp.einsum('bhqd,bhkd->bhqk', qb.astype(jnp.float32), kf) * scale
        mask = kpos[None, :] < qpos[:, None]
        log_keep = jnp.where(mask, jax.nn.log_sigmoid(-z), 0.0)
        rest = lax.cumsum(log_keep, axis=3, reverse=True) - log_keep
        a = jnp.where(mask, jnp.exp(jax.nn.log_sigmoid(z) + rest), 0.0)
        return jnp.einsum('bhqk,bhkd->bhqd', a, vf)

    out = lax.map(one_block, (to_blocks(q), jnp.arange(nb)))
    return from_blocks(out).astype(q.dtype)


def forgetting_attention(q, k, v, log_f):
    s_len = q.shape[2]
    nb = s_len // Q_BLOCK
    scale = 1.0 / math.sqrt(HEAD_DIM)
    kf = k.astype(jnp.float32)
    vf = v.astype(jnp.float32)
    c = jnp.cumsum(log_f, axis=-1)
    kpos = jnp.arange(s_len)

    def one_block(args):
        qb, cq, bi = args
        qpos = bi * Q_BLOCK + jnp.arange(Q_BLOCK)
        z = jnp.einsum('bhqd,bhkd->bhqk', qb.astype(jnp.float32), kf) * scale
        z = z + cq[..., :, None] - c[..., None, :]
        mask = kpos[None, :] <= qpos[:, None]
        z = jnp.where(mask, z, -jnp.inf)
        p = jax.nn.softmax(z, axis=-1)
        return jnp.einsum('bhqk,bhkd->bhqd', p, vf)

    out = lax.map(one_block, (to_blocks(q), to_blocks(c), jnp.arange(nb)))
    return from_blocks(out).astype(q.dtype)


def hierarchical_moe(h, w_rg, b_rg, w_re, b_re, w1, w3, w2):
    b, s, d = h.shape
    hf = h.reshape(b * s, d)
    g_logits = (hf @ w_rg).astype(jnp.float32) + b_rg.astype(jnp.float32)
    g_prob = jax.nn.softmax(g_logits, axis=-1)
    g_idx = jnp.argmax(g_prob, axis=-1)
    g_w = jnp.take_along_axis(g_prob, g_idx[:, None], axis=1)

    e_logits = (hf @ w_re).astype(jnp.float32) + b_re.astype(jnp.float32)
    e_logits = e_logits.reshape(-1, N_GROUPS, EXPERTS_PER_GROUP)
    e_sel = jnp.take_along_axis(e_logits, g_idx[:, None, None], axis=1)[:, 0]
    e_prob = jax.nn.softmax(e_sel, axis=-1)
    top_v, top_i = lax.top_k(e_prob, TOP_K_IN_GROUP)
    top_v = top_v / jnp.sum(top_v, axis=-1, keepdims=True)
    weights = g_w * top_v
    expert_id = g_idx[:, None] * EXPERTS_PER_GROUP + top_i
    combine = jnp.sum(jax.nn.one_hot(expert_id, N_EXPERTS, dtype=jnp.float32)
                      * weights[..., None], axis=1)

    y = jnp.zeros(hf.shape, jnp.float32)
    for e in range(N_EXPERTS):
        hidden = jax.nn.silu(hf @ w1[e]) * (hf @ w3[e])
        y = y + combine[:, e:e + 1] * (hidden @ w2[e]).astype(jnp.float32)
    return y.reshape(b, s, d).astype(h.dtype)


def setup_inputs(seed: int = 0) -> dict:
    key = jax.random.key(seed)
    ks = jax.random.split(key, 20)
    nrm = jax.random.normal
    L = DEPTH
    x = nrm(ks[0], (BATCH, SEQ, D_MODEL), jnp.float32)
    norm_attn = 1.0 + 0.02 * nrm(ks[1], (L, D_MODEL), jnp.float32)
    w_in = nrm(ks[2], (L, D_MODEL, IN_COLS), jnp.float32) * D_MODEL ** -0.5
    b_forget = 2.0 + 0.1 * nrm(ks[3], (L, N_FOX_HEADS), jnp.float32)
    w_o_sb = nrm(ks[4], (L, SB_WIDTH, D_MODEL), jnp.float32) * SB_WIDTH ** -0.5
    w_o_fox = nrm(ks[5], (L, FOX_WIDTH, D_MODEL), jnp.float32) * FOX_WIDTH ** -0.5
    w_out = nrm(ks[6], (L, D_MODEL, D_MODEL), jnp.float32) * D_MODEL ** -0.5
    norm_ffn = 1.0 + 0.02 * nrm(ks[7], (L, D_MODEL), jnp.float32)
    w_router_group = nrm(ks[8], (L, D_MODEL, N_GROUPS), jnp.float32) * D_MODEL ** -0.5
    b_router_group = 0.01 * nrm(ks[9], (L, N_GROUPS), jnp.float32)
    w_router_expert = nrm(ks[10], (L, D_MODEL, N_EXPERTS), jnp.float32) * D_MODEL ** -0.5
    b_router_expert = 0.01 * nrm(ks[11], (L, N_EXPERTS), jnp.float32)
    w1 = nrm(ks[12], (L, N_EXPERTS, D_MODEL, D_EXPERT), jnp.float32) * D_MODEL ** -0.5
    w3 = nrm(ks[13], (L, N_EXPERTS, D_MODEL, D_EXPERT), jnp.float32) * D_MODEL ** -0.5
    w2 = nrm(ks[14], (L, N_EXPERTS, D_EXPERT, D_MODEL), jnp.float32) * D_EXPERT ** -0.5
    norm_final = 1.0 + 0.02 * nrm(ks[15], (D_MODEL,), jnp.float32)
    return {"x": x, "norm_attn": norm_attn, "w_in": w_in, "b_forget": b_forget,
            "w_o_sb": w_o_sb, "w_o_fox": w_o_fox, "w_out": w_out, "norm_ffn": norm_ffn,
            "w_router_group": w_router_group, "b_router_group": b_router_group,
            "w_router_expert": w_router_expert, "b_router_expert": b_router_expert,
            "w1": w1, "w3": w3, "w2": w2, "norm_final": norm_final}


def reference(x, norm_attn, w_in, b_forget, w_o_sb, w_o_fox, w_out, norm_ffn,
              w_router_group, b_router_group, w_router_expert, b_router_expert,
              w1, w3, w2, norm_final):
    for l in range(DEPTH):
        h = rmsnorm(x, norm_attn[l])
        proj = h @ w_in[l]
        q_sb, k_sb, v_sb, q_fx, k_fx, v_fx, f_logit, g_sb, g_fx = jnp.split(
            proj, SPLIT_POINTS, axis=-1)

        o_sb = stick_breaking_attention(split_heads(q_sb, N_SB_HEADS),
                                        split_heads(k_sb, N_SB_HEADS),
                                        split_heads(v_sb, N_SB_HEADS))
        log_f = jax.nn.log_sigmoid((f_logit + b_forget[l]).astype(jnp.float32))
        log_f = log_f.transpose(0, 2, 1)
        o_fx = forgetting_attention(split_heads(q_fx, N_FOX_HEADS),
                                    split_heads(k_fx, N_FOX_HEADS),
                                    split_heads(v_fx, N_FOX_HEADS), log_f)

        y_sb = merge_heads(o_sb) @ w_o_sb[l]
        y_fx = merge_heads(o_fx) @ w_o_fox[l]
        mixed = jax.nn.sigmoid(g_sb) * y_sb + jax.nn.sigmoid(g_fx) * y_fx
        x = x + mixed @ w_out[l]

        h = rmsnorm(x, norm_ffn[l])
        x = x + hierarchical_moe(h, w_router_group[l], b_router_group[l],
                                 w_router_expert[l], b_router_expert[l],
                                 w1[l], w3[l], w2[l])
    return rmsnorm(x, norm_final)
```

```python
from contextlib import ExitStack
import numpy as np
import concourse.bass as bass
import concourse.mybir as mybir
from concourse.bass_utils import run_bass_kernel_spmd

F32 = mybir.dt.float32
BF16 = mybir.dt.bfloat16
I32 = mybir.dt.int32
AF = mybir.ActivationFunctionType
ALU = mybir.AluOpType

S = 4096
D = 1024
NT = S // 128
NQ = S // 512
HD = 64
NH = 8
INC = 5128
C_QSB, C_KSB, C_VSB, C_QFX, C_KFX, C_VFX, C_F, C_GSB, C_GFX = 0, 512, 1024, 1536, 2048, 2560, 3072, 3080, 4104
NE = 32
DE = 256
EPS = 1e-6
NEGBIG = -30000.0
FOX_DUMMY = 1
FILL_SB = 0
FILL_FX = 0

ENGS = ("pe", "act", "dve", "pool", "sp")
GEN = 30000


class Buf:
    __slots__ = ("name", "writer", "readers", "excl")

    def __init__(self, name="", excl=False):
        self.name = name
        self.writer = None
        self.readers = []
        self.excl = excl


class Op:
    __slots__ = ("eng", "fn", "idx", "deps", "signal", "sig", "dma", "dkey", "dval")

    def __init__(self, eng, fn, idx, dma):
        self.eng = eng
        self.fn = fn
        self.idx = idx
        self.deps = []
        self.signal = False
        self.sig = None
        self.dma = dma
        self.dkey = None
        self.dval = None


class Prog:
    def __init__(self, nc):
        self.nc = nc
        self.ops = {e: [] for e in ENGS}
        self.known = {e: {f: -1 for f in ENGS} for e in ENGS}
        self.known_dma = {e: {} for e in ENGS}
        self.dma_cnt = {}
        self.last_dma = {}
        self.pending = {e: [] for e in ENGS}

    def barrier(self):
        lasts = []
        for e in ENGS:
            for o in reversed(self.ops[e]):
                if not o.dma and o.fn is not None:
                    lasts.append(o)
                    break
        lasts += list(self.last_dma.values())
        for e in ENGS:
            self.pending[e] = list(lasts)

    def op(self, eng, fn, reads=(), writes=(), dma_key=None, ninc=1):
        o = Op(eng, fn, len(self.ops[eng]), dma_key is not None)
        cand = []
        for b in reads:
            if b.writer is not None:
                cand.append((b.writer, "raw"))
            if b.excl:
                for r in b.readers:
                    if r.eng != eng:
                        cand.append((r, "rar"))
        for b in writes:
            if b.writer is not None:
                cand.append((b.writer, "waw"))
            for r in b.readers:
                cand.append((r, "war"))
        best = {}
        dma_deps = {}
        if self.pending[eng]:
            for p in self.pending[eng]:
                if p.dma or p.eng != eng:
                    cand.append((p, "raw"))
            self.pending[eng] = []
        for (p, kind) in cand:
            if p is o:
                continue
            if p.dma:
                if self.known_dma[eng].get(p.dkey, 0) >= p.dval:
                    continue
                if p.dkey not in dma_deps or dma_deps[p.dkey].dval < p.dval:
                    dma_deps[p.dkey] = p
                continue
            if p.eng == eng:
                if eng == "pe":
                    continue
            if self.known[eng][p.eng] >= p.idx:
                continue
            if p.eng not in best or best[p.eng].idx < p.idx:
                best[p.eng] = p
        for k, p in dma_deps.items():
            o.deps.append(p)
            self.known_dma[eng][k] = p.dval
        for f, p in best.items():
            o.deps.append(p)
            p.signal = True
            self.known[eng][f] = p.idx
        if o.dma:
            o.dkey = dma_key
            self.dma_cnt[dma_key] = self.dma_cnt.get(dma_key, 0) + 16 * ninc
            o.dval = self.dma_cnt[dma_key]
            self.last_dma[dma_key] = o
        for b in reads:
            b.readers.append(o)
        for b in writes:
            b.writer = o
            b.readers = []
        self.ops[eng].append(o)
        return o

    def emit(self):
        nc = self.nc
        with ExitStack() as st:
            sems = {}
            for e in ENGS:
                n = 0
                for o in self.ops[e]:
                    if o.signal and not o.dma:
                        o.sig = n
                        n += 1
                ngen = max(1, (n + GEN - 1) // GEN)
                sems[e] = [st.enter_context(nc.semaphore(f"s_{e}_{g}")) for g in range(ngen)]
            dsem = {}
            for k in self.dma_cnt:
                dsem[k] = st.enter_context(nc.semaphore(f"d_{len(dsem)}"))
            block = st.enter_context(nc.Block())
            handles = {"pe": block.tensor, "act": block.scalar, "dve": block.vector,
                       "pool": block.gpsimd, "sp": block.sync}

            def run(e):
                ops = self.ops[e]
                if not ops:
                    return

                def body(eng):
                    for o in ops:
                        for p in o.deps:
                            if p.dma:
                                eng.wait_ge(dsem[p.dkey], p.dval)
                            else:
                                eng.wait_ge(sems[p.eng][p.sig // GEN], p.sig % GEN + 1)
                        if o.fn is None:
                            continue
                        ins = o.fn(eng)
                        if o.dma:
                            if not isinstance(ins, (list, tuple)):
                                ins = [ins]
                            for i_ in ins:
                                i_.then_inc(dsem[o.dkey], 16)
                        elif o.signal:
                            ins.then_inc(sems[e][o.sig // GEN], 1)
                handles[e](body)
            for e in ENGS:
                run(e)


def build_program(debug=False, upto=None):
    nc = bass.Bass("TRN2", target_bir_lowering=False)
    dram_in = lambda n, s: nc.dram_tensor(n, list(s), F32, kind="ExternalInput").ap()
    x_d = dram_in("x", (S, D))
    norm_attn_d = dram_in("norm_attn", (1, D))
    w_in_d = dram_in("w_in", (D, INC))
    b_forget_d = dram_in("b_forget", (1, NH))
    w_o_sb_d = dram_in("w_o_sb", (512, D))
    w_o_fox_d = dram_in("w_o_fox", (512, D))
    w_out_d = dram_in("w_out", (D, D))
    norm_ffn_d = dram_in("norm_ffn", (1, D))
    w_rg_d = dram_in("w_router_group", (D, 4))
    b_rg_d = dram_in("b_router_group", (1, 4))
    w_re_d = dram_in("w_router_expert", (D, NE))
    b_re_d = dram_in("b_router_expert", (1, NE))
    w1_d = dram_in("w1", (NE, D, DE))
    w3_d = dram_in("w3", (NE, D, DE))
    w2_d = dram_in("w2", (NE, DE, D))
    norm_final_d = dram_in("norm_final", (1, D))
    out_d = nc.dram_tensor("out", [S, D], F32, kind="ExternalOutput").ap()
    oT_d = nc.dram_tensor("oT_scr", [16, HD, S], BF16, kind="Internal").ap()
    x2_d = nc.dram_tensor("x2_scr", [S, D], F32, kind="Internal").ap()
    NSLOT = 64
    SLOTR = 256
    NSUB = SLOTR // 128
    SHIFT = 8
    WROW = 2 * 8 * DE + 2 * D
    Wb_d = nc.dram_tensor("Wb_scr", [NE * 128, WROW], BF16, kind="Internal").ap()
    Xs_d = nc.dram_tensor("Xs_scr", [NSLOT * SLOTR, D], BF16, kind="Internal").ap()
    Ys_d = nc.dram_tensor("Ys_scr", [NSLOT * SLOTR, D], F32, kind="Internal").ap()
    cparts_d = nc.dram_tensor("cparts_scr", [NH, 3, S], BF16, kind="Internal").ap()
    ncparts_d = nc.dram_tensor("ncparts_scr", [NH, 3, S], BF16, kind="Internal").ap()
    dbg_d = None
    if debug:
        dbg_d = nc.dram_tensor("dbg", [S, D], F32, kind="ExternalOutput").ap()

    P = Prog(nc)
    with ExitStack() as st:
        stk = [st]

        def sb(name, shape, dt):
            return stk[-1].enter_context(nc.sbuf_tensor(name, list(shape), dt))

        banks = [st.enter_context(nc.psum_tensor(f"bank{i}", [128, 512], F32)) for i in range(8)]
        bankB = [Buf(f"bank{i}", excl=True) for i in range(8)]

        def MM(out, lhsT, rhs, start, stop, reads, writes, **kw):
            return P.op("pe", lambda e: e.matmul(out, lhsT=lhsT, rhs=rhs, start=start, stop=stop, **kw), reads, writes)

        def TR(out, in_, ident, reads, writes):
            return P.op("pe", lambda e: e.transpose(out=out, in_=in_, identity=ident), reads, writes)

        def ACT(out, in_, func, reads, writes, **kw):
            return P.op("act", lambda e: e.activation(out=out, in_=in_, func=func, **kw), reads, writes)

        def TT(eng, out, in0, in1, op, reads, writes):
            return P.op(eng, lambda e: e.tensor_tensor(out=out, in0=in0, in1=in1, op=op), reads, writes)

        def TS(eng, out, in0, s1, s2, op0, op1, reads, writes, **kw):
            if op1 is None:
                return P.op(eng, lambda e: e.tensor_scalar(out=out, in0=in0, scalar1=s1, scalar2=None, op0=op0, **kw), reads, writes)
            return P.op(eng, lambda e: e.tensor_scalar(out=out, in0=in0, scalar1=s1, scalar2=s2, op0=op0, op1=op1, **kw), reads, writes)

        def STT(out, in0, scalar, in1, op0, op1, reads, writes):
            return P.op("dve", lambda e: e.scalar_tensor_tensor(out=out, in0=in0, scalar=scalar, in1=in1, op0=op0, op1=op1), reads, writes)

        def CP(eng, out, in_, reads, writes):
            if eng == "act":
                return P.op("act", lambda e: e.copy(out=out, in_=in_), reads, writes)
            return P.op(eng, lambda e: e.tensor_copy(out=out, in_=in_), reads, writes)

        def MEMSET(eng, ap, val, writes):
            return P.op(eng, lambda e: e.memset(ap, val), (), writes)

        def DMA(q, out, in_, reads, writes, key, **kw):
            return P.op(q, lambda e: e.dma_start(out=out, in_=in_, **kw), reads, writes, dma_key=key)

        ones_bf = sb("ones_bf", (128, 128), BF16)
        negones_bf = sb("negones_bf", (128, 128), BF16)
        negbig_bf = sb("negbig_bf", (128, 128), BF16)
        ident_bf = sb("ident_bf", (128, 128), BF16)
        negbigI = sb("negbigI", (128, 128), BF16)
        negtri = sb("negtri", (128, 128), BF16)
        ones513 = sb("ones513", (128, 513), BF16)
        M0 = sb("M0", (128, 513), BF16)
        ones_f32 = sb("ones_f32", (128, 128), F32)
        ident_f32 = sb("ident_f32", (128, 128), F32)
        eps_t = sb("eps_t", (128, 1), F32)
        Bc = Buf("consts")
        Bg1 = Buf("g1")

        P.op("pool", lambda e: e.memset(ones_bf[:], 1.0), (), [Bc])
        P.op("pool", lambda e: e.memset(negones_bf[:], -1.0), (), [Bc])
        P.op("pool", lambda e: e.memset(negbig_bf[:], NEGBIG), (), [Bc])
        P.op("pool", lambda e: e.memset(ones513[:], 1.0), (), [Bc])
        P.op("pool", lambda e: e.memset(ones_f32[:], 1.0), (), [Bc])
        P.op("pool", lambda e: e.memset(eps_t[:], EPS), (), [Bc])
        Bc2 = Buf("consts2")
        P.op("pool", lambda e: e.affine_select(out=ident_bf[:], in_=ones_bf[:], pattern=[[-1, 128]], compare_op=ALU.is_equal,
                                               fill=0.0, base=0, channel_multiplier=1), [Bc], [Bc2])
        P.op("pool", lambda e: e.affine_select(out=ident_f32[:], in_=ones_f32[:], pattern=[[-1, 128]], compare_op=ALU.is_equal,
                                               fill=0.0, base=0, channel_multiplier=1), [Bc], [Bc2])
        P.op("pool", lambda e: e.affine_select(out=negbigI[:], in_=negbig_bf[:], pattern=[[-1, 128]], compare_op=ALU.is_equal,
                                               fill=0.0, base=0, channel_multiplier=1), [Bc], [Bc2])
        P.op("pool", lambda e: e.affine_select(out=negtri[:], in_=negones_bf[:], pattern=[[-1, 128]], compare_op=ALU.is_ge,
                                               fill=0.0, base=0, channel_multiplier=1), [Bc], [Bc2])
        P.op("pool", lambda e: e.affine_select(out=M0[:], in_=ones513[:], pattern=[[-1, 513]], compare_op=ALU.is_ge,
                                               fill=0.0, base=0, channel_multiplier=1), [Bc], [Bc2])
        P.op("pool", lambda e: e.affine_select(out=SL_bf[:], in_=ones_bf[:], pattern=[[1, 128]], compare_op=ALU.is_ge,
                                               fill=0.0, base=-1, channel_multiplier=-1), [Bc], [Bc2])

        hT = sb("hT", (128, 8, S), BF16)
        BhT = [Buf(f"hT{t}") for t in range(NQ)]
        g3 = sb("g3", (128, D), F32)
        M1 = sb("M1", (128, NT, NE), BF16)
        M2 = sb("M2", (128, NT, NE), BF16)
        Rk = sb("Rk", (128, NT, NE), F32)
        Wg = sb("Wg", (128, NT, 2), F32)
        IDX = sb("IDX", (128, NT, 2), I32)
        WI = sb("WI", (128, 64), I32)
        SL_bf = sb("SL_bf", (128, 128), BF16)

        def rms_rstd(xs_ap, junk_ap, ss_ap, ln_ap, rstd_ap, rd, wr_junk, wr_stat):
            P.op("act", lambda e: e.activation(out=junk_ap, in_=xs_ap, func=AF.Square, accum_out=ss_ap), rd, [wr_junk, wr_stat])
            ACT(ln_ap, ss_ap, AF.Ln, [wr_stat, Bc], [wr_stat], scale=1.0 / D, bias=eps_t[:, 0:1])
            ACT(rstd_ap, ln_ap, AF.Exp, [wr_stat], [wr_stat], scale=-0.5)

        NXS = 2
        Bxs = [Buf(f"xs{i}") for i in range(NXS)]
        junk = sb("junk", (128, D), BF16)
        Bjunk = Buf("junk")
        stat = [sb(f"stat{i}", (128, 4), F32) for i in range(2)]
        Bstat = [Buf(f"stat{i}") for i in range(2)]
        stk.append(ExitStack())
        g1 = sb("g1", (128, D), F32)
        DMA("sp", g1[:], norm_attn_d[0:1, :].partition_broadcast(128), (), [Bg1], "g1")
        xs = [sb(f"xsa{i}", (128, D), F32) for i in range(NXS)]
        hb = [sb(f"hb{i}", (128, D), BF16) for i in range(2)]
        Bhb = [Buf(f"hb{i}") for i in range(2)]

        def run_pipeline(stage_lists, bg, every=6):
            n = len(stage_lists)
            nst = max(len(x) for x in stage_lists)
            bgi = 0
            for s in range(n + nst - 1):
                for k in range(nst):
                    t = s - k
                    if 0 <= t < n and k < len(stage_lists[t]) and stage_lists[t][k] is not None:
                        stage_lists[t][k]()
                if bg and s % every == 3 and bgi < len(bg):
                    bg[bgi]()
                    bgi += 1
            while bg and bgi < len(bg):
                bg[bgi]()
                bgi += 1

        def phase1():
            def front(tt):
                s3 = tt % NXS
                s2 = tt % 2
                DMA("sp", xs[s3][:], x_d[tt * 128:(tt + 1) * 128, :], (), [Bxs[s3]], f"xs{s3}")
                rms_rstd(xs[s3][:], junk[:], stat[s2][:, 0:1], stat[s2][:, 1:2], stat[s2][:, 2:3],
                         [Bxs[s3]], Bjunk, Bstat[s2])
                STT(hb[s2][:], xs[s3][:], stat[s2][:, 2:3], g1[:], ALU.mult, ALU.mult,
                    [Bxs[s3], Bstat[s2], Bg1], [Bhb[s2]])

            def back(tt):
                s2 = tt % 2
                bk = 6 + (tt % 2)
                pv = banks[bk][:].bitcast(BF16)
                for c in range(8):
                    TR(pv[:, c * 128:(c + 1) * 128], hb[s2][:, c * 128:(c + 1) * 128], ident_bf[:],
                       [Bhb[s2], Bc2], [bankB[bk]])
                CP("act", hT[:, :, tt * 128:(tt + 1) * 128], pv[:, :].rearrange("p (c t) -> p c t", c=8),
                   [bankB[bk]], [BhT[tt // 4]])
            steps = [[lambda tt=tt: front(tt), lambda tt=tt: back(tt)] for tt in range(NT)]
            run_pipeline(steps, None)

        phase1()
        P.barrier()
        stk.pop().close()

        NSL = 3
        stk.append(ExitStack())
        QTa = [sb(f"QTa{i}", (128, S), BF16) for i in range(NSL)]
        KTa = [sb(f"KTa{i}", (128, S), BF16) for i in range(NSL)]
        Vt = [sb(f"Vt{i}", (128, NT, HD + 1), BF16) for i in range(NSL)]
        wqk = [sb(f"wqk{i}", (128, 8, 130), BF16) for i in range(NSL)]
        wv = [sb(f"wv{i}", (128, 8, HD), BF16) for i in range(NSL)]
        BQ = [Buf(f"Q{i}") for i in range(NSL)]
        BK = [Buf(f"K{i}") for i in range(NSL)]
        BQaug = [Buf(f"Qaug{i}") for i in range(NSL)]
        BKaug = [Buf(f"Kaug{i}") for i in range(NSL)]
        BV = [Buf(f"V{i}") for i in range(NSL)]
        BVones = [Buf(f"Vones{i}") for i in range(NSL)]
        Bwqk = [Buf(f"wqk{i}") for i in range(NSL)]
        Bwv = [Buf(f"wv{i}") for i in range(NSL)]
        ostS = sb("ostS", (64, S), BF16)
        ostF = sb("ostF", (64, S), BF16)
        BostS = Buf("ostS")
        BostF = Buf("ostF")
        wstage = [sb(f"wstage{i}", (128, 2048), BF16) for i in range(1)]
        Bwstage = [Buf(f"wstage{i}") for i in range(1)]
        BWb = [Buf(f"Wb{e}") for e in range(NE)]

        def conv_expert(ex):
            def f():
                srcs = [w1_d[ex].rearrange("(c p) n -> p c n", p=128), w3_d[ex].rearrange("(c p) n -> p c n", p=128),
                        w2_d[ex].rearrange("(k p) n -> p k n", p=128)]
                pats = ["p (c n) -> p c n", "p (c n) -> p c n", "p (k n) -> p k n"]
                for part in range(3):
                    kw = {"c": 8} if part < 2 else {"k": 2}
                    P.op("pool", lambda e, part=part, kw=kw: e.dma_start(
                        out=wstage[0][:, :].rearrange(pats[part], **kw), in_=srcs[part]),
                        (), [Bwstage[0]], dma_key="wstg0")
                    DMA("sp", Wb_d[ex * 128:(ex + 1) * 128, part * 2048:(part + 1) * 2048], wstage[0][:, :],
                        [Bwstage[0]], [BWb[ex]], "wbst0")
            return f
        BoT_d = [Buf(f"oTd{h}") for h in range(16)]

        for i in range(NSL):
            P.op("pool", lambda e, i=i: e.memset(Vt[i][:, :, HD:HD + 1], 1.0), (), [BVones[i]])
            P.op("pool", lambda e, i=i: e.memset(wqk[i][:, :, 128:130], 0.0), (), [BVones[i]])

        e_sb = [sb(f"e_sb{i}", (128, 512), F32) for i in range(2)]
        sp_bf = [sb(f"sp_bf{i}", (128, 512), BF16) for i in range(2)]
        arg_sb = [sb(f"arg_sb{i}", (128, 512), F32) for i in range(2)]
        A_bf = [sb(f"A_bf{i}", (128, 512), BF16) for i in range(3)]
        P_bf = [sb(f"P_bf{i}", (128, 512), BF16) for i in range(3)]
        BPb = [Buf() for _ in range(3)]
        oraw = [sb(f"oraw{i}", (128, 512), F32) for i in range(2)]
        Boraw = [Buf() for _ in range(2)]
        C_sb = [sb(f"C_sb{i}", (128, 512), F32) for i in range(2)]
        Be = [Buf() for _ in range(2)]
        Bsp = [Buf() for _ in range(2)]
        Barg = [Buf() for _ in range(2)]
        BA = [Buf() for _ in range(3)]
        BC = [Buf() for _ in range(2)]

        wf = sb("wf", (128, 8, NH), BF16)
        Bwf = Buf("wf")
        fexp = [sb(f"fexp{i}", (NH, 512), F32) for i in range(1)] * 2
        cpt = [sb(f"cpt{i}", (NH, 512), F32) for i in range(2)]
        r1 = fexp
        cpp = [sb(f"cpp{i}", (NH, 3, 512), BF16) for i in range(1)] * 2
        negb = sb("negb", (NH, 1), F32)
        Bnegb = Buf("negb")
        Bfexp = [Buf()] * 2
        Bcpt = [Buf() for _ in range(2)]
        Br1 = Bfexp
        Bcpp = [Buf()] * 2
        Bncpp = [Buf()] * 2
        Bcparts = [Buf(f"cparts_d{t}") for t in range(NQ)]

        def load_head_weights(hd, sl):
            typ, h = divmod(hd, NH)
            cq = (C_QSB if typ == 0 else C_QFX) + h * HD
            ck = (C_KSB if typ == 0 else C_KFX) + h * HD
            cv = (C_VSB if typ == 0 else C_VFX) + h * HD
            P.op("pool", lambda e: [
                e.dma_start(out=wqk[sl][:, :, 0:HD], in_=w_in_d[:, cq:cq + HD].rearrange("(c p) n -> p c n", p=128)),
                e.dma_start(out=wqk[sl][:, :, HD:2 * HD], in_=w_in_d[:, ck:ck + HD].rearrange("(c p) n -> p c n", p=128)),
            ], (), [Bwqk[sl]], dma_key=f"wqk{sl}", ninc=2)
            P.op("pool", lambda e: e.dma_start(out=wv[sl][:, :, :], in_=w_in_d[:, cv:cv + HD].rearrange("(c p) n -> p c n", p=128)),
                 (), [Bwv[sl]], dma_key=f"wv{sl}")

        pj_rot = [0]

        def proj_chunks(hd, sl):
            typ, h = divmod(hd, NH)
            chunks = []

            def qk_chunk(T, which):
                def f():
                    bk = 7
                    lo = 0 if which == "q" else HD
                    for c in range(8):
                        MM(banks[bk][0:HD + 1, :], wqk[sl][:, c, lo:lo + HD + 1], hT[:, c, T * 512:(T + 1) * 512],
                           c == 0, c == 7, [Bwqk[sl], BhT[T], BVones[sl]], [bankB[bk]])
                    if which == "q":
                        TS("dve", QTa[sl][0:HD, T * 512:(T + 1) * 512], banks[bk][0:HD, :], 0.125, None, ALU.mult, None,
                           [bankB[bk]], [BQ[sl]])
                    else:
                        CP("dve", KTa[sl][0:HD, T * 512:(T + 1) * 512], banks[bk][0:HD, :], [bankB[bk]], [BK[sl]])
                return f

            def v_chunk(g):
                def f():
                    bk = 7
                    for u in range(8):
                        tt = g * 8 + u
                        for c in range(8):
                            MM(banks[bk][:, u * HD:(u + 1) * HD], hT[:, c, tt * 128:(tt + 1) * 128], wv[sl][:, c, :],
                               c == 0, c == 7, [Bwv[sl], BhT[tt // 4]], [bankB[bk]])
                    CP("dve", Vt[sl][:, g * 8:(g + 1) * 8, 0:HD], banks[bk][:, :].rearrange("p (u d) -> p u d", u=8),
                       [bankB[bk]], [BV[sl]])
                return f

            for T in range(NQ):
                chunks.append(qk_chunk(T, "k"))
            for g in range(4):
                chunks.append(v_chunk(g))
            for T in range(NQ):
                chunks.append(qk_chunk(T, "q"))
            if typ == 0:
                def zpad():
                    P.op("pool", lambda e: e.memset(QTa[sl][64:128, :], 0.0), (), [BQaug[sl]])
                    P.op("pool", lambda e: e.memset(KTa[sl][64:128, :], 0.0), (), [BKaug[sl]])
                chunks.append(zpad)
            if typ == 1:
                def aug():
                    P.op("pool", lambda e: e.memset(QTa[sl][64:70, :], 1.0), (), [BQaug[sl]])
                    P.op("pool", lambda e: e.memset(KTa[sl][64:70, :], -1.0), (), [BKaug[sl]])
                    DMA("sp", QTa[sl][64:67, :], cparts_d[h, :, :], Bcparts, [BQaug[sl]], f"qaug{sl}")
                    DMA("sp", KTa[sl][67:70, :], cparts_d[h, :, :], Bcparts, [BKaug[sl]], f"kaug{sl}")
                chunks.append(aug)
            return chunks

        def fox_prep():
            P.op("pool", lambda e: e.dma_start(out=wf[:, :, :], in_=w_in_d[:, C_F:C_F + NH].rearrange("(c p) n -> p c n", p=128)),
                 (), [Bwf], dma_key="wf")
            DMA("sp", negb[:, 0:1], b_forget_d[0:1, :].rearrange("o h -> h o"), (), [Bnegb], "negb")
            TS("dve", negb[:, 0:1], negb[:, 0:1], -1.0, None, ALU.mult, None, [Bnegb], [Bnegb])
            for T in range(NQ):
                bk = 7
                s2 = T % 2
                for c in range(8):
                    MM(banks[bk][0:NH, :], wf[:, c, :], hT[:, c, T * 512:(T + 1) * 512], c == 0, c == 7,
                       [Bwf, BhT[T]], [bankB[bk]])
                ACT(fexp[s2][:, :], banks[bk][0:NH, :], AF.Exp, [bankB[bk], Bnegb], [Bfexp[s2]],
                    scale=-1.0, bias=negb[:, 0:1])
                ACT(fexp[s2][:, :], fexp[s2][:, :], AF.Ln, [Bfexp[s2]], [Bfexp[s2]], bias=1.0)
                init = 0.0 if T == 0 else cpt[1 - s2][:, 511:512]
                rds = [Bfexp[s2]] + ([Bcpt[1 - s2]] if T > 0 else [])
                P.op("dve", lambda e, s2=s2, init=init: e.tensor_tensor_scan(
                    out=cpt[s2][:, :], data0=fexp[s2][:, :], data1=fexp[s2][:, :], initial=init,
                    op0=ALU.add, op1=ALU.max), rds, [Bcpt[s2]])
                CP("dve", cpp[s2][:, 0, :], cpt[s2][:, :], [Bcpt[s2]], [Bcpp[s2]])
                TT("dve", r1[s2][:, :], cpt[s2][:, :], cpp[s2][:, 0, :], ALU.subtract, [Bcpt[s2], Bcpp[s2]], [Br1[s2]])
                CP("dve", cpp[s2][:, 1, :], r1[s2][:, :], [Br1[s2]], [Bcpp[s2]])
                TT("dve", r1[s2][:, :], r1[s2][:, :], cpp[s2][:, 1, :], ALU.subtract, [Br1[s2], Bcpp[s2]], [Br1[s2]])
                CP("dve", cpp[s2][:, 2, :], r1[s2][:, :], [Br1[s2]], [Bcpp[s2]])
                P.op("sp", lambda e, s2=s2, T=T: [
                    e.dma_start(out=cparts_d[:, :, T * 512:(T + 1) * 512], in_=cpp[s2][:, :, :]),
                ], [Bcpp[s2]], [Bcparts[T]], dma_key="cpst0", ninc=1)

        ZB = [0, 1]
        AB = [2, 3]
        CSB = 4
        OB = 5

        cnt = {"z": 0, "a": 0, "A": 0, "c": 0, "e": 0}

        def sb_steps(hd, sl):
            steps = []
            ZA = [0, 1]
            CSB_ = 2
            ob = 3
            for j in range(NQ):
                cslot = cnt["c"] % 2
                cnt["c"] += 1
                order = list(range(4 * j + 3, -1, -1))
                for n_, i in enumerate(order):
                    m = i - 4 * j
                    off = max(0, m) * 128
                    diag = m >= 0
                    first = n_ == 0
                    last = i == 0
                    zs = cnt["z"] % 2
                    cnt["z"] += 1
                    As = cnt["A"] % 3
                    cnt["A"] += 1
                    kT = KTa[sl][0:128, i * 128:(i + 1) * 128]
                    qT = QTa[sl][0:128, j * 512 + off:(j + 1) * 512]
                    msk = M0[:, 0:512 - off]
                    W = slice(off, 512)

                    def st0(zs=zs, kT=kT, qT=qT, msk=msk, W=W, diag=diag, first=first, cslot=cslot, off=off, n_=n_):
                        if n_ == 2:
                            MEMSET("pool", C_sb[1 - cslot][:], 0.0, [BC[1 - cslot]])
                        zb = ZA[zs]
                        MM(banks[zb][:, W], kT, qT, True, not diag, [BK[sl], BQ[sl], BKaug[sl], BQaug[sl]], [bankB[zb]])
                        if diag:
                            MM(banks[zb][:, off:off + 128], negbigI[:], M0[:, 0:128], False, True, [Bc2], [bankB[zb]])
                        ACT(e_sb[zs][:, W], banks[zb][:, W], AF.Exp, [bankB[zb]], [Be[zs]])
                        ACT(sp_bf[zs][:, W], e_sb[zs][:, W], AF.Ln, [Be[zs]], [Bsp[zs]], bias=1.0)

                    def st1(zs=zs, As=As, W=W, first=first, last=last, cslot=cslot):
                        ab = ZA[zs]
                        MM(banks[ab][:, W], negtri[:], sp_bf[zs][:, W], False, True, [Bsp[zs], Bc2], [bankB[ab]],
                           skip_group_check=True)
                        if not last:
                            MM(banks[CSB_][:, W], negones_bf[:], sp_bf[zs][:, W], True, True, [Bsp[zs], Bc], [bankB[CSB_]])
                        if first:
                            ACT(A_bf[As][:, W], banks[ab][:, W], AF.Exp, [bankB[ab]], [BA[As]])
                        else:
                            TT("dve", arg_sb[zs][:, W], banks[ab][:, W], C_sb[cslot][:, W], ALU.add,
                               [bankB[ab], BC[cslot]], [Barg[zs]])
                            ACT(A_bf[As][:, W], arg_sb[zs][:, W], AF.Exp, [Barg[zs]], [BA[As]])
                        if not last:
                            TT("dve", C_sb[cslot][:, W], banks[CSB_][:, W], C_sb[cslot][:, W], ALU.add,
                               [bankB[CSB_], BC[cslot]], [BC[cslot]])
                        for _ in range(FILL_SB):
                            MM(banks[7][:, :], negtri[:], M0[:, 0:512], True, True, [Bc2], [bankB[7]])

                    def st2(As=As, i=i, j=j, W=W, first=first, last=last):
                        MM(banks[ob][0:HD + 1, W], Vt[sl][:, i, 0:HD + 1], A_bf[As][:, W], first, last,
                           [BV[sl], BVones[sl], BA[As]], [bankB[ob]], skip_group_check=True)
                        if last:
                            CP("dve", ostS[0:HD, j * 512:(j + 1) * 512], banks[ob][0:HD, :], [bankB[ob]], [BostS])
                            if j == NQ - 1:
                                DMA("sp", oT_d[hd, :, :], ostS[:, :], [BostS], [BoT_d[hd]], "ostS")
                    steps.append([st0, st1, st2])
            return steps

        def fox_steps(hd, sl):
            steps = []
            SBK = [4, 5]
            ob = 6
            MB = 7
            for j in range(NQ):
                nk = 4 * j + 4
                orot = j % 2
                for i in range(nk):
                    m = i - 4 * j
                    off = max(0, m) * 128
                    diag = m >= 0
                    first = i == 0
                    last = i == nk - 1
                    zs = cnt["fz"] % 2
                    cnt["fz"] += 1
                    As = cnt["fA"] % 3
                    cnt["fA"] += 1
                    kT = KTa[sl][0:70, i * 128:(i + 1) * 128]
                    qT = QTa[sl][0:70, j * 512 + off:(j + 1) * 512]
                    msk = M0[:, 1:1 + 512 - off]
                    W = slice(off, 512)

                    def st0(zs=zs, As=As, kT=kT, qT=qT, msk=msk, W=W, diag=diag, off=off):
                        zb = SBK[zs]
                        MM(banks[zb][:, W], kT, qT, True, not diag, [BK[sl], BQ[sl], BKaug[sl], BQaug[sl]], [bankB[zb]])
                        if diag:
                            MM(banks[zb][:, off:off + 128], negbigI[:], M0[:, 1:129], False, True, [Bc2], [bankB[zb]])

                    def stE(zs=zs, As=As, W=W):
                        zb = SBK[zs]
                        ACT(P_bf[As][:, W], banks[zb][:, W], AF.Exp, [bankB[zb]], [BPb[As]])

                    def st1(As=As, i=i, j=j, W=W, first=first, last=last, orot=orot):
                        MM(banks[ob][0:HD + 1, W], Vt[sl][:, i, 0:HD + 1], P_bf[As][:, W], first, last,
                           [BV[sl], BVones[sl], BPb[As]], [bankB[ob]])
                        if last:
                            CP("dve", oraw[orot][0:HD + 1, :], banks[ob][0:HD + 1, :], [bankB[ob]], [Boraw[orot]])

                    def stN(j=j, orot=orot, last=last):
                        if not last:
                            return
                        ACT(oraw[orot][64:65, :], oraw[orot][64:65, :], AF.Ln, [Boraw[orot]], [Boraw[orot]])
                        ACT(oraw[orot][64:65, :], oraw[orot][64:65, :], AF.Exp, [Boraw[orot]], [Boraw[orot]], scale=-1.0)
                        MM(banks[MB][0:HD, :], ones_f32[64:65, 0:HD], oraw[orot][64:65, :], True, True,
                           [Boraw[orot], Bc], [bankB[MB]])
                        TT("dve", ostF[0:HD, j * 512:(j + 1) * 512], banks[MB][0:HD, :], oraw[orot][0:HD, :], ALU.mult,
                           [bankB[MB], Boraw[orot]], [BostF])
                        if j == NQ - 1:
                            DMA("sp", oT_d[hd, :, :], ostF[:, :], [BostF], [BoT_d[hd]], "ostF")
                    steps.append([st0, stE, st1, stN])
            return steps

        cnt["fz"] = 0
        cnt["fA"] = 0
        HS = 144
        LAG = HS // 2
        seq = []
        for h in range(NH):
            seq += [h, NH + h]
        sched = {}

        def add_bg(it, f):
            sched.setdefault(max(it, 0), []).append(f)

        for n, hd in enumerate(seq):
            sl = n % 3
            chunks = [lambda hd=hd, sl=sl: load_head_weights(hd, sl)]
            if n == 1:
                chunks.append(fox_prep)
            chunks += proj_chunks(hd, sl)
            chunks = chunks[:5] + [conv_expert(2 * n)] + chunks[5:13] + [conv_expert(2 * n + 1)] + chunks[13:]
            h = n // 2
            start = HS * h if n % 2 == 0 else HS * h + LAG
            base = start - LAG + 5
            nch = len(chunks)
            for ci, f in enumerate(chunks):
                add_bg(base + (ci * (LAG - 8)) // nch, f)
        sb_lists = {}
        fx_lists = {}

        def sb_step(sidx):
            if sidx < 0 or sidx >= HS * NH:
                return None
            h, r = divmod(sidx, HS)
            if h not in sb_lists:
                sb_lists[h] = sb_steps(h, (2 * h) % 3)
                sb_lists.pop(h - 2, None)
            return sb_lists[h][r]

        def fx_step(fidx):
            if fidx < 0 or fidx >= HS * NH:
                return None
            h, r = divmod(fidx, HS)
            if h not in fx_lists:
                fx_lists[h] = fox_steps(NH + h, (2 * h + 1) % 3)
                fx_lists.pop(h - 2, None)
            return fx_lists[h][r]

        def run_stage(stp, k):
            if stp is not None and stp[k] is not None:
                stp[k]()

        MEMSET("pool", C_sb[0][:], 0.0, [BC[0]])
        for it in sorted(k for k in sched if k <= 0):
            for f in sched.pop(it):
                f()
        total_p = HS * NH + LAG
        for p_ in range(total_p + 4):
            f_ = p_ - LAG
            run_stage(fx_step(f_ - 1), 1)
            run_stage(sb_step(p_), 0)
            run_stage(fx_step(f_), 0)
            run_stage(sb_step(p_ - 1), 1)
            run_stage(fx_step(f_ - 1), 2)
            run_stage(sb_step(p_ - 2), 2)
            run_stage(fx_step(f_ - 2), 3)
            for f in sched.pop(p_, []):
                f()
        for it in sorted(sched):
            for f in sched[it]:
                f()

        P.barrier()
        stk.pop().close()
        stk.append(ExitStack())
        xs = [sb(f"xsb{i}", (128, D), F32) for i in range(NXS)]
        wosb = sb("wosb", (128, 4, D), BF16)
        wofx = sb("wofx", (128, 4, D), BF16)
        wout = sb("wout", (128, 8, D), BF16)
        wg = sb("wg", (128, 8, 2 * D), BF16)
        Bw3 = Buf("w3")
        P.op("pool", lambda e: [
            e.dma_start(out=wosb[:, :, :], in_=w_o_sb_d.rearrange("(c p) n -> p c n", p=128)),
            e.dma_start(out=wofx[:, :, :], in_=w_o_fox_d.rearrange("(c p) n -> p c n", p=128)),
            e.dma_start(out=wout[:, :, :], in_=w_out_d.rearrange("(c p) n -> p c n", p=128)),
        ] + [e.dma_start(out=wg[:, c, :], in_=w_in_d[c * 128:(c + 1) * 128, C_GSB:C_GSB + 2 * D]) for c in range(8)],
            (), [Bw3], dma_key="w3", ninc=11)
        oTt = [sb(f"oTt{i}", (128, 8, 512), BF16) for i in range(2)]
        BoTt = [Buf() for _ in range(2)]
        sig = [sb(f"sig{i}", (128, 512), BF16) for i in range(4)]
        Bsig = [Buf() for _ in range(4)]
        tmp = [sb(f"tmp{i}", (128, 512), F32) for i in range(2)]
        Btmp = [Buf() for _ in range(2)]
        mixT = [sb(f"mixT{i}", (128, 8, 512), BF16) for i in range(2)]
        BmixT = [Buf() for _ in range(2)]
        x2s = [sb(f"x2s{i}", (128, D), F32) for i in range(2)]
        Bx2s = [Buf() for _ in range(2)]
        Bx2d = [Buf(f"x2d{t}") for t in range(NT)]
        Bdbg = [Buf(f"dbg{t}") for t in range(NT)]

        def phase3():
            def load_oT(T):
                s2 = T % 2
                P.op("sp", lambda e: e.dma_start(
                    out=oTt[s2][:, :, :],
                    in_=oT_d[:, :, T * 512:(T + 1) * 512].rearrange("(c two) d t -> (two d) c t", two=2)),
                    BoT_d, [BoTt[s2]], dma_key=f"oTt{s2}")
            load_oT(0)
            for T in range(NQ):
                s2 = T % 2
                if T + 1 < NQ:
                    load_oT(T + 1)
                for dc in range(8):
                    dsl = slice(dc * 128, (dc + 1) * 128)
                    b0 = 0 if dc % 2 == 0 else 4
                    for c in range(4):
                        MM(banks[b0][:, :], wosb[:, c, dsl], oTt[s2][:, c, :], c == 0, c == 3, [Bw3, BoTt[s2]], [bankB[b0]])
                    for c in range(4):
                        MM(banks[b0 + 1][:, :], wofx[:, c, dsl], oTt[s2][:, 4 + c, :], c == 0, c == 3, [Bw3, BoTt[s2]], [bankB[b0 + 1]])
                    for c in range(8):
                        MM(banks[b0 + 2][:, :], wg[:, c, dsl], hT[:, c, T * 512:(T + 1) * 512], c == 0, c == 7,
                           [Bw3, BhT[T]], [bankB[b0 + 2]])
                    for c in range(8):
                        MM(banks[b0 + 3][:, :], wg[:, c, D + dc * 128:D + (dc + 1) * 128], hT[:, c, T * 512:(T + 1) * 512],
                           c == 0, c == 7, [Bw3, BhT[T]], [bankB[b0 + 3]])
                    sa = (dc % 2) * 2
                    ACT(sig[sa][:, :], banks[b0 + 2][:, :], AF.Sigmoid, [bankB[b0 + 2]], [Bsig[sa]])
                    ACT(sig[sa + 1][:, :], banks[b0 + 3][:, :], AF.Sigmoid, [bankB[b0 + 3]], [Bsig[sa + 1]])
                    t2 = dc % 2
                    TT("dve", tmp[t2][:, :], banks[b0][:, :], sig[sa][:, :], ALU.mult, [bankB[b0], Bsig[sa]], [Btmp[t2]])
                    TT("dve", sig[sa + 1][:, :], banks[b0 + 1][:, :], sig[sa + 1][:, :], ALU.mult,
                       [bankB[b0 + 1], Bsig[sa + 1]], [Bsig[sa + 1]])
                    TT("dve", mixT[s2][:, dc, :], tmp[t2][:, :], sig[sa + 1][:, :], ALU.add,
                       [Btmp[t2], Bsig[sa + 1]], [BmixT[s2]])
                for u in range(4):
                    tt = T * 4 + u
                    s3 = tt % NXS
                    xq = tt % 2
                    DMA("sp", xs[s3][:], x_d[tt * 128:(tt + 1) * 128, :], (), [Bxs[s3]], f"xs{s3}")
                    bb = 0 if tt % 2 == 0 else 4
                    for half in range(2):
                        for c in range(8):
                            MM(banks[bb + half][:, :], mixT[s2][:, c, u * 128:(u + 1) * 128], wout[:, c, half * 512:(half + 1) * 512],
                               c == 0, c == 7, [BmixT[s2], Bw3], [bankB[bb + half]])
                    for half in range(2):
                        TT("dve", x2s[xq][:, half * 512:(half + 1) * 512], banks[bb + half][:, :],
                           xs[s3][:, half * 512:(half + 1) * 512], ALU.add, [bankB[bb + half], Bxs[s3]], [Bx2s[xq]])
                    DMA("act", x2_d[tt * 128:(tt + 1) * 128, :], x2s[xq][:], [Bx2s[xq]], [Bx2d[tt]], f"x2st{xq}")
                    if debug and upto is None:
                        DMA("sp", dbg_d[tt * 128:(tt + 1) * 128, :], x2s[xq][:], [Bx2s[xq]], [Bdbg[tt]], f"dbgst{xq}")

        phase3()

        P.barrier()
        stk.pop().close()
        stk.append(ExitStack())
        BIGR = 1.0e4
        xs = [sb(f"xsc{i}", (128, D), F32) for i in range(NXS)]
        g2 = sb("g2", (128, D), F32)
        Bg2 = Buf("g2")
        DMA("sp", g2[:], norm_ffn_d[0:1, :].partition_broadcast(128), (), [Bg2], "g2")
        Bg3 = Buf("g3")
        DMA("sp", g3[:], norm_final_d[0:1, :].partition_broadcast(128), (), [Bg3], "g3")
        wr = sb("wr", (128, 8, 36), F32)
        rbias = sb("rbias", (128, 36), F32)
        Bwr = Buf("wr")
        P.op("sp", lambda e: [
            e.dma_start(out=wr[:, :, 0:4], in_=w_rg_d.rearrange("(c p) n -> p c n", p=128)),
            e.dma_start(out=wr[:, :, 4:36], in_=w_re_d.rearrange("(c p) n -> p c n", p=128)),
            e.dma_start(out=rbias[:, 0:4], in_=b_rg_d[0:1, :].partition_broadcast(128)),
            e.dma_start(out=rbias[:, 4:36], in_=b_re_d[0:1, :].partition_broadcast(128)),
        ], (), [Bwr], dma_key="wr", ninc=4)
        h2b = hT[:].rearrange("p c t -> p (c t)")
        Bh2b = [Buf(f"h2b{t}") for t in range(NT)]
        BM = [Buf(f"M{t}") for t in range(NT)]
        BRk = [Buf(f"Rk{t}") for t in range(NT)]
        BWg = [Buf(f"Wg{t}") for t in range(NT)]
        h2f = [sb(f"h2f{i}", (128, D), F32) for i in range(2)]
        Bh2f = [Buf() for _ in range(2)]
        h2Tf = [sb(f"h2Tf{i}", (128, 8, 128), F32) for i in range(2)]
        Bh2Tf = [Buf() for _ in range(2)]
        rg = [sb(f"rg{i}", (128, 640), F32) for i in range(2)]
        Brg = [Buf() for _ in range(2)]
        msel4 = [sb(f"msel4_{i}", (128, 4, NE), BF16) for i in range(2)]
        msel = [sb(f"msel{i}", (128, NE), BF16) for i in range(2)]
        Bmsel = [Buf() for _ in range(2)]
        mcum = [sb(f"mcum{i}", (128, NE), BF16) for i in range(2)]
        Bmcum = [Buf() for _ in range(2)]

        def phase3b():
            steps = []
            for tt in range(NT):
                steps.append([lambda tt=tt: p3b_front(tt), lambda tt=tt: p3b_mid(tt),
                              (lambda tt=tt: p3b_chain(tt // 4)) if tt % 4 == 3 else None])
            run_pipeline(steps, None)

        def p3b_front(tt):
            if True:
                s3 = tt % NXS
                s2 = tt % 2
                DMA("sp", xs[s3][:], x2_d[tt * 128:(tt + 1) * 128, :], [Bx2d[tt]], [Bxs[s3]], f"xs{s3}")
                rms_rstd(xs[s3][:], junk[:], stat[s2][:, 0:1], stat[s2][:, 1:2], stat[s2][:, 2:3],
                         [Bxs[s3]], Bjunk, Bstat[s2])
                STT(h2f[s2][:], xs[s3][:], stat[s2][:, 2:3], g2[:], ALU.mult, ALU.mult,
                    [Bxs[s3], Bstat[s2], Bg2], [Bh2f[s2]])
                CP("pool", h2b[:, tt * D:(tt + 1) * D], h2f[s2][:, :], [Bh2f[s2]], [Bh2b[tt]])

        def p3b_mid(tt):
            if True:
                s2 = tt % 2
                bA = 0 if s2 == 0 else 4
                for c in range(8):
                    bk = bA + c // 4
                    TR(banks[bk][:, (c % 4) * 128:(c % 4 + 1) * 128], h2f[s2][:, c * 128:(c + 1) * 128], ident_f32[:],
                       [Bh2f[s2], Bc2], [bankB[bk]])
                for hh in range(2):
                    bk = bA + hh
                    CP("act", h2Tf[s2][:, hh * 4:(hh + 1) * 4, :], banks[bk][:, :].rearrange("p (c t) -> p c t", c=4),
                       [bankB[bk]], [Bh2Tf[s2]])
                bR = bA + 2
                for c in range(8):
                    MM(banks[bR][:, 0:36], h2Tf[s2][:, c, :], wr[:, c, :], c == 0, c == 7, [Bh2Tf[s2], Bwr], [bankB[bR]])
                gp = (tt // 4) % 2
                g_ = tt % 4
                TT("dve", rg[gp][:, g_ * 36:(g_ + 1) * 36], banks[bR][:, 0:36], rbias[:, :], ALU.add,
                   [bankB[bR], Bwr], [Brg[gp]])

        def p3b_chain(grp):
            G = 4
            gp = grp % 2
            r = rg[gp]
            B_ = [Brg[gp]]
            t0_ = grp * G
            X = mybir.AxisListType.X

            def v(lo, n, *dims):
                ap = r[:, lo:lo + n]
                if len(dims) == 2:
                    return ap.rearrange("p (a b) -> p a b", a=dims[0])
                if len(dims) == 3:
                    return ap.rearrange("p (a b c) -> p a b c", a=dims[0], b=dims[1])
                return ap
            lg = v(0, G * 36, G, 36)
            gl = lg[:, :, 0:4]
            el = lg[:, :, 4:36]
            gmax = v(144, G)
            gmask = v(148, G * 4, G, 4)
            gd = v(164, G * 4, G, 4)
            gsum = v(180, G)
            gw = v(184, G)
            pen = v(188, G * 4, G, 4)
            elm = v(204, G * NE, G, NE)
            elm4 = v(204, G * NE, G, 4, 8)
            m1 = v(332, G)
            mask1 = v(336, G * NE, G, NE)
            m2 = v(464, G)
            mask2 = v(468, G * NE, G, NE)
            dd = v(596, G)
            ee = v(600, G)
            w1 = v(604, G)
            w2 = v(608, G)

            def bc(ap, shape):
                return ap.unsqueeze(len(ap.shape)).broadcast_to(shape)
            P.op("dve", lambda e: e.tensor_reduce(out=gmax, in_=gl, axis=X, op=ALU.max), B_, B_)
            TT("dve", gmask, gl, bc(gmax, [128, G, 4]), ALU.is_equal, B_, B_)
            TT("dve", gd, gl, bc(gmax, [128, G, 4]), ALU.subtract, B_, B_)
            ACT(gd, gd, AF.Exp, B_, B_)
            P.op("dve", lambda e: e.tensor_reduce(out=gsum, in_=gd, axis=X, op=ALU.add), B_, B_)
            P.op("dve", lambda e: e.reciprocal(out=gw, in_=gsum), B_, B_)
            TS("dve", pen, gmask, BIGR, -BIGR, ALU.mult, ALU.add, B_, B_)
            TT("dve", elm4, el.rearrange("p g (a b) -> p g a b", a=4), bc(pen, [128, G, 4, 8]), ALU.add, B_, B_)
            P.op("dve", lambda e: e.tensor_reduce(out=m1, in_=elm, axis=X, op=ALU.max), B_, B_)
            TT("dve", mask1, elm, bc(m1, [128, G, NE]), ALU.is_equal, B_, B_)
            STT(elm, mask1, -3.0 * BIGR, elm, ALU.mult, ALU.add, B_, B_)
            P.op("dve", lambda e: e.tensor_reduce(out=m2, in_=elm, axis=X, op=ALU.max), B_, B_)
            TT("dve", mask2, elm, bc(m2, [128, G, NE]), ALU.is_equal, B_, B_)
            TT("dve", dd, m1, m2, ALU.subtract, B_, B_)
            ACT(ee, dd, AF.Exp, B_, B_, scale=-1.0)
            TS("dve", w1, ee, 1.0, None, ALU.add, None, B_, B_)
            P.op("dve", lambda e: e.reciprocal(out=w1, in_=w1), B_, B_)
            TT("dve", w2, ee, w1, ALU.mult, B_, B_)
            BWgs = [BWg[t0_ + g] for g in range(G)]
            BMs = [BM[t0_ + g] for g in range(G)]
            TT("dve", Wg[:, t0_:t0_ + G, 0], w1, gw, ALU.mult, B_, BWgs)
            TT("dve", Wg[:, t0_:t0_ + G, 1], w2, gw, ALU.mult, B_, BWgs)
            CP("dve", M1[:, t0_:t0_ + G, :], mask1, B_, BMs)
            CP("dve", M2[:, t0_:t0_ + G, :], mask2, B_, BMs)
            TT("dve", msel4[gp][:, :, :], mask1, mask2, ALU.add, B_, [Bmsel[gp]])
            bK = 3 if gp == 0 else 7
            for g in range(G):
                tt = t0_ + g
                s2 = tt % 2
                if tt == 0:
                    CP("dve", mcum[s2][:, :], msel4[gp][:, g, :], [Bmsel[gp]], [Bmcum[s2]])
                else:
                    TT("dve", mcum[s2][:, :], mcum[1 - s2][:, :], msel4[gp][:, g, :], ALU.add,
                       [Bmcum[1 - s2], Bmsel[gp]], [Bmcum[s2]])
                MM(banks[bK][:, g * NE:(g + 1) * NE], SL_bf[:], msel4[gp][:, g, :], True, tt == 0, [Bmsel[gp], Bc2], [bankB[bK]])
                if tt > 0:
                    MM(banks[bK][:, g * NE:(g + 1) * NE], ones_bf[:], mcum[1 - s2][:, :], False, True,
                       [Bmcum[1 - s2], Bc], [bankB[bK]])
            CP("dve", Rk[:, t0_:t0_ + G, :], banks[bK][:, 0:G * NE].rearrange("p (g e) -> p g e", g=G),
               [bankB[bK]], [BRk[t0_ + g] for g in range(G)])

        phase3b()
        if upto == "3b":
            DMA("sp", dbg_d[0:128, :], Rk[:, :, :].rearrange("p t e -> p (t e)"), BRk, [Bdbg[0]], "dbgcmb")

        cnt = sb("cnt", (128, NE), F32)
        cnti = sb("cnti", (128, NE), I32)
        pcf = sb("pcf", (128, NE), F32)
        cend = sb("cend", (128, NE), F32)
        offs = sb("offs", (128, NE), F32)
        sstart_i = sb("sstart_i", (128, NSLOT), I32)
        sstart = sb("sstart", (128, NSLOT), F32)
        eidf = sb("eidf", (128, NSLOT), F32)
        pidx_i = sb("pidx_i", (128, 1), I32)
        pidx = sb("pidx", (128, 1), F32)
        posall = sb("posall", (128, NT * NE), F32)
        pmall = sb("pmall", (128, NT * NE), F32)
        pfall = sb("pfall", (128, 2, NT), F32)
        Bpb = Buf("passB")
        Bpos = [Buf() for _ in range(2)]
        BIDX = [Buf(f"IDX{t}") for t in range(NT)]
        BWI = Buf("WI")
        BXs = Buf("Xs_d")
        BXs_t = [Buf(f"Xs_t{t}") for t in range(NT)]

        def passB():
            lastm = (NT - 1) % 2
            MM(banks[0][:, 0:NE], ones_bf[:], mcum[lastm][:, :], True, True, [Bmcum[lastm], Bc], [bankB[0]])
            B_ = [Bpb]
            TS("dve", cnti[:, :], banks[0][:, 0:NE], float(SLOTR - 1), None, ALU.add, None, [bankB[0]], B_)
            TS("dve", cnti[:, :], cnti[:, :], SHIFT, None, ALU.logical_shift_right, None, B_, B_)
            TS("dve", cnti[:, :], cnti[:, :], SHIFT, None, ALU.logical_shift_left, None, B_, B_)
            CP("dve", pcf[:, :], cnti[:, :], B_, B_)
            P.op("dve", lambda e: e.tensor_tensor_scan(out=cend[:, :], data0=pcf[:, :], data1=pcf[:, :], initial=0.0,
                                                       op0=ALU.add, op1=ALU.max), B_, B_)
            TT("dve", offs[:, :], cend[:, :], pcf[:, :], ALU.subtract, B_, B_)
            P.op("pool", lambda e: e.iota(sstart_i[:, :], pattern=[[SLOTR, NSLOT]], base=0, channel_multiplier=0), (), B_)
            P.op("pool", lambda e: e.iota(pidx_i[:, :], pattern=[[0, 1]], base=0, channel_multiplier=1), (), B_)
            CP("dve", sstart[:, :], sstart_i[:, :], B_, B_)
            CP("dve", pidx[:, :], pidx_i[:, :], B_, B_)
            for ex in range(NE):
                if ex == 0:
                    TS("dve", eidf[:, :], sstart[:, :], cend[:, 0:1], None, ALU.is_ge, None, B_, B_)
                else:
                    STT(eidf[:, :], sstart[:, :], cend[:, ex:ex + 1], eidf[:, :], ALU.is_ge, ALU.add, B_, B_)
            TS("dve", eidf[:, :], eidf[:, :], float(NE - 1), 128.0, ALU.min, ALU.mult, B_, B_)
            TS("dve", WI[:, :], eidf[:, :], pidx[:, 0:1], None, ALU.add, None, B_, [BWI])
            Bp = [Bpos[0]]
            pos3 = posall[:, :].rearrange("p (t e) -> p t e", e=NE)
            pm3 = pmall[:, :].rearrange("p (t e) -> p t e", e=NE)
            TT("dve", pos3, Rk[:, :, :], offs[:, :].unsqueeze(1).broadcast_to([128, NT, NE]), ALU.add, BRk + [Bpb], Bp)
            TT("dve", pm3, pos3, M1[:, :, :], ALU.mult, Bp + BM, Bp)
            P.op("dve", lambda e: e.tensor_reduce(out=pfall[:, 0, :], in_=pm3, axis=mybir.AxisListType.X, op=ALU.add), Bp, Bp)
            TT("dve", pm3, pos3, M2[:, :, :], ALU.mult, Bp + BM, Bp)
            P.op("dve", lambda e: e.tensor_reduce(out=pfall[:, 1, :], in_=pm3, axis=mybir.AxisListType.X, op=ALU.add), Bp, Bp)
            CP("dve", IDX[:, :, :].rearrange("p t c -> p c t"), pfall[:, :, :], Bp, BIDX)
            for tt in range(NT):
                for ch in range(2):
                    P.op("pool", lambda e, tt=tt, ch=ch: e.indirect_dma_start(
                        out=Xs_d[:, :], out_offset=bass.IndirectOffsetOnAxis(ap=IDX[:, tt, ch:ch + 1], axis=0),
                        in_=h2b[:, tt * D:(tt + 1) * D], in_offset=None),
                        [BIDX[tt], Bh2b[tt]], [BXs_t[tt]] if ch else [BXs], dma_key="scat")

        passB()
        if upto == "pb":
            DMA("sp", dbg_d[0:128, 0:64], IDX[:, :, :].rearrange("p t c -> p (t c)").bitcast(F32), BIDX, [Bdbg[0]], "dbgidx")
            DMA("sp", dbg_d[128:256, 0:NSLOT], WI[:, :].bitcast(F32), [BWI], [Bdbg[1]], "dbgwi")
            DMA("sp", dbg_d[256:384, 0:64], Wg[:, :, :].rearrange("p t c -> p (t c)"), BWg, [Bdbg[2]], "dbgwg")

        P.barrier()
        stk.pop().close()
        stk.append(ExitStack())
        NWS = 4
        wsl = [sb(f"wsl{i}", (128, WROW), BF16) for i in range(NWS)]
        Bwsl = [Buf() for _ in range(NWS)]
        xsl = [sb(f"xsl{i}", (128, NSUB, D), BF16) for i in range(2)]
        Bxsl = [Buf() for _ in range(2)]
        XsT = [sb(f"XsT{i}", (128, 8, SLOTR), BF16) for i in range(2)]
        BXsT = [[Buf(), Buf()] for _ in range(2)]
        silb = [sb(f"silb{i}", (128, SLOTR), BF16) for i in range(4)]
        Bsilb = [Buf() for _ in range(4)]
        hidT = [sb(f"hidT{i}", (128, 2, SLOTR), BF16) for i in range(2)]
        BhidT = [Buf() for _ in range(2)]
        ysb = [sb(f"ysb{i}", (128, D), F32) for i in range(3)]
        Bysb = [[Buf(), Buf()] for _ in range(3)]
        BYs = [Buf(f"Ys{i}") for i in range(NSLOT)]
        Bout = [Buf(f"out{t}") for t in range(NT)]

        def slot_loop():
            yrot = [0]
            steps = []
            for i in range(NSLOT):
                ws = i % NWS
                r2 = i % 2

                def stL(i=i, ws=ws, r2=r2):
                    P.op("pool", lambda e: e.indirect_dma_start(
                        out=wsl[ws][:, :], out_offset=None, in_=Wb_d[:, :],
                        in_offset=bass.IndirectOffsetOnAxis(ap=WI[:, i:i + 1], axis=0)),
                        [BWI] + BWb, [Bwsl[ws]], dma_key=f"wsl{ws}")
                    DMA("sp", xsl[r2][:, :, :], Xs_d[i * SLOTR:(i + 1) * SLOTR, :].rearrange("(s p) d -> p s d", p=128),
                        [BXs] + BXs_t, [Bxsl[r2]], f"xsl{r2}")

                def stT(i=i, ws=ws, r2=r2):
                    for hb_ in range(2):
                        bk = hb_
                        tv = banks[bk][:].bitcast(BF16)
                        for cc in range(4):
                            c = hb_ * 4 + cc
                            for sub in range(NSUB):
                                TR(tv[:, cc * SLOTR + sub * 128:cc * SLOTR + (sub + 1) * 128], xsl[r2][:, sub, c * 128:(c + 1) * 128],
                                   ident_bf[:], [Bxsl[r2], Bc2], [bankB[bk]])
                        CP("act" if hb_ == 0 else "dve", XsT[r2][:, hb_ * 4:hb_ * 4 + 4, :],
                           tv[:, :].rearrange("p (c t) -> p c t", c=4), [bankB[bk]], [BXsT[r2][hb_]])

                def stAB(i=i, ws=ws, r2=r2):
                    for m in range(2):
                        ba = 2 + 2 * r2 + m
                        sb_ = 2 * r2 + m
                        for c in range(8):
                            MM(banks[ba][:, 0:SLOTR], wsl[ws][:, c * DE + m * 128:c * DE + (m + 1) * 128], XsT[r2][:, c, :], c == 0, c == 7,
                               [Bwsl[ws]] + BXsT[r2], [bankB[ba]])
                        for c in range(8):
                            MM(banks[ba][:, SLOTR:2 * SLOTR], wsl[ws][:, 8 * DE + c * DE + m * 128:8 * DE + c * DE + (m + 1) * 128],
                               XsT[r2][:, c, :], c == 0, c == 7, [Bwsl[ws]] + BXsT[r2], [bankB[ba]])
                        ACT(silb[sb_][:, :], banks[ba][:, 0:SLOTR], AF.Silu, [bankB[ba]], [Bsilb[sb_]])
                        TT("dve", hidT[r2][:, m, :], banks[ba][:, SLOTR:2 * SLOTR], silb[sb_][:, :], ALU.mult,
                           [bankB[ba], Bsilb[sb_]], [BhidT[r2]])

                def stY(i=i, ws=ws, r2=r2):
                    for sub in range(NSUB):
                        yr = yrot[0] % 3
                        yrot[0] += 1
                        for half in range(2):
                            by = 6 + half
                            for m in range(2):
                                MM(banks[by][:, :], hidT[r2][:, m, sub * 128:(sub + 1) * 128],
                                   wsl[ws][:, 16 * DE + m * D + half * 512:16 * DE + m * D + (half + 1) * 512],
                                   m == 0, m == 1, [BhidT[r2], Bwsl[ws]], [bankB[by]])
                            CP("act" if half == 0 else "dve", ysb[yr][:, half * 512:(half + 1) * 512], banks[by][:, :],
                               [bankB[by]], [Bysb[yr][half]])
                        DMA("act", Ys_d[i * SLOTR + sub * 128:i * SLOTR + (sub + 1) * 128, :], ysb[yr][:, :], Bysb[yr], [BYs[i]],
                            f"yst{yr}")
                steps.append([stL, stT, stAB, stY])
            run_pipeline(steps, None)

        def combine():
            def load(tt):
                s3 = tt % NXS
                s2 = tt % 3
                DMA("sp", xs[s3][:], x2_d[tt * 128:(tt + 1) * 128, :], [Bx2d[tt]], [Bxs[s3]], f"xs{s3}")
                P.op("pool", lambda e: e.indirect_dma_start(
                    out=y1[s2][:, :], out_offset=None, in_=Ys_d[:, :],
                    in_offset=bass.IndirectOffsetOnAxis(ap=IDX[:, tt, 0:1], axis=0)),
                    [BIDX[tt]] + BYs, [By1[s2]], dma_key=f"g1_{s2}")
                P.op("pool", lambda e: e.indirect_dma_start(
                    out=y2[s2][:, :], out_offset=None, in_=Ys_d[:, :],
                    in_offset=bass.IndirectOffsetOnAxis(ap=IDX[:, tt, 1:2], axis=0)),
                    [BIDX[tt]] + BYs, [By2[s2]], dma_key=f"g2_{s2}")
            load(0)
            for tt in range(NT):
                s3 = tt % NXS
                s2 = tt % 3
                STT(x3[s2][:, :], y1[s2][:, :], Wg[:, tt, 0:1], xs[s3][:, :], ALU.mult, ALU.add,
                    [By1[s2], BWg[tt], Bxs[s3]], [Bx3[s2]])
                if tt + 1 < NT:
                    load(tt + 1)
                STT(x3[s2][:, :], y2[s2][:, :], Wg[:, tt, 1:2], x3[s2][:, :], ALU.mult, ALU.add,
                    [By2[s2], BWg[tt], Bx3[s2]], [Bx3[s2]])
                rms_rstd(x3[s2][:], junk[:], stat[tt % 2][:, 0:1], stat[tt % 2][:, 1:2], stat[tt % 2][:, 2:3],
                         [Bx3[s2]], Bjunk, Bstat[tt % 2])
                STT(x3[s2][:], x3[s2][:], stat[tt % 2][:, 2:3], g3[:], ALU.mult, ALU.mult,
                    [Bx3[s2], Bstat[tt % 2], Bg3], [Bx3[s2]])
                DMA("act", out_d[tt * 128:(tt + 1) * 128, :], x3[s2][:], [Bx3[s2]], [Bout[tt]], f"outst{s2}")

        if upto not in ("3b", "pb"):
            slot_loop()
        P.barrier()
        stk.pop().close()
        stk.append(ExitStack())
        xs = [sb(f"xsd{i}", (128, D), F32) for i in range(NXS)]
        y1 = [sb(f"y1_{i}", (128, D), F32) for i in range(3)]
        y2 = [sb(f"y2_{i}", (128, D), F32) for i in range(3)]
        By1 = [Buf() for _ in range(3)]
        By2 = [Buf() for _ in range(3)]
        x3 = [sb(f"x3_{i}", (128, D), F32) for i in range(3)]
        Bx3 = [Buf() for _ in range(3)]
        if upto not in ("3b", "pb"):
            combine()

        P.op("sp", None, reads=(Bout if upto is None else []) + (Bdbg if debug else []))
        P.barrier()
        stk.pop().close()
        P.emit()
    return nc


_NC_CACHE = {}


def kernel(x, norm_attn, w_in, b_forget, w_o_sb, w_o_fox, w_out, norm_ffn,
           w_router_group, b_router_group, w_router_expert, b_router_expert,
           w1, w3, w2, norm_final, _debug=False, _upto=None, _cores=8):
    f32 = lambda a: np.ascontiguousarray(np.asarray(a, dtype=np.float32))
    nc = build_program(debug=_debug, upto=_upto)
    shared = {
        "norm_attn": f32(norm_attn).reshape(1, D), "w_in": f32(w_in)[0], "b_forget": f32(b_forget).reshape(1, NH),
        "w_o_sb": f32(w_o_sb)[0], "w_o_fox": f32(w_o_fox)[0], "w_out": f32(w_out)[0],
        "norm_ffn": f32(norm_ffn).reshape(1, D), "w_router_group": f32(w_router_group)[0],
        "b_router_group": f32(b_router_group).reshape(1, 4), "w_router_expert": f32(w_router_expert)[0],
        "b_router_expert": f32(b_router_expert).reshape(1, NE), "w1": f32(w1)[0], "w3": f32(w3)[0], "w2": f32(w2)[0],
        "norm_final": f32(norm_final).reshape(1, D),
    }
    xf = f32(x)
    in_maps = [dict(shared, x=xf[b]) for b in range(_cores)]
    res = run_bass_kernel_spmd(nc, in_maps, core_ids=list(range(_cores)))
    if _debug:
        return (np.stack([res.results[b]["out"] for b in range(_cores)], axis=0),
                np.stack([res.results[b]["dbg"] for b in range(_cores)], axis=0))
    return np.stack([res.results[b]["out"] for b in range(_cores)], axis=0)
```

```python
from contextlib import ExitStack
import numpy as np
import concourse.bass as bass
import concourse.mybir as mybir
from concourse.bass_utils import run_bass_kernel_spmd

F32 = mybir.dt.float32
BF16 = mybir.dt.bfloat16
I32 = mybir.dt.int32
AF = mybir.ActivationFunctionType
ALU = mybir.AluOpType

S = 4096
D = 1024
NT = S // 128
NQ = S // 512
HD = 64
NH = 8
INC = 5128
C_QSB, C_KSB, C_VSB, C_QFX, C_KFX, C_VFX, C_F, C_GSB, C_GFX = 0, 512, 1024, 1536, 2048, 2560, 3072, 3080, 4104
NE = 32
DE = 256
EPS = 1e-6
NEGBIG = -30000.0
FOX_DUMMY = 1
FILL_SB = 0
FILL_FX = 0

ENGS = ("pe", "act", "dve", "pool", "sp")
GEN = 30000


class Buf:
    __slots__ = ("name", "writer", "readers", "excl")

    def __init__(self, name="", excl=False):
        self.name = name
        self.writer = None
        self.readers = []
        self.excl = excl


class Op:
    __slots__ = ("eng", "fn", "idx", "deps", "signal", "sig", "dma", "dkey", "dval")

    def __init__(self, eng, fn, idx, dma):
        self.eng = eng
        self.fn = fn
        self.idx = idx
        self.deps = []
        self.signal = False
        self.sig = None
        self.dma = dma
        self.dkey = None
        self.dval = None


class Prog:
    def __init__(self, nc):
        self.nc = nc
        self.ops = {e: [] for e in ENGS}
        self.known = {e: {f: -1 for f in ENGS} for e in ENGS}
        self.known_dma = {e: {} for e in ENGS}
        self.dma_cnt = {}
        self.last_dma = {}
        self.pending = {e: [] for e in ENGS}

    def barrier(self):
        lasts = []
        for e in ENGS:
            for o in reversed(self.ops[e]):
                if not o.dma and o.fn is not None:
                    lasts.append(o)
                    break
        lasts += list(self.last_dma.values())
        for e in ENGS:
            self.pending[e] = list(lasts)

    def op(self, eng, fn, reads=(), writes=(), dma_key=None, ninc=1):
        o = Op(eng, fn, len(self.ops[eng]), dma_key is not None)
        cand = []
        for b in reads:
            if b.writer is not None:
                cand.append((b.writer, "raw"))
            if b.excl:
                for r in b.readers:
                    if r.eng != eng:
                        cand.append((r, "rar"))
        for b in writes:
            if b.writer is not None:
                cand.append((b.writer, "waw"))
            for r in b.readers:
                cand.append((r, "war"))
        best = {}
        dma_deps = {}
        if self.pending[eng]:
            for p in self.pending[eng]:
                if p.dma or p.eng != eng:
                    cand.append((p, "raw"))
            self.pending[eng] = []
        for (p, kind) in cand:
            if p is o:
                continue
            if p.dma:
                if self.known_dma[eng].get(p.dkey, 0) >= p.dval:
                    continue
                if p.dkey not in dma_deps or dma_deps[p.dkey].dval < p.dval:
                    dma_deps[p.dkey] = p
                continue
            if p.eng == eng:
                if eng == "pe":
                    continue
            if self.known[eng][p.eng] >= p.idx:
                continue
            if p.eng not in best or best[p.eng].idx < p.idx:
                best[p.eng] = p
        for k, p in dma_deps.items():
            o.deps.append(p)
            self.known_dma[eng][k] = p.dval
        for f, p in best.items():
            o.deps.append(p)
            p.signal = True
            self.known[eng][f] = p.idx
        if o.dma:
            o.dkey = dma_key
            self.dma_cnt[dma_key] = self.dma_cnt.get(dma_key, 0) + 16 * ninc
            o.dval = self.dma_cnt[dma_key]
            self.last_dma[dma_key] = o
        for b in reads:
            b.readers.append(o)
        for b in writes:
            b.writer = o
            b.readers = []
        self.ops[eng].append(o)
        return o

    def emit(self):
        nc = self.nc
        with ExitStack() as st:
            sems = {}
            for e in ENGS:
                n = 0
                for o in self.ops[e]:
                    if o.signal and not o.dma:
                        o.sig = n
                        n += 1
                ngen = max(1, (n + GEN - 1) // GEN)
                sems[e] = [st.enter_context(nc.semaphore(f"s_{e}_{g}")) for g in range(ngen)]
            dsem = {}
            for k in self.dma_cnt:
                dsem[k] = st.enter_context(nc.semaphore(f"d_{len(dsem)}"))
            block = st.enter_context(nc.Block())
            handles = {"pe": block.tensor, "act": block.scalar, "dve": block.vector,
                       "pool": block.gpsimd, "sp": block.sync}

            def run(e):
                ops = self.ops[e]
                if not ops:
                    return

                def body(eng):
                    for o in ops:
                        for p in o.deps:
                            if p.dma:
                                eng.wait_ge(dsem[p.dkey], p.dval)
                            else:
                                eng.wait_ge(sems[p.eng][p.sig // GEN], p.sig % GEN + 1)
                        if o.fn is None:
                            continue
                        ins = o.fn(eng)
                        if o.dma:
                            if not isinstance(ins, (list, tuple)):
                                ins = [ins]
                            for i_ in ins:
                                i_.then_inc(dsem[o.dkey], 16)
                        elif o.signal:
                            ins.then_inc(sems[e][o.sig // GEN], 1)
                handles[e](body)
            for e in ENGS:
                run(e)


def build_program(debug=False, upto=None):
    nc = bass.Bass("TRN2", target_bir_lowering=False)
    dram_in = lambda n, s: nc.dram_tensor(n, list(s), F32, kind="ExternalInput").ap()
    x_d = dram_in("x", (S, D))
    norm_attn_d = dram_in("norm_attn", (1, D))
    w_in_d = dram_in("w_in", (D, INC))
    b_forget_d = dram_in("b_forget", (1, NH))
    w_o_sb_d = dram_in("w_o_sb", (512, D))
    w_o_fox_d = dram_in("w_o_fox", (512, D))
    w_out_d = dram_in("w_out", (D, D))
    norm_ffn_d = dram_in("norm_ffn", (1, D))
    w_rg_d = dram_in("w_router_group", (D, 4))
    b_rg_d = dram_in("b_router_group", (1, 4))
    w_re_d = dram_in("w_router_expert", (D, NE))
    b_re_d = dram_in("b_router_expert", (1, NE))
    w1_d = dram_in("w1", (NE, D, DE))
    w3_d = dram_in("w3", (NE, D, DE))
    w2_d = dram_in("w2", (NE, DE, D))
    norm_final_d = dram_in("norm_final", (1, D))
    out_d = nc.dram_tensor("out", [S, D], F32, kind="ExternalOutput").ap()
    oT_d = nc.dram_tensor("oT_scr", [16, HD, S], BF16, kind="Internal").ap()
    x2_d = nc.dram_tensor("x2_scr", [S, D], F32, kind="Internal").ap()
    NSLOT = 64
    SLOTR = 256
    NSUB = SLOTR // 128
    SHIFT = 8
    WROW = 2 * 8 * DE + 2 * D
    Wb_d = nc.dram_tensor("Wb_scr", [NE * 128, WROW], BF16, kind="Internal").ap()
    Xs_d = nc.dram_tensor("Xs_scr", [NSLOT * SLOTR, D], BF16, kind="Internal").ap()
    Ys_d = nc.dram_tensor("Ys_scr", [NSLOT * SLOTR, D], F32, kind="Internal").ap()
    cparts_d = nc.dram_tensor("cparts_scr", [NH, 3, S], BF16, kind="Internal").ap()
    ncparts_d = nc.dram_tensor("ncparts_scr", [NH, 3, S], BF16, kind="Internal").ap()
    dbg_d = None
    if debug:
        dbg_d = nc.dram_tensor("dbg", [S, D], F32, kind="ExternalOutput").ap()

    P = Prog(nc)
    with ExitStack() as st:
        stk = [st]

        def sb(name, shape, dt):
            return stk[-1].enter_context(nc.sbuf_tensor(name, list(shape), dt))

        banks = [st.enter_context(nc.psum_tensor(f"bank{i}", [128, 512], F32)) for i in range(8)]
        bankB = [Buf(f"bank{i}", excl=True) for i in range(8)]

        def MM(out, lhsT, rhs, start, stop, reads, writes, **kw):
            return P.op("pe", lambda e: e.matmul(out, lhsT=lhsT, rhs=rhs, start=start, stop=stop, **kw), reads, writes)

        def TR(out, in_, ident, reads, writes):
            return P.op("pe", lambda e: e.transpose(out=out, in_=in_, identity=ident), reads, writes)

        def ACT(out, in_, func, reads, writes, **kw):
            return P.op("act", lambda e: e.activation(out=out, in_=in_, func=func, **kw), reads, writes)

        def TT(eng, out, in0, in1, op, reads, writes):
            return P.op(eng, lambda e: e.tensor_tensor(out=out, in0=in0, in1=in1, op=op), reads, writes)

        def TS(eng, out, in0, s1, s2, op0, op1, reads, writes, **kw):
            if op1 is None:
                return P.op(eng, lambda e: e.tensor_scalar(out=out, in0=in0, scalar1=s1, scalar2=None, op0=op0, **kw), reads, writes)
            return P.op(eng, lambda e: e.tensor_scalar(out=out, in0=in0, scalar1=s1, scalar2=s2, op0=op0, op1=op1, **kw), reads, writes)

        def STT(out, in0, scalar, in1, op0, op1, reads, writes):
            return P.op("dve", lambda e: e.scalar_tensor_tensor(out=out, in0=in0, scalar=scalar, in1=in1, op0=op0, op1=op1), reads, writes)

        def CP(eng, out, in_, reads, writes):
            if eng == "act":
                return P.op("act", lambda e: e.copy(out=out, in_=in_), reads, writes)
            return P.op(eng, lambda e: e.tensor_copy(out=out, in_=in_), reads, writes)

        def MEMSET(eng, ap, val, writes):
            return P.op(eng, lambda e: e.memset(ap, val), (), writes)

        def DMA(q, out, in_, reads, writes, key, **kw):
            return P.op(q, lambda e: e.dma_start(out=out, in_=in_, **kw), reads, writes, dma_key=key)

        ones_bf = sb("ones_bf", (128, 128), BF16)
        negones_bf = sb("negones_bf", (128, 128), BF16)
        negbig_bf = sb("negbig_bf", (128, 128), BF16)
        ident_bf = sb("ident_bf", (128, 128), BF16)
        negbigI = sb("negbigI", (128, 128), BF16)
        negtri = sb("negtri", (128, 128), BF16)
        ones513 = sb("ones513", (128, 513), BF16)
        M0 = sb("M0", (128, 513), BF16)
        ones_f32 = sb("ones_f32", (128, 128), F32)
        ident_f32 = sb("ident_f32", (128, 128), F32)
        eps_t = sb("eps_t", (128, 1), F32)
        Bc = Buf("consts")
        Bg1 = Buf("g1")

        P.op("pool", lambda e: e.memset(ones_bf[:], 1.0), (), [Bc])
        P.op("pool", lambda e: e.memset(negones_bf[:], -1.0), (), [Bc])
        P.op("pool", lambda e: e.memset(negbig_bf[:], NEGBIG), (), [Bc])
        P.op("pool", lambda e: e.memset(ones513[:], 1.0), (), [Bc])
        P.op("pool", lambda e: e.memset(ones_f32[:], 1.0), (), [Bc])
        P.op("pool", lambda e: e.memset(eps_t[:], EPS), (), [Bc])
        Bc2 = Buf("consts2")
        P.op("pool", lambda e: e.affine_select(out=ident_bf[:], in_=ones_bf[:], pattern=[[-1, 128]], compare_op=ALU.is_equal,
                                               fill=0.0, base=0, channel_multiplier=1), [Bc], [Bc2])
        P.op("pool", lambda e: e.affine_select(out=ident_f32[:], in_=ones_f32[:], pattern=[[-1, 128]], compare_op=ALU.is_equal,
                                               fill=0.0, base=0, channel_multiplier=1), [Bc], [Bc2])
        P.op("pool", lambda e: e.affine_select(out=negbigI[:], in_=negbig_bf[:], pattern=[[-1, 128]], compare_op=ALU.is_equal,
                                               fill=0.0, base=0, channel_multiplier=1), [Bc], [Bc2])
        P.op("pool", lambda e: e.affine_select(out=negtri[:], in_=negones_bf[:], pattern=[[-1, 128]], compare_op=ALU.is_ge,
                                               fill=0.0, base=0, channel_multiplier=1), [Bc], [Bc2])
        P.op("pool", lambda e: e.affine_select(out=M0[:], in_=ones513[:], pattern=[[-1, 513]], compare_op=ALU.is_ge,
                                               fill=0.0, base=0, channel_multiplier=1), [Bc], [Bc2])
        P.op("pool", lambda e: e.affine_select(out=SL_bf[:], in_=ones_bf[:], pattern=[[1, 128]], compare_op=ALU.is_ge,
                                               fill=0.0, base=-1, channel_multiplier=-1), [Bc], [Bc2])

        hT = sb("hT", (128, 8, S), BF16)
        BhT = [Buf(f"hT{t}") for t in range(NQ)]
        g3 = sb("g3", (128, D), F32)
        M1 = sb("M1", (128, NT, NE), BF16)
        M2 = sb("M2", (128, NT, NE), BF16)
        Rk = sb("Rk", (128, NT, NE), F32)
        Wg = sb("Wg", (128, NT, 2), F32)
        IDX = sb("IDX", (128, NT, 2), I32)
        WI = sb("WI", (128, 64), I32)
        SL_bf = sb("SL_bf", (128, 128), BF16)

        def rms_rstd(xs_ap, junk_ap, ss_ap, ln_ap, rstd_ap, rd, wr_junk, wr_stat):
            P.op("act", lambda e: e.activation(out=junk_ap, in_=xs_ap, func=AF.Square, accum_out=ss_ap), rd, [wr_junk, wr_stat])
            ACT(ln_ap, ss_ap, AF.Ln, [wr_stat, Bc], [wr_stat], scale=1.0 / D, bias=eps_t[:, 0:1])
            ACT(rstd_ap, ln_ap, AF.Exp, [wr_stat], [wr_stat], scale=-0.5)

        NXS = 2
        Bxs = [Buf(f"xs{i}") for i in range(NXS)]
        junk = sb("junk", (128, D), BF16)
        Bjunk = Buf("junk")
        stat = [sb(f"stat{i}", (128, 4), F32) for i in range(2)]
        Bstat = [Buf(f"stat{i}") for i in range(2)]
        stk.append(ExitStack())
        g1 = sb("g1", (128, D), F32)
        DMA("sp", g1[:], norm_attn_d[0:1, :].partition_broadcast(128), (), [Bg1], "g1")
        xs = [sb(f"xsa{i}", (128, D), F32) for i in range(NXS)]
        hb = [sb(f"hb{i}", (128, D), BF16) for i in range(2)]
        Bhb = [Buf(f"hb{i}") for i in range(2)]

        def run_pipeline(stage_lists, bg, every=6):
            n = len(stage_lists)
            nst = max(len(x) for x in stage_lists)
            bgi = 0
            for s in range(n + nst - 1):
                for k in range(nst):
                    t = s - k
                    if 0 <= t < n and k < len(stage_lists[t]) and stage_lists[t][k] is not None:
                        stage_lists[t][k]()
                if bg and s % every == 3 and bgi < len(bg):
                    bg[bgi]()
                    bgi += 1
            while bg and bgi < len(bg):
                bg[bgi]()
                bgi += 1

        def phase1():
            def front(tt):
                s3 = tt % NXS
                s2 = tt % 2
                DMA("sp", xs[s3][:], x_d[tt * 128:(tt + 1) * 128, :], (), [Bxs[s3]], f"xs{s3}")
                rms_rstd(xs[s3][:], junk[:], stat[s2][:, 0:1], stat[s2][:, 1:2], stat[s2][:, 2:3],
                         [Bxs[s3]], Bjunk, Bstat[s2])
                STT(hb[s2][:], xs[s3][:], stat[s2][:, 2:3], g1[:], ALU.mult, ALU.mult,
                    [Bxs[s3], Bstat[s2], Bg1], [Bhb[s2]])

            def back(tt):
                s2 = tt % 2
                bk = 6 + (tt % 2)
                pv = banks[bk][:].bitcast(BF16)
                for c in range(8):
                    TR(pv[:, c * 128:(c + 1) * 128], hb[s2][:, c * 128:(c + 1) * 128], ident_bf[:],
                       [Bhb[s2], Bc2], [bankB[bk]])
                CP("act", hT[:, :, tt * 128:(tt + 1) * 128], pv[:, :].rearrange("p (c t) -> p c t", c=8),
                   [bankB[bk]], [BhT[tt // 4]])
            steps = [[lambda tt=tt: front(tt), lambda tt=tt: back(tt)] for tt in range(NT)]
            run_pipeline(steps, None)

        phase1()
        P.barrier()
        stk.pop().close()

        NSL = 3
        stk.append(ExitStack())
        QTa = [sb(f"QTa{i}", (128, S), BF16) for i in range(NSL)]
        KTa = [sb(f"KTa{i}", (128, S), BF16) for i in range(NSL)]
        Vt = [sb(f"Vt{i}", (128, NT, HD + 1), BF16) for i in range(NSL)]
        wqk = [sb(f"wqk{i}", (128, 8, 130), BF16) for i in range(NSL)]
        wv = [sb(f"wv{i}", (128, 8, HD), BF16) for i in range(NSL)]
        BQ = [Buf(f"Q{i}") for i in range(NSL)]
        BK = [Buf(f"K{i}") for i in range(NSL)]
        BQaug = [Buf(f"Qaug{i}") for i in range(NSL)]
        BKaug = [Buf(f"Kaug{i}") for i in range(NSL)]
        BV = [Buf(f"V{i}") for i in range(NSL)]
        BVones = [Buf(f"Vones{i}") for i in range(NSL)]
        Bwqk = [Buf(f"wqk{i}") for i in range(NSL)]
        Bwv = [Buf(f"wv{i}") for i in range(NSL)]
        ostS = sb("ostS", (64, S), BF16)
        ostF = sb("ostF", (64, S), BF16)
        BostS = Buf("ostS")
        BostF = Buf("ostF")
        wstage = [sb(f"wstage{i}", (128, 2048), BF16) for i in range(1)]
        Bwstage = [Buf(f"wstage{i}") for i in range(1)]
        BWb = [Buf(f"Wb{e}") for e in range(NE)]

        def conv_expert(ex):
            def f():
                srcs = [w1_d[ex].rearrange("(c p) n -> p c n", p=128), w3_d[ex].rearrange("(c p) n -> p c n", p=128),
                        w2_d[ex].rearrange("(k p) n -> p k n", p=128)]
                pats = ["p (c n) -> p c n", "p (c n) -> p c n", "p (k n) -> p k n"]
                for part in range(3):
                    kw = {"c": 8} if part < 2 else {"k": 2}
                    P.op("pool", lambda e, part=part, kw=kw: e.dma_start(
                        out=wstage[0][:, :].rearrange(pats[part], **kw), in_=srcs[part]),
                        (), [Bwstage[0]], dma_key="wstg0")
                    DMA("sp", Wb_d[ex * 128:(ex + 1) * 128, part * 2048:(part + 1) * 2048], wstage[0][:, :],
                        [Bwstage[0]], [BWb[ex]], "wbst0")
            return f
        BoT_d = [Buf(f"oTd{h}") for h in range(16)]

        for i in range(NSL):
            P.op("pool", lambda e, i=i: e.memset(Vt[i][:, :, HD:HD + 1], 1.0), (), [BVones[i]])
            P.op("pool", lambda e, i=i: e.memset(wqk[i][:, :, 128:130], 0.0), (), [BVones[i]])

        e_sb = [sb(f"e_sb{i}", (128, 512), F32) for i in range(2)]
        sp_bf = [sb(f"sp_bf{i}", (128, 512), BF16) for i in range(2)]
        arg_sb = [sb(f"arg_sb{i}", (128, 512), F32) for i in range(2)]
        A_bf = [sb(f"A_bf{i}", (128, 512), BF16) for i in range(3)]
        P_bf = [sb(f"P_bf{i}", (128, 512), BF16) for i in range(3)]
        BPb = [Buf() for _ in range(3)]
        oraw = [sb(f"oraw{i}", (128, 512), F32) for i in range(2)]
        Boraw = [Buf() for _ in range(2)]
        C_sb = [sb(f"C_sb{i}", (128, 512), F32) for i in range(2)]
        Be = [Buf() for _ in range(2)]
        Bsp = [Buf() for _ in range(2)]
        Barg = [Buf() for _ in range(2)]
        BA = [Buf() for _ in range(3)]
        BC = [Buf() for _ in range(2)]

        wf = sb("wf", (128, 8, NH), BF16)
        Bwf = Buf("wf")
        fexp = [sb(f"fexp{i}", (NH, 512), F32) for i in range(1)] * 2
        cpt = [sb(f"cpt{i}", (NH, 512), F32) for i in range(2)]
        r1 = fexp
        cpp = [sb(f"cpp{i}", (NH, 3, 512), BF16) for i in range(1)] * 2
        negb = sb("negb", (NH, 1), F32)
        Bnegb = Buf("negb")
        Bfexp = [Buf()] * 2
        Bcpt = [Buf() for _ in range(2)]
        Br1 = Bfexp
        Bcpp = [Buf()] * 2
        Bncpp = [Buf()] * 2
        Bcparts = [Buf(f"cparts_d{t}") for t in range(NQ)]

        def load_head_weights(hd, sl):
            typ, h = divmod(hd, NH)
            cq = (C_QSB if typ == 0 else C_QFX) + h * HD
            ck = (C_KSB if typ == 0 else C_KFX) + h * HD
            cv = (C_VSB if typ == 0 else C_VFX) + h * HD
            P.op("pool", lambda e: [
                e.dma_start(out=wqk[sl][:, :, 0:HD], in_=w_in_d[:, cq:cq + HD].rearrange("(c p) n -> p c n", p=128)),
                e.dma_start(out=wqk[sl][:, :, HD:2 * HD], in_=w_in_d[:, ck:ck + HD].rearrange("(c p) n -> p c n", p=128)),
            ], (), [Bwqk[sl]], dma_key=f"wqk{sl}", ninc=2)
            P.op("pool", lambda e: e.dma_start(out=wv[sl][:, :, :], in_=w_in_d[:, cv:cv + HD].rearrange("(c p) n -> p c n", p=128)),
                 (), [Bwv[sl]], dma_key=f"wv{sl}")

        pj_rot = [0]

        def proj_chunks(hd, sl):
            typ, h = divmod(hd, NH)
            chunks = []
            bk = 7

            def qk_chunk(T, which):
                lo = 0 if which == "q" else HD
                out = []
                for c in range(8):
                    out.append(lambda c=c: MM(banks[bk][0:HD + 1, :], wqk[sl][:, c, lo:lo + HD + 1],
                                              hT[:, c, T * 512:(T + 1) * 512], c == 0, c == 7,
                                              [Bwqk[sl], BhT[T], BVones[sl]], [bankB[bk]]))
                if which == "q":
                    out.append(lambda: TS("dve", QTa[sl][0:HD, T * 512:(T + 1) * 512], banks[bk][0:HD, :], 0.125, None,
                                          ALU.mult, None, [bankB[bk]], [BQ[sl]]))
                else:
                    out.append(lambda: CP("dve", KTa[sl][0:HD, T * 512:(T + 1) * 512], banks[bk][0:HD, :],
                                          [bankB[bk]], [BK[sl]]))
                return out

            def v_chunk(g):
                out = []
                for u in range(8):
                    def f(u=u):
                        tt = g * 8 + u
                        for c in range(8):
                            MM(banks[bk][:, u * HD:(u + 1) * HD], hT[:, c, tt * 128:(tt + 1) * 128], wv[sl][:, c, :],
                               c == 0, c == 7, [Bwv[sl], BhT[tt // 4]], [bankB[bk]])
                    out.append(f)
                out.append(lambda: CP("dve", Vt[sl][:, g * 8:(g + 1) * 8, 0:HD],
                                      banks[bk][:, :].rearrange("p (u d) -> p u d", u=8), [bankB[bk]], [BV[sl]]))
                return out

            for T in range(NQ):
                chunks += qk_chunk(T, "k")
            for g in range(4):
                chunks += v_chunk(g)
            for T in range(NQ):
                chunks += qk_chunk(T, "q")
            if typ == 0:
                def zpad():
                    P.op("pool", lambda e: e.memset(QTa[sl][64:128, :], 0.0), (), [BQaug[sl]])
                    P.op("pool", lambda e: e.memset(KTa[sl][64:128, :], 0.0), (), [BKaug[sl]])
                chunks.append(zpad)
            if typ == 1:
                def aug():
                    P.op("pool", lambda e: e.memset(QTa[sl][64:70, :], 1.0), (), [BQaug[sl]])
                    P.op("pool", lambda e: e.memset(KTa[sl][64:70, :], -1.0), (), [BKaug[sl]])
                    DMA("sp", QTa[sl][64:67, :], cparts_d[h, :, :], Bcparts, [BQaug[sl]], f"qaug{sl}")
                    DMA("sp", KTa[sl][67:70, :], cparts_d[h, :, :], Bcparts, [BKaug[sl]], f"kaug{sl}")
                chunks.append(aug)
            return chunks

        def fox_prep():
            P.op("pool", lambda e: e.dma_start(out=wf[:, :, :], in_=w_in_d[:, C_F:C_F + NH].rearrange("(c p) n -> p c n", p=128)),
                 (), [Bwf], dma_key="wf")
            DMA("sp", negb[:, 0:1], b_forget_d[0:1, :].rearrange("o h -> h o"), (), [Bnegb], "negb")
            TS("dve", negb[:, 0:1], negb[:, 0:1], -1.0, None, ALU.mult, None, [Bnegb], [Bnegb])
            for T in range(NQ):
                bk = 7
                s2 = T % 2
                for c in range(8):
                    MM(banks[bk][0:NH, :], wf[:, c, :], hT[:, c, T * 512:(T + 1) * 512], c == 0, c == 7,
                       [Bwf, BhT[T]], [bankB[bk]])
                ACT(fexp[s2][:, :], banks[bk][0:NH, :], AF.Exp, [bankB[bk], Bnegb], [Bfexp[s2]],
                    scale=-1.0, bias=negb[:, 0:1])
                ACT(fexp[s2][:, :], fexp[s2][:, :], AF.Ln, [Bfexp[s2]], [Bfexp[s2]], bias=1.0)
                init = 0.0 if T == 0 else cpt[1 - s2][:, 511:512]
                rds = [Bfexp[s2]] + ([Bcpt[1 - s2]] if T > 0 else [])
                P.op("dve", lambda e, s2=s2, init=init: e.tensor_tensor_scan(
                    out=cpt[s2][:, :], data0=fexp[s2][:, :], data1=fexp[s2][:, :], initial=init,
                    op0=ALU.add, op1=ALU.max), rds, [Bcpt[s2]])
                CP("dve", cpp[s2][:, 0, :], cpt[s2][:, :], [Bcpt[s2]], [Bcpp[s2]])
                TT("dve", r1[s2][:, :], cpt[s2][:, :], cpp[s2][:, 0, :], ALU.subtract, [Bcpt[s2], Bcpp[s2]], [Br1[s2]])
                CP("dve", cpp[s2][:, 1, :], r1[s2][:, :], [Br1[s2]], [Bcpp[s2]])
                TT("dve", r1[s2][:, :], r1[s2][:, :], cpp[s2][:, 1, :], ALU.subtract, [Br1[s2], Bcpp[s2]], [Br1[s2]])
                CP("dve", cpp[s2][:, 2, :], r1[s2][:, :], [Br1[s2]], [Bcpp[s2]])
                P.op("sp", lambda e, s2=s2, T=T: [
                    e.dma_start(out=cparts_d[:, :, T * 512:(T + 1) * 512], in_=cpp[s2][:, :, :]),
                ], [Bcpp[s2]], [Bcparts[T]], dma_key="cpst0", ninc=1)

        ZB = [0, 1]
        AB = [2, 3]
        CSB = 4
        OB = 5

        cnt = {"z": 0, "a": 0, "A": 0, "c": 0, "e": 0}

        def sb_steps(hd, sl):
            steps = []
            ZA = [0, 1]
            CSB_ = 2
            ob = 3
            for j in range(NQ):
                cslot = cnt["c"] % 2
                cnt["c"] += 1
                order = list(range(4 * j + 3, -1, -1))
                for n_, i in enumerate(order):
                    m = i - 4 * j
                    off = max(0, m) * 128
                    diag = m >= 0
                    first = n_ == 0
                    last = i == 0
                    zs = cnt["z"] % 2
                    cnt["z"] += 1
                    As = cnt["A"] % 3
                    cnt["A"] += 1
                    kT = KTa[sl][0:128, i * 128:(i + 1) * 128]
                    qT = QTa[sl][0:128, j * 512 + off:(j + 1) * 512]
                    msk = M0[:, 0:512 - off]
                    W = slice(off, 512)

                    def st0(zs=zs, kT=kT, qT=qT, msk=msk, W=W, diag=diag, first=first, cslot=cslot, off=off):
                        if first:
                            MEMSET("pool", C_sb[cslot][:], 0.0, [BC[cslot]])
                        zb = ZA[zs]
                        MM(banks[zb][:, W], kT, qT, True, not diag, [BK[sl], BQ[sl], BKaug[sl], BQaug[sl]], [bankB[zb]])
                        if diag:
                            MM(banks[zb][:, off:off + 128], negbigI[:], M0[:, 0:128], False, True, [Bc2], [bankB[zb]])
                        ACT(e_sb[zs][:, W], banks[zb][:, W], AF.Exp, [bankB[zb]], [Be[zs]])
                        ACT(sp_bf[zs][:, W], e_sb[zs][:, W], AF.Ln, [Be[zs]], [Bsp[zs]], bias=1.0)

                    def st1(zs=zs, As=As, W=W, first=first, last=last, cslot=cslot):
                        ab = ZA[zs]
                        MM(banks[ab][:, W], negtri[:], sp_bf[zs][:, W], False, True, [Bsp[zs], Bc2], [bankB[ab]],
                           skip_group_check=True)
                        if not last:
                            MM(banks[CSB_][:, W], negones_bf[:], sp_bf[zs][:, W], True, True, [Bsp[zs], Bc], [bankB[CSB_]])
                        if first:
                            ACT(A_bf[As][:, W], banks[ab][:, W], AF.Exp, [bankB[ab]], [BA[As]])
                        else:
                            TT("dve", arg_sb[zs][:, W], banks[ab][:, W], C_sb[cslot][:, W], ALU.add,
                               [bankB[ab], BC[cslot]], [Barg[zs]])
                            ACT(A_bf[As][:, W], arg_sb[zs][:, W], AF.Exp, [Barg[zs]], [BA[As]])
                        if not last:
                            TT("dve", C_sb[cslot][:, W], banks[CSB_][:, W], C_sb[cslot][:, W], ALU.add,
                               [bankB[CSB_], BC[cslot]], [BC[cslot]])
                        for _ in range(FILL_SB):
                            MM(banks[7][:, :], negtri[:], M0[:, 0:512], True, True, [Bc2], [bankB[7]])

                    def st2(As=As, i=i, j=j, W=W, first=first, last=last):
                        MM(banks[ob][0:HD + 1, W], Vt[sl][:, i, 0:HD + 1], A_bf[As][:, W], first, last,
                           [BV[sl], BVones[sl], BA[As]], [bankB[ob]], skip_group_check=True)
                        if last:
                            CP("dve", ostS[0:HD, j * 512:(j + 1) * 512], banks[ob][0:HD, :], [bankB[ob]], [BostS])
                            if j == NQ - 1:
                                DMA("sp", oT_d[hd, :, :], ostS[:, :], [BostS], [BoT_d[hd]], "ostS")
                    steps.append([st0, st1, st2])
            return steps

        def fox_steps(hd, sl):
            steps = []
            SBK = [4, 5]
            ob = 6
            MB = 2
            for j in range(NQ):
                nk = 4 * j + 4
                orot = j % 2
                for i in range(nk):
                    m = i - 4 * j
                    off = max(0, m) * 128
                    diag = m >= 0
                    first = i == 0
                    last = i == nk - 1
                    zs = cnt["fz"] % 2
                    cnt["fz"] += 1
                    As = cnt["fA"] % 3
                    cnt["fA"] += 1
                    kT = KTa[sl][0:70, i * 128:(i + 1) * 128]
                    qT = QTa[sl][0:70, j * 512 + off:(j + 1) * 512]
                    msk = M0[:, 1:1 + 512 - off]
                    W = slice(off, 512)

                    def st0(zs=zs, As=As, kT=kT, qT=qT, msk=msk, W=W, diag=diag, off=off):
                        zb = SBK[zs]
                        MM(banks[zb][:, W], kT, qT, True, not diag, [BK[sl], BQ[sl], BKaug[sl], BQaug[sl]], [bankB[zb]])
                        if diag:
                            MM(banks[zb][:, off:off + 128], negbigI[:], M0[:, 1:129], False, True, [Bc2], [bankB[zb]])

                    def stE(zs=zs, As=As, W=W):
                        zb = SBK[zs]
                        ACT(P_bf[As][:, W], banks[zb][:, W], AF.Exp, [bankB[zb]], [BPb[As]])

                    def st1(As=As, i=i, j=j, W=W, first=first, last=last, orot=orot):
                        MM(banks[ob][0:HD + 1, W], Vt[sl][:, i, 0:HD + 1], P_bf[As][:, W], first, last,
                           [BV[sl], BVones[sl], BPb[As]], [bankB[ob]])
                        if last:
                            CP("dve", oraw[orot][0:HD + 1, :], banks[ob][0:HD + 1, :], [bankB[ob]], [Boraw[orot]])

                    def stN(j=j, orot=orot, last=last):
                        if not last:
                            return
                        ACT(oraw[orot][64:65, :], oraw[orot][64:65, :], AF.Ln, [Boraw[orot]], [Boraw[orot]])
                        ACT(oraw[orot][64:65, :], oraw[orot][64:65, :], AF.Exp, [Boraw[orot]], [Boraw[orot]], scale=-1.0)
                        MM(banks[MB][0:HD, :], ones_f32[64:65, 0:HD], oraw[orot][64:65, :], True, True,
                           [Boraw[orot], Bc], [bankB[MB]])
                        TT("dve", ostF[0:HD, j * 512:(j + 1) * 512], banks[MB][0:HD, :], oraw[orot][0:HD, :], ALU.mult,
                           [bankB[MB], Boraw[orot]], [BostF])
                        if j == NQ - 1:
                            DMA("sp", oT_d[hd, :, :], ostF[:, :], [BostF], [BoT_d[hd]], "ostF")
                    steps.append([st0, stE, st1, stN])
            return steps

        cnt["fz"] = 0
        cnt["fA"] = 0
        HS = 144
        LAG = HS // 2
        seq = []
        for h in range(NH):
            seq += [h, NH + h]
        sched = {}

        def add_bg(it, f):
            sched.setdefault(max(it, 0), []).append(f)

        for n, hd in enumerate(seq):
            sl = n % 3
            chunks = [lambda hd=hd, sl=sl: load_head_weights(hd, sl)]
            if n == 1:
                chunks.append(fox_prep)
            chunks += proj_chunks(hd, sl)
            k1 = len(chunks) // 3
            chunks = chunks[:k1] + [conv_expert(2 * n)] + chunks[k1:2 * k1] + [conv_expert(2 * n + 1)] + chunks[2 * k1:]
            h = n // 2
            start = HS * h if n % 2 == 0 else HS * h + LAG
            base = start - LAG + 5
            nch = len(chunks)
            for ci, f in enumerate(chunks):
                add_bg(base + (ci * (LAG - 8)) // nch, f)
        sb_lists = {}
        fx_lists = {}

        def sb_step(sidx):
            if sidx < 0 or sidx >= HS * NH:
                return None
            h, r = divmod(sidx, HS)
            if h not in sb_lists:
                sb_lists[h] = sb_steps(h, (2 * h) % 3)
                sb_lists.pop(h - 2, None)
            return sb_lists[h][r]

        def fx_step(fidx):
            if fidx < 0 or fidx >= HS * NH:
                return None
            h, r = divmod(fidx, HS)
            if h not in fx_lists:
                fx_lists[h] = fox_steps(NH + h, (2 * h + 1) % 3)
                fx_lists.pop(h - 2, None)
            return fx_lists[h][r]

        def run_stage(stp, k):
            if stp is not None and stp[k] is not None:
                stp[k]()

        for it in sorted(k for k in sched if k <= 0):
            for f in sched.pop(it):
                f()
        total_p = HS * NH + LAG
        for p_ in range(total_p + 4):
            f_ = p_ - LAG
            run_stage(fx_step(f_ - 1), 1)
            run_stage(sb_step(p_), 0)
            run_stage(fx_step(f_), 0)
            run_stage(sb_step(p_ - 1), 1)
            run_stage(fx_step(f_ - 1), 2)
            run_stage(sb_step(p_ - 2), 2)
            run_stage(fx_step(f_ - 2), 3)
            for f in sched.pop(p_, []):
                f()
        for it in sorted(sched):
            for f in sched[it]:
                f()

        P.barrier()
        stk.pop().close()
        stk.append(ExitStack())
        xs = [sb(f"xsb{i}", (128, D), F32) for i in range(NXS)]
        wosb = sb("wosb", (128, 4, D), BF16)
        wofx = sb("wofx", (128, 4, D), BF16)
        wout = sb("wout", (128, 8, D), BF16)
        wg = sb("wg", (128, 8, 2 * D), BF16)
        Bw3 = Buf("w3")
        P.op("pool", lambda e: [
            e.dma_start(out=wosb[:, :, :], in_=w_o_sb_d.rearrange("(c p) n -> p c n", p=128)),
            e.dma_start(out=wofx[:, :, :], in_=w_o_fox_d.rearrange("(c p) n -> p c n", p=128)),
            e.dma_start(out=wout[:, :, :], in_=w_out_d.rearrange("(c p) n -> p c n", p=128)),
        ] + [e.dma_start(out=wg[:, c, :], in_=w_in_d[c * 128:(c + 1) * 128, C_GSB:C_GSB + 2 * D]) for c in range(8)],
            (), [Bw3], dma_key="w3", ninc=11)
        oTt = [sb(f"oTt{i}", (128, 8, 512), BF16) for i in range(2)]
        BoTt = [Buf() for _ in range(2)]
        sig = [sb(f"sig{i}", (128, 512), BF16) for i in range(4)]
        Bsig = [Buf() for _ in range(4)]
        tmp = [sb(f"tmp{i}", (128, 512), F32) for i in range(2)]
        Btmp = [Buf() for _ in range(2)]
        mixT = [sb(f"mixT{i}", (128, 8, 512), BF16) for i in range(2)]
        BmixT = [Buf() for _ in range(2)]
        x2s = [sb(f"x2s{i}", (128, D), F32) for i in range(2)]
        Bx2s = [Buf() for _ in range(2)]
        Bx2d = [Buf(f"x2d{t}") for t in range(NT)]
        Bdbg = [Buf(f"dbg{t}") for t in range(NT)]

        def phase3():
            def load_oT(T):
                s2 = T % 2
                P.op("sp", lambda e: e.dma_start(
                    out=oTt[s2][:, :, :],
                    in_=oT_d[:, :, T * 512:(T + 1) * 512].rearrange("(c two) d t -> (two d) c t", two=2)),
                    BoT_d, [BoTt[s2]], dma_key=f"oTt{s2}")
            load_oT(0)
            for T in range(NQ):
                s2 = T % 2
                if T + 1 < NQ:
                    load_oT(T + 1)
                for dc in range(8):
                    dsl = slice(dc * 128, (dc + 1) * 128)
                    b0 = 0 if dc % 2 == 0 else 4
                    for c in range(4):
                        MM(banks[b0][:, :], wosb[:, c, dsl], oTt[s2][:, c, :], c == 0, c == 3, [Bw3, BoTt[s2]], [bankB[b0]])
                    for c in range(4):
                        MM(banks[b0 + 1][:, :], wofx[:, c, dsl], oTt[s2][:, 4 + c, :], c == 0, c == 3, [Bw3, BoTt[s2]], [bankB[b0 + 1]])
                    for c in range(8):
                        MM(banks[b0 + 2][:, :], wg[:, c, dsl], hT[:, c, T * 512:(T + 1) * 512], c == 0, c == 7,
                           [Bw3, BhT[T]], [bankB[b0 + 2]])
                    for c in range(8):
                        MM(banks[b0 + 3][:, :], wg[:, c, D + dc * 128:D + (dc + 1) * 128], hT[:, c, T * 512:(T + 1) * 512],
                           c == 0, c == 7, [Bw3, BhT[T]], [bankB[b0 + 3]])
                    sa = (dc % 2) * 2
                    ACT(sig[sa][:, :], banks[b0 + 2][:, :], AF.Sigmoid, [bankB[b0 + 2]], [Bsig[sa]])
                    ACT(sig[sa + 1][:, :], banks[b0 + 3][:, :], AF.Sigmoid, [bankB[b0 + 3]], [Bsig[sa + 1]])
                    t2 = dc % 2
                    TT("dve", tmp[t2][:, :], banks[b0][:, :], sig[sa][:, :], ALU.mult, [bankB[b0], Bsig[sa]], [Btmp[t2]])
                    TT("dve", sig[sa + 1][:, :], banks[b0 + 1][:, :], sig[sa + 1][:, :], ALU.mult,
                       [bankB[b0 + 1], Bsig[sa + 1]], [Bsig[sa + 1]])
                    TT("dve", mixT[s2][:, dc, :], tmp[t2][:, :], sig[sa + 1][:, :], ALU.add,
                       [Btmp[t2], Bsig[sa + 1]], [BmixT[s2]])
                for u in range(4):
                    tt = T * 4 + u
                    s3 = tt % NXS
                    xq = tt % 2
                    DMA("sp", xs[s3][:], x_d[tt * 128:(tt + 1) * 128, :], (), [Bxs[s3]], f"xs{s3}")
                    bb = 0 if tt % 2 == 0 else 4
                    for half in range(2):
                        for c in range(8):
                            MM(banks[bb + half][:, :], mixT[s2][:, c, u * 128:(u + 1) * 128], wout[:, c, half * 512:(half + 1) * 512],
                               c == 0, c == 7, [BmixT[s2], Bw3], [bankB[bb + half]])
                    for half in range(2):
                        TT("dve", x2s[xq][:, half * 512:(half + 1) * 512], banks[bb + half][:, :],
                           xs[s3][:, half * 512:(half + 1) * 512], ALU.add, [bankB[bb + half], Bxs[s3]], [Bx2s[xq]])
                    DMA("act", x2_d[tt * 128:(tt + 1) * 128, :], x2s[xq][:], [Bx2s[xq]], [Bx2d[tt]], f"x2st{xq}")
                    if debug and upto is None:
                        DMA("sp", dbg_d[tt * 128:(tt + 1) * 128, :], x2s[xq][:], [Bx2s[xq]], [Bdbg[tt]], f"dbgst{xq}")

        phase3()

        P.barrier()
        stk.pop().close()
        stk.append(ExitStack())
        BIGR = 1.0e4
        xs = [sb(f"xsc{i}", (128, D), F32) for i in range(NXS)]
        g2 = sb("g2", (128, D), F32)
        Bg2 = Buf("g2")
        DMA("sp", g2[:], norm_ffn_d[0:1, :].partition_broadcast(128), (), [Bg2], "g2")
        Bg3 = Buf("g3")
        DMA("sp", g3[:], norm_final_d[0:1, :].partition_broadcast(128), (), [Bg3], "g3")
        wr = sb("wr", (128, 8, 36), F32)
        rbias = sb("rbias", (128, 36), F32)
        Bwr = Buf("wr")
        P.op("sp", lambda e: [
            e.dma_start(out=wr[:, :, 0:4], in_=w_rg_d.rearrange("(c p) n -> p c n", p=128)),
            e.dma_start(out=wr[:, :, 4:36], in_=w_re_d.rearrange("(c p) n -> p c n", p=128)),
            e.dma_start(out=rbias[:, 0:4], in_=b_rg_d[0:1, :].partition_broadcast(128)),
            e.dma_start(out=rbias[:, 4:36], in_=b_re_d[0:1, :].partition_broadcast(128)),
        ], (), [Bwr], dma_key="wr", ninc=4)
        h2b = hT[:].rearrange("p c t -> p (c t)")
        Bh2b = [Buf(f"h2b{t}") for t in range(NT)]
        BM = [Buf(f"M{t}") for t in range(NT)]
        BRk = [Buf(f"Rk{t}") for t in range(NT)]
        BWg = [Buf(f"Wg{t}") for t in range(NT)]
        h2f = [sb(f"h2f{i}", (128, D), F32) for i in range(2)]
        Bh2f = [Buf() for _ in range(2)]
        h2Tf = [sb(f"h2Tf{i}", (128, 8, 128), F32) for i in range(2)]
        Bh2Tf = [Buf() for _ in range(2)]
        rg = [sb(f"rg{i}", (128, 640), F32) for i in range(2)]
        Brg = [Buf() for _ in range(2)]
        msel4 = [sb(f"msel4_{i}", (128, 4, NE), BF16) for i in range(2)]
        msel = [sb(f"msel{i}", (128, NE), BF16) for i in range(2)]
        Bmsel = [Buf() for _ in range(2)]
        mcum = [sb(f"mcum{i}", (128, NE), BF16) for i in range(2)]
        Bmcum = [Buf() for _ in range(2)]

        def phase3b():
            steps = []
            for tt in range(NT):
                steps.append([lambda tt=tt: p3b_front(tt), lambda tt=tt: p3b_mid(tt),
                              (lambda tt=tt: p3b_chain(tt // 4)) if tt % 4 == 3 else None])
            run_pipeline(steps, None)

        def p3b_front(tt):
            if True:
                s3 = tt % NXS
                s2 = tt % 2
                DMA("sp", xs[s3][:], x2_d[tt * 128:(tt + 1) * 128, :], [Bx2d[tt]], [Bxs[s3]], f"xs{s3}")
                rms_rstd(xs[s3][:], junk[:], stat[s2][:, 0:1], stat[s2][:, 1:2], stat[s2][:, 2:3],
                         [Bxs[s3]], Bjunk, Bstat[s2])
                STT(h2f[s2][:], xs[s3][:], stat[s2][:, 2:3], g2[:], ALU.mult, ALU.mult,
                    [Bxs[s3], Bstat[s2], Bg2], [Bh2f[s2]])
                CP("pool", h2b[:, tt * D:(tt + 1) * D], h2f[s2][:, :], [Bh2f[s2]], [Bh2b[tt]])

        def p3b_mid(tt):
            if True:
                s2 = tt % 2
                bA = 0 if s2 == 0 else 4
                for c in range(8):
                    bk = bA + c // 4
                    TR(banks[bk][:, (c % 4) * 128:(c % 4 + 1) * 128], h2f[s2][:, c * 128:(c + 1) * 128], ident_f32[:],
                       [Bh2f[s2], Bc2], [bankB[bk]])
                for hh in range(2):
                    bk = bA + hh
                    CP("act", h2Tf[s2][:, hh * 4:(hh + 1) * 4, :], banks[bk][:, :].rearrange("p (c t) -> p c t", c=4),
                       [bankB[bk]], [Bh2Tf[s2]])
                bR = bA + 2
                for c in range(8):
                    MM(banks[bR][:, 0:36], h2Tf[s2][:, c, :], wr[:, c, :], c == 0, c == 7, [Bh2Tf[s2], Bwr], [bankB[bR]])
                gp = (tt // 4) % 2
                g_ = tt % 4
                TT("dve", rg[gp][:, g_ * 36:(g_ + 1) * 36], banks[bR][:, 0:36], rbias[:, :], ALU.add,
                   [bankB[bR], Bwr], [Brg[gp]])

        def p3b_chain(grp):
            G = 4
            gp = grp % 2
            r = rg[gp]
            B_ = [Brg[gp]]
            t0_ = grp * G
            X = mybir.AxisListType.X

            def v(lo, n, *dims):
                ap = r[:, lo:lo + n]
                if len(dims) == 2:
                    return ap.rearrange("p (a b) -> p a b", a=dims[0])
                if len(dims) == 3:
                    return ap.rearrange("p (a b c) -> p a b c", a=dims[0], b=dims[1])
                return ap
            lg = v(0, G * 36, G, 36)
            gl = lg[:, :, 0:4]
            el = lg[:, :, 4:36]
            gmax = v(144, G)
            gmask = v(148, G * 4, G, 4)
            gd = v(164, G * 4, G, 4)
            gsum = v(180, G)
            gw = v(184, G)
            pen = v(188, G * 4, G, 4)
            elm = v(204, G * NE, G, NE)
            elm4 = v(204, G * NE, G, 4, 8)
            m1 = v(332, G)
            mask1 = v(336, G * NE, G, NE)
            m2 = v(464, G)
            mask2 = v(468, G * NE, G, NE)
            dd = v(596, G)
            ee = v(600, G)
            w1 = v(604, G)
            w2 = v(608, G)

            def bc(ap, shape):
                return ap.unsqueeze(len(ap.shape)).broadcast_to(shape)
            P.op("dve", lambda e: e.tensor_reduce(out=gmax, in_=gl, axis=X, op=ALU.max), B_, B_)
            TT("dve", gmask, gl, bc(gmax, [128, G, 4]), ALU.is_equal, B_, B_)
            TT("dve", gd, gl, bc(gmax, [128, G, 4]), ALU.subtract, B_, B_)
            ACT(gd, gd, AF.Exp, B_, B_)
            P.op("dve", lambda e: e.tensor_reduce(out=gsum, in_=gd, axis=X, op=ALU.add), B_, B_)
            P.op("dve", lambda e: e.reciprocal(out=gw, in_=gsum), B_, B_)
            TS("dve", pen, gmask, BIGR, -BIGR, ALU.mult, ALU.add, B_, B_)
            TT("dve", elm4, el.rearrange("p g (a b) -> p g a b", a=4), bc(pen, [128, G, 4, 8]), ALU.add, B_, B_)
            P.op("dve", lambda e: e.tensor_reduce(out=m1, in_=elm, axis=X, op=ALU.max), B_, B_)
            TT("dve", mask1, elm, bc(m1, [128, G, NE]), ALU.is_equal, B_, B_)
            STT(elm, mask1, -3.0 * BIGR, elm, ALU.mult, ALU.add, B_, B_)
            P.op("dve", lambda e: e.tensor_reduce(out=m2, in_=elm, axis=X, op=ALU.max), B_, B_)
            TT("dve", mask2, elm, bc(m2, [128, G, NE]), ALU.is_equal, B_, B_)
            TT("dve", dd, m1, m2, ALU.subtract, B_, B_)
            ACT(ee, dd, AF.Exp, B_, B_, scale=-1.0)
            TS("dve", w1, ee, 1.0, None, ALU.add, None, B_, B_)
            P.op("dve", lambda e: e.reciprocal(out=w1, in_=w1), B_, B_)
            TT("dve", w2, ee, w1, ALU.mult, B_, B_)
            BWgs = [BWg[t0_ + g] for g in range(G)]
            BMs = [BM[t0_ + g] for g in range(G)]
            TT("dve", Wg[:, t0_:t0_ + G, 0], w1, gw, ALU.mult, B_, BWgs)
            TT("dve", Wg[:, t0_:t0_ + G, 1], w2, gw, ALU.mult, B_, BWgs)
            CP("dve", M1[:, t0_:t0_ + G, :], mask1, B_, BMs)
            CP("dve", M2[:, t0_:t0_ + G, :], mask2, B_, BMs)
            TT("dve", msel4[gp][:, :, :], mask1, mask2, ALU.add, B_, [Bmsel[gp]])
            bK = 3 if gp == 0 else 7
            for g in range(G):
                tt = t0_ + g
                s2 = tt % 2
                if tt == 0:
                    CP("dve", mcum[s2][:, :], msel4[gp][:, g, :], [Bmsel[gp]], [Bmcum[s2]])
                else:
                    TT("dve", mcum[s2][:, :], mcum[1 - s2][:, :], msel4[gp][:, g, :], ALU.add,
                       [Bmcum[1 - s2], Bmsel[gp]], [Bmcum[s2]])
                MM(banks[bK][:, g * NE:(g + 1) * NE], SL_bf[:], msel4[gp][:, g, :], True, tt == 0, [Bmsel[gp], Bc2], [bankB[bK]])
                if tt > 0:
                    MM(banks[bK][:, g * NE:(g + 1) * NE], ones_bf[:], mcum[1 - s2][:, :], False, True,
                       [Bmcum[1 - s2], Bc], [bankB[bK]])
            CP("dve", Rk[:, t0_:t0_ + G, :], banks[bK][:, 0:G * NE].rearrange("p (g e) -> p g e", g=G),
               [bankB[bK]], [BRk[t0_ + g] for g in range(G)])

        phase3b()
        if upto == "3b":
            DMA("sp", dbg_d[0:128, :], Rk[:, :, :].rearrange("p t e -> p (t e)"), BRk, [Bdbg[0]], "dbgcmb")

        cnt = sb("cnt", (128, NE), F32)
        cnti = sb("cnti", (128, NE), I32)
        pcf = sb("pcf", (128, NE), F32)
        cend = sb("cend", (128, NE), F32)
        offs = sb("offs", (128, NE), F32)
        sstart_i = sb("sstart_i", (128, NSLOT), I32)
        sstart = sb("sstart", (128, NSLOT), F32)
        eidf = sb("eidf", (128, NSLOT), F32)
        pidx_i = sb("pidx_i", (128, 1), I32)
        pidx = sb("pidx", (128, 1), F32)
        posall = sb("posall", (128, NT * NE), F32)
        pmall = sb("pmall", (128, NT * NE), F32)
        pfall = sb("pfall", (128, 2, NT), F32)
        Bpb = Buf("passB")
        Bpos = [Buf() for _ in range(2)]
        BIDX = [Buf(f"IDX{t}") for t in range(NT)]
        BWI = Buf("WI")
        BXs = Buf("Xs_d")
        BXs_t = [Buf(f"Xs_t{t}") for t in range(NT)]

        def passB():
            lastm = (NT - 1) % 2
            MM(banks[0][:, 0:NE], ones_bf[:], mcum[lastm][:, :], True, True, [Bmcum[lastm], Bc], [bankB[0]])
            B_ = [Bpb]
            TS("dve", cnti[:, :], banks[0][:, 0:NE], float(SLOTR - 1), None, ALU.add, None, [bankB[0]], B_)
            TS("dve", cnti[:, :], cnti[:, :], SHIFT, None, ALU.logical_shift_right, None, B_, B_)
            TS("dve", cnti[:, :], cnti[:, :], SHIFT, None, ALU.logical_shift_left, None, B_, B_)
            CP("dve", pcf[:, :], cnti[:, :], B_, B_)
            P.op("dve", lambda e: e.tensor_tensor_scan(out=cend[:, :], data0=pcf[:, :], data1=pcf[:, :], initial=0.0,
                                                       op0=ALU.add, op1=ALU.max), B_, B_)
            TT("dve", offs[:, :], cend[:, :], pcf[:, :], ALU.subtract, B_, B_)
            P.op("pool", lambda e: e.iota(sstart_i[:, :], pattern=[[SLOTR, NSLOT]], base=0, channel_multiplier=0), (), B_)
            P.op("pool", lambda e: e.iota(pidx_i[:, :], pattern=[[0, 1]], base=0, channel_multiplier=1), (), B_)
            CP("dve", sstart[:, :], sstart_i[:, :], B_, B_)
            CP("dve", pidx[:, :], pidx_i[:, :], B_, B_)
            for ex in range(NE):
                if ex == 0:
                    TS("dve", eidf[:, :], sstart[:, :], cend[:, 0:1], None, ALU.is_ge, None, B_, B_)
                else:
                    STT(eidf[:, :], sstart[:, :], cend[:, ex:ex + 1], eidf[:, :], ALU.is_ge, ALU.add, B_, B_)
            TS("dve", eidf[:, :], eidf[:, :], float(NE - 1), 128.0, ALU.min, ALU.mult, B_, B_)
            TS("dve", WI[:, :], eidf[:, :], pidx[:, 0:1], None, ALU.add, None, B_, [BWI])
            Bp = [Bpos[0]]
            pos3 = posall[:, :].rearrange("p (t e) -> p t e", e=NE)
            pm3 = pmall[:, :].rearrange("p (t e) -> p t e", e=NE)
            TT("dve", pos3, Rk[:, :, :], offs[:, :].unsqueeze(1).broadcast_to([128, NT, NE]), ALU.add, BRk + [Bpb], Bp)
            TT("dve", pm3, pos3, M1[:, :, :], ALU.mult, Bp + BM, Bp)
            P.op("dve", lambda e: e.tensor_reduce(out=pfall[:, 0, :], in_=pm3, axis=mybir.AxisListType.X, op=ALU.add), Bp, Bp)
            TT("dve", pm3, pos3, M2[:, :, :], ALU.mult, Bp + BM, Bp)
            P.op("dve", lambda e: e.tensor_reduce(out=pfall[:, 1, :], in_=pm3, axis=mybir.AxisListType.X, op=ALU.add), Bp, Bp)
            CP("dve", IDX[:, :, :].rearrange("p t c -> p c t"), pfall[:, :, :], Bp, BIDX)
            for tt in range(NT):
                for ch in range(2):
                    P.op("pool", lambda e, tt=tt, ch=ch: e.indirect_dma_start(
                        out=Xs_d[:, :], out_offset=bass.IndirectOffsetOnAxis(ap=IDX[:, tt, ch:ch + 1], axis=0),
                        in_=h2b[:, tt * D:(tt + 1) * D], in_offset=None),
                        [BIDX[tt], Bh2b[tt]], [BXs_t[tt]] if ch else [BXs], dma_key="scat")

        passB()
        if upto == "pb":
            DMA("sp", dbg_d[0:128, 0:64], IDX[:, :, :].rearrange("p t c -> p (t c)").bitcast(F32), BIDX, [Bdbg[0]], "dbgidx")
            DMA("sp", dbg_d[128:256, 0:NSLOT], WI[:, :].bitcast(F32), [BWI], [Bdbg[1]], "dbgwi")
            DMA("sp", dbg_d[256:384, 0:64], Wg[:, :, :].rearrange("p t c -> p (t c)"), BWg, [Bdbg[2]], "dbgwg")

        P.barrier()
        stk.pop().close()
        stk.append(ExitStack())
        NWS = 4
        wsl = [sb(f"wsl{i}", (128, WROW), BF16) for i in range(NWS)]
        Bwsl = [Buf() for _ in range(NWS)]
        xsl = [sb(f"xsl{i}", (128, NSUB, D), BF16) for i in range(2)]
        Bxsl = [Buf() for _ in range(2)]
        XsT = [sb(f"XsT{i}", (128, 8, SLOTR), BF16) for i in range(2)]
        BXsT = [[Buf(), Buf()] for _ in range(2)]
        silb = [sb(f"silb{i}", (128, SLOTR), BF16) for i in range(4)]
        Bsilb = [Buf() for _ in range(4)]
        hidT = [sb(f"hidT{i}", (128, 2, SLOTR), BF16) for i in range(2)]
        BhidT = [Buf() for _ in range(2)]
        ysb = [sb(f"ysb{i}", (128, D), F32) for i in range(3)]
        Bysb = [[Buf(), Buf()] for _ in range(3)]
        BYs = [Buf(f"Ys{i}") for i in range(NSLOT)]
        Bout = [Buf(f"out{t}") for t in range(NT)]

        def slot_loop():
            yrot = [0]
            steps = []
            for i in range(NSLOT):
                ws = i % NWS
                r2 = i % 2

                def stL(i=i, ws=ws, r2=r2):
                    P.op("pool", lambda e: e.indirect_dma_start(
                        out=wsl[ws][:, :], out_offset=None, in_=Wb_d[:, :],
                        in_offset=bass.IndirectOffsetOnAxis(ap=WI[:, i:i + 1], axis=0)),
                        [BWI] + BWb, [Bwsl[ws]], dma_key=f"wsl{ws}")
                    DMA("sp", xsl[r2][:, :, :], Xs_d[i * SLOTR:(i + 1) * SLOTR, :].rearrange("(s p) d -> p s d", p=128),
                        [BXs] + BXs_t, [Bxsl[r2]], f"xsl{r2}")

                def stT(i=i, ws=ws, r2=r2):
                    for hb_ in range(2):
                        bk = hb_
                        tv = banks[bk][:].bitcast(BF16)
                        for cc in range(4):
                            c = hb_ * 4 + cc
                            for sub in range(NSUB):
                                TR(tv[:, cc * SLOTR + sub * 128:cc * SLOTR + (sub + 1) * 128], xsl[r2][:, sub, c * 128:(c + 1) * 128],
                                   ident_bf[:], [Bxsl[r2], Bc2], [bankB[bk]])
                        CP("act" if hb_ == 0 else "dve", XsT[r2][:, hb_ * 4:hb_ * 4 + 4, :],
                           tv[:, :].rearrange("p (c t) -> p c t", c=4), [bankB[bk]], [BXsT[r2][hb_]])

                def stAB(i=i, ws=ws, r2=r2):
                    for m in range(2):
                        ba = 2 + 2 * r2 + m
                        sb_ = 2 * r2 + m
                        for c in range(8):
                            MM(banks[ba][:, 0:SLOTR], wsl[ws][:, c * DE + m * 128:c * DE + (m + 1) * 128], XsT[r2][:, c, :], c == 0, c == 7,
                               [Bwsl[ws]] + BXsT[r2], [bankB[ba]])
                        for c in range(8):
                            MM(banks[ba][:, SLOTR:2 * SLOTR], wsl[ws][:, 8 * DE + c * DE + m * 128:8 * DE + c * DE + (m + 1) * 128],
                               XsT[r2][:, c, :], c == 0, c == 7, [Bwsl[ws]] + BXsT[r2], [bankB[ba]])
                        ACT(silb[sb_][:, :], banks[ba][:, 0:SLOTR], AF.Silu, [bankB[ba]], [Bsilb[sb_]])
                        TT("dve", hidT[r2][:, m, :], banks[ba][:, SLOTR:2 * SLOTR], silb[sb_][:, :], ALU.mult,
                           [bankB[ba], Bsilb[sb_]], [BhidT[r2]])

                def stY(i=i, ws=ws, r2=r2):
                    for sub in range(NSUB):
                        yr = yrot[0] % 3
                        yrot[0] += 1
                        for half in range(2):
                            by = 6 + half
                            for m in range(2):
                                MM(banks[by][:, :], hidT[r2][:, m, sub * 128:(sub + 1) * 128],
                                   wsl[ws][:, 16 * DE + m * D + half * 512:16 * DE + m * D + (half + 1) * 512],
                                   m == 0, m == 1, [BhidT[r2], Bwsl[ws]], [bankB[by]])
                            CP("act" if half == 0 else "dve", ysb[yr][:, half * 512:(half + 1) * 512], banks[by][:, :],
                               [bankB[by]], [Bysb[yr][half]])
                        DMA("act", Ys_d[i * SLOTR + sub * 128:i * SLOTR + (sub + 1) * 128, :], ysb[yr][:, :], Bysb[yr], [BYs[i]],
                            f"yst{yr}")
                steps.append([stL, stT, stAB, stY])
            run_pipeline(steps, None)

        def combine():
            def load(tt):
                s3 = tt % NXS
                s2 = tt % 3
                DMA("sp", xs[s3][:], x2_d[tt * 128:(tt + 1) * 128, :], [Bx2d[tt]], [Bxs[s3]], f"xs{s3}")
                P.op("pool", lambda e: e.indirect_dma_start(
                    out=y1[s2][:, :], out_offset=None, in_=Ys_d[:, :],
                    in_offset=bass.IndirectOffsetOnAxis(ap=IDX[:, tt, 0:1], axis=0)),
                    [BIDX[tt]] + BYs, [By1[s2]], dma_key=f"g1_{s2}")
                P.op("pool", lambda e: e.indirect_dma_start(
                    out=y2[s2][:, :], out_offset=None, in_=Ys_d[:, :],
                    in_offset=bass.IndirectOffsetOnAxis(ap=IDX[:, tt, 1:2], axis=0)),
                    [BIDX[tt]] + BYs, [By2[s2]], dma_key=f"g2_{s2}")
            load(0)
            for tt in range(NT):
                s3 = tt % NXS
                s2 = tt % 3
                STT(x3[s2][:, :], y1[s2][:, :], Wg[:, tt, 0:1], xs[s3][:, :], ALU.mult, ALU.add,
                    [By1[s2], BWg[tt], Bxs[s3]], [Bx3[s2]])
                if tt + 1 < NT:
                    load(tt + 1)
                STT(x3[s2][:, :], y2[s2][:, :], Wg[:, tt, 1:2], x3[s2][:, :], ALU.mult, ALU.add,
                    [By2[s2], BWg[tt], Bx3[s2]], [Bx3[s2]])
                rms_rstd(x3[s2][:], junk[:], stat[tt % 2][:, 0:1], stat[tt % 2][:, 1:2], stat[tt % 2][:, 2:3],
                         [Bx3[s2]], Bjunk, Bstat[tt % 2])
                STT(x3[s2][:], x3[s2][:], stat[tt % 2][:, 2:3], g3[:], ALU.mult, ALU.mult,
                    [Bx3[s2], Bstat[tt % 2], Bg3], [Bx3[s2]])
                DMA("act", out_d[tt * 128:(tt + 1) * 128, :], x3[s2][:], [Bx3[s2]], [Bout[tt]], f"outst{s2}")

        if upto not in ("3b", "pb"):
            slot_loop()
        P.barrier()
        stk.pop().close()
        stk.append(ExitStack())
        xs = [sb(f"xsd{i}", (128, D), F32) for i in range(NXS)]
        y1 = [sb(f"y1_{i}", (128, D), F32) for i in range(3)]
        y2 = [sb(f"y2_{i}", (128, D), F32) for i in range(3)]
        By1 = [Buf() for _ in range(3)]
        By2 = [Buf() for _ in range(3)]
        x3 = [sb(f"x3_{i}", (128, D), F32) for i in range(3)]
        Bx3 = [Buf() for _ in range(3)]
        if upto not in ("3b", "pb"):
            combine()

        P.op("sp", None, reads=(Bout if upto is None else []) + (Bdbg if debug else []))
        P.barrier()
        stk.pop().close()
        P.emit()
    return nc


_NC_CACHE = {}


def kernel(x, norm_attn, w_in, b_forget, w_o_sb, w_o_fox, w_out, norm_ffn,
           w_router_group, b_router_group, w_router_expert, b_router_expert,
           w1, w3, w2, norm_final, _debug=False, _upto=None, _cores=8):
    f32 = lambda a: np.ascontiguousarray(np.asarray(a, dtype=np.float32))
    nc = build_program(debug=_debug, upto=_upto)
    shared = {
        "norm_attn": f32(norm_attn).reshape(1, D), "w_in": f32(w_in)[0], "b_forget": f32(b_forget).reshape(1, NH),
        "w_o_sb": f32(w_o_sb)[0], "w_o_fox": f32(w_o_fox)[0], "w_out": f32(w_out)[0],
        "norm_ffn": f32(norm_ffn).reshape(1, D), "w_router_group": f32(w_router_group)[0],
        "b_router_group": f32(b_router_group).reshape(1, 4), "w_router_expert": f32(w_router_expert)[0],
        "b_router_expert": f32(b_router_expert).reshape(1, NE), "w1": f32(w1)[0], "w3": f32(w3)[0], "w2": f32(w2)[0],
        "norm_final": f32(norm_final).reshape(1, D),
    }
    xf = f32(x)
    in_maps = [dict(shared, x=xf[b]) for b in range(_cores)]
    res = run_bass_kernel_spmd(nc, in_maps, core_ids=list(range(_cores)))
    if _debug:
        return (np.stack([res.results[b]["out"] for b in range(_cores)], axis=0),
                np.stack([res.results[b]["dbg"] for b in range(_cores)], axis=0))
    return np.stack([res.results[b]["out"] for b in range(_cores)], axis=0)
```

```python
from contextlib import ExitStack
import numpy as np
import concourse.bass as bass
import concourse.mybir as mybir
from concourse.bass_utils import run_bass_kernel_spmd

F32 = mybir.dt.float32
BF16 = mybir.dt.bfloat16
I32 = mybir.dt.int32
AF = mybir.ActivationFunctionType
ALU = mybir.AluOpType

S = 4096
D = 1024
NT = S // 128
NQ = S // 512
HD = 64
NH = 8
INC = 5128
C_QSB, C_KSB, C_VSB, C_QFX, C_KFX, C_VFX, C_F, C_GSB, C_GFX = 0, 512, 1024, 1536, 2048, 2560, 3072, 3080, 4104
NE = 32
DE = 256
EPS = 1e-6
NEGBIG = -30000.0
FOX_DUMMY = 1
FILL_SB = 0
FILL_FX = 0

ENGS = ("pe", "act", "dve", "pool", "sp")
GEN = 30000


class Buf:
    __slots__ = ("name", "writer", "readers", "excl")

    def __init__(self, name="", excl=False):
        self.name = name
        self.writer = None
        self.readers = []
        self.excl = excl


class Op:
    __slots__ = ("eng", "fn", "idx", "deps", "signal", "sig", "dma", "dkey", "dval")

    def __init__(self, eng, fn, idx, dma):
        self.eng = eng
        self.fn = fn
        self.idx = idx
        self.deps = []
        self.signal = False
        self.sig = None
        self.dma = dma
        self.dkey = None
        self.dval = None


class Prog:
    def __init__(self, nc):
        self.nc = nc
        self.ops = {e: [] for e in ENGS}
        self.known = {e: {f: -1 for f in ENGS} for e in ENGS}
        self.known_dma = {e: {} for e in ENGS}
        self.dma_cnt = {}
        self.last_dma = {}
        self.pending = {e: [] for e in ENGS}

    def barrier(self):
        lasts = []
        for e in ENGS:
            for o in reversed(self.ops[e]):
                if not o.dma and o.fn is not None:
                    lasts.append(o)
                    break
        lasts += list(self.last_dma.values())
        for e in ENGS:
            self.pending[e] = list(lasts)

    def op(self, eng, fn, reads=(), writes=(), dma_key=None, ninc=1):
        o = Op(eng, fn, len(self.ops[eng]), dma_key is not None)
        cand = []
        for b in reads:
            if b.writer is not None:
                cand.append((b.writer, "raw"))
            if b.excl:
                for r in b.readers:
                    if r.eng != eng:
                        cand.append((r, "rar"))
        for b in writes:
            if b.writer is not None:
                cand.append((b.writer, "waw"))
            for r in b.readers:
                cand.append((r, "war"))
        best = {}
        dma_deps = {}
        if self.pending[eng]:
            for p in self.pending[eng]:
                if p.dma or p.eng != eng:
                    cand.append((p, "raw"))
            self.pending[eng] = []
        for (p, kind) in cand:
            if p is o:
                continue
            if p.dma:
                if self.known_dma[eng].get(p.dkey, 0) >= p.dval:
                    continue
                if p.dkey not in dma_deps or dma_deps[p.dkey].dval < p.dval:
                    dma_deps[p.dkey] = p
                continue
            if p.eng == eng:
                if eng == "pe":
                    continue
            if self.known[eng][p.eng] >= p.idx:
                continue
            if p.eng not in best or best[p.eng].idx < p.idx:
                best[p.eng] = p
        for k, p in dma_deps.items():
            o.deps.append(p)
            self.known_dma[eng][k] = p.dval
        for f, p in best.items():
            o.deps.append(p)
            p.signal = True
            self.known[eng][f] = p.idx
        if o.dma:
            o.dkey = dma_key
            self.dma_cnt[dma_key] = self.dma_cnt.get(dma_key, 0) + 16 * ninc
            o.dval = self.dma_cnt[dma_key]
            self.last_dma[dma_key] = o
        for b in reads:
            b.readers.append(o)
        for b in writes:
            b.writer = o
            b.readers = []
        self.ops[eng].append(o)
        return o

    def emit(self):
        nc = self.nc
        with ExitStack() as st:
            sems = {}
            for e in ENGS:
                n = 0
                for o in self.ops[e]:
                    if o.signal and not o.dma:
                        o.sig = n
                        n += 1
                ngen = max(1, (n + GEN - 1) // GEN)
                sems[e] = [st.enter_context(nc.semaphore(f"s_{e}_{g}")) for g in range(ngen)]
            dsem = {}
            for k in self.dma_cnt:
                dsem[k] = st.enter_context(nc.semaphore(f"d_{len(dsem)}"))
            block = st.enter_context(nc.Block())
            handles = {"pe": block.tensor, "act": block.scalar, "dve": block.vector,
                       "pool": block.gpsimd, "sp": block.sync}

            def run(e):
                ops = self.ops[e]
                if not ops:
                    return

                def body(eng):
                    for o in ops:
                        for p in o.deps:
                            if p.dma:
                                eng.wait_ge(dsem[p.dkey], p.dval)
                            else:
                                eng.wait_ge(sems[p.eng][p.sig // GEN], p.sig % GEN + 1)
                        if o.fn is None:
                            continue
                        ins = o.fn(eng)
                        if o.dma:
                            if not isinstance(ins, (list, tuple)):
                                ins = [ins]
                            for i_ in ins:
                                i_.then_inc(dsem[o.dkey], 16)
                        elif o.signal:
                            ins.then_inc(sems[e][o.sig // GEN], 1)
                handles[e](body)
            for e in ENGS:
                run(e)


def build_program(debug=False, upto=None):
    nc = bass.Bass("TRN2", target_bir_lowering=False)
    dram_in = lambda n, s: nc.dram_tensor(n, list(s), F32, kind="ExternalInput").ap()
    x_d = dram_in("x", (S, D))
    norm_attn_d = dram_in("norm_attn", (1, D))
    w_in_d = dram_in("w_in", (D, INC))
    b_forget_d = dram_in("b_forget", (1, NH))
    w_o_sb_d = dram_in("w_o_sb", (512, D))
    w_o_fox_d = dram_in("w_o_fox", (512, D))
    w_out_d = dram_in("w_out", (D, D))
    norm_ffn_d = dram_in("norm_ffn", (1, D))
    w_rg_d = dram_in("w_router_group", (D, 4))
    b_rg_d = dram_in("b_router_group", (1, 4))
    w_re_d = dram_in("w_router_expert", (D, NE))
    b_re_d = dram_in("b_router_expert", (1, NE))
    w1_d = dram_in("w1", (NE, D, DE))
    w3_d = dram_in("w3", (NE, D, DE))
    w2_d = dram_in("w2", (NE, DE, D))
    norm_final_d = dram_in("norm_final", (1, D))
    out_d = nc.dram_tensor("out", [S, D], F32, kind="ExternalOutput").ap()
    oT_d = nc.dram_tensor("oT_scr", [16, HD, S], BF16, kind="Internal").ap()
    x2_d = nc.dram_tensor("x2_scr", [S, D], F32, kind="Internal").ap()
    NSLOT = 64
    SLOTR = 256
    NSUB = SLOTR // 128
    SHIFT = 8
    WROW = 2 * 8 * DE + 2 * D
    Wb_d = nc.dram_tensor("Wb_scr", [NE * 128, WROW], BF16, kind="Internal").ap()
    Xs_d = nc.dram_tensor("Xs_scr", [NSLOT * SLOTR, D], BF16, kind="Internal").ap()
    Ys_d = nc.dram_tensor("Ys_scr", [NSLOT * SLOTR, D], F32, kind="Internal").ap()
    cparts_d = nc.dram_tensor("cparts_scr", [NH, 3, S], BF16, kind="Internal").ap()
    ncparts_d = nc.dram_tensor("ncparts_scr", [NH, 3, S], BF16, kind="Internal").ap()
    dbg_d = None
    if debug:
        dbg_d = nc.dram_tensor("dbg", [S, D], F32, kind="ExternalOutput").ap()

    P = Prog(nc)
    with ExitStack() as st:
        stk = [st]

        def sb(name, shape, dt):
            return stk[-1].enter_context(nc.sbuf_tensor(name, list(shape), dt))

        banks = [st.enter_context(nc.psum_tensor(f"bank{i}", [128, 512], F32)) for i in range(8)]
        bankB = [Buf(f"bank{i}", excl=True) for i in range(8)]

        def MM(out, lhsT, rhs, start, stop, reads, writes, **kw):
            return P.op("pe", lambda e: e.matmul(out, lhsT=lhsT, rhs=rhs, start=start, stop=stop, **kw), reads, writes)

        def TR(out, in_, ident, reads, writes):
            return P.op("pe", lambda e: e.transpose(out=out, in_=in_, identity=ident), reads, writes)

        def ACT(out, in_, func, reads, writes, **kw):
            return P.op("act", lambda e: e.activation(out=out, in_=in_, func=func, **kw), reads, writes)

        def TT(eng, out, in0, in1, op, reads, writes):
            return P.op(eng, lambda e: e.tensor_tensor(out=out, in0=in0, in1=in1, op=op), reads, writes)

        def TS(eng, out, in0, s1, s2, op0, op1, reads, writes, **kw):
            if op1 is None:
                return P.op(eng, lambda e: e.tensor_scalar(out=out, in0=in0, scalar1=s1, scalar2=None, op0=op0, **kw), reads, writes)
            return P.op(eng, lambda e: e.tensor_scalar(out=out, in0=in0, scalar1=s1, scalar2=s2, op0=op0, op1=op1, **kw), reads, writes)

        def STT(out, in0, scalar, in1, op0, op1, reads, writes):
            return P.op("dve", lambda e: e.scalar_tensor_tensor(out=out, in0=in0, scalar=scalar, in1=in1, op0=op0, op1=op1), reads, writes)

        def CP(eng, out, in_, reads, writes):
            if eng == "act":
                return P.op("act", lambda e: e.copy(out=out, in_=in_), reads, writes)
            return P.op(eng, lambda e: e.tensor_copy(out=out, in_=in_), reads, writes)

        def MEMSET(eng, ap, val, writes):
            return P.op(eng, lambda e: e.memset(ap, val), (), writes)

        def DMA(q, out, in_, reads, writes, key, **kw):
            return P.op(q, lambda e: e.dma_start(out=out, in_=in_, **kw), reads, writes, dma_key=key)

        ones_bf = sb("ones_bf", (128, 128), BF16)
        negones_bf = sb("negones_bf", (128, 128), BF16)
        negbig_bf = sb("negbig_bf", (128, 128), BF16)
        ident_bf = sb("ident_bf", (128, 128), BF16)
        negbigI = sb("negbigI", (128, 128), BF16)
        negtri = sb("negtri", (128, 128), BF16)
        ones513 = sb("ones513", (128, 513), BF16)
        M0 = sb("M0", (128, 513), BF16)
        ones_f32 = sb("ones_f32", (128, 128), F32)
        ident_f32 = sb("ident_f32", (128, 128), F32)
        eps_t = sb("eps_t", (128, 1), F32)
        Bc = Buf("consts")
        Bg1 = Buf("g1")

        P.op("pool", lambda e: e.memset(ones_bf[:], 1.0), (), [Bc])
        P.op("pool", lambda e: e.memset(negones_bf[:], -1.0), (), [Bc])
        P.op("pool", lambda e: e.memset(negbig_bf[:], NEGBIG), (), [Bc])
        P.op("pool", lambda e: e.memset(ones513[:], 1.0), (), [Bc])
        P.op("pool", lambda e: e.memset(ones_f32[:], 1.0), (), [Bc])
        P.op("pool", lambda e: e.memset(eps_t[:], EPS), (), [Bc])
        Bc2 = Buf("consts2")
        P.op("pool", lambda e: e.affine_select(out=ident_bf[:], in_=ones_bf[:], pattern=[[-1, 128]], compare_op=ALU.is_equal,
                                               fill=0.0, base=0, channel_multiplier=1), [Bc], [Bc2])
        P.op("pool", lambda e: e.affine_select(out=ident_f32[:], in_=ones_f32[:], pattern=[[-1, 128]], compare_op=ALU.is_equal,
                                               fill=0.0, base=0, channel_multiplier=1), [Bc], [Bc2])
        P.op("pool", lambda e: e.affine_select(out=negbigI[:], in_=negbig_bf[:], pattern=[[-1, 128]], compare_op=ALU.is_equal,
                                               fill=0.0, base=0, channel_multiplier=1), [Bc], [Bc2])
        P.op("pool", lambda e: e.affine_select(out=negtri[:], in_=negones_bf[:], pattern=[[-1, 128]], compare_op=ALU.is_ge,
                                               fill=0.0, base=0, channel_multiplier=1), [Bc], [Bc2])
        P.op("pool", lambda e: e.affine_select(out=M0[:], in_=ones513[:], pattern=[[-1, 513]], compare_op=ALU.is_ge,
                                               fill=0.0, base=0, channel_multiplier=1), [Bc], [Bc2])
        P.op("pool", lambda e: e.affine_select(out=SL_bf[:], in_=ones_bf[:], pattern=[[1, 128]], compare_op=ALU.is_ge,
                                               fill=0.0, base=-1, channel_multiplier=-1), [Bc], [Bc2])

        hT = sb("hT", (128, 8, S), BF16)
        BhT = [Buf(f"hT{t}") for t in range(NQ)]
        g3 = sb("g3", (128, D), F32)
        M1 = sb("M1", (128, NT, NE), BF16)
        M2 = sb("M2", (128, NT, NE), BF16)
        Rk = sb("Rk", (128, NT, NE), F32)
        Wg = sb("Wg", (128, NT, 2), F32)
        IDX = sb("IDX", (128, NT, 2), I32)
        WI = sb("WI", (128, 64), I32)
        SL_bf = sb("SL_bf", (128, 128), BF16)

        def rms_rstd(xs_ap, junk_ap, ss_ap, ln_ap, rstd_ap, rd, wr_junk, wr_stat):
            P.op("act", lambda e: e.activation(out=junk_ap, in_=xs_ap, func=AF.Square, accum_out=ss_ap), rd, [wr_junk, wr_stat])
            ACT(ln_ap, ss_ap, AF.Ln, [wr_stat, Bc], [wr_stat], scale=1.0 / D, bias=eps_t[:, 0:1])
            ACT(rstd_ap, ln_ap, AF.Exp, [wr_stat], [wr_stat], scale=-0.5)

        NXS = 2
        Bxs = [Buf(f"xs{i}") for i in range(NXS)]
        junk = sb("junk", (128, D), BF16)
        Bjunk = Buf("junk")
        stat = [sb(f"stat{i}", (128, 4), F32) for i in range(2)]
        Bstat = [Buf(f"stat{i}") for i in range(2)]
        stk.append(ExitStack())
        g1 = sb("g1", (128, D), F32)
        DMA("sp", g1[:], norm_attn_d[0:1, :].partition_broadcast(128), (), [Bg1], "g1")
        xs = [sb(f"xsa{i}", (128, D), F32) for i in range(NXS)]
        hb = [sb(f"hb{i}", (128, D), BF16) for i in range(2)]
        Bhb = [Buf(f"hb{i}") for i in range(2)]

        def run_pipeline(stage_lists, bg, every=6):
            n = len(stage_lists)
            nst = max(len(x) for x in stage_lists)
            bgi = 0
            for s in range(n + nst - 1):
                for k in range(nst):
                    t = s - k
                    if 0 <= t < n and k < len(stage_lists[t]) and stage_lists[t][k] is not None:
                        stage_lists[t][k]()
                if bg and s % every == 3 and bgi < len(bg):
                    bg[bgi]()
                    bgi += 1
            while bg and bgi < len(bg):
                bg[bgi]()
                bgi += 1

        def phase1():
            def front(tt):
                s3 = tt % NXS
                s2 = tt % 2
                DMA("sp", xs[s3][:], x_d[tt * 128:(tt + 1) * 128, :], (), [Bxs[s3]], f"xs{s3}")
                rms_rstd(xs[s3][:], junk[:], stat[s2][:, 0:1], stat[s2][:, 1:2], stat[s2][:, 2:3],
                         [Bxs[s3]], Bjunk, Bstat[s2])
                STT(hb[s2][:], xs[s3][:], stat[s2][:, 2:3], g1[:], ALU.mult, ALU.mult,
                    [Bxs[s3], Bstat[s2], Bg1], [Bhb[s2]])

            def back(tt):
                s2 = tt % 2
                bk = 6 + (tt % 2)
                pv = banks[bk][:].bitcast(BF16)
                for c in range(8):
                    TR(pv[:, c * 128:(c + 1) * 128], hb[s2][:, c * 128:(c + 1) * 128], ident_bf[:],
                       [Bhb[s2], Bc2], [bankB[bk]])
                CP("act", hT[:, :, tt * 128:(tt + 1) * 128], pv[:, :].rearrange("p (c t) -> p c t", c=8),
                   [bankB[bk]], [BhT[tt // 4]])
            steps = [[lambda tt=tt: front(tt), lambda tt=tt: back(tt)] for tt in range(NT)]
            run_pipeline(steps, None)

        phase1()
        P.barrier()
        stk.pop().close()

        NSL = 3
        stk.append(ExitStack())
        QTa = [sb(f"QTa{i}", (128, S), BF16) for i in range(NSL)]
        KTa = [sb(f"KTa{i}", (128, S), BF16) for i in range(NSL)]
        Vt = [sb(f"Vt{i}", (128, NT, HD + 1), BF16) for i in range(NSL)]
        wqk = [sb(f"wqk{i}", (128, 8, 130), BF16) for i in range(NSL)]
        wv = [sb(f"wv{i}", (128, 8, HD), BF16) for i in range(NSL)]
        BQ = [Buf(f"Q{i}") for i in range(NSL)]
        BK = [Buf(f"K{i}") for i in range(NSL)]
        BQaug = [Buf(f"Qaug{i}") for i in range(NSL)]
        BKaug = [Buf(f"Kaug{i}") for i in range(NSL)]
        BV = [Buf(f"V{i}") for i in range(NSL)]
        BVones = [Buf(f"Vones{i}") for i in range(NSL)]
        Bwqk = [Buf(f"wqk{i}") for i in range(NSL)]
        Bwv = [Buf(f"wv{i}") for i in range(NSL)]
        ostS = sb("ostS", (64, S), BF16)
        ostF = sb("ostF", (64, S), BF16)
        BostS = Buf("ostS")
        BostF = Buf("ostF")
        wstage = [sb(f"wstage{i}", (128, 2048), BF16) for i in range(1)]
        Bwstage = [Buf(f"wstage{i}") for i in range(1)]
        BWb = [Buf(f"Wb{e}") for e in range(NE)]

        def conv_expert(ex):
            def f():
                srcs = [w1_d[ex].rearrange("(c p) n -> p c n", p=128), w3_d[ex].rearrange("(c p) n -> p c n", p=128),
                        w2_d[ex].rearrange("(k p) n -> p k n", p=128)]
                pats = ["p (c n) -> p c n", "p (c n) -> p c n", "p (k n) -> p k n"]
                for part in range(3):
                    kw = {"c": 8} if part < 2 else {"k": 2}
                    P.op("pool", lambda e, part=part, kw=kw: e.dma_start(
                        out=wstage[0][:, :].rearrange(pats[part], **kw), in_=srcs[part]),
                        (), [Bwstage[0]], dma_key="wstg0")
                    DMA("sp", Wb_d[ex * 128:(ex + 1) * 128, part * 2048:(part + 1) * 2048], wstage[0][:, :],
                        [Bwstage[0]], [BWb[ex]], "wbst0")
            return f
        BoT_d = [Buf(f"oTd{h}") for h in range(16)]

        for i in range(NSL):
            P.op("pool", lambda e, i=i: e.memset(Vt[i][:, :, HD:HD + 1], 1.0), (), [BVones[i]])
            P.op("pool", lambda e, i=i: e.memset(wqk[i][:, :, 128:130], 0.0), (), [BVones[i]])

        e_sb = [sb(f"e_sb{i}", (128, 512), F32) for i in range(2)]
        sp_bf = [sb(f"sp_bf{i}", (128, 512), BF16) for i in range(2)]
        arg_sb = [sb(f"arg_sb{i}", (128, 512), F32) for i in range(2)]
        A_bf = [sb(f"A_bf{i}", (128, 512), BF16) for i in range(3)]
        P_bf = [sb(f"P_bf{i}", (128, 512), BF16) for i in range(3)]
        BPb = [Buf() for _ in range(3)]
        oraw = [sb(f"oraw{i}", (128, 512), F32) for i in range(2)]
        Boraw = [Buf() for _ in range(2)]
        C_sb = [sb(f"C_sb{i}", (128, 512), F32) for i in range(2)]
        Be = [Buf() for _ in range(2)]
        Bsp = [Buf() for _ in range(2)]
        Barg = [Buf() for _ in range(2)]
        BA = [Buf() for _ in range(3)]
        BC = [Buf() for _ in range(2)]

        wf = sb("wf", (128, 8, NH), BF16)
        Bwf = Buf("wf")
        fexp = [sb(f"fexp{i}", (NH, 512), F32) for i in range(1)] * 2
        cpt = [sb(f"cpt{i}", (NH, 512), F32) for i in range(2)]
        r1 = fexp
        cpp = [sb(f"cpp{i}", (NH, 3, 512), BF16) for i in range(1)] * 2
        negb = sb("negb", (NH, 1), F32)
        Bnegb = Buf("negb")
        Bfexp = [Buf()] * 2
        Bcpt = [Buf() for _ in range(2)]
        Br1 = Bfexp
        Bcpp = [Buf()] * 2
        Bncpp = [Buf()] * 2
        Bcparts = [Buf(f"cparts_d{t}") for t in range(NQ)]

        def load_head_weights(hd, sl):
            typ, h = divmod(hd, NH)
            cq = (C_QSB if typ == 0 else C_QFX) + h * HD
            ck = (C_KSB if typ == 0 else C_KFX) + h * HD
            cv = (C_VSB if typ == 0 else C_VFX) + h * HD
            P.op("pool", lambda e: [
                e.dma_start(out=wqk[sl][:, :, 0:HD], in_=w_in_d[:, cq:cq + HD].rearrange("(c p) n -> p c n", p=128)),
                e.dma_start(out=wqk[sl][:, :, HD:2 * HD], in_=w_in_d[:, ck:ck + HD].rearrange("(c p) n -> p c n", p=128)),
            ], (), [Bwqk[sl]], dma_key=f"wqk{sl}", ninc=2)
            P.op("pool", lambda e: e.dma_start(out=wv[sl][:, :, :], in_=w_in_d[:, cv:cv + HD].rearrange("(c p) n -> p c n", p=128)),
                 (), [Bwv[sl]], dma_key=f"wv{sl}")

        pj_rot = [0]

        def proj_chunks(hd, sl):
            typ, h = divmod(hd, NH)
            chunks = []
            bk = 7

            def qk_chunk(T, which):
                lo = 0 if which == "q" else HD
                out = []
                for c in range(8):
                    out.append(lambda c=c: MM(banks[bk][0:HD + 1, :], wqk[sl][:, c, lo:lo + HD + 1],
                                              hT[:, c, T * 512:(T + 1) * 512], c == 0, c == 7,
                                              [Bwqk[sl], BhT[T], BVones[sl]], [bankB[bk]]))
                if which == "q":
                    out.append(lambda: TS("dve", QTa[sl][0:HD, T * 512:(T + 1) * 512], banks[bk][0:HD, :], 0.125, None,
                                          ALU.mult, None, [bankB[bk]], [BQ[sl]]))
                else:
                    out.append(lambda: CP("dve", KTa[sl][0:HD, T * 512:(T + 1) * 512], banks[bk][0:HD, :],
                                          [bankB[bk]], [BK[sl]]))
                return out

            def v_chunk(g):
                out = []
                for u in range(8):
                    def f(u=u):
                        tt = g * 8 + u
                        for c in range(8):
                            MM(banks[bk][:, u * HD:(u + 1) * HD], hT[:, c, tt * 128:(tt + 1) * 128], wv[sl][:, c, :],
                               c == 0, c == 7, [Bwv[sl], BhT[tt // 4]], [bankB[bk]])
                    out.append(f)
                out.append(lambda: CP("dve", Vt[sl][:, g * 8:(g + 1) * 8, 0:HD],
                                      banks[bk][:, :].rearrange("p (u d) -> p u d", u=8), [bankB[bk]], [BV[sl]]))
                return out

            for T in range(NQ):
                chunks += qk_chunk(T, "k")
            for g in range(4):
                chunks += v_chunk(g)
            for T in range(NQ):
                chunks += qk_chunk(T, "q")
            if typ == 0:
                def zpad():
                    P.op("pool", lambda e: e.memset(QTa[sl][64:128, :], 0.0), (), [BQaug[sl]])
                    P.op("pool", lambda e: e.memset(KTa[sl][64:128, :], 0.0), (), [BKaug[sl]])
                chunks.append(zpad)
            if typ == 1:
                def aug():
                    P.op("pool", lambda e: e.memset(QTa[sl][64:70, :], 1.0), (), [BQaug[sl]])
                    P.op("pool", lambda e: e.memset(KTa[sl][64:70, :], -1.0), (), [BKaug[sl]])
                    DMA("sp", QTa[sl][64:67, :], cparts_d[h, :, :], Bcparts, [BQaug[sl]], f"qaug{sl}")
                    DMA("sp", KTa[sl][67:70, :], cparts_d[h, :, :], Bcparts, [BKaug[sl]], f"kaug{sl}")
                chunks.append(aug)
            return chunks

        def fox_prep():
            P.op("pool", lambda e: e.dma_start(out=wf[:, :, :], in_=w_in_d[:, C_F:C_F + NH].rearrange("(c p) n -> p c n", p=128)),
                 (), [Bwf], dma_key="wf")
            DMA("sp", negb[:, 0:1], b_forget_d[0:1, :].rearrange("o h -> h o"), (), [Bnegb], "negb")
            TS("dve", negb[:, 0:1], negb[:, 0:1], -1.0, None, ALU.mult, None, [Bnegb], [Bnegb])
            for T in range(NQ):
                bk = 7
                s2 = T % 2
                for c in range(8):
                    MM(banks[bk][0:NH, :], wf[:, c, :], hT[:, c, T * 512:(T + 1) * 512], c == 0, c == 7,
                       [Bwf, BhT[T]], [bankB[bk]])
                ACT(fexp[s2][:, :], banks[bk][0:NH, :], AF.Exp, [bankB[bk], Bnegb], [Bfexp[s2]],
                    scale=-1.0, bias=negb[:, 0:1])
                ACT(fexp[s2][:, :], fexp[s2][:, :], AF.Ln, [Bfexp[s2]], [Bfexp[s2]], bias=1.0)
                init = 0.0 if T == 0 else cpt[1 - s2][:, 511:512]
                rds = [Bfexp[s2]] + ([Bcpt[1 - s2]] if T > 0 else [])
                P.op("dve", lambda e, s2=s2, init=init: e.tensor_tensor_scan(
                    out=cpt[s2][:, :], data0=fexp[s2][:, :], data1=fexp[s2][:, :], initial=init,
                    op0=ALU.add, op1=ALU.max), rds, [Bcpt[s2]])
                CP("dve", cpp[s2][:, 0, :], cpt[s2][:, :], [Bcpt[s2]], [Bcpp[s2]])
                TT("dve", r1[s2][:, :], cpt[s2][:, :], cpp[s2][:, 0, :], ALU.subtract, [Bcpt[s2], Bcpp[s2]], [Br1[s2]])
                CP("dve", cpp[s2][:, 1, :], r1[s2][:, :], [Br1[s2]], [Bcpp[s2]])
                TT("dve", r1[s2][:, :], r1[s2][:, :], cpp[s2][:, 1, :], ALU.subtract, [Br1[s2], Bcpp[s2]], [Br1[s2]])
                CP("dve", cpp[s2][:, 2, :], r1[s2][:, :], [Br1[s2]], [Bcpp[s2]])
                P.op("sp", lambda e, s2=s2, T=T: [
                    e.dma_start(out=cparts_d[:, :, T * 512:(T + 1) * 512], in_=cpp[s2][:, :, :]),
                ], [Bcpp[s2]], [Bcparts[T]], dma_key="cpst0", ninc=1)

        ZB = [0, 1]
        AB = [2, 3]
        CSB = 4
        OB = 5

        cnt = {"z": 0, "a": 0, "A": 0, "c": 0, "e": 0}

        def sb_steps(hd, sl):
            steps = []
            ZA = [0, 1]
            CSB_ = 2
            ob = 3
            for j in range(NQ):
                cslot = cnt["c"] % 2
                cnt["c"] += 1
                order = list(range(4 * j + 3, -1, -1))
                for n_, i in enumerate(order):
                    m = i - 4 * j
                    off = max(0, m) * 128
                    diag = m >= 0
                    first = n_ == 0
                    last = i == 0
                    zs = cnt["z"] % 2
                    cnt["z"] += 1
                    As = cnt["A"] % 3
                    cnt["A"] += 1
                    kT = KTa[sl][0:128, i * 128:(i + 1) * 128]
                    qT = QTa[sl][0:128, j * 512 + off:(j + 1) * 512]
                    msk = M0[:, 0:512 - off]
                    W = slice(off, 512)

                    def st0(zs=zs, kT=kT, qT=qT, msk=msk, W=W, diag=diag, first=first, cslot=cslot, off=off):
                        if first:
                            MEMSET("pool", C_sb[cslot][:], 0.0, [BC[cslot]])
                        zb = ZA[zs]
                        MM(banks[zb][:, W], kT, qT, True, not diag, [BK[sl], BQ[sl], BKaug[sl], BQaug[sl]], [bankB[zb]])
                        if diag:
                            MM(banks[zb][:, off:off + 128], negbigI[:], M0[:, 0:128], False, True, [Bc2], [bankB[zb]])
                        ACT(e_sb[zs][:, W], banks[zb][:, W], AF.Exp, [bankB[zb]], [Be[zs]])
                        ACT(sp_bf[zs][:, W], e_sb[zs][:, W], AF.Ln, [Be[zs]], [Bsp[zs]], bias=1.0)

                    def st1(zs=zs, As=As, W=W, first=first, last=last, cslot=cslot):
                        ab = ZA[zs]
                        MM(banks[ab][:, W], negtri[:], sp_bf[zs][:, W], False, True, [Bsp[zs], Bc2], [bankB[ab]],
                           skip_group_check=True)
                        if not last:
                            MM(banks[CSB_][:, W], negones_bf[:], sp_bf[zs][:, W], True, True, [Bsp[zs], Bc], [bankB[CSB_]])
                        if first:
                            ACT(A_bf[As][:, W], banks[ab][:, W], AF.Exp, [bankB[ab]], [BA[As]])
                        else:
                            TT("dve", arg_sb[zs][:, W], banks[ab][:, W], C_sb[cslot][:, W], ALU.add,
                               [bankB[ab], BC[cslot]], [Barg[zs]])
                            ACT(A_bf[As][:, W], arg_sb[zs][:, W], AF.Exp, [Barg[zs]], [BA[As]])
                        if not last:
                            TT("dve", C_sb[cslot][:, W], banks[CSB_][:, W], C_sb[cslot][:, W], ALU.add,
                               [bankB[CSB_], BC[cslot]], [BC[cslot]])
                        for _ in range(FILL_SB):
                            MM(banks[7][:, :], negtri[:], M0[:, 0:512], True, True, [Bc2], [bankB[7]])

                    def st2(As=As, i=i, j=j, W=W, first=first, last=last):
                        MM(banks[ob][0:HD + 1, W], Vt[sl][:, i, 0:HD + 1], A_bf[As][:, W], first, last,
                           [BV[sl], BVones[sl], BA[As]], [bankB[ob]], skip_group_check=True)
                        if last:
                            CP("dve", ostS[0:HD, j * 512:(j + 1) * 512], banks[ob][0:HD, :], [bankB[ob]], [BostS])
                            if j == NQ - 1:
                                DMA("sp", oT_d[hd, :, :], ostS[:, :], [BostS], [BoT_d[hd]], "ostS")
                    steps.append([st0, st1, st2])
            return steps

        def fox_steps(hd, sl):
            steps = []
            SBK = [4, 5]
            ob = 6
            MB = 2
            for j in range(NQ):
                nk = 4 * j + 4
                orot = j % 2
                for i in range(nk):
                    m = i - 4 * j
                    off = max(0, m) * 128
                    diag = m >= 0
                    first = i == 0
                    last = i == nk - 1
                    zs = cnt["fz"] % 2
                    cnt["fz"] += 1
                    As = cnt["fA"] % 3
                    cnt["fA"] += 1
                    kT = KTa[sl][0:70, i * 128:(i + 1) * 128]
                    qT = QTa[sl][0:70, j * 512 + off:(j + 1) * 512]
                    msk = M0[:, 1:1 + 512 - off]
                    W = slice(off, 512)

                    def st0(zs=zs, As=As, kT=kT, qT=qT, msk=msk, W=W, diag=diag, off=off):
                        zb = SBK[zs]
                        MM(banks[zb][:, W], kT, qT, True, not diag, [BK[sl], BQ[sl], BKaug[sl], BQaug[sl]], [bankB[zb]])
                        if diag:
                            MM(banks[zb][:, off:off + 128], negbigI[:], M0[:, 1:129], False, True, [Bc2], [bankB[zb]])

                    def stE(zs=zs, As=As, W=W):
                        zb = SBK[zs]
                        ACT(P_bf[As][:, W], banks[zb][:, W], AF.Exp, [bankB[zb]], [BPb[As]])

                    def st1(As=As, i=i, j=j, W=W, first=first, last=last, orot=orot):
                        MM(banks[ob][0:HD + 1, W], Vt[sl][:, i, 0:HD + 1], P_bf[As][:, W], first, last,
                           [BV[sl], BVones[sl], BPb[As]], [bankB[ob]])
                        if last:
                            CP("dve", oraw[orot][0:HD + 1, :], banks[ob][0:HD + 1, :], [bankB[ob]], [Boraw[orot]])

                    def stN(j=j, orot=orot, last=last):
                        if not last:
                            return
                        ACT(oraw[orot][64:65, :], oraw[orot][64:65, :], AF.Ln, [Boraw[orot]], [Boraw[orot]])
                        ACT(oraw[orot][64:65, :], oraw[orot][64:65, :], AF.Exp, [Boraw[orot]], [Boraw[orot]], scale=-1.0)

                    def stM(j=j, orot=orot, last=last):
                        if not last:
                            return
                        MM(banks[MB][0:HD, :], ones_f32[64:65, 0:HD], oraw[orot][64:65, :], True, True,
                           [Boraw[orot], Bc], [bankB[MB]])
                        TT("dve", ostF[0:HD, j * 512:(j + 1) * 512], banks[MB][0:HD, :], oraw[orot][0:HD, :], ALU.mult,
                           [bankB[MB], Boraw[orot]], [BostF])
                        if j == NQ - 1:
                            DMA("sp", oT_d[hd, :, :], ostF[:, :], [BostF], [BoT_d[hd]], "ostF")
                    steps.append([st0, stE, st1, stN, stM])
            return steps

        cnt["fz"] = 0
        cnt["fA"] = 0
        HS = 144
        LAG = HS // 2
        seq = []
        for h in range(NH):
            seq += [h, NH + h]
        sched = {}

        def add_bg(it, f):
            sched.setdefault(max(it, 0), []).append(f)

        for n, hd in enumerate(seq):
            sl = n % 3
            chunks = [lambda hd=hd, sl=sl: load_head_weights(hd, sl)]
            if n == 1:
                chunks.append(fox_prep)
            chunks += proj_chunks(hd, sl)
            k1 = len(chunks) // 3
            chunks = chunks[:k1] + [conv_expert(2 * n)] + chunks[k1:2 * k1] + [conv_expert(2 * n + 1)] + chunks[2 * k1:]
            h = n // 2
            start = HS * h if n % 2 == 0 else HS * h + LAG
            base = start - LAG + 5
            nch = len(chunks)
            for ci, f in enumerate(chunks):
                add_bg(base + (ci * (LAG - 8)) // nch, f)
        sb_lists = {}
        fx_lists = {}

        def sb_step(sidx):
            if sidx < 0 or sidx >= HS * NH:
                return None
            h, r = divmod(sidx, HS)
            if h not in sb_lists:
                sb_lists[h] = sb_steps(h, (2 * h) % 3)
                sb_lists.pop(h - 2, None)
            return sb_lists[h][r]

        def fx_step(fidx):
            if fidx < 0 or fidx >= HS * NH:
                return None
            h, r = divmod(fidx, HS)
            if h not in fx_lists:
                fx_lists[h] = fox_steps(NH + h, (2 * h + 1) % 3)
                fx_lists.pop(h - 2, None)
            return fx_lists[h][r]

        def run_stage(stp, k):
            if stp is not None and stp[k] is not None:
                stp[k]()

        for it in sorted(k for k in sched if k <= 0):
            for f in sched.pop(it):
                f()
        total_p = HS * NH + LAG
        for p_ in range(total_p + 5):
            f_ = p_ - LAG
            run_stage(fx_step(f_ - 1), 1)
            run_stage(sb_step(p_), 0)
            run_stage(fx_step(f_), 0)
            run_stage(sb_step(p_ - 1), 1)
            run_stage(fx_step(f_ - 1), 2)
            run_stage(sb_step(p_ - 2), 2)
            run_stage(fx_step(f_ - 2), 3)
            run_stage(fx_step(f_ - 3), 4)
            for f in sched.pop(p_, []):
                f()
        for it in sorted(sched):
            for f in sched[it]:
                f()

        P.barrier()
        stk.pop().close()
        stk.append(ExitStack())
        xs = [sb(f"xsb{i}", (128, D), F32) for i in range(NXS)]
        wosb = sb("wosb", (128, 4, D), BF16)
        wofx = sb("wofx", (128, 4, D), BF16)
        wout = sb("wout", (128, 8, D), BF16)
        wg = sb("wg", (128, 8, 2 * D), BF16)
        Bw3 = Buf("w3")
        P.op("pool", lambda e: [
            e.dma_start(out=wosb[:, :, :], in_=w_o_sb_d.rearrange("(c p) n -> p c n", p=128)),
            e.dma_start(out=wofx[:, :, :], in_=w_o_fox_d.rearrange("(c p) n -> p c n", p=128)),
            e.dma_start(out=wout[:, :, :], in_=w_out_d.rearrange("(c p) n -> p c n", p=128)),
        ] + [e.dma_start(out=wg[:, c, :], in_=w_in_d[c * 128:(c + 1) * 128, C_GSB:C_GSB + 2 * D]) for c in range(8)],
            (), [Bw3], dma_key="w3", ninc=11)
        oTt = [sb(f"oTt{i}", (128, 8, 512), BF16) for i in range(2)]
        BoTt = [Buf() for _ in range(2)]
        sig = [sb(f"sig{i}", (128, 512), BF16) for i in range(4)]
        Bsig = [Buf() for _ in range(4)]
        tmp = [sb(f"tmp{i}", (128, 512), F32) for i in range(2)]
        Btmp = [Buf() for _ in range(2)]
        mixT = [sb(f"mixT{i}", (128, 8, 512), BF16) for i in range(2)]
        BmixT = [Buf() for _ in range(2)]
        x2s = [sb(f"x2s{i}", (128, D), F32) for i in range(2)]
        Bx2s = [Buf() for _ in range(2)]
        Bx2d = [Buf(f"x2d{t}") for t in range(NT)]
        Bdbg = [Buf(f"dbg{t}") for t in range(NT)]

        def phase3():
            def load_oT(T):
                s2 = T % 2
                P.op("sp", lambda e: e.dma_start(
                    out=oTt[s2][:, :, :],
                    in_=oT_d[:, :, T * 512:(T + 1) * 512].rearrange("(c two) d t -> (two d) c t", two=2)),
                    BoT_d, [BoTt[s2]], dma_key=f"oTt{s2}")
            load_oT(0)
            for T in range(NQ):
                s2 = T % 2
                if T + 1 < NQ:
                    load_oT(T + 1)
                for dc in range(8):
                    dsl = slice(dc * 128, (dc + 1) * 128)
                    b0 = 0 if dc % 2 == 0 else 4
                    for c in range(4):
                        MM(banks[b0][:, :], wosb[:, c, dsl], oTt[s2][:, c, :], c == 0, c == 3, [Bw3, BoTt[s2]], [bankB[b0]])
                    for c in range(4):
                        MM(banks[b0 + 1][:, :], wofx[:, c, dsl], oTt[s2][:, 4 + c, :], c == 0, c == 3, [Bw3, BoTt[s2]], [bankB[b0 + 1]])
                    for c in range(8):
                        MM(banks[b0 + 2][:, :], wg[:, c, dsl], hT[:, c, T * 512:(T + 1) * 512], c == 0, c == 7,
                           [Bw3, BhT[T]], [bankB[b0 + 2]])
                    for c in range(8):
                        MM(banks[b0 + 3][:, :], wg[:, c, D + dc * 128:D + (dc + 1) * 128], hT[:, c, T * 512:(T + 1) * 512],
                           c == 0, c == 7, [Bw3, BhT[T]], [bankB[b0 + 3]])
                    sa = (dc % 2) * 2
                    ACT(sig[sa][:, :], banks[b0 + 2][:, :], AF.Sigmoid, [bankB[b0 + 2]], [Bsig[sa]])
                    ACT(sig[sa + 1][:, :], banks[b0 + 3][:, :], AF.Sigmoid, [bankB[b0 + 3]], [Bsig[sa + 1]])
                    t2 = dc % 2
                    TT("dve", tmp[t2][:, :], banks[b0][:, :], sig[sa][:, :], ALU.mult, [bankB[b0], Bsig[sa]], [Btmp[t2]])
                    TT("dve", sig[sa + 1][:, :], banks[b0 + 1][:, :], sig[sa + 1][:, :], ALU.mult,
                       [bankB[b0 + 1], Bsig[sa + 1]], [Bsig[sa + 1]])
                    TT("dve", mixT[s2][:, dc, :], tmp[t2][:, :], sig[sa + 1][:, :], ALU.add,
                       [Btmp[t2], Bsig[sa + 1]], [BmixT[s2]])
                for u in range(4):
                    tt = T * 4 + u
                    s3 = tt % NXS
                    xq = tt % 2
                    DMA("sp", xs[s3][:], x_d[tt * 128:(tt + 1) * 128, :], (), [Bxs[s3]], f"xs{s3}")
                    bb = 0 if tt % 2 == 0 else 4
                    for half in range(2):
                        for c in range(8):
                            MM(banks[bb + half][:, :], mixT[s2][:, c, u * 128:(u + 1) * 128], wout[:, c, half * 512:(half + 1) * 512],
                               c == 0, c == 7, [BmixT[s2], Bw3], [bankB[bb + half]])
                    for half in range(2):
                        TT("dve", x2s[xq][:, half * 512:(half + 1) * 512], banks[bb + half][:, :],
                           xs[s3][:, half * 512:(half + 1) * 512], ALU.add, [bankB[bb + half], Bxs[s3]], [Bx2s[xq]])
                    DMA("act", x2_d[tt * 128:(tt + 1) * 128, :], x2s[xq][:], [Bx2s[xq]], [Bx2d[tt]], f"x2st{xq}")
                    if debug and upto is None:
                        DMA("sp", dbg_d[tt * 128:(tt + 1) * 128, :], x2s[xq][:], [Bx2s[xq]], [Bdbg[tt]], f"dbgst{xq}")

        phase3()

        P.barrier()
        stk.pop().close()
        stk.append(ExitStack())
        BIGR = 1.0e4
        xs = [sb(f"xsc{i}", (128, D), F32) for i in range(NXS)]
        g2 = sb("g2", (128, D), F32)
        Bg2 = Buf("g2")
        DMA("sp", g2[:], norm_ffn_d[0:1, :].partition_broadcast(128), (), [Bg2], "g2")
        Bg3 = Buf("g3")
        DMA("sp", g3[:], norm_final_d[0:1, :].partition_broadcast(128), (), [Bg3], "g3")
        wr = sb("wr", (128, 8, 36), F32)
        rbias = sb("rbias", (128, 36), F32)
        Bwr = Buf("wr")
        P.op("sp", lambda e: [
            e.dma_start(out=wr[:, :, 0:4], in_=w_rg_d.rearrange("(c p) n -> p c n", p=128)),
            e.dma_start(out=wr[:, :, 4:36], in_=w_re_d.rearrange("(c p) n -> p c n", p=128)),
            e.dma_start(out=rbias[:, 0:4], in_=b_rg_d[0:1, :].partition_broadcast(128)),
            e.dma_start(out=rbias[:, 4:36], in_=b_re_d[0:1, :].partition_broadcast(128)),
        ], (), [Bwr], dma_key="wr", ninc=4)
        h2b = hT[:].rearrange("p c t -> p (c t)")
        Bh2b = [Buf(f"h2b{t}") for t in range(NT)]
        BM = [Buf(f"M{t}") for t in range(NT)]
        BRk = [Buf(f"Rk{t}") for t in range(NT)]
        BWg = [Buf(f"Wg{t}") for t in range(NT)]
        h2f = [sb(f"h2f{i}", (128, D), F32) for i in range(2)]
        Bh2f = [Buf() for _ in range(2)]
        h2Tf = [sb(f"h2Tf{i}", (128, 8, 128), F32) for i in range(2)]
        Bh2Tf = [Buf() for _ in range(2)]
        rg = [sb(f"rg{i}", (128, 640), F32) for i in range(2)]
        Brg = [Buf() for _ in range(2)]
        msel4 = [sb(f"msel4_{i}", (128, 4, NE), BF16) for i in range(2)]
        msel = [sb(f"msel{i}", (128, NE), BF16) for i in range(2)]
        Bmsel = [Buf() for _ in range(2)]
        mcum = [sb(f"mcum{i}", (128, NE), BF16) for i in range(2)]
        Bmcum = [Buf() for _ in range(2)]

        def phase3b():
            steps = []
            for tt in range(NT):
                steps.append([lambda tt=tt: p3b_front(tt), lambda tt=tt: p3b_mid(tt),
                              (lambda tt=tt: p3b_chain(tt // 4)) if tt % 4 == 3 else None])
            run_pipeline(steps, None)

        def p3b_front(tt):
            if True:
                s3 = tt % NXS
                s2 = tt % 2
                DMA("sp", xs[s3][:], x2_d[tt * 128:(tt + 1) * 128, :], [Bx2d[tt]], [Bxs[s3]], f"xs{s3}")
                rms_rstd(xs[s3][:], junk[:], stat[s2][:, 0:1], stat[s2][:, 1:2], stat[s2][:, 2:3],
                         [Bxs[s3]], Bjunk, Bstat[s2])
                STT(h2f[s2][:], xs[s3][:], stat[s2][:, 2:3], g2[:], ALU.mult, ALU.mult,
                    [Bxs[s3], Bstat[s2], Bg2], [Bh2f[s2]])
                CP("pool", h2b[:, tt * D:(tt + 1) * D], h2f[s2][:, :], [Bh2f[s2]], [Bh2b[tt]])

        def p3b_mid(tt):
            if True:
                s2 = tt % 2
                bA = 0 if s2 == 0 else 4
                for c in range(8):
                    bk = bA + c // 4
                    TR(banks[bk][:, (c % 4) * 128:(c % 4 + 1) * 128], h2f[s2][:, c * 128:(c + 1) * 128], ident_f32[:],
                       [Bh2f[s2], Bc2], [bankB[bk]])
                for hh in range(2):
                    bk = bA + hh
                    CP("act", h2Tf[s2][:, hh * 4:(hh + 1) * 4, :], banks[bk][:, :].rearrange("p (c t) -> p c t", c=4),
                       [bankB[bk]], [Bh2Tf[s2]])
                bR = bA + 2
                for c in range(8):
                    MM(banks[bR][:, 0:36], h2Tf[s2][:, c, :], wr[:, c, :], c == 0, c == 7, [Bh2Tf[s2], Bwr], [bankB[bR]])
                gp = (tt // 4) % 2
                g_ = tt % 4
                TT("dve", rg[gp][:, g_ * 36:(g_ + 1) * 36], banks[bR][:, 0:36], rbias[:, :], ALU.add,
                   [bankB[bR], Bwr], [Brg[gp]])

        def p3b_chain(grp):
            G = 4
            gp = grp % 2
            r = rg[gp]
            B_ = [Brg[gp]]
            t0_ = grp * G
            X = mybir.AxisListType.X

            def v(lo, n, *dims):
                ap = r[:, lo:lo + n]
                if len(dims) == 2:
                    return ap.rearrange("p (a b) -> p a b", a=dims[0])
                if len(dims) == 3:
                    return ap.rearrange("p (a b c) -> p a b c", a=dims[0], b=dims[1])
                return ap
            lg = v(0, G * 36, G, 36)
            gl = lg[:, :, 0:4]
            el = lg[:, :, 4:36]
            gmax = v(144, G)
            gmask = v(148, G * 4, G, 4)
            gd = v(164, G * 4, G, 4)
            gsum = v(180, G)
            gw = v(184, G)
            pen = v(188, G * 4, G, 4)
            elm = v(204, G * NE, G, NE)
            elm4 = v(204, G * NE, G, 4, 8)
            m1 = v(332, G)
            mask1 = v(336, G * NE, G, NE)
            m2 = v(464, G)
            mask2 = v(468, G * NE, G, NE)
            dd = v(596, G)
            ee = v(600, G)
            w1 = v(604, G)
            w2 = v(608, G)

            def bc(ap, shape):
                return ap.unsqueeze(len(ap.shape)).broadcast_to(shape)
            P.op("dve", lambda e: e.tensor_reduce(out=gmax, in_=gl, axis=X, op=ALU.max), B_, B_)
            TT("dve", gmask, gl, bc(gmax, [128, G, 4]), ALU.is_equal, B_, B_)
            TT("dve", gd, gl, bc(gmax, [128, G, 4]), ALU.subtract, B_, B_)
            ACT(gd, gd, AF.Exp, B_, B_)
            P.op("dve", lambda e: e.tensor_reduce(out=gsum, in_=gd, axis=X, op=ALU.add), B_, B_)
            P.op("dve", lambda e: e.reciprocal(out=gw, in_=gsum), B_, B_)
            TS("dve", pen, gmask, BIGR, -BIGR, ALU.mult, ALU.add, B_, B_)
            TT("dve", elm4, el.rearrange("p g (a b) -> p g a b", a=4), bc(pen, [128, G, 4, 8]), ALU.add, B_, B_)
            P.op("dve", lambda e: e.tensor_reduce(out=m1, in_=elm, axis=X, op=ALU.max), B_, B_)
            TT("dve", mask1, elm, bc(m1, [128, G, NE]), ALU.is_equal, B_, B_)
            STT(elm, mask1, -3.0 * BIGR, elm, ALU.mult, ALU.add, B_, B_)
            P.op("dve", lambda e: e.tensor_reduce(out=m2, in_=elm, axis=X, op=ALU.max), B_, B_)
            TT("dve", mask2, elm, bc(m2, [128, G, NE]), ALU.is_equal, B_, B_)
            TT("dve", dd, m1, m2, ALU.subtract, B_, B_)
            ACT(ee, dd, AF.Exp, B_, B_, scale=-1.0)
            TS("dve", w1, ee, 1.0, None, ALU.add, None, B_, B_)
            P.op("dve", lambda e: e.reciprocal(out=w1, in_=w1), B_, B_)
            TT("dve", w2, ee, w1, ALU.mult, B_, B_)
            BWgs = [BWg[t0_ + g] for g in range(G)]
            BMs = [BM[t0_ + g] for g in range(G)]
            TT("dve", Wg[:, t0_:t0_ + G, 0], w1, gw, ALU.mult, B_, BWgs)
            TT("dve", Wg[:, t0_:t0_ + G, 1], w2, gw, ALU.mult, B_, BWgs)
            CP("dve", M1[:, t0_:t0_ + G, :], mask1, B_, BMs)
            CP("dve", M2[:, t0_:t0_ + G, :], mask2, B_, BMs)
            TT("dve", msel4[gp][:, :, :], mask1, mask2, ALU.add, B_, [Bmsel[gp]])
            bK = 3 if gp == 0 else 7
            for g in range(G):
                tt = t0_ + g
                s2 = tt % 2
                if tt == 0:
                    CP("dve", mcum[s2][:, :], msel4[gp][:, g, :], [Bmsel[gp]], [Bmcum[s2]])
                else:
                    TT("dve", mcum[s2][:, :], mcum[1 - s2][:, :], msel4[gp][:, g, :], ALU.add,
                       [Bmcum[1 - s2], Bmsel[gp]], [Bmcum[s2]])
                MM(banks[bK][:, g * NE:(g + 1) * NE], SL_bf[:], msel4[gp][:, g, :], True, tt == 0, [Bmsel[gp], Bc2], [bankB[bK]])
                if tt > 0:
                    MM(banks[bK][:, g * NE:(g + 1) * NE], ones_bf[:], mcum[1 - s2][:, :], False, True,
                       [Bmcum[1 - s2], Bc], [bankB[bK]])
            CP("dve", Rk[:, t0_:t0_ + G, :], banks[bK][:, 0:G * NE].rearrange("p (g e) -> p g e", g=G),
               [bankB[bK]], [BRk[t0_ + g] for g in range(G)])

        phase3b()
        if upto == "3b":
            DMA("sp", dbg_d[0:128, :], Rk[:, :, :].rearrange("p t e -> p (t e)"), BRk, [Bdbg[0]], "dbgcmb")

        cnt = sb("cnt", (128, NE), F32)
        cnti = sb("cnti", (128, NE), I32)
        pcf = sb("pcf", (128, NE), F32)
        cend = sb("cend", (128, NE), F32)
        offs = sb("offs", (128, NE), F32)
        sstart_i = sb("sstart_i", (128, NSLOT), I32)
        sstart = sb("sstart", (128, NSLOT), F32)
        eidf = sb("eidf", (128, NSLOT), F32)
        pidx_i = sb("pidx_i", (128, 1), I32)
        pidx = sb("pidx", (128, 1), F32)
        posall = sb("posall", (128, NT * NE), F32)
        pmall = sb("pmall", (128, NT * NE), F32)
        pfall = sb("pfall", (128, 2, NT), F32)
        Bpb = Buf("passB")
        Bpos = [Buf() for _ in range(2)]
        BIDX = [Buf(f"IDX{t}") for t in range(NT)]
        BWI = Buf("WI")
        BXs = Buf("Xs_d")
        BXs_t = [Buf(f"Xs_t{t}") for t in range(NT)]

        def passB():
            lastm = (NT - 1) % 2
            MM(banks[0][:, 0:NE], ones_bf[:], mcum[lastm][:, :], True, True, [Bmcum[lastm], Bc], [bankB[0]])
            B_ = [Bpb]
            TS("dve", cnti[:, :], banks[0][:, 0:NE], float(SLOTR - 1), None, ALU.add, None, [bankB[0]], B_)
            TS("dve", cnti[:, :], cnti[:, :], SHIFT, None, ALU.logical_shift_right, None, B_, B_)
            TS("dve", cnti[:, :], cnti[:, :], SHIFT, None, ALU.logical_shift_left, None, B_, B_)
            CP("dve", pcf[:, :], cnti[:, :], B_, B_)
            P.op("dve", lambda e: e.tensor_tensor_scan(out=cend[:, :], data0=pcf[:, :], data1=pcf[:, :], initial=0.0,
                                                       op0=ALU.add, op1=ALU.max), B_, B_)
            TT("dve", offs[:, :], cend[:, :], pcf[:, :], ALU.subtract, B_, B_)
            P.op("pool", lambda e: e.iota(sstart_i[:, :], pattern=[[SLOTR, NSLOT]], base=0, channel_multiplier=0), (), B_)
            P.op("pool", lambda e: e.iota(pidx_i[:, :], pattern=[[0, 1]], base=0, channel_multiplier=1), (), B_)
            CP("dve", sstart[:, :], sstart_i[:, :], B_, B_)
            CP("dve", pidx[:, :], pidx_i[:, :], B_, B_)
            for ex in range(NE):
                if ex == 0:
                    TS("dve", eidf[:, :], sstart[:, :], cend[:, 0:1], None, ALU.is_ge, None, B_, B_)
                else:
                    STT(eidf[:, :], sstart[:, :], cend[:, ex:ex + 1], eidf[:, :], ALU.is_ge, ALU.add, B_, B_)
            TS("dve", eidf[:, :], eidf[:, :], float(NE - 1), 128.0, ALU.min, ALU.mult, B_, B_)
            TS("dve", WI[:, :], eidf[:, :], pidx[:, 0:1], None, ALU.add, None, B_, [BWI])
            Bp = [Bpos[0]]
            pos3 = posall[:, :].rearrange("p (t e) -> p t e", e=NE)
            pm3 = pmall[:, :].rearrange("p (t e) -> p t e", e=NE)
            TT("dve", pos3, Rk[:, :, :], offs[:, :].unsqueeze(1).broadcast_to([128, NT, NE]), ALU.add, BRk + [Bpb], Bp)
            TT("dve", pm3, pos3, M1[:, :, :], ALU.mult, Bp + BM, Bp)
            P.op("dve", lambda e: e.tensor_reduce(out=pfall[:, 0, :], in_=pm3, axis=mybir.AxisListType.X, op=ALU.add), Bp, Bp)
            TT("dve", pm3, pos3, M2[:, :, :], ALU.mult, Bp + BM, Bp)
            P.op("dve", lambda e: e.tensor_reduce(out=pfall[:, 1, :], in_=pm3, axis=mybir.AxisListType.X, op=ALU.add), Bp, Bp)
            CP("dve", IDX[:, :, :].rearrange("p t c -> p c t"), pfall[:, :, :], Bp, BIDX)
            for tt in range(NT):
                for ch in range(2):
                    P.op("pool", lambda e, tt=tt, ch=ch: e.indirect_dma_start(
                        out=Xs_d[:, :], out_offset=bass.IndirectOffsetOnAxis(ap=IDX[:, tt, ch:ch + 1], axis=0),
                        in_=h2b[:, tt * D:(tt + 1) * D], in_offset=None),
                        [BIDX[tt], Bh2b[tt]], [BXs_t[tt]] if ch else [BXs], dma_key="scat")

        passB()
        if upto == "pb":
            DMA("sp", dbg_d[0:128, 0:64], IDX[:, :, :].rearrange("p t c -> p (t c)").bitcast(F32), BIDX, [Bdbg[0]], "dbgidx")
            DMA("sp", dbg_d[128:256, 0:NSLOT], WI[:, :].bitcast(F32), [BWI], [Bdbg[1]], "dbgwi")
            DMA("sp", dbg_d[256:384, 0:64], Wg[:, :, :].rearrange("p t c -> p (t c)"), BWg, [Bdbg[2]], "dbgwg")

        P.barrier()
        stk.pop().close()
        stk.append(ExitStack())
        NWS = 4
        wsl = [sb(f"wsl{i}", (128, WROW), BF16) for i in range(NWS)]
        Bwsl = [Buf() for _ in range(NWS)]
        xsl = [sb(f"xsl{i}", (128, NSUB, D), BF16) for i in range(2)]
        Bxsl = [Buf() for _ in range(2)]
        XsT = [sb(f"XsT{i}", (128, 8, SLOTR), BF16) for i in range(2)]
        BXsT = [[Buf(), Buf()] for _ in range(2)]
        silb = [sb(f"silb{i}", (128, SLOTR), BF16) for i in range(4)]
        Bsilb = [Buf() for _ in range(4)]
        hidT = [sb(f"hidT{i}", (128, 2, SLOTR), BF16) for i in range(2)]
        BhidT = [Buf() for _ in range(2)]
        ysb = [sb(f"ysb{i}", (128, D), F32) for i in range(3)]
        Bysb = [[Buf(), Buf()] for _ in range(3)]
        BYs = [Buf(f"Ys{i}") for i in range(NSLOT)]
        Bout = [Buf(f"out{t}") for t in range(NT)]

        def slot_loop():
            yrot = [0]
            steps = []
            for i in range(NSLOT):
                ws = i % NWS
                r2 = i % 2

                def stL(i=i, ws=ws, r2=r2):
                    P.op("pool", lambda e: e.indirect_dma_start(
                        out=wsl[ws][:, :], out_offset=None, in_=Wb_d[:, :],
                        in_offset=bass.IndirectOffsetOnAxis(ap=WI[:, i:i + 1], axis=0)),
                        [BWI] + BWb, [Bwsl[ws]], dma_key=f"wsl{ws}")
                    DMA("sp", xsl[r2][:, :, :], Xs_d[i * SLOTR:(i + 1) * SLOTR, :].rearrange("(s p) d -> p s d", p=128),
                        [BXs] + BXs_t, [Bxsl[r2]], f"xsl{r2}")

                def stT(i=i, ws=ws, r2=r2):
                    for hb_ in range(2):
                        bk = hb_
                        tv = banks[bk][:].bitcast(BF16)
                        for cc in range(4):
                            c = hb_ * 4 + cc
                            for sub in range(NSUB):
                                TR(tv[:, cc * SLOTR + sub * 128:cc * SLOTR + (sub + 1) * 128], xsl[r2][:, sub, c * 128:(c + 1) * 128],
                                   ident_bf[:], [Bxsl[r2], Bc2], [bankB[bk]])
                        CP("act" if hb_ == 0 else "dve", XsT[r2][:, hb_ * 4:hb_ * 4 + 4, :],
                           tv[:, :].rearrange("p (c t) -> p c t", c=4), [bankB[bk]], [BXsT[r2][hb_]])

                def stAB(i=i, ws=ws, r2=r2):
                    for m in range(2):
                        ba = 2 + 2 * r2 + m
                        sb_ = 2 * r2 + m
                        for c in range(8):
                            MM(banks[ba][:, 0:SLOTR], wsl[ws][:, c * DE + m * 128:c * DE + (m + 1) * 128], XsT[r2][:, c, :], c == 0, c == 7,
                               [Bwsl[ws]] + BXsT[r2], [bankB[ba]])
                        for c in range(8):
                            MM(banks[ba][:, SLOTR:2 * SLOTR], wsl[ws][:, 8 * DE + c * DE + m * 128:8 * DE + c * DE + (m + 1) * 128],
                               XsT[r2][:, c, :], c == 0, c == 7, [Bwsl[ws]] + BXsT[r2], [bankB[ba]])
                        ACT(silb[sb_][:, :], banks[ba][:, 0:SLOTR], AF.Silu, [bankB[ba]], [Bsilb[sb_]])
                        TT("dve", hidT[r2][:, m, :], banks[ba][:, SLOTR:2 * SLOTR], silb[sb_][:, :], ALU.mult,
                           [bankB[ba], Bsilb[sb_]], [BhidT[r2]])

                def stY(i=i, ws=ws, r2=r2):
                    for sub in range(NSUB):
                        yr = yrot[0] % 3
                        yrot[0] += 1
                        for half in range(2):
                            by = 6 + half
                            for m in range(2):
                                MM(banks[by][:, :], hidT[r2][:, m, sub * 128:(sub + 1) * 128],
                                   wsl[ws][:, 16 * DE + m * D + half * 512:16 * DE + m * D + (half + 1) * 512],
                                   m == 0, m == 1, [BhidT[r2], Bwsl[ws]], [bankB[by]])
                            CP("act" if half == 0 else "dve", ysb[yr][:, half * 512:(half + 1) * 512], banks[by][:, :],
                               [bankB[by]], [Bysb[yr][half]])
                        DMA("act", Ys_d[i * SLOTR + sub * 128:i * SLOTR + (sub + 1) * 128, :], ysb[yr][:, :], Bysb[yr], [BYs[i]],
                            f"yst{yr}")
                steps.append([stL, stT, stAB, stY])
            run_pipeline(steps, None)

        def combine():
            def load(tt):
                s3 = tt % NXS
                s2 = tt % 3
                DMA("sp", xs[s3][:], x2_d[tt * 128:(tt + 1) * 128, :], [Bx2d[tt]], [Bxs[s3]], f"xs{s3}")
                P.op("pool", lambda e: e.indirect_dma_start(
                    out=y1[s2][:, :], out_offset=None, in_=Ys_d[:, :],
                    in_offset=bass.IndirectOffsetOnAxis(ap=IDX[:, tt, 0:1], axis=0)),
                    [BIDX[tt]] + BYs, [By1[s2]], dma_key=f"g1_{s2}")
                P.op("pool", lambda e: e.indirect_dma_start(
                    out=y2[s2][:, :], out_offset=None, in_=Ys_d[:, :],
                    in_offset=bass.IndirectOffsetOnAxis(ap=IDX[:, tt, 1:2], axis=0)),
                    [BIDX[tt]] + BYs, [By2[s2]], dma_key=f"g2_{s2}")
            load(0)
            for tt in range(NT):
                s3 = tt % NXS
                s2 = tt % 3
                STT(x3[s2][:, :], y1[s2][:, :], Wg[:, tt, 0:1], xs[s3][:, :], ALU.mult, ALU.add,
                    [By1[s2], BWg[tt], Bxs[s3]], [Bx3[s2]])
                if tt + 1 < NT:
                    load(tt + 1)
                STT(x3[s2][:, :], y2[s2][:, :], Wg[:, tt, 1:2], x3[s2][:, :], ALU.mult, ALU.add,
                    [By2[s2], BWg[tt], Bx3[s2]], [Bx3[s2]])
                rms_rstd(x3[s2][:], junk[:], stat[tt % 2][:, 0:1], stat[tt % 2][:, 1:2], stat[tt % 2][:, 2:3],
                         [Bx3[s2]], Bjunk, Bstat[tt % 2])
                STT(x3[s2][:], x3[s2][:], stat[tt % 2][:, 2:3], g3[:], ALU.mult, ALU.mult,
                    [Bx3[s2], Bstat[tt % 2], Bg3], [Bx3[s2]])
                DMA("act", out_d[tt * 128:(tt + 1) * 128, :], x3[s2][:], [Bx3[s2]], [Bout[tt]], f"outst{s2}")

        if upto not in ("3b", "pb"):
            slot_loop()
        P.barrier()
        stk.pop().close()
        stk.append(ExitStack())
        xs = [sb(f"xsd{i}", (128, D), F32) for i in range(NXS)]
        y1 = [sb(f"y1_{i}", (128, D), F32) for i in range(3)]
        y2 = [sb(f"y2_{i}", (128, D), F32) for i in range(3)]
        By1 = [Buf() for _ in range(3)]
        By2 = [Buf() for _ in range(3)]
        x3 = [sb(f"x3_{i}", (128, D), F32) for i in range(3)]
        Bx3 = [Buf() for _ in range(3)]
        if upto not in ("3b", "pb"):
            combine()

        P.op("sp", None, reads=(Bout if upto is None else []) + (Bdbg if debug else []))
        P.barrier()
        stk.pop().close()
        P.emit()
    return nc


_NC_CACHE = {}


def kernel(x, norm_attn, w_in, b_forget, w_o_sb, w_o_fox, w_out, norm_ffn,
           w_router_group, b_router_group, w_router_expert, b_router_expert,
           w1, w3, w2, norm_final, _debug=False, _upto=None, _cores=8):
    f32 = lambda a: np.ascontiguousarray(np.asarray(a, dtype=np.float32))
    nc = build_program(debug=_debug, upto=_upto)
    shared = {
        "norm_attn": f32(norm_attn).reshape(1, D), "w_in": f32(w_in)[0], "b_forget": f32(b_forget).reshape(1, NH),
        "w_o_sb": f32(w_o_sb)[0], "w_o_fox": f32(w_o_fox)[0], "w_out": f32(w_out)[0],
        "norm_ffn": f32(norm_ffn).reshape(1, D), "w_router_group": f32(w_router_group)[0],
        "b_router_group": f32(b_router_group).reshape(1, 4), "w_router_expert": f32(w_router_expert)[0],
        "b_router_expert": f32(b_router_expert).reshape(1, NE), "w1": f32(w1)[0], "w3": f32(w3)[0], "w2": f32(w2)[0],
        "norm_final": f32(norm_final).reshape(1, D),
    }
    xf = f32(x)
    in_maps = [dict(shared, x=xf[b]) for b in range(_cores)]
    res = run_bass_kernel_spmd(nc, in_maps, core_ids=list(range(_cores)))
    if _debug:
        return (np.stack([res.results[b]["out"] for b in range(_cores)], axis=0),
                np.stack([res.results[b]["dbg"] for b in range(_cores)], axis=0))
    return np.stack([res.results[b]["out"] for b in range(_cores)], axis=0)
```

```python
from contextlib import ExitStack
import numpy as np
import concourse.bass as bass
import concourse.mybir as mybir
from concourse.bass_utils import run_bass_kernel_spmd

F32 = mybir.dt.float32
BF16 = mybir.dt.bfloat16
I32 = mybir.dt.int32
AF = mybir.ActivationFunctionType
ALU = mybir.AluOpType

S = 4096
D = 1024
NT = S // 128
NQ = S // 512
HD = 64
NH = 8
INC = 5128
C_QSB, C_KSB, C_VSB, C_QFX, C_KFX, C_VFX, C_F, C_GSB, C_GFX = 0, 512, 1024, 1536, 2048, 2560, 3072, 3080, 4104
NE = 32
DE = 256
EPS = 1e-6
NEGBIG = -30000.0
FOX_DUMMY = 1
FILL_SB = 0
FILL_FX = 0

ENGS = ("pe", "act", "dve", "pool", "sp")
GEN = 30000


class Buf:
    __slots__ = ("name", "writer", "readers", "excl")

    def __init__(self, name="", excl=False):
        self.name = name
        self.writer = None
        self.readers = []
        self.excl = excl


class Op:
    __slots__ = ("eng", "fn", "idx", "deps", "signal", "sig", "dma", "dkey", "dval")

    def __init__(self, eng, fn, idx, dma):
        self.eng = eng
        self.fn = fn
        self.idx = idx
        self.deps = []
        self.signal = False
        self.sig = None
        self.dma = dma
        self.dkey = None
        self.dval = None


class Prog:
    def __init__(self, nc):
        self.nc = nc
        self.ops = {e: [] for e in ENGS}
        self.known = {e: {f: -1 for f in ENGS} for e in ENGS}
        self.known_dma = {e: {} for e in ENGS}
        self.dma_cnt = {}
        self.last_dma = {}
        self.pending = {e: [] for e in ENGS}

    def barrier(self):
        lasts = []
        for e in ENGS:
            for o in reversed(self.ops[e]):
                if not o.dma and o.fn is not None:
                    lasts.append(o)
                    break
        lasts += list(self.last_dma.values())
        for e in ENGS:
            self.pending[e] = list(lasts)

    def op(self, eng, fn, reads=(), writes=(), dma_key=None, ninc=1):
        o = Op(eng, fn, len(self.ops[eng]), dma_key is not None)
        cand = []
        for b in reads:
            if b.writer is not None:
                cand.append((b.writer, "raw"))
            if b.excl:
                for r in b.readers:
                    if r.eng != eng:
                        cand.append((r, "rar"))
        for b in writes:
            if b.writer is not None:
                cand.append((b.writer, "waw"))
            for r in b.readers:
                cand.append((r, "war"))
        best = {}
        dma_deps = {}
        if self.pending[eng]:
            for p in self.pending[eng]:
                if p.dma or p.eng != eng:
                    cand.append((p, "raw"))
            self.pending[eng] = []
        for (p, kind) in cand:
            if p is o:
                continue
            if p.dma:
                if self.known_dma[eng].get(p.dkey, 0) >= p.dval:
                    continue
                if p.dkey not in dma_deps or dma_deps[p.dkey].dval < p.dval:
                    dma_deps[p.dkey] = p
                continue
            if p.eng == eng:
                if eng == "pe":
                    continue
            if self.known[eng][p.eng] >= p.idx:
                continue
            if p.eng not in best or best[p.eng].idx < p.idx:
                best[p.eng] = p
        for k, p in dma_deps.items():
            o.deps.append(p)
            self.known_dma[eng][k] = p.dval
        for f, p in best.items():
            o.deps.append(p)
            p.signal = True
            self.known[eng][f] = p.idx
        if o.dma:
            o.dkey = dma_key
            self.dma_cnt[dma_key] = self.dma_cnt.get(dma_key, 0) + 16 * ninc
            o.dval = self.dma_cnt[dma_key]
            self.last_dma[dma_key] = o
        for b in reads:
            b.readers.append(o)
        for b in writes:
            b.writer = o
            b.readers = []
        self.ops[eng].append(o)
        return o

    def emit(self):
        nc = self.nc
        with ExitStack() as st:
            sems = {}
            for e in ENGS:
                n = 0
                for o in self.ops[e]:
                    if o.signal and not o.dma:
                        o.sig = n
                        n += 1
                ngen = max(1, (n + GEN - 1) // GEN)
                sems[e] = [st.enter_context(nc.semaphore(f"s_{e}_{g}")) for g in range(ngen)]
            dsem = {}
            for k in self.dma_cnt:
                dsem[k] = st.enter_context(nc.semaphore(f"d_{len(dsem)}"))
            block = st.enter_context(nc.Block())
            handles = {"pe": block.tensor, "act": block.scalar, "dve": block.vector,
                       "pool": block.gpsimd, "sp": block.sync}

            def run(e):
                ops = self.ops[e]
                if not ops:
                    return

                def body(eng):
                    for o in ops:
                        for p in o.deps:
                            if p.dma:
                                eng.wait_ge(dsem[p.dkey], p.dval)
                            else:
                                eng.wait_ge(sems[p.eng][p.sig // GEN], p.sig % GEN + 1)
                        if o.fn is None:
                            continue
                        ins = o.fn(eng)
                        if o.dma:
                            if not isinstance(ins, (list, tuple)):
                                ins = [ins]
                            for i_ in ins:
                                i_.then_inc(dsem[o.dkey], 16)
                        elif o.signal:
                            ins.then_inc(sems[e][o.sig // GEN], 1)
                handles[e](body)
            for e in ENGS:
                run(e)


def build_program(debug=False, upto=None):
    nc = bass.Bass("TRN2", target_bir_lowering=False)
    dram_in = lambda n, s: nc.dram_tensor(n, list(s), F32, kind="ExternalInput").ap()
    x_d = dram_in("x", (S, D))
    norm_attn_d = dram_in("norm_attn", (1, D))
    w_in_d = dram_in("w_in", (D, INC))
    b_forget_d = dram_in("b_forget", (1, NH))
    w_o_sb_d = dram_in("w_o_sb", (512, D))
    w_o_fox_d = dram_in("w_o_fox", (512, D))
    w_out_d = dram_in("w_out", (D, D))
    norm_ffn_d = dram_in("norm_ffn", (1, D))
    w_rg_d = dram_in("w_router_group", (D, 4))
    b_rg_d = dram_in("b_router_group", (1, 4))
    w_re_d = dram_in("w_router_expert", (D, NE))
    b_re_d = dram_in("b_router_expert", (1, NE))
    w1_d = dram_in("w1", (NE, D, DE))
    w3_d = dram_in("w3", (NE, D, DE))
    w2_d = dram_in("w2", (NE, DE, D))
    norm_final_d = dram_in("norm_final", (1, D))
    out_d = nc.dram_tensor("out", [S, D], F32, kind="ExternalOutput").ap()
    oT_d = nc.dram_tensor("oT_scr", [16, HD, S], BF16, kind="Internal").ap()
    x2_d = nc.dram_tensor("x2_scr", [S, D], F32, kind="Internal").ap()
    NSLOT = 64
    SLOTR = 256
    NSUB = SLOTR // 128
    SHIFT = 8
    WROW = 2 * 8 * DE + 2 * D
    Wb_d = nc.dram_tensor("Wb_scr", [NE * 128, WROW], BF16, kind="Internal").ap()
    Xs_d = nc.dram_tensor("Xs_scr", [NSLOT * SLOTR, D], BF16, kind="Internal").ap()
    Ys_d = nc.dram_tensor("Ys_scr", [NSLOT * SLOTR, D], F32, kind="Internal").ap()
    cparts_d = nc.dram_tensor("cparts_scr", [NH, 3, S], BF16, kind="Internal").ap()
    ncparts_d = nc.dram_tensor("ncparts_scr", [NH, 3, S], BF16, kind="Internal").ap()
    dbg_d = None
    if debug:
        dbg_d = nc.dram_tensor("dbg", [S, D], F32, kind="ExternalOutput").ap()

    P = Prog(nc)
    with ExitStack() as st:
        stk = [st]

        def sb(name, shape, dt):
            return stk[-1].enter_context(nc.sbuf_tensor(name, list(shape), dt))

        banks = [st.enter_context(nc.psum_tensor(f"bank{i}", [128, 512], F32)) for i in range(8)]
        bankB = [Buf(f"bank{i}", excl=True) for i in range(8)]

        def MM(out, lhsT, rhs, start, stop, reads, writes, **kw):
            return P.op("pe", lambda e: e.matmul(out, lhsT=lhsT, rhs=rhs, start=start, stop=stop, **kw), reads, writes)

        def TR(out, in_, ident, reads, writes):
            return P.op("pe", lambda e: e.transpose(out=out, in_=in_, identity=ident), reads, writes)

        def ACT(out, in_, func, reads, writes, **kw):
            return P.op("act", lambda e: e.activation(out=out, in_=in_, func=func, **kw), reads, writes)

        def TT(eng, out, in0, in1, op, reads, writes):
            return P.op(eng, lambda e: e.tensor_tensor(out=out, in0=in0, in1=in1, op=op), reads, writes)

        def TS(eng, out, in0, s1, s2, op0, op1, reads, writes, **kw):
            if op1 is None:
                return P.op(eng, lambda e: e.tensor_scalar(out=out, in0=in0, scalar1=s1, scalar2=None, op0=op0, **kw), reads, writes)
            return P.op(eng, lambda e: e.tensor_scalar(out=out, in0=in0, scalar1=s1, scalar2=s2, op0=op0, op1=op1, **kw), reads, writes)

        def STT(out, in0, scalar, in1, op0, op1, reads, writes):
            return P.op("dve", lambda e: e.scalar_tensor_tensor(out=out, in0=in0, scalar=scalar, in1=in1, op0=op0, op1=op1), reads, writes)

        def CP(eng, out, in_, reads, writes):
            if eng == "act":
                return P.op("act", lambda e: e.copy(out=out, in_=in_), reads, writes)
            return P.op(eng, lambda e: e.tensor_copy(out=out, in_=in_), reads, writes)

        def MEMSET(eng, ap, val, writes):
            return P.op(eng, lambda e: e.memset(ap, val), (), writes)

        def DMA(q, out, in_, reads, writes, key, **kw):
            return P.op(q, lambda e: e.dma_start(out=out, in_=in_, **kw), reads, writes, dma_key=key)

        ones_bf = sb("ones_bf", (128, 128), BF16)
        negones_bf = sb("negones_bf", (128, 128), BF16)
        negbig_bf = sb("negbig_bf", (128, 128), BF16)
        ident_bf = sb("ident_bf", (128, 128), BF16)
        negbigI = sb("negbigI", (128, 128), BF16)
        negtri = sb("negtri", (128, 128), BF16)
        ones513 = sb("ones513", (128, 513), BF16)
        M0 = sb("M0", (128, 513), BF16)
        ones_f32 = sb("ones_f32", (128, 128), F32)
        ident_f32 = sb("ident_f32", (128, 128), F32)
        eps_t = sb("eps_t", (128, 1), F32)
        Bc = Buf("consts")
        Bg1 = Buf("g1")

        P.op("pool", lambda e: e.memset(ones_bf[:], 1.0), (), [Bc])
        P.op("pool", lambda e: e.memset(negones_bf[:], -1.0), (), [Bc])
        P.op("pool", lambda e: e.memset(negbig_bf[:], NEGBIG), (), [Bc])
        P.op("pool", lambda e: e.memset(ones513[:], 1.0), (), [Bc])
        P.op("pool", lambda e: e.memset(ones_f32[:], 1.0), (), [Bc])
        P.op("pool", lambda e: e.memset(eps_t[:], EPS), (), [Bc])
        Bc2 = Buf("consts2")
        P.op("pool", lambda e: e.affine_select(out=ident_bf[:], in_=ones_bf[:], pattern=[[-1, 128]], compare_op=ALU.is_equal,
                                               fill=0.0, base=0, channel_multiplier=1), [Bc], [Bc2])
        P.op("pool", lambda e: e.affine_select(out=ident_f32[:], in_=ones_f32[:], pattern=[[-1, 128]], compare_op=ALU.is_equal,
                                               fill=0.0, base=0, channel_multiplier=1), [Bc], [Bc2])
        P.op("pool", lambda e: e.affine_select(out=negbigI[:], in_=negbig_bf[:], pattern=[[-1, 128]], compare_op=ALU.is_equal,
                                               fill=0.0, base=0, channel_multiplier=1), [Bc], [Bc2])
        P.op("pool", lambda e: e.affine_select(out=negtri[:], in_=negones_bf[:], pattern=[[-1, 128]], compare_op=ALU.is_ge,
                                               fill=0.0, base=0, channel_multiplier=1), [Bc], [Bc2])
        P.op("pool", lambda e: e.affine_select(out=M0[:], in_=ones513[:], pattern=[[-1, 513]], compare_op=ALU.is_ge,
                                               fill=0.0, base=0, channel_multiplier=1), [Bc], [Bc2])
        P.op("pool", lambda e: e.affine_select(out=SL_bf[:], in_=ones_bf[:], pattern=[[1, 128]], compare_op=ALU.is_ge,
                                               fill=0.0, base=-1, channel_multiplier=-1), [Bc], [Bc2])

        hT = sb("hT", (128, 8, S), BF16)
        BhT = [Buf(f"hT{t}") for t in range(NQ)]
        g3 = sb("g3", (128, D), F32)
        M1 = sb("M1", (128, NT, NE), BF16)
        M2 = sb("M2", (128, NT, NE), BF16)
        Rk = sb("Rk", (128, NT, NE), F32)
        Wg = sb("Wg", (128, NT, 2), F32)
        IDX = sb("IDX", (128, NT, 2), I32)
        WI = sb("WI", (128, 64), I32)
        SL_bf = sb("SL_bf", (128, 128), BF16)

        def rms_rstd(xs_ap, junk_ap, ss_ap, ln_ap, rstd_ap, rd, wr_junk, wr_stat):
            P.op("act", lambda e: e.activation(out=junk_ap, in_=xs_ap, func=AF.Square, accum_out=ss_ap), rd, [wr_junk, wr_stat])
            ACT(ln_ap, ss_ap, AF.Ln, [wr_stat, Bc], [wr_stat], scale=1.0 / D, bias=eps_t[:, 0:1])
            ACT(rstd_ap, ln_ap, AF.Exp, [wr_stat], [wr_stat], scale=-0.5)

        NXS = 2
        Bxs = [Buf(f"xs{i}") for i in range(NXS)]
        junk = sb("junk", (128, D), BF16)
        Bjunk = Buf("junk")
        stat = [sb(f"stat{i}", (128, 4), F32) for i in range(2)]
        Bstat = [Buf(f"stat{i}") for i in range(2)]
        stk.append(ExitStack())
        g1 = sb("g1", (128, D), F32)
        DMA("sp", g1[:], norm_attn_d[0:1, :].partition_broadcast(128), (), [Bg1], "g1")
        xs = [sb(f"xsa{i}", (128, D), F32) for i in range(NXS)]
        hb = [sb(f"hb{i}", (128, D), BF16) for i in range(2)]
        Bhb = [Buf(f"hb{i}") for i in range(2)]

        def run_pipeline(stage_lists, bg, every=6):
            n = len(stage_lists)
            nst = max(len(x) for x in stage_lists)
            bgi = 0
            for s in range(n + nst - 1):
                for k in range(nst):
                    t = s - k
                    if 0 <= t < n and k < len(stage_lists[t]) and stage_lists[t][k] is not None:
                        stage_lists[t][k]()
                if bg and s % every == 3 and bgi < len(bg):
                    bg[bgi]()
                    bgi += 1
            while bg and bgi < len(bg):
                bg[bgi]()
                bgi += 1

        def phase1():
            def front(tt):
                s3 = tt % NXS
                s2 = tt % 2
                DMA("sp", xs[s3][:], x_d[tt * 128:(tt + 1) * 128, :], (), [Bxs[s3]], f"xs{s3}")
                rms_rstd(xs[s3][:], junk[:], stat[s2][:, 0:1], stat[s2][:, 1:2], stat[s2][:, 2:3],
                         [Bxs[s3]], Bjunk, Bstat[s2])
                STT(hb[s2][:], xs[s3][:], stat[s2][:, 2:3], g1[:], ALU.mult, ALU.mult,
                    [Bxs[s3], Bstat[s2], Bg1], [Bhb[s2]])

            def back(tt):
                s2 = tt % 2
                bk = 6 + (tt % 2)
                pv = banks[bk][:].bitcast(BF16)
                for c in range(8):
                    TR(pv[:, c * 128:(c + 1) * 128], hb[s2][:, c * 128:(c + 1) * 128], ident_bf[:],
                       [Bhb[s2], Bc2], [bankB[bk]])
                CP("act", hT[:, :, tt * 128:(tt + 1) * 128], pv[:, :].rearrange("p (c t) -> p c t", c=8),
                   [bankB[bk]], [BhT[tt // 4]])
            steps = [[lambda tt=tt: front(tt), lambda tt=tt: back(tt)] for tt in range(NT)]
            run_pipeline(steps, None)

        phase1()
        P.barrier()
        stk.pop().close()

        NSL = 3
        stk.append(ExitStack())
        QTa = [sb(f"QTa{i}", (128, S), BF16) for i in range(NSL)]
        KTa = [sb(f"KTa{i}", (128, S), BF16) for i in range(NSL)]
        Vt = [sb(f"Vt{i}", (128, NT, HD + 1), BF16) for i in range(NSL)]
        wqk = [sb(f"wqk{i}", (128, 8, 130), BF16) for i in range(NSL)]
        wv = [sb(f"wv{i}", (128, 8, HD), BF16) for i in range(NSL)]
        BQ = [Buf(f"Q{i}") for i in range(NSL)]
        BK = [Buf(f"K{i}") for i in range(NSL)]
        BQaug = [Buf(f"Qaug{i}") for i in range(NSL)]
        BKaug = [Buf(f"Kaug{i}") for i in range(NSL)]
        BV = [Buf(f"V{i}") for i in range(NSL)]
        BVones = [Buf(f"Vones{i}") for i in range(NSL)]
        Bwqk = [Buf(f"wqk{i}") for i in range(NSL)]
        Bwv = [Buf(f"wv{i}") for i in range(NSL)]
        ostS = sb("ostS", (64, S), BF16)
        ostF = sb("ostF", (64, S), BF16)
        BostS = Buf("ostS")
        BostF = Buf("ostF")
        wstage = [sb(f"wstage{i}", (128, 2048), BF16) for i in range(1)]
        Bwstage = [Buf(f"wstage{i}") for i in range(1)]
        BWb = [Buf(f"Wb{e}") for e in range(NE)]

        def conv_expert(ex):
            def f():
                srcs = [w1_d[ex].rearrange("(c p) n -> p c n", p=128), w3_d[ex].rearrange("(c p) n -> p c n", p=128),
                        w2_d[ex].rearrange("(k p) n -> p k n", p=128)]
                pats = ["p (c n) -> p c n", "p (c n) -> p c n", "p (k n) -> p k n"]
                for part in range(3):
                    kw = {"c": 8} if part < 2 else {"k": 2}
                    P.op("pool", lambda e, part=part, kw=kw: e.dma_start(
                        out=wstage[0][:, :].rearrange(pats[part], **kw), in_=srcs[part]),
                        (), [Bwstage[0]], dma_key="wstg0")
                    DMA("sp", Wb_d[ex * 128:(ex + 1) * 128, part * 2048:(part + 1) * 2048], wstage[0][:, :],
                        [Bwstage[0]], [BWb[ex]], "wbst0")
            return f
        BoT_d = [Buf(f"oTd{h}") for h in range(16)]

        for i in range(NSL):
            P.op("pool", lambda e, i=i: e.memset(Vt[i][:, :, HD:HD + 1], 1.0), (), [BVones[i]])
            P.op("pool", lambda e, i=i: e.memset(wqk[i][:, :, 128:130], 0.0), (), [BVones[i]])

        e_sb = [sb(f"e_sb{i}", (128, 512), F32) for i in range(2)]
        sp_bf = [sb(f"sp_bf{i}", (128, 512), BF16) for i in range(2)]
        arg_sb = [sb(f"arg_sb{i}", (128, 512), F32) for i in range(2)]
        A_bf = [sb(f"A_bf{i}", (128, 512), BF16) for i in range(3)]
        P_bf = [sb(f"P_bf{i}", (128, 512), BF16) for i in range(3)]
        BPb = [Buf() for _ in range(3)]
        oraw = [sb(f"oraw{i}", (128, 512), F32) for i in range(2)]
        Boraw = [Buf() for _ in range(2)]
        C_sb = [sb(f"C_sb{i}", (128, 512), F32) for i in range(2)]
        Be = [Buf() for _ in range(2)]
        Bsp = [Buf() for _ in range(2)]
        Barg = [Buf() for _ in range(2)]
        BA = [Buf() for _ in range(3)]
        BC = [Buf() for _ in range(2)]

        wf = sb("wf", (128, 8, NH), BF16)
        Bwf = Buf("wf")
        fexp = [sb(f"fexp{i}", (NH, 512), F32) for i in range(1)] * 2
        cpt = [sb(f"cpt{i}", (NH, 512), F32) for i in range(2)]
        r1 = fexp
        cpp = [sb(f"cpp{i}", (NH, 3, 512), BF16) for i in range(1)] * 2
        negb = sb("negb", (NH, 1), F32)
        Bnegb = Buf("negb")
        Bfexp = [Buf()] * 2
        Bcpt = [Buf() for _ in range(2)]
        Br1 = Bfexp
        Bcpp = [Buf()] * 2
        Bncpp = [Buf()] * 2
        Bcparts = [Buf(f"cparts_d{t}") for t in range(NQ)]

        def load_head_weights(hd, sl):
            typ, h = divmod(hd, NH)
            cq = (C_QSB if typ == 0 else C_QFX) + h * HD
            ck = (C_KSB if typ == 0 else C_KFX) + h * HD
            cv = (C_VSB if typ == 0 else C_VFX) + h * HD
            P.op("pool", lambda e: [
                e.dma_start(out=wqk[sl][:, :, 0:HD], in_=w_in_d[:, cq:cq + HD].rearrange("(c p) n -> p c n", p=128)),
                e.dma_start(out=wqk[sl][:, :, HD:2 * HD], in_=w_in_d[:, ck:ck + HD].rearrange("(c p) n -> p c n", p=128)),
            ], (), [Bwqk[sl]], dma_key=f"wqk{sl}", ninc=2)
            P.op("pool", lambda e: e.dma_start(out=wv[sl][:, :, :], in_=w_in_d[:, cv:cv + HD].rearrange("(c p) n -> p c n", p=128)),
                 (), [Bwv[sl]], dma_key=f"wv{sl}")

        pj_rot = [0]

        def proj_chunks(hd, sl):
            typ, h = divmod(hd, NH)
            chunks = []
            bk = 7

            def qk_chunk(T, which):
                lo = 0 if which == "q" else HD
                out = []
                for c in range(8):
                    out.append(lambda c=c: MM(banks[bk][0:HD + 1, :], wqk[sl][:, c, lo:lo + HD + 1],
                                              hT[:, c, T * 512:(T + 1) * 512], c == 0, c == 7,
                                              [Bwqk[sl], BhT[T], BVones[sl]], [bankB[bk]]))
                if which == "q":
                    out.append(lambda: TS("dve", QTa[sl][0:HD, T * 512:(T + 1) * 512], banks[bk][0:HD, :], 0.125, None,
                                          ALU.mult, None, [bankB[bk]], [BQ[sl]]))
                else:
                    out.append(lambda: CP("dve", KTa[sl][0:HD, T * 512:(T + 1) * 512], banks[bk][0:HD, :],
                                          [bankB[bk]], [BK[sl]]))
                return out

            def v_chunk(g):
                out = []
                for u in range(8):
                    def f(u=u):
                        tt = g * 8 + u
                        for c in range(8):
                            MM(banks[bk][:, u * HD:(u + 1) * HD], hT[:, c, tt * 128:(tt + 1) * 128], wv[sl][:, c, :],
                               c == 0, c == 7, [Bwv[sl], BhT[tt // 4]], [bankB[bk]])
                    out.append(f)
                out.append(lambda: CP("dve", Vt[sl][:, g * 8:(g + 1) * 8, 0:HD],
                                      banks[bk][:, :].rearrange("p (u d) -> p u d", u=8), [bankB[bk]], [BV[sl]]))
                return out

            for T in range(NQ):
                chunks += qk_chunk(T, "k")
            for g in range(4):
                chunks += v_chunk(g)
            for T in range(NQ):
                chunks += qk_chunk(T, "q")
            if typ == 0:
                def zpad():
                    P.op("pool", lambda e: e.memset(QTa[sl][64:128, :], 0.0), (), [BQaug[sl]])
                    P.op("pool", lambda e: e.memset(KTa[sl][64:128, :], 0.0), (), [BKaug[sl]])
                chunks.append(zpad)
            if typ == 1:
                def aug():
                    P.op("pool", lambda e: e.memset(QTa[sl][64:70, :], 1.0), (), [BQaug[sl]])
                    P.op("pool", lambda e: e.memset(KTa[sl][64:70, :], -1.0), (), [BKaug[sl]])
                    DMA("sp", QTa[sl][64:67, :], cparts_d[h, :, :], Bcparts, [BQaug[sl]], f"qaug{sl}")
                    DMA("sp", KTa[sl][67:70, :], cparts_d[h, :, :], Bcparts, [BKaug[sl]], f"kaug{sl}")
                chunks.append(aug)
            return chunks

        def fox_prep():
            P.op("pool", lambda e: e.dma_start(out=wf[:, :, :], in_=w_in_d[:, C_F:C_F + NH].rearrange("(c p) n -> p c n", p=128)),
                 (), [Bwf], dma_key="wf")
            DMA("sp", negb[:, 0:1], b_forget_d[0:1, :].rearrange("o h -> h o"), (), [Bnegb], "negb")
            TS("dve", negb[:, 0:1], negb[:, 0:1], -1.0, None, ALU.mult, None, [Bnegb], [Bnegb])
            for T in range(NQ):
                bk = 7
                s2 = T % 2
                for c in range(8):
                    MM(banks[bk][0:NH, :], wf[:, c, :], hT[:, c, T * 512:(T + 1) * 512], c == 0, c == 7,
                       [Bwf, BhT[T]], [bankB[bk]])
                ACT(fexp[s2][:, :], banks[bk][0:NH, :], AF.Exp, [bankB[bk], Bnegb], [Bfexp[s2]],
                    scale=-1.0, bias=negb[:, 0:1])
                ACT(fexp[s2][:, :], fexp[s2][:, :], AF.Ln, [Bfexp[s2]], [Bfexp[s2]], bias=1.0)
                init = 0.0 if T == 0 else cpt[1 - s2][:, 511:512]
                rds = [Bfexp[s2]] + ([Bcpt[1 - s2]] if T > 0 else [])
                P.op("dve", lambda e, s2=s2, init=init: e.tensor_tensor_scan(
                    out=cpt[s2][:, :], data0=fexp[s2][:, :], data1=fexp[s2][:, :], initial=init,
                    op0=ALU.add, op1=ALU.max), rds, [Bcpt[s2]])
                CP("dve", cpp[s2][:, 0, :], cpt[s2][:, :], [Bcpt[s2]], [Bcpp[s2]])
                TT("dve", r1[s2][:, :], cpt[s2][:, :], cpp[s2][:, 0, :], ALU.subtract, [Bcpt[s2], Bcpp[s2]], [Br1[s2]])
                CP("dve", cpp[s2][:, 1, :], r1[s2][:, :], [Br1[s2]], [Bcpp[s2]])
                TT("dve", r1[s2][:, :], r1[s2][:, :], cpp[s2][:, 1, :], ALU.subtract, [Br1[s2], Bcpp[s2]], [Br1[s2]])
                CP("dve", cpp[s2][:, 2, :], r1[s2][:, :], [Br1[s2]], [Bcpp[s2]])
                P.op("sp", lambda e, s2=s2, T=T: [
                    e.dma_start(out=cparts_d[:, :, T * 512:(T + 1) * 512], in_=cpp[s2][:, :, :]),
                ], [Bcpp[s2]], [Bcparts[T]], dma_key="cpst0", ninc=1)

        ZB = [0, 1]
        AB = [2, 3]
        CSB = 4
        OB = 5

        cnt = {"z": 0, "a": 0, "A": 0, "c": 0, "e": 0}

        def sb_steps(hd, sl):
            steps = []
            ZA = [0, 1]
            CSB_ = 2
            ob = 3
            for j in range(NQ):
                cslot = cnt["c"] % 2
                cnt["c"] += 1
                order = list(range(4 * j + 3, -1, -1))
                for n_, i in enumerate(order):
                    m = i - 4 * j
                    off = max(0, m) * 128
                    diag = m >= 0
                    first = n_ == 0
                    last = i == 0
                    zs = cnt["z"] % 2
                    cnt["z"] += 1
                    As = cnt["A"] % 3
                    cnt["A"] += 1
                    kT = KTa[sl][0:128, i * 128:(i + 1) * 128]
                    qT = QTa[sl][0:128, j * 512 + off:(j + 1) * 512]
                    msk = M0[:, 0:512 - off]
                    W = slice(off, 512)

                    def st0(zs=zs, kT=kT, qT=qT, msk=msk, W=W, diag=diag, first=first, cslot=cslot, off=off):
                        if first:
                            MEMSET("pool", C_sb[cslot][:], 0.0, [BC[cslot]])
                        zb = ZA[zs]
                        MM(banks[zb][:, W], kT, qT, True, not diag, [BK[sl], BQ[sl], BKaug[sl], BQaug[sl]], [bankB[zb]])
                        if diag:
                            MM(banks[zb][:, off:off + 128], negbigI[:], M0[:, 0:128], False, True, [Bc2], [bankB[zb]])
                        ACT(e_sb[zs][:, W], banks[zb][:, W], AF.Exp, [bankB[zb]], [Be[zs]])
                        ACT(sp_bf[zs][:, W], e_sb[zs][:, W], AF.Ln, [Be[zs]], [Bsp[zs]], bias=1.0)

                    def st1(zs=zs, As=As, W=W, first=first, last=last, cslot=cslot):
                        ab = ZA[zs]
                        MM(banks[ab][:, W], negtri[:], sp_bf[zs][:, W], False, True, [Bsp[zs], Bc2], [bankB[ab]],
                           skip_group_check=True)
                        if not last:
                            MM(banks[CSB_][:, W], negones_bf[:], sp_bf[zs][:, W], True, True, [Bsp[zs], Bc], [bankB[CSB_]])
                        if first:
                            ACT(A_bf[As][:, W], banks[ab][:, W], AF.Exp, [bankB[ab]], [BA[As]])
                        else:
                            TT("dve", arg_sb[zs][:, W], banks[ab][:, W], C_sb[cslot][:, W], ALU.add,
                               [bankB[ab], BC[cslot]], [Barg[zs]])
                            ACT(A_bf[As][:, W], arg_sb[zs][:, W], AF.Exp, [Barg[zs]], [BA[As]])
                        if not last:
                            TT("dve", C_sb[cslot][:, W], banks[CSB_][:, W], C_sb[cslot][:, W], ALU.add,
                               [bankB[CSB_], BC[cslot]], [BC[cslot]])
                        for _ in range(FILL_SB):
                            MM(banks[7][:, :], negtri[:], M0[:, 0:512], True, True, [Bc2], [bankB[7]])

                    def st2(As=As, i=i, j=j, W=W, first=first, last=last):
                        MM(banks[ob][0:HD + 1, W], Vt[sl][:, i, 0:HD + 1], A_bf[As][:, W], first, last,
                           [BV[sl], BVones[sl], BA[As]], [bankB[ob]], skip_group_check=True)
                        if last:
                            CP("dve", ostS[0:HD, j * 512:(j + 1) * 512], banks[ob][0:HD, :], [bankB[ob]], [BostS])
                            if j == NQ - 1:
                                DMA("sp", oT_d[hd, :, :], ostS[:, :], [BostS], [BoT_d[hd]], "ostS")
                    steps.append([st0, st1, st2])
            return steps

        def fox_steps(hd, sl):
            steps = []
            SBK = [4, 5]
            ob = 6
            MB = 2
            for j in range(NQ):
                nk = 4 * j + 4
                orot = j % 2
                for i in range(nk):
                    m = i - 4 * j
                    off = max(0, m) * 128
                    diag = m >= 0
                    first = i == 0
                    last = i == nk - 1
                    zs = cnt["fz"] % 2
                    cnt["fz"] += 1
                    As = cnt["fA"] % 3
                    cnt["fA"] += 1
                    kT = KTa[sl][0:70, i * 128:(i + 1) * 128]
                    qT = QTa[sl][0:70, j * 512 + off:(j + 1) * 512]
                    msk = M0[:, 1:1 + 512 - off]
                    W = slice(off, 512)

                    def st0(zs=zs, As=As, kT=kT, qT=qT, msk=msk, W=W, diag=diag, off=off):
                        zb = SBK[zs]
                        MM(banks[zb][:, W], kT, qT, True, not diag, [BK[sl], BQ[sl], BKaug[sl], BQaug[sl]], [bankB[zb]])
                        if diag:
                            MM(banks[zb][:, off:off + 128], negbigI[:], M0[:, 1:129], False, True, [Bc2], [bankB[zb]])

                    def stE(zs=zs, As=As, W=W):
                        zb = SBK[zs]
                        ACT(P_bf[As][:, W], banks[zb][:, W], AF.Exp, [bankB[zb]], [BPb[As]])

                    def st1(As=As, i=i, j=j, W=W, first=first, last=last, orot=orot):
                        MM(banks[ob][0:HD + 1, W], Vt[sl][:, i, 0:HD + 1], P_bf[As][:, W], first, last,
                           [BV[sl], BVones[sl], BPb[As]], [bankB[ob]])
                        if last:
                            CP("dve", oraw[orot][0:HD + 1, :], banks[ob][0:HD + 1, :], [bankB[ob]], [Boraw[orot]])

                    def stN(j=j, orot=orot, last=last):
                        if not last:
                            return
                        ACT(oraw[orot][64:65, :], oraw[orot][64:65, :], AF.Ln, [Boraw[orot]], [Boraw[orot]])
                        ACT(oraw[orot][64:65, :], oraw[orot][64:65, :], AF.Exp, [Boraw[orot]], [Boraw[orot]], scale=-1.0)

                    def stM(j=j, orot=orot, last=last):
                        if not last:
                            return
                        MM(banks[MB][0:HD, :], ones_f32[64:65, 0:HD], oraw[orot][64:65, :], True, True,
                           [Boraw[orot], Bc], [bankB[MB]])
                        TT("dve", ostF[0:HD, j * 512:(j + 1) * 512], banks[MB][0:HD, :], oraw[orot][0:HD, :], ALU.mult,
                           [bankB[MB], Boraw[orot]], [BostF])
                        if j == NQ - 1:
                            DMA("sp", oT_d[hd, :, :], ostF[:, :], [BostF], [BoT_d[hd]], "ostF")
                    steps.append([st0, stE, st1, stN, stM])
            return steps

        cnt["fz"] = 0
        cnt["fA"] = 0
        HS = 144
        LAG = HS // 2
        seq = []
        for h in range(NH):
            seq += [h, NH + h]
        sched = {}

        def add_bg(it, f):
            sched.setdefault(max(it, 0), []).append(f)

        for n, hd in enumerate(seq):
            sl = n % 3
            chunks = [lambda hd=hd, sl=sl: load_head_weights(hd, sl)]
            if n == 1:
                chunks.append(fox_prep)
            chunks += proj_chunks(hd, sl)
            k1 = len(chunks) // 3
            chunks = chunks[:k1] + [conv_expert(2 * n)] + chunks[k1:2 * k1] + [conv_expert(2 * n + 1)] + chunks[2 * k1:]
            h = n // 2
            start = HS * h if n % 2 == 0 else HS * h + LAG
            base = start - LAG + 5
            nch = len(chunks)
            for ci, f in enumerate(chunks):
                add_bg(base + (ci * (LAG - 8)) // nch, f)
        sb_lists = {}
        fx_lists = {}

        def sb_step(sidx):
            if sidx < 0 or sidx >= HS * NH:
                return None
            h, r = divmod(sidx, HS)
            if h not in sb_lists:
                sb_lists[h] = sb_steps(h, (2 * h) % 3)
                sb_lists.pop(h - 2, None)
            return sb_lists[h][r]

        def fx_step(fidx):
            if fidx < 0 or fidx >= HS * NH:
                return None
            h, r = divmod(fidx, HS)
            if h not in fx_lists:
                fx_lists[h] = fox_steps(NH + h, (2 * h + 1) % 3)
                fx_lists.pop(h - 2, None)
            return fx_lists[h][r]

        def run_stage(stp, k):
            if stp is not None and stp[k] is not None:
                stp[k]()

        for it in sorted(k for k in sched if k <= 0):
            for f in sched.pop(it):
                f()
        total_p = HS * NH + LAG
        for p_ in range(total_p + 5):
            f_ = p_ - LAG
            run_stage(fx_step(f_ - 1), 1)
            run_stage(sb_step(p_), 0)
            run_stage(fx_step(f_), 0)
            run_stage(sb_step(p_ - 1), 1)
            run_stage(fx_step(f_ - 1), 2)
            run_stage(sb_step(p_ - 2), 2)
            run_stage(fx_step(f_ - 2), 3)
            run_stage(fx_step(f_ - 3), 4)
            for f in sched.pop(p_, []):
                f()
        for it in sorted(sched):
            for f in sched[it]:
                f()

        P.barrier()
        stk.pop().close()
        stk.append(ExitStack())
        xs = [sb(f"xsb{i}", (128, D), F32) for i in range(NXS)]
        wosb = sb("wosb", (128, 4, D), BF16)
        wofx = sb("wofx", (128, 4, D), BF16)
        wout = sb("wout", (128, 8, D), BF16)
        wg = sb("wg", (128, 8, 2 * D), BF16)
        Bw3 = Buf("w3")
        P.op("pool", lambda e: [
            e.dma_start(out=wosb[:, :, :], in_=w_o_sb_d.rearrange("(c p) n -> p c n", p=128)),
            e.dma_start(out=wofx[:, :, :], in_=w_o_fox_d.rearrange("(c p) n -> p c n", p=128)),
            e.dma_start(out=wout[:, :, :], in_=w_out_d.rearrange("(c p) n -> p c n", p=128)),
        ] + [e.dma_start(out=wg[:, c, :], in_=w_in_d[c * 128:(c + 1) * 128, C_GSB:C_GSB + 2 * D]) for c in range(8)],
            (), [Bw3], dma_key="w3", ninc=11)
        oTt = [sb(f"oTt{i}", (128, 8, 512), BF16) for i in range(2)]
        BoTt = [Buf() for _ in range(2)]
        sig = [sb(f"sig{i}", (128, 512), BF16) for i in range(4)]
        Bsig = [Buf() for _ in range(4)]
        tmp = [sb(f"tmp{i}", (128, 512), F32) for i in range(2)]
        Btmp = [Buf() for _ in range(2)]
        mixT = [sb(f"mixT{i}", (128, 8, 512), BF16) for i in range(2)]
        BmixT = [Buf() for _ in range(2)]
        x2s = [sb(f"x2s{i}", (128, D), F32) for i in range(2)]
        Bx2s = [Buf() for _ in range(2)]
        Bx2d = [Buf(f"x2d{t}") for t in range(NT)]
        Bdbg = [Buf(f"dbg{t}") for t in range(NT)]

        def phase3():
            def load_oT(T):
                s2 = T % 2
                P.op("sp", lambda e: e.dma_start(
                    out=oTt[s2][:, :, :],
                    in_=oT_d[:, :, T * 512:(T + 1) * 512].rearrange("(c two) d t -> (two d) c t", two=2)),
                    BoT_d, [BoTt[s2]], dma_key=f"oTt{s2}")
            load_oT(0)
            for T in range(NQ):
                s2 = T % 2
                if T + 1 < NQ:
                    load_oT(T + 1)
                for dc in range(8):
                    dsl = slice(dc * 128, (dc + 1) * 128)
                    b0 = 0 if dc % 2 == 0 else 4
                    for c in range(4):
                        MM(banks[b0][:, :], wosb[:, c, dsl], oTt[s2][:, c, :], c == 0, c == 3, [Bw3, BoTt[s2]], [bankB[b0]])
                    for c in range(4):
                        MM(banks[b0 + 1][:, :], wofx[:, c, dsl], oTt[s2][:, 4 + c, :], c == 0, c == 3, [Bw3, BoTt[s2]], [bankB[b0 + 1]])
                    for c in range(8):
                        MM(banks[b0 + 2][:, :], wg[:, c, dsl], hT[:, c, T * 512:(T + 1) * 512], c == 0, c == 7,
                           [Bw3, BhT[T]], [bankB[b0 + 2]])
                    for c in range(8):
                        MM(banks[b0 + 3][:, :], wg[:, c, D + dc * 128:D + (dc + 1) * 128], hT[:, c, T * 512:(T + 1) * 512],
                           c == 0, c == 7, [Bw3, BhT[T]], [bankB[b0 + 3]])
                    sa = (dc % 2) * 2
                    ACT(sig[sa][:, :], banks[b0 + 2][:, :], AF.Sigmoid, [bankB[b0 + 2]], [Bsig[sa]])
                    ACT(sig[sa + 1][:, :], banks[b0 + 3][:, :], AF.Sigmoid, [bankB[b0 + 3]], [Bsig[sa + 1]])
                    t2 = dc % 2
                    TT("dve", tmp[t2][:, :], banks[b0][:, :], sig[sa][:, :], ALU.mult, [bankB[b0], Bsig[sa]], [Btmp[t2]])
                    TT("dve", sig[sa + 1][:, :], banks[b0 + 1][:, :], sig[sa + 1][:, :], ALU.mult,
                       [bankB[b0 + 1], Bsig[sa + 1]], [Bsig[sa + 1]])
                    TT("dve", mixT[s2][:, dc, :], tmp[t2][:, :], sig[sa + 1][:, :], ALU.add,
                       [Btmp[t2], Bsig[sa + 1]], [BmixT[s2]])
                for u in range(4):
                    tt = T * 4 + u
                    s3 = tt % NXS
                    xq = tt % 2
                    DMA("sp", xs[s3][:], x_d[tt * 128:(tt + 1) * 128, :], (), [Bxs[s3]], f"xs{s3}")
                    bb = 0 if tt % 2 == 0 else 4
                    for half in range(2):
                        for c in range(8):
                            MM(banks[bb + half][:, :], mixT[s2][:, c, u * 128:(u + 1) * 128], wout[:, c, half * 512:(half + 1) * 512],
                               c == 0, c == 7, [BmixT[s2], Bw3], [bankB[bb + half]])
                    for half in range(2):
                        TT("dve", x2s[xq][:, half * 512:(half + 1) * 512], banks[bb + half][:, :],
                           xs[s3][:, half * 512:(half + 1) * 512], ALU.add, [bankB[bb + half], Bxs[s3]], [Bx2s[xq]])
                    DMA("act", x2_d[tt * 128:(tt + 1) * 128, :], x2s[xq][:], [Bx2s[xq]], [Bx2d[tt]], f"x2st{xq}")
                    if debug and upto is None:
                        DMA("sp", dbg_d[tt * 128:(tt + 1) * 128, :], x2s[xq][:], [Bx2s[xq]], [Bdbg[tt]], f"dbgst{xq}")

        phase3()

        P.barrier()
        stk.pop().close()
        stk.append(ExitStack())
        BIGR = 1.0e4
        xs = [sb(f"xsc{i}", (128, D), F32) for i in range(NXS)]
        g2 = sb("g2", (128, D), F32)
        Bg2 = Buf("g2")
        DMA("sp", g2[:], norm_ffn_d[0:1, :].partition_broadcast(128), (), [Bg2], "g2")
        Bg3 = Buf("g3")
        DMA("sp", g3[:], norm_final_d[0:1, :].partition_broadcast(128), (), [Bg3], "g3")
        wr = sb("wr", (128, 8, 36), F32)
        rbias = sb("rbias", (128, 36), F32)
        Bwr = Buf("wr")
        P.op("sp", lambda e: [
            e.dma_start(out=wr[:, :, 0:4], in_=w_rg_d.rearrange("(c p) n -> p c n", p=128)),
            e.dma_start(out=wr[:, :, 4:36], in_=w_re_d.rearrange("(c p) n -> p c n", p=128)),
            e.dma_start(out=rbias[:, 0:4], in_=b_rg_d[0:1, :].partition_broadcast(128)),
            e.dma_start(out=rbias[:, 4:36], in_=b_re_d[0:1, :].partition_broadcast(128)),
        ], (), [Bwr], dma_key="wr", ninc=4)
        h2b = hT[:].rearrange("p c t -> p (c t)")
        Bh2b = [Buf(f"h2b{t}") for t in range(NT)]
        BM = [Buf(f"M{t}") for t in range(NT)]
        BRk = [Buf(f"Rk{t}") for t in range(NT)]
        BWg = [Buf(f"Wg{t}") for t in range(NT)]
        h2f = [sb(f"h2f{i}", (128, D), F32) for i in range(2)]
        Bh2f = [Buf() for _ in range(2)]
        h2Tf = [sb(f"h2Tf{i}", (128, 8, 128), F32) for i in range(2)]
        Bh2Tf = [Buf() for _ in range(2)]
        rg = [sb(f"rg{i}", (128, 640), F32) for i in range(2)]
        Brg = [Buf() for _ in range(2)]
        msel4 = [sb(f"msel4_{i}", (128, 4, NE), BF16) for i in range(2)]
        msel = [sb(f"msel{i}", (128, NE), BF16) for i in range(2)]
        Bmsel = [Buf() for _ in range(2)]
        mcum = [sb(f"mcum{i}", (128, NE), BF16) for i in range(2)]
        Bmcum = [Buf() for _ in range(2)]

        def phase3b():
            steps = []
            for tt in range(NT):
                steps.append([lambda tt=tt: p3b_front(tt), lambda tt=tt: p3b_mid(tt),
                              (lambda tt=tt: p3b_chain(tt // 4)) if tt % 4 == 3 else None])
            run_pipeline(steps, None)

        def p3b_front(tt):
            if True:
                s3 = tt % NXS
                s2 = tt % 2
                DMA("sp", xs[s3][:], x2_d[tt * 128:(tt + 1) * 128, :], [Bx2d[tt]], [Bxs[s3]], f"xs{s3}")
                rms_rstd(xs[s3][:], junk[:], stat[s2][:, 0:1], stat[s2][:, 1:2], stat[s2][:, 2:3],
                         [Bxs[s3]], Bjunk, Bstat[s2])
                STT(h2f[s2][:], xs[s3][:], stat[s2][:, 2:3], g2[:], ALU.mult, ALU.mult,
                    [Bxs[s3], Bstat[s2], Bg2], [Bh2f[s2]])
                CP("pool", h2b[:, tt * D:(tt + 1) * D], h2f[s2][:, :], [Bh2f[s2]], [Bh2b[tt]])

        def p3b_mid(tt):
            if True:
                s2 = tt % 2
                bA = 0 if s2 == 0 else 4
                for c in range(8):
                    bk = bA + c // 4
                    TR(banks[bk][:, (c % 4) * 128:(c % 4 + 1) * 128], h2f[s2][:, c * 128:(c + 1) * 128], ident_f32[:],
                       [Bh2f[s2], Bc2], [bankB[bk]])
                for hh in range(2):
                    bk = bA + hh
                    CP("act", h2Tf[s2][:, hh * 4:(hh + 1) * 4, :], banks[bk][:, :].rearrange("p (c t) -> p c t", c=4),
                       [bankB[bk]], [Bh2Tf[s2]])
                bR = bA + 2
                for c in range(8):
                    MM(banks[bR][:, 0:36], h2Tf[s2][:, c, :], wr[:, c, :], c == 0, c == 7, [Bh2Tf[s2], Bwr], [bankB[bR]])
                gp = (tt // 4) % 2
                g_ = tt % 4
                TT("dve", rg[gp][:, g_ * 36:(g_ + 1) * 36], banks[bR][:, 0:36], rbias[:, :], ALU.add,
                   [bankB[bR], Bwr], [Brg[gp]])

        def p3b_chain(grp):
            G = 4
            gp = grp % 2
            r = rg[gp]
            B_ = [Brg[gp]]
            t0_ = grp * G
            X = mybir.AxisListType.X

            def v(lo, n, *dims):
                ap = r[:, lo:lo + n]
                if len(dims) == 2:
                    return ap.rearrange("p (a b) -> p a b", a=dims[0])
                if len(dims) == 3:
                    return ap.rearrange("p (a b c) -> p a b c", a=dims[0], b=dims[1])
                return ap
            lg = v(0, G * 36, G, 36)
            gl = lg[:, :, 0:4]
            el = lg[:, :, 4:36]
            gmax = v(144, G)
            gmask = v(148, G * 4, G, 4)
            gd = v(164, G * 4, G, 4)
            gsum = v(180, G)
            gw = v(184, G)
            pen = v(188, G * 4, G, 4)
            elm = v(204, G * NE, G, NE)
            elm4 = v(204, G * NE, G, 4, 8)
            m1 = v(332, G)
            mask1 = v(336, G * NE, G, NE)
            m2 = v(464, G)
            mask2 = v(468, G * NE, G, NE)
            dd = v(596, G)
            ee = v(600, G)
            w1 = v(604, G)
            w2 = v(608, G)

            def bc(ap, shape):
                return ap.unsqueeze(len(ap.shape)).broadcast_to(shape)
            P.op("dve", lambda e: e.tensor_reduce(out=gmax, in_=gl, axis=X, op=ALU.max), B_, B_)
            TT("dve", gmask, gl, bc(gmax, [128, G, 4]), ALU.is_equal, B_, B_)
            TT("dve", gd, gl, bc(gmax, [128, G, 4]), ALU.subtract, B_, B_)
            ACT(gd, gd, AF.Exp, B_, B_)
            P.op("dve", lambda e: e.tensor_reduce(out=gsum, in_=gd, axis=X, op=ALU.add), B_, B_)
            P.op("dve", lambda e: e.reciprocal(out=gw, in_=gsum), B_, B_)
            TS("dve", pen, gmask, BIGR, -BIGR, ALU.mult, ALU.add, B_, B_)
            TT("dve", elm4, el.rearrange("p g (a b) -> p g a b", a=4), bc(pen, [128, G, 4, 8]), ALU.add, B_, B_)
            P.op("dve", lambda e: e.tensor_reduce(out=m1, in_=elm, axis=X, op=ALU.max), B_, B_)
            TT("dve", mask1, elm, bc(m1, [128, G, NE]), ALU.is_equal, B_, B_)
            STT(elm, mask1, -3.0 * BIGR, elm, ALU.mult, ALU.add, B_, B_)
            P.op("dve", lambda e: e.tensor_reduce(out=m2, in_=elm, axis=X, op=ALU.max), B_, B_)
            TT("dve", mask2, elm, bc(m2, [128, G, NE]), ALU.is_equal, B_, B_)
            TT("dve", dd, m1, m2, ALU.subtract, B_, B_)
            ACT(ee, dd, AF.Exp, B_, B_, scale=-1.0)
            TS("dve", w1, ee, 1.0, None, ALU.add, None, B_, B_)
            P.op("dve", lambda e: e.reciprocal(out=w1, in_=w1), B_, B_)
            TT("dve", w2, ee, w1, ALU.mult, B_, B_)
            BWgs = [BWg[t0_ + g] for g in range(G)]
            BMs = [BM[t0_ + g] for g in range(G)]
            TT("dve", Wg[:, t0_:t0_ + G, 0], w1, gw, ALU.mult, B_, BWgs)
            TT("dve", Wg[:, t0_:t0_ + G, 1], w2, gw, ALU.mult, B_, BWgs)
            CP("dve", M1[:, t0_:t0_ + G, :], mask1, B_, BMs)
            CP("dve", M2[:, t0_:t0_ + G, :], mask2, B_, BMs)
            TT("dve", msel4[gp][:, :, :], mask1, mask2, ALU.add, B_, [Bmsel[gp]])
            bK = 3 if gp == 0 else 7
            for g in range(G):
                tt = t0_ + g
                s2 = tt % 2
                if tt == 0:
                    CP("dve", mcum[s2][:, :], msel4[gp][:, g, :], [Bmsel[gp]], [Bmcum[s2]])
                else:
                    TT("dve", mcum[s2][:, :], mcum[1 - s2][:, :], msel4[gp][:, g, :], ALU.add,
                       [Bmcum[1 - s2], Bmsel[gp]], [Bmcum[s2]])
                MM(banks[bK][:, g * NE:(g + 1) * NE], SL_bf[:], msel4[gp][:, g, :], True, tt == 0, [Bmsel[gp], Bc2], [bankB[bK]])
                if tt > 0:
                    MM(banks[bK][:, g * NE:(g + 1) * NE], ones_bf[:], mcum[1 - s2][:, :], False, True,
                       [Bmcum[1 - s2], Bc], [bankB[bK]])
            CP("dve", Rk[:, t0_:t0_ + G, :], banks[bK][:, 0:G * NE].rearrange("p (g e) -> p g e", g=G),
               [bankB[bK]], [BRk[t0_ + g] for g in range(G)])

        phase3b()
        if upto == "3b":
            DMA("sp", dbg_d[0:128, :], Rk[:, :, :].rearrange("p t e -> p (t e)"), BRk, [Bdbg[0]], "dbgcmb")

        cnt = sb("cnt", (128, NE), F32)
        cnti = sb("cnti", (128, NE), I32)
        pcf = sb("pcf", (128, NE), F32)
        cend = sb("cend", (128, NE), F32)
        offs = sb("offs", (128, NE), F32)
        sstart_i = sb("sstart_i", (128, NSLOT), I32)
        sstart = sb("sstart", (128, NSLOT), F32)
        eidf = sb("eidf", (128, NSLOT), F32)
        pidx_i = sb("pidx_i", (128, 1), I32)
        pidx = sb("pidx", (128, 1), F32)
        posall = sb("posall", (128, NT * NE), F32)
        pmall = sb("pmall", (128, NT * NE), F32)
        pfall = sb("pfall", (128, 2, NT), F32)
        Bpb = Buf("passB")
        Bpos = [Buf() for _ in range(2)]
        BIDX = [Buf(f"IDX{t}") for t in range(NT)]
        BWI = Buf("WI")
        BXs = Buf("Xs_d")
        BXs_t = [Buf(f"Xs_t{t}") for t in range(NT)]

        def passB():
            lastm = (NT - 1) % 2
            MM(banks[0][:, 0:NE], ones_bf[:], mcum[lastm][:, :], True, True, [Bmcum[lastm], Bc], [bankB[0]])
            B_ = [Bpb]
            TS("dve", cnti[:, :], banks[0][:, 0:NE], float(SLOTR - 1), None, ALU.add, None, [bankB[0]], B_)
            TS("dve", cnti[:, :], cnti[:, :], SHIFT, None, ALU.logical_shift_right, None, B_, B_)
            TS("dve", cnti[:, :], cnti[:, :], SHIFT, None, ALU.logical_shift_left, None, B_, B_)
            CP("dve", pcf[:, :], cnti[:, :], B_, B_)
            P.op("dve", lambda e: e.tensor_tensor_scan(out=cend[:, :], data0=pcf[:, :], data1=pcf[:, :], initial=0.0,
                                                       op0=ALU.add, op1=ALU.max), B_, B_)
            TT("dve", offs[:, :], cend[:, :], pcf[:, :], ALU.subtract, B_, B_)
            P.op("pool", lambda e: e.iota(sstart_i[:, :], pattern=[[SLOTR, NSLOT]], base=0, channel_multiplier=0), (), B_)
            P.op("pool", lambda e: e.iota(pidx_i[:, :], pattern=[[0, 1]], base=0, channel_multiplier=1), (), B_)
            CP("dve", sstart[:, :], sstart_i[:, :], B_, B_)
            CP("dve", pidx[:, :], pidx_i[:, :], B_, B_)
            for ex in range(NE):
                if ex == 0:
                    TS("dve", eidf[:, :], sstart[:, :], cend[:, 0:1], None, ALU.is_ge, None, B_, B_)
                else:
                    STT(eidf[:, :], sstart[:, :], cend[:, ex:ex + 1], eidf[:, :], ALU.is_ge, ALU.add, B_, B_)
            TS("dve", eidf[:, :], eidf[:, :], float(NE - 1), 128.0, ALU.min, ALU.mult, B_, B_)
            TS("dve", WI[:, :], eidf[:, :], pidx[:, 0:1], None, ALU.add, None, B_, [BWI])
            Bp = [Bpos[0]]
            pos3 = posall[:, :].rearrange("p (t e) -> p t e", e=NE)
            pm3 = pmall[:, :].rearrange("p (t e) -> p t e", e=NE)
            TT("dve", pos3, Rk[:, :, :], offs[:, :].unsqueeze(1).broadcast_to([128, NT, NE]), ALU.add, BRk + [Bpb], Bp)
            TT("dve", pm3, pos3, M1[:, :, :], ALU.mult, Bp + BM, Bp)
            P.op("dve", lambda e: e.tensor_reduce(out=pfall[:, 0, :], in_=pm3, axis=mybir.AxisListType.X, op=ALU.add), Bp, Bp)
            TT("dve", pm3, pos3, M2[:, :, :], ALU.mult, Bp + BM, Bp)
            P.op("dve", lambda e: e.tensor_reduce(out=pfall[:, 1, :], in_=pm3, axis=mybir.AxisListType.X, op=ALU.add), Bp, Bp)
            CP("dve", IDX[:, :, :].rearrange("p t c -> p c t"), pfall[:, :, :], Bp, BIDX)
            for tt in range(NT):
                for ch in range(2):
                    P.op("pool", lambda e, tt=tt, ch=ch: e.indirect_dma_start(
                        out=Xs_d[:, :], out_offset=bass.IndirectOffsetOnAxis(ap=IDX[:, tt, ch:ch + 1], axis=0),
                        in_=h2b[:, tt * D:(tt + 1) * D], in_offset=None),
                        [BIDX[tt], Bh2b[tt]], [BXs_t[tt]] if ch else [BXs], dma_key="scat")

        passB()
        if upto == "pb":
            DMA("sp", dbg_d[0:128, 0:64], IDX[:, :, :].rearrange("p t c -> p (t c)").bitcast(F32), BIDX, [Bdbg[0]], "dbgidx")
            DMA("sp", dbg_d[128:256, 0:NSLOT], WI[:, :].bitcast(F32), [BWI], [Bdbg[1]], "dbgwi")
            DMA("sp", dbg_d[256:384, 0:64], Wg[:, :, :].rearrange("p t c -> p (t c)"), BWg, [Bdbg[2]], "dbgwg")

        P.barrier()
        stk.pop().close()
        stk.append(ExitStack())
        NWS = 4
        wsl = [sb(f"wsl{i}", (128, WROW), BF16) for i in range(NWS)]
        Bwsl = [Buf() for _ in range(NWS)]
        xsl = [sb(f"xsl{i}", (128, NSUB, D), BF16) for i in range(2)]
        Bxsl = [Buf() for _ in range(2)]
        XsT = [sb(f"XsT{i}", (128, 8, SLOTR), BF16) for i in range(2)]
        BXsT = [[Buf(), Buf()] for _ in range(2)]
        silb = [sb(f"silb{i}", (128, SLOTR), BF16) for i in range(4)]
        Bsilb = [Buf() for _ in range(4)]
        hidT = [sb(f"hidT{i}", (128, 2, SLOTR), BF16) for i in range(2)]
        BhidT = [Buf() for _ in range(2)]
        ysb = [sb(f"ysb{i}", (128, D), F32) for i in range(3)]
        Bysb = [[Buf(), Buf()] for _ in range(3)]
        BYs = [Buf(f"Ys{i}") for i in range(NSLOT)]
        Bout = [Buf(f"out{t}") for t in range(NT)]

        def slot_loop():
            yrot = [0]
            steps = []
            for i in range(NSLOT):
                ws = i % NWS
                r2 = i % 2

                def stL(i=i, ws=ws, r2=r2):
                    P.op("pool", lambda e: e.indirect_dma_start(
                        out=wsl[ws][:, :], out_offset=None, in_=Wb_d[:, :],
                        in_offset=bass.IndirectOffsetOnAxis(ap=WI[:, i:i + 1], axis=0)),
                        [BWI] + BWb, [Bwsl[ws]], dma_key=f"wsl{ws}")
                    DMA("sp", xsl[r2][:, :, :], Xs_d[i * SLOTR:(i + 1) * SLOTR, :].rearrange("(s p) d -> p s d", p=128),
                        [BXs] + BXs_t, [Bxsl[r2]], f"xsl{r2}")

                def stT(i=i, ws=ws, r2=r2):
                    for hb_ in range(2):
                        bk = hb_
                        tv = banks[bk][:].bitcast(BF16)
                        for cc in range(4):
                            c = hb_ * 4 + cc
                            for sub in range(NSUB):
                                TR(tv[:, cc * SLOTR + sub * 128:cc * SLOTR + (sub + 1) * 128], xsl[r2][:, sub, c * 128:(c + 1) * 128],
                                   ident_bf[:], [Bxsl[r2], Bc2], [bankB[bk]])
                        CP("act" if hb_ == 0 else "dve", XsT[r2][:, hb_ * 4:hb_ * 4 + 4, :],
                           tv[:, :].rearrange("p (c t) -> p c t", c=4), [bankB[bk]], [BXsT[r2][hb_]])

                def stAB(i=i, ws=ws, r2=r2):
                    for m in range(2):
                        ba = 2 + 2 * r2 + m
                        sb_ = 2 * r2 + m
                        for c in range(8):
                            MM(banks[ba][:, 0:SLOTR], wsl[ws][:, c * DE + m * 128:c * DE + (m + 1) * 128], XsT[r2][:, c, :], c == 0, c == 7,
                               [Bwsl[ws]] + BXsT[r2], [bankB[ba]])
                        for c in range(8):
                            MM(banks[ba][:, SLOTR:2 * SLOTR], wsl[ws][:, 8 * DE + c * DE + m * 128:8 * DE + c * DE + (m + 1) * 128],
                               XsT[r2][:, c, :], c == 0, c == 7, [Bwsl[ws]] + BXsT[r2], [bankB[ba]])
                        ACT(silb[sb_][:, :], banks[ba][:, 0:SLOTR], AF.Silu, [bankB[ba]], [Bsilb[sb_]])
                        TT("dve", hidT[r2][:, m, :], banks[ba][:, SLOTR:2 * SLOTR], silb[sb_][:, :], ALU.mult,
                           [bankB[ba], Bsilb[sb_]], [BhidT[r2]])

                def stY(i=i, ws=ws, r2=r2):
                    for sub in range(NSUB):
                        yr = yrot[0] % 3
                        yrot[0] += 1
                        for half in range(2):
                            by = 6 + half
                            for m in range(2):
                                MM(banks[by][:, :], hidT[r2][:, m, sub * 128:(sub + 1) * 128],
                                   wsl[ws][:, 16 * DE + m * D + half * 512:16 * DE + m * D + (half + 1) * 512],
                                   m == 0, m == 1, [BhidT[r2], Bwsl[ws]], [bankB[by]])
                            CP("act" if half == 0 else "dve", ysb[yr][:, half * 512:(half + 1) * 512], banks[by][:, :],
                               [bankB[by]], [Bysb[yr][half]])
                        DMA("sp", Ys_d[i * SLOTR + sub * 128:i * SLOTR + (sub + 1) * 128, :], ysb[yr][:, :], Bysb[yr], [BYs[i]],
                            f"yst{yr}")
                steps.append([stL, stT, stAB, stY])
            run_pipeline(steps, None)

        def combine():
            def load(tt):
                s3 = tt % NXS
                s2 = tt % 3
                DMA("sp", xs[s3][:], x2_d[tt * 128:(tt + 1) * 128, :], [Bx2d[tt]], [Bxs[s3]], f"xs{s3}")
                P.op("pool", lambda e: e.indirect_dma_start(
                    out=y1[s2][:, :], out_offset=None, in_=Ys_d[:, :],
                    in_offset=bass.IndirectOffsetOnAxis(ap=IDX[:, tt, 0:1], axis=0)),
                    [BIDX[tt]] + BYs, [By1[s2]], dma_key=f"g1_{s2}")
                P.op("pool", lambda e: e.indirect_dma_start(
                    out=y2[s2][:, :], out_offset=None, in_=Ys_d[:, :],
                    in_offset=bass.IndirectOffsetOnAxis(ap=IDX[:, tt, 1:2], axis=0)),
                    [BIDX[tt]] + BYs, [By2[s2]], dma_key=f"g2_{s2}")
            load(0)
            for tt in range(NT):
                s3 = tt % NXS
                s2 = tt % 3
                STT(x3[s2][:, :], y1[s2][:, :], Wg[:, tt, 0:1], xs[s3][:, :], ALU.mult, ALU.add,
                    [By1[s2], BWg[tt], Bxs[s3]], [Bx3[s2]])
                if tt + 1 < NT:
                    load(tt + 1)
                STT(x3[s2][:, :], y2[s2][:, :], Wg[:, tt, 1:2], x3[s2][:, :], ALU.mult, ALU.add,
                    [By2[s2], BWg[tt], Bx3[s2]], [Bx3[s2]])
                rms_rstd(x3[s2][:], junk[:], stat[tt % 2][:, 0:1], stat[tt % 2][:, 1:2], stat[tt % 2][:, 2:3],
                         [Bx3[s2]], Bjunk, Bstat[tt % 2])
                STT(x3[s2][:], x3[s2][:], stat[tt % 2][:, 2:3], g3[:], ALU.mult, ALU.mult,
                    [Bx3[s2], Bstat[tt % 2], Bg3], [Bx3[s2]])
                DMA("sp", out_d[tt * 128:(tt + 1) * 128, :], x3[s2][:], [Bx3[s2]], [Bout[tt]], f"outst{s2}")

        if upto not in ("3b", "pb"):
            slot_loop()
        P.barrier()
        stk.pop().close()
        stk.append(ExitStack())
        xs = [sb(f"xsd{i}", (128, D), F32) for i in range(NXS)]
        y1 = [sb(f"y1_{i}", (128, D), F32) for i in range(3)]
        y2 = [sb(f"y2_{i}", (128, D), F32) for i in range(3)]
        By1 = [Buf() for _ in range(3)]
        By2 = [Buf() for _ in range(3)]
        x3 = [sb(f"x3_{i}", (128, D), F32) for i in range(3)]
        Bx3 = [Buf() for _ in range(3)]
        if upto not in ("3b", "pb"):
            combine()

        P.op("sp", None, reads=(Bout if upto is None else []) + (Bdbg if debug else []))
        P.barrier()
        stk.pop().close()
        P.emit()
    return nc


_NC_CACHE = {}


def kernel(x, norm_attn, w_in, b_forget, w_o_sb, w_o_fox, w_out, norm_ffn,
           w_router_group, b_router_group, w_router_expert, b_router_expert,
           w1, w3, w2, norm_final, _debug=False, _upto=None, _cores=8):
    f32 = lambda a: np.ascontiguousarray(np.asarray(a, dtype=np.float32))
    nc = build_program(debug=_debug, upto=_upto)
    shared = {
        "norm_attn": f32(norm_attn).reshape(1, D), "w_in": f32(w_in)[0], "b_forget": f32(b_forget).reshape(1, NH),
        "w_o_sb": f32(w_o_sb)[0], "w_o_fox": f32(w_o_fox)[0], "w_out": f32(w_out)[0],
        "norm_ffn": f32(norm_ffn).reshape(1, D), "w_router_group": f32(w_router_group)[0],
        "b_router_group": f32(b_router_group).reshape(1, 4), "w_router_expert": f32(w_router_expert)[0],
        "b_router_expert": f32(b_router_expert).reshape(1, NE), "w1": f32(w1)[0], "w3": f32(w3)[0], "w2": f32(w2)[0],
        "norm_final": f32(norm_final).reshape(1, D),
    }
    xf = f32(x)
    in_maps = [dict(shared, x=xf[b]) for b in range(_cores)]
    res = run_bass_kernel_spmd(nc, in_maps, core_ids=list(range(_cores)))
    if _debug:
        return (np.stack([res.results[b]["out"] for b in range(_cores)], axis=0),
                np.stack([res.results[b]["dbg"] for b in range(_cores)], axis=0))
    return np.stack([res.results[b]["out"] for b in range(_cores)], axis=0)
```

```python
from contextlib import ExitStack
import numpy as np
import concourse.bass as bass
import concourse.mybir as mybir
from concourse.bass_utils import run_bass_kernel_spmd

F32 = mybir.dt.float32
BF16 = mybir.dt.bfloat16
I32 = mybir.dt.int32
AF = mybir.ActivationFunctionType
ALU = mybir.AluOpType

S = 4096
D = 1024
NT = S // 128
NQ = S // 512
HD = 64
NH = 8
INC = 5128
C_QSB, C_KSB, C_VSB, C_QFX, C_KFX, C_VFX, C_F, C_GSB, C_GFX = 0, 512, 1024, 1536, 2048, 2560, 3072, 3080, 4104
NE = 32
DE = 256
EPS = 1e-6
NEGBIG = -30000.0
FOX_DUMMY = 1
FILL_SB = 0
FILL_FX = 0

ENGS = ("pe", "act", "dve", "pool", "sp")
GEN = 30000


class Buf:
    __slots__ = ("name", "writer", "readers", "excl")

    def __init__(self, name="", excl=False):
        self.name = name
        self.writer = None
        self.readers = []
        self.excl = excl


class Op:
    __slots__ = ("eng", "fn", "idx", "deps", "signal", "sig", "dma", "dkey", "dval")

    def __init__(self, eng, fn, idx, dma):
        self.eng = eng
        self.fn = fn
        self.idx = idx
        self.deps = []
        self.signal = False
        self.sig = None
        self.dma = dma
        self.dkey = None
        self.dval = None


class Prog:
    def __init__(self, nc):
        self.nc = nc
        self.ops = {e: [] for e in ENGS}
        self.known = {e: {f: -1 for f in ENGS} for e in ENGS}
        self.known_dma = {e: {} for e in ENGS}
        self.dma_cnt = {}
        self.last_dma = {}
        self.pending = {e: [] for e in ENGS}

    def barrier(self):
        lasts = []
        for e in ENGS:
            for o in reversed(self.ops[e]):
                if not o.dma and o.fn is not None:
                    lasts.append(o)
                    break
        lasts += list(self.last_dma.values())
        for e in ENGS:
            self.pending[e] = list(lasts)

    def op(self, eng, fn, reads=(), writes=(), dma_key=None, ninc=1):
        o = Op(eng, fn, len(self.ops[eng]), dma_key is not None)
        cand = []
        for b in reads:
            if b.writer is not None:
                cand.append((b.writer, "raw"))
            if b.excl:
                for r in b.readers:
                    if r.eng != eng:
                        cand.append((r, "rar"))
        for b in writes:
            if b.writer is not None:
                cand.append((b.writer, "waw"))
            for r in b.readers:
                cand.append((r, "war"))
        best = {}
        dma_deps = {}
        if self.pending[eng]:
            for p in self.pending[eng]:
                if p.dma or p.eng != eng:
                    cand.append((p, "raw"))
            self.pending[eng] = []
        for (p, kind) in cand:
            if p is o:
                continue
            if p.dma:
                if self.known_dma[eng].get(p.dkey, 0) >= p.dval:
                    continue
                if p.dkey not in dma_deps or dma_deps[p.dkey].dval < p.dval:
                    dma_deps[p.dkey] = p
                continue
            if p.eng == eng:
                if eng == "pe":
                    continue
            if self.known[eng][p.eng] >= p.idx:
                continue
            if p.eng not in best or best[p.eng].idx < p.idx:
                best[p.eng] = p
        for k, p in dma_deps.items():
            o.deps.append(p)
            self.known_dma[eng][k] = p.dval
        for f, p in best.items():
            o.deps.append(p)
            p.signal = True
            self.known[eng][f] = p.idx
        if o.dma:
            o.dkey = dma_key
            self.dma_cnt[dma_key] = self.dma_cnt.get(dma_key, 0) + 16 * ninc
            o.dval = self.dma_cnt[dma_key]
            self.last_dma[dma_key] = o
        for b in reads:
            b.readers.append(o)
        for b in writes:
            b.writer = o
            b.readers = []
        self.ops[eng].append(o)
        return o

    def emit(self):
        nc = self.nc
        with ExitStack() as st:
            sems = {}
            for e in ENGS:
                n = 0
                for o in self.ops[e]:
                    if o.signal and not o.dma:
                        o.sig = n
                        n += 1
                ngen = max(1, (n + GEN - 1) // GEN)
                sems[e] = [st.enter_context(nc.semaphore(f"s_{e}_{g}")) for g in range(ngen)]
            dsem = {}
            for k in self.dma_cnt:
                dsem[k] = st.enter_context(nc.semaphore(f"d_{len(dsem)}"))
            block = st.enter_context(nc.Block())
            handles = {"pe": block.tensor, "act": block.scalar, "dve": block.vector,
                       "pool": block.gpsimd, "sp": block.sync}

            def run(e):
                ops = self.ops[e]
                if not ops:
                    return

                def body(eng):
                    for o in ops:
                        for p in o.deps:
                            if p.dma:
                                eng.wait_ge(dsem[p.dkey], p.dval)
                            else:
                                eng.wait_ge(sems[p.eng][p.sig // GEN], p.sig % GEN + 1)
                        if o.fn is None:
                            continue
                        ins = o.fn(eng)
                        if o.dma:
                            if not isinstance(ins, (list, tuple)):
                                ins = [ins]
                            for i_ in ins:
                                i_.then_inc(dsem[o.dkey], 16)
                        elif o.signal:
                            ins.then_inc(sems[e][o.sig // GEN], 1)
                handles[e](body)
            for e in ENGS:
                run(e)


def build_program(debug=False, upto=None):
    nc = bass.Bass("TRN2", target_bir_lowering=False)
    dram_in = lambda n, s: nc.dram_tensor(n, list(s), F32, kind="ExternalInput").ap()
    x_d = dram_in("x", (S, D))
    norm_attn_d = dram_in("norm_attn", (1, D))
    w_in_d = dram_in("w_in", (D, INC))
    b_forget_d = dram_in("b_forget", (1, NH))
    w_o_sb_d = dram_in("w_o_sb", (512, D))
    w_o_fox_d = dram_in("w_o_fox", (512, D))
    w_out_d = dram_in("w_out", (D, D))
    norm_ffn_d = dram_in("norm_ffn", (1, D))
    w_rg_d = dram_in("w_router_group", (D, 4))
    b_rg_d = dram_in("b_router_group", (1, 4))
    w_re_d = dram_in("w_router_expert", (D, NE))
    b_re_d = dram_in("b_router_expert", (1, NE))
    w1_d = dram_in("w1", (NE, D, DE))
    w3_d = dram_in("w3", (NE, D, DE))
    w2_d = dram_in("w2", (NE, DE, D))
    norm_final_d = dram_in("norm_final", (1, D))
    out_d = nc.dram_tensor("out", [S, D], F32, kind="ExternalOutput").ap()
    oT_d = nc.dram_tensor("oT_scr", [16, HD, S], BF16, kind="Internal").ap()
    x2_d = nc.dram_tensor("x2_scr", [S, D], F32, kind="Internal").ap()
    NSLOT = 64
    SLOTR = 256
    NSUB = SLOTR // 128
    SHIFT = 8
    WROW = 2 * 8 * DE + 2 * D
    Wb_d = nc.dram_tensor("Wb_scr", [NE * 128, WROW], BF16, kind="Internal").ap()
    Xs_d = nc.dram_tensor("Xs_scr", [NSLOT * SLOTR, D], BF16, kind="Internal").ap()
    Ys_d = nc.dram_tensor("Ys_scr", [NSLOT * SLOTR, D], F32, kind="Internal").ap()
    cparts_d = nc.dram_tensor("cparts_scr", [NH, 3, S], BF16, kind="Internal").ap()
    ncparts_d = nc.dram_tensor("ncparts_scr", [NH, 3, S], BF16, kind="Internal").ap()
    dbg_d = None
    if debug:
        dbg_d = nc.dram_tensor("dbg", [S, D], F32, kind="ExternalOutput").ap()

    P = Prog(nc)
    with ExitStack() as st:
        stk = [st]

        def sb(name, shape, dt):
            return stk[-1].enter_context(nc.sbuf_tensor(name, list(shape), dt))

        banks = [st.enter_context(nc.psum_tensor(f"bank{i}", [128, 512], F32)) for i in range(8)]
        bankB = [Buf(f"bank{i}", excl=True) for i in range(8)]

        def MM(out, lhsT, rhs, start, stop, reads, writes, **kw):
            return P.op("pe", lambda e: e.matmul(out, lhsT=lhsT, rhs=rhs, start=start, stop=stop, **kw), reads, writes)

        def TR(out, in_, ident, reads, writes):
            return P.op("pe", lambda e: e.transpose(out=out, in_=in_, identity=ident), reads, writes)

        def ACT(out, in_, func, reads, writes, **kw):
            return P.op("act", lambda e: e.activation(out=out, in_=in_, func=func, **kw), reads, writes)

        def TT(eng, out, in0, in1, op, reads, writes):
            return P.op(eng, lambda e: e.tensor_tensor(out=out, in0=in0, in1=in1, op=op), reads, writes)

        def TS(eng, out, in0, s1, s2, op0, op1, reads, writes, **kw):
            if op1 is None:
                return P.op(eng, lambda e: e.tensor_scalar(out=out, in0=in0, scalar1=s1, scalar2=None, op0=op0, **kw), reads, writes)
            return P.op(eng, lambda e: e.tensor_scalar(out=out, in0=in0, scalar1=s1, scalar2=s2, op0=op0, op1=op1, **kw), reads, writes)

        def STT(out, in0, scalar, in1, op0, op1, reads, writes):
            return P.op("dve", lambda e: e.scalar_tensor_tensor(out=out, in0=in0, scalar=scalar, in1=in1, op0=op0, op1=op1), reads, writes)

        def CP(eng, out, in_, reads, writes):
            if eng == "act":
                return P.op("act", lambda e: e.copy(out=out, in_=in_), reads, writes)
            return P.op(eng, lambda e: e.tensor_copy(out=out, in_=in_), reads, writes)

        def MEMSET(eng, ap, val, writes):
            return P.op(eng, lambda e: e.memset(ap, val), (), writes)

        def DMA(q, out, in_, reads, writes, key, **kw):
            return P.op(q, lambda e: e.dma_start(out=out, in_=in_, **kw), reads, writes, dma_key=key)

        ones_bf = sb("ones_bf", (128, 128), BF16)
        negones_bf = sb("negones_bf", (128, 128), BF16)
        negbig_bf = sb("negbig_bf", (128, 128), BF16)
        ident_bf = sb("ident_bf", (128, 128), BF16)
        negbigI = sb("negbigI", (128, 128), BF16)
        negtri = sb("negtri", (128, 128), BF16)
        ones513 = sb("ones513", (128, 513), BF16)
        M0 = sb("M0", (128, 513), BF16)
        ones_f32 = sb("ones_f32", (128, 128), F32)
        ident_f32 = sb("ident_f32", (128, 128), F32)
        eps_t = sb("eps_t", (128, 1), F32)
        Bc = Buf("consts")
        Bg1 = Buf("g1")

        P.op("pool", lambda e: e.memset(ones_bf[:], 1.0), (), [Bc])
        P.op("pool", lambda e: e.memset(negones_bf[:], -1.0), (), [Bc])
        P.op("pool", lambda e: e.memset(negbig_bf[:], NEGBIG), (), [Bc])
        P.op("pool", lambda e: e.memset(ones513[:], 1.0), (), [Bc])
        P.op("pool", lambda e: e.memset(ones_f32[:], 1.0), (), [Bc])
        P.op("pool", lambda e: e.memset(eps_t[:], EPS), (), [Bc])
        Bc2 = Buf("consts2")
        P.op("pool", lambda e: e.affine_select(out=ident_bf[:], in_=ones_bf[:], pattern=[[-1, 128]], compare_op=ALU.is_equal,
                                               fill=0.0, base=0, channel_multiplier=1), [Bc], [Bc2])
        P.op("pool", lambda e: e.affine_select(out=ident_f32[:], in_=ones_f32[:], pattern=[[-1, 128]], compare_op=ALU.is_equal,
                                               fill=0.0, base=0, channel_multiplier=1), [Bc], [Bc2])
        P.op("pool", lambda e: e.affine_select(out=negbigI[:], in_=negbig_bf[:], pattern=[[-1, 128]], compare_op=ALU.is_equal,
                                               fill=0.0, base=0, channel_multiplier=1), [Bc], [Bc2])
        P.op("pool", lambda e: e.affine_select(out=negtri[:], in_=negones_bf[:], pattern=[[-1, 128]], compare_op=ALU.is_ge,
                                               fill=0.0, base=0, channel_multiplier=1), [Bc], [Bc2])
        P.op("pool", lambda e: e.affine_select(out=M0[:], in_=ones513[:], pattern=[[-1, 513]], compare_op=ALU.is_ge,
                                               fill=0.0, base=0, channel_multiplier=1), [Bc], [Bc2])
        P.op("pool", lambda e: e.affine_select(out=SL_bf[:], in_=ones_bf[:], pattern=[[1, 128]], compare_op=ALU.is_ge,
                                               fill=0.0, base=-1, channel_multiplier=-1), [Bc], [Bc2])

        hT = sb("hT", (128, 8, S), BF16)
        BhT = [Buf(f"hT{t}") for t in range(NQ)]
        g3 = sb("g3", (128, D), F32)
        M1 = sb("M1", (128, NT, NE), BF16)
        M2 = sb("M2", (128, NT, NE), BF16)
        Rk = sb("Rk", (128, NT, NE), F32)
        Wg = sb("Wg", (128, NT, 2), F32)
        IDX = sb("IDX", (128, NT, 2), I32)
        WI = sb("WI", (128, 64), I32)
        SL_bf = sb("SL_bf", (128, 128), BF16)

        def rms_rstd(xs_ap, junk_ap, ss_ap, ln_ap, rstd_ap, rd, wr_junk, wr_stat):
            P.op("act", lambda e: e.activation(out=junk_ap, in_=xs_ap, func=AF.Square, accum_out=ss_ap), rd, [wr_junk, wr_stat])
            ACT(ln_ap, ss_ap, AF.Ln, [wr_stat, Bc], [wr_stat], scale=1.0 / D, bias=eps_t[:, 0:1])
            ACT(rstd_ap, ln_ap, AF.Exp, [wr_stat], [wr_stat], scale=-0.5)

        NXS = 2
        Bxs = [Buf(f"xs{i}") for i in range(NXS)]
        junk = sb("junk", (128, D), BF16)
        Bjunk = Buf("junk")
        stat = [sb(f"stat{i}", (128, 4), F32) for i in range(2)]
        Bstat = [Buf(f"stat{i}") for i in range(2)]
        stk.append(ExitStack())
        g1 = sb("g1", (128, D), F32)
        DMA("sp", g1[:], norm_attn_d[0:1, :].partition_broadcast(128), (), [Bg1], "g1")
        xs = [sb(f"xsa{i}", (128, D), F32) for i in range(NXS)]
        hb = [sb(f"hb{i}", (128, D), BF16) for i in range(2)]
        Bhb = [Buf(f"hb{i}") for i in range(2)]

        def run_pipeline(stage_lists, bg, every=6):
            n = len(stage_lists)
            nst = max(len(x) for x in stage_lists)
            bgi = 0
            for s in range(n + nst - 1):
                for k in range(nst):
                    t = s - k
                    if 0 <= t < n and k < len(stage_lists[t]) and stage_lists[t][k] is not None:
                        stage_lists[t][k]()
                if bg and s % every == 3 and bgi < len(bg):
                    bg[bgi]()
                    bgi += 1
            while bg and bgi < len(bg):
                bg[bgi]()
                bgi += 1

        def phase1():
            def front(tt):
                s3 = tt % NXS
                s2 = tt % 2
                DMA("sp", xs[s3][:], x_d[tt * 128:(tt + 1) * 128, :], (), [Bxs[s3]], f"xs{s3}")
                rms_rstd(xs[s3][:], junk[:], stat[s2][:, 0:1], stat[s2][:, 1:2], stat[s2][:, 2:3],
                         [Bxs[s3]], Bjunk, Bstat[s2])
                STT(hb[s2][:], xs[s3][:], stat[s2][:, 2:3], g1[:], ALU.mult, ALU.mult,
                    [Bxs[s3], Bstat[s2], Bg1], [Bhb[s2]])

            def back(tt):
                s2 = tt % 2
                bk = 6 + (tt % 2)
                pv = banks[bk][:].bitcast(BF16)
                for c in range(8):
                    TR(pv[:, c * 128:(c + 1) * 128], hb[s2][:, c * 128:(c + 1) * 128], ident_bf[:],
                       [Bhb[s2], Bc2], [bankB[bk]])
                CP("act", hT[:, :, tt * 128:(tt + 1) * 128], pv[:, :].rearrange("p (c t) -> p c t", c=8),
                   [bankB[bk]], [BhT[tt // 4]])
            steps = [[lambda tt=tt: front(tt), lambda tt=tt: back(tt)] for tt in range(NT)]
            run_pipeline(steps, None)

        phase1()
        P.barrier()
        stk.pop().close()

        NSL = 3
        stk.append(ExitStack())
        QTa = [sb(f"QTa{i}", (128, S), BF16) for i in range(NSL)]
        KTa = [sb(f"KTa{i}", (128, S), BF16) for i in range(NSL)]
        Vt = [sb(f"Vt{i}", (128, NT, HD + 1), BF16) for i in range(NSL)]
        wqk = [sb(f"wqk{i}", (128, 8, 130), BF16) for i in range(NSL)]
        wv = [sb(f"wv{i}", (128, 8, HD), BF16) for i in range(NSL)]
        BQ = [Buf(f"Q{i}") for i in range(NSL)]
        BK = [Buf(f"K{i}") for i in range(NSL)]
        BQaug = [Buf(f"Qaug{i}") for i in range(NSL)]
        BKaug = [Buf(f"Kaug{i}") for i in range(NSL)]
        BV = [Buf(f"V{i}") for i in range(NSL)]
        BVones = [Buf(f"Vones{i}") for i in range(NSL)]
        Bwqk = [Buf(f"wqk{i}") for i in range(NSL)]
        Bwv = [Buf(f"wv{i}") for i in range(NSL)]
        ostS = sb("ostS", (64, S), BF16)
        ostF = sb("ostF", (64, S), BF16)
        BostS = Buf("ostS")
        BostF = Buf("ostF")
        wstage = [sb(f"wstage{i}", (128, 2048), BF16) for i in range(1)]
        Bwstage = [Buf(f"wstage{i}") for i in range(1)]
        BWb = [Buf(f"Wb{e}") for e in range(NE)]

        def conv_expert(ex):
            def f():
                srcs = [w1_d[ex].rearrange("(c p) n -> p c n", p=128), w3_d[ex].rearrange("(c p) n -> p c n", p=128),
                        w2_d[ex].rearrange("(k p) n -> p k n", p=128)]
                pats = ["p (c n) -> p c n", "p (c n) -> p c n", "p (k n) -> p k n"]
                for part in range(3):
                    kw = {"c": 8} if part < 2 else {"k": 2}
                    P.op("pool", lambda e, part=part, kw=kw: e.dma_start(
                        out=wstage[0][:, :].rearrange(pats[part], **kw), in_=srcs[part]),
                        (), [Bwstage[0]], dma_key="wstg0")
                    DMA("sp", Wb_d[ex * 128:(ex + 1) * 128, part * 2048:(part + 1) * 2048], wstage[0][:, :],
                        [Bwstage[0]], [BWb[ex]], "wbst0")
            return f
        BoT_d = [Buf(f"oTd{h}") for h in range(16)]

        for i in range(NSL):
            P.op("pool", lambda e, i=i: e.memset(Vt[i][:, :, HD:HD + 1], 1.0), (), [BVones[i]])
            P.op("pool", lambda e, i=i: e.memset(wqk[i][:, :, 128:130], 0.0), (), [BVones[i]])

        e_sb = [sb(f"e_sb{i}", (128, 512), F32) for i in range(2)]
        sp_bf = [sb(f"sp_bf{i}", (128, 512), BF16) for i in range(2)]
        arg_sb = [sb(f"arg_sb{i}", (128, 512), F32) for i in range(2)]
        A_bf = [sb(f"A_bf{i}", (128, 512), BF16) for i in range(3)]
        P_bf = [sb(f"P_bf{i}", (128, 512), BF16) for i in range(3)]
        BPb = [Buf() for _ in range(3)]
        oraw = [sb(f"oraw{i}", (128, 512), F32) for i in range(2)]
        Boraw = [Buf() for _ in range(2)]
        C_sb = [sb(f"C_sb{i}", (128, 512), F32) for i in range(2)]
        Be = [Buf() for _ in range(2)]
        Bsp = [Buf() for _ in range(2)]
        Barg = [Buf() for _ in range(2)]
        BA = [Buf() for _ in range(3)]
        BC = [Buf() for _ in range(2)]

        wf = sb("wf", (128, 8, NH), BF16)
        Bwf = Buf("wf")
        fexp = [sb(f"fexp{i}", (NH, 512), F32) for i in range(1)] * 2
        cpt = [sb(f"cpt{i}", (NH, 512), F32) for i in range(2)]
        r1 = fexp
        cpp = [sb(f"cpp{i}", (NH, 3, 512), BF16) for i in range(1)] * 2
        negb = sb("negb", (NH, 1), F32)
        Bnegb = Buf("negb")
        Bfexp = [Buf()] * 2
        Bcpt = [Buf() for _ in range(2)]
        Br1 = Bfexp
        Bcpp = [Buf()] * 2
        Bncpp = [Buf()] * 2
        Bcparts = [Buf(f"cparts_d{t}") for t in range(NQ)]

        def load_head_weights(hd, sl):
            typ, h = divmod(hd, NH)
            cq = (C_QSB if typ == 0 else C_QFX) + h * HD
            ck = (C_KSB if typ == 0 else C_KFX) + h * HD
            cv = (C_VSB if typ == 0 else C_VFX) + h * HD
            P.op("pool", lambda e: [
                e.dma_start(out=wqk[sl][:, :, 0:HD], in_=w_in_d[:, cq:cq + HD].rearrange("(c p) n -> p c n", p=128)),
                e.dma_start(out=wqk[sl][:, :, HD:2 * HD], in_=w_in_d[:, ck:ck + HD].rearrange("(c p) n -> p c n", p=128)),
            ], (), [Bwqk[sl]], dma_key=f"wqk{sl}", ninc=2)
            P.op("pool", lambda e: e.dma_start(out=wv[sl][:, :, :], in_=w_in_d[:, cv:cv + HD].rearrange("(c p) n -> p c n", p=128)),
                 (), [Bwv[sl]], dma_key=f"wv{sl}")

        pj_rot = [0]

        def proj_chunks(hd, sl):
            typ, h = divmod(hd, NH)
            chunks = []
            bk = 7

            def qk_chunk(T, which):
                lo = 0 if which == "q" else HD
                out = []
                for c in range(8):
                    out.append(lambda c=c: MM(banks[bk][0:HD + 1, :], wqk[sl][:, c, lo:lo + HD + 1],
                                              hT[:, c, T * 512:(T + 1) * 512], c == 0, c == 7,
                                              [Bwqk[sl], BhT[T], BVones[sl]], [bankB[bk]]))
                if which == "q":
                    out.append(lambda: TS("dve", QTa[sl][0:HD, T * 512:(T + 1) * 512], banks[bk][0:HD, :], 0.125, None,
                                          ALU.mult, None, [bankB[bk]], [BQ[sl]]))
                else:
                    out.append(lambda: CP("dve", KTa[sl][0:HD, T * 512:(T + 1) * 512], banks[bk][0:HD, :],
                                          [bankB[bk]], [BK[sl]]))
                return out

            def v_chunk(g):
                out = []
                for u in range(8):
                    def f(u=u):
                        tt = g * 8 + u
                        for c in range(8):
                            MM(banks[bk][:, u * HD:(u + 1) * HD], hT[:, c, tt * 128:(tt + 1) * 128], wv[sl][:, c, :],
                               c == 0, c == 7, [Bwv[sl], BhT[tt // 4]], [bankB[bk]])
                    out.append(f)
                out.append(lambda: CP("dve", Vt[sl][:, g * 8:(g + 1) * 8, 0:HD],
                                      banks[bk][:, :].rearrange("p (u d) -> p u d", u=8), [bankB[bk]], [BV[sl]]))
                return out

            for T in range(NQ):
                chunks += qk_chunk(T, "k")
            for g in range(4):
                chunks += v_chunk(g)
            for T in range(NQ):
                chunks += qk_chunk(T, "q")
            if typ == 0:
                def zpad():
                    P.op("pool", lambda e: e.memset(QTa[sl][64:128, :], 0.0), (), [BQaug[sl]])
                    P.op("pool", lambda e: e.memset(KTa[sl][64:128, :], 0.0), (), [BKaug[sl]])
                chunks.append(zpad)
            if typ == 1:
                def aug():
                    P.op("pool", lambda e: e.memset(QTa[sl][64:70, :], 1.0), (), [BQaug[sl]])
                    P.op("pool", lambda e: e.memset(KTa[sl][64:70, :], -1.0), (), [BKaug[sl]])
                    DMA("sp", QTa[sl][64:67, :], cparts_d[h, :, :], Bcparts, [BQaug[sl]], f"qaug{sl}")
                    DMA("sp", KTa[sl][67:70, :], cparts_d[h, :, :], Bcparts, [BKaug[sl]], f"kaug{sl}")
                chunks.append(aug)
            return chunks

        def fox_prep():
            P.op("pool", lambda e: e.dma_start(out=wf[:, :, :], in_=w_in_d[:, C_F:C_F + NH].rearrange("(c p) n -> p c n", p=128)),
                 (), [Bwf], dma_key="wf")
            DMA("sp", negb[:, 0:1], b_forget_d[0:1, :].rearrange("o h -> h o"), (), [Bnegb], "negb")
            TS("dve", negb[:, 0:1], negb[:, 0:1], -1.0, None, ALU.mult, None, [Bnegb], [Bnegb])
            for T in range(NQ):
                bk = 7
                s2 = T % 2
                for c in range(8):
                    MM(banks[bk][0:NH, :], wf[:, c, :], hT[:, c, T * 512:(T + 1) * 512], c == 0, c == 7,
                       [Bwf, BhT[T]], [bankB[bk]])
                ACT(fexp[s2][:, :], banks[bk][0:NH, :], AF.Exp, [bankB[bk], Bnegb], [Bfexp[s2]],
                    scale=-1.0, bias=negb[:, 0:1])
                ACT(fexp[s2][:, :], fexp[s2][:, :], AF.Ln, [Bfexp[s2]], [Bfexp[s2]], bias=1.0)
                init = 0.0 if T == 0 else cpt[1 - s2][:, 511:512]
                rds = [Bfexp[s2]] + ([Bcpt[1 - s2]] if T > 0 else [])
                P.op("dve", lambda e, s2=s2, init=init: e.tensor_tensor_scan(
                    out=cpt[s2][:, :], data0=fexp[s2][:, :], data1=fexp[s2][:, :], initial=init,
                    op0=ALU.add, op1=ALU.max), rds, [Bcpt[s2]])
                CP("dve", cpp[s2][:, 0, :], cpt[s2][:, :], [Bcpt[s2]], [Bcpp[s2]])
                TT("dve", r1[s2][:, :], cpt[s2][:, :], cpp[s2][:, 0, :], ALU.subtract, [Bcpt[s2], Bcpp[s2]], [Br1[s2]])
                CP("dve", cpp[s2][:, 1, :], r1[s2][:, :], [Br1[s2]], [Bcpp[s2]])
                TT("dve", r1[s2][:, :], r1[s2][:, :], cpp[s2][:, 1, :], ALU.subtract, [Br1[s2], Bcpp[s2]], [Br1[s2]])
                CP("dve", cpp[s2][:, 2, :], r1[s2][:, :], [Br1[s2]], [Bcpp[s2]])
                P.op("sp", lambda e, s2=s2, T=T: [
                    e.dma_start(out=cparts_d[:, :, T * 512:(T + 1) * 512], in_=cpp[s2][:, :, :]),
                ], [Bcpp[s2]], [Bcparts[T]], dma_key="cpst0", ninc=1)

        ZB = [0, 1]
        AB = [2, 3]
        CSB = 4
        OB = 5

        cnt = {"z": 0, "a": 0, "A": 0, "c": 0, "e": 0}

        def sb_steps(hd, sl):
            steps = []
            ZA = [0, 1]
            CSB_ = 2
            ob = 3
            for j in range(NQ):
                cslot = cnt["c"] % 2
                cnt["c"] += 1
                order = list(range(4 * j + 3, -1, -1))
                for n_, i in enumerate(order):
                    m = i - 4 * j
                    off = max(0, m) * 128
                    diag = m >= 0
                    first = n_ == 0
                    last = i == 0
                    zs = cnt["z"] % 2
                    cnt["z"] += 1
                    As = cnt["A"] % 3
                    cnt["A"] += 1
                    kT = KTa[sl][0:128, i * 128:(i + 1) * 128]
                    qT = QTa[sl][0:128, j * 512 + off:(j + 1) * 512]
                    msk = M0[:, 0:512 - off]
                    W = slice(off, 512)

                    def st0(zs=zs, kT=kT, qT=qT, msk=msk, W=W, diag=diag, first=first, cslot=cslot, off=off):
                        if first:
                            MEMSET("pool", C_sb[cslot][:], 0.0, [BC[cslot]])
                        zb = ZA[zs]
                        MM(banks[zb][:, W], kT, qT, True, not diag, [BK[sl], BQ[sl], BKaug[sl], BQaug[sl]], [bankB[zb]])
                        if diag:
                            MM(banks[zb][:, off:off + 128], negbigI[:], M0[:, 0:128], False, True, [Bc2], [bankB[zb]])
                        ACT(e_sb[zs][:, W], banks[zb][:, W], AF.Exp, [bankB[zb]], [Be[zs]])
                        ACT(sp_bf[zs][:, W], e_sb[zs][:, W], AF.Ln, [Be[zs]], [Bsp[zs]], bias=1.0)

                    def st1(zs=zs, As=As, W=W, first=first, last=last, cslot=cslot):
                        ab = ZA[zs]
                        MM(banks[ab][:, W], negtri[:], sp_bf[zs][:, W], False, True, [Bsp[zs], Bc2], [bankB[ab]],
                           skip_group_check=True)
                        if not last:
                            MM(banks[CSB_][:, W], negones_bf[:], sp_bf[zs][:, W], True, True, [Bsp[zs], Bc], [bankB[CSB_]])
                        if first:
                            ACT(A_bf[As][:, W], banks[ab][:, W], AF.Exp, [bankB[ab]], [BA[As]])
                        else:
                            TT("dve", arg_sb[zs][:, W], banks[ab][:, W], C_sb[cslot][:, W], ALU.add,
                               [bankB[ab], BC[cslot]], [Barg[zs]])
                            ACT(A_bf[As][:, W], arg_sb[zs][:, W], AF.Exp, [Barg[zs]], [BA[As]])
                        if not last:
                            TT("dve", C_sb[cslot][:, W], banks[CSB_][:, W], C_sb[cslot][:, W], ALU.add,
                               [bankB[CSB_], BC[cslot]], [BC[cslot]])
                        for _ in range(FILL_SB):
                            MM(banks[7][:, :], negtri[:], M0[:, 0:512], True, True, [Bc2], [bankB[7]])

                    def st2(As=As, i=i, j=j, W=W, first=first, last=last):
                        MM(banks[ob][0:HD + 1, W], Vt[sl][:, i, 0:HD + 1], A_bf[As][:, W], first, last,
                           [BV[sl], BVones[sl], BA[As]], [bankB[ob]], skip_group_check=True)
                        if last:
                            CP("dve", ostS[0:HD, j * 512:(j + 1) * 512], banks[ob][0:HD, :], [bankB[ob]], [BostS])
                            if j == NQ - 1:
                                DMA("sp", oT_d[hd, :, :], ostS[:, :], [BostS], [BoT_d[hd]], "ostS")
                    steps.append([st0, st1, st2])
            return steps

        def fox_steps(hd, sl):
            steps = []
            SBK = [4, 5]
            ob = 6
            MB = 2
            for j in range(NQ):
                nk = 4 * j + 4
                orot = j % 2
                for i in range(nk):
                    m = i - 4 * j
                    off = max(0, m) * 128
                    diag = m >= 0
                    first = i == 0
                    last = i == nk - 1
                    zs = cnt["fz"] % 2
                    cnt["fz"] += 1
                    As = cnt["fA"] % 3
                    cnt["fA"] += 1
                    kT = KTa[sl][0:70, i * 128:(i + 1) * 128]
                    qT = QTa[sl][0:70, j * 512 + off:(j + 1) * 512]
                    msk = M0[:, 1:1 + 512 - off]
                    W = slice(off, 512)

                    def st0(zs=zs, As=As, kT=kT, qT=qT, msk=msk, W=W, diag=diag, off=off):
                        zb = SBK[zs]
                        MM(banks[zb][:, W], kT, qT, True, not diag, [BK[sl], BQ[sl], BKaug[sl], BQaug[sl]], [bankB[zb]])
                        if diag:
                            MM(banks[zb][:, off:off + 128], negbigI[:], M0[:, 1:129], False, True, [Bc2], [bankB[zb]])

                    def stE(zs=zs, As=As, W=W):
                        zb = SBK[zs]
                        ACT(P_bf[As][:, W], banks[zb][:, W], AF.Exp, [bankB[zb]], [BPb[As]])

                    def st1(As=As, i=i, j=j, W=W, first=first, last=last, orot=orot):
                        MM(banks[ob][0:HD + 1, W], Vt[sl][:, i, 0:HD + 1], P_bf[As][:, W], first, last,
                           [BV[sl], BVones[sl], BPb[As]], [bankB[ob]])
                        if last:
                            CP("dve", oraw[orot][0:HD + 1, :], banks[ob][0:HD + 1, :], [bankB[ob]], [Boraw[orot]])

                    def stN(j=j, orot=orot, last=last):
                        if not last:
                            return
                        ACT(oraw[orot][64:65, :], oraw[orot][64:65, :], AF.Ln, [Boraw[orot]], [Boraw[orot]])
                        ACT(oraw[orot][64:65, :], oraw[orot][64:65, :], AF.Exp, [Boraw[orot]], [Boraw[orot]], scale=-1.0)

                    def stM(j=j, orot=orot, last=last):
                        if not last:
                            return
                        MM(banks[MB][0:HD, :], ones_f32[64:65, 0:HD], oraw[orot][64:65, :], True, True,
                           [Boraw[orot], Bc], [bankB[MB]])
                        TT("dve", ostF[0:HD, j * 512:(j + 1) * 512], banks[MB][0:HD, :], oraw[orot][0:HD, :], ALU.mult,
                           [bankB[MB], Boraw[orot]], [BostF])
                        if j == NQ - 1:
                            DMA("sp", oT_d[hd, :, :], ostF[:, :], [BostF], [BoT_d[hd]], "ostF")
                    steps.append([st0, stE, st1, stN, stM])
            return steps

        cnt["fz"] = 0
        cnt["fA"] = 0
        HS = 144
        LAG = HS // 2
        seq = []
        for h in range(NH):
            seq += [h, NH + h]
        sched = {}

        def add_bg(it, f):
            sched.setdefault(max(it, 0), []).append(f)

        for n, hd in enumerate(seq):
            sl = n % 3
            chunks = [lambda hd=hd, sl=sl: load_head_weights(hd, sl)]
            if n == 1:
                chunks.append(fox_prep)
            chunks += proj_chunks(hd, sl)
            k1 = len(chunks) // 3
            chunks = chunks[:k1] + [conv_expert(2 * n)] + chunks[k1:2 * k1] + [conv_expert(2 * n + 1)] + chunks[2 * k1:]
            h = n // 2
            start = HS * h if n % 2 == 0 else HS * h + LAG
            base = start - LAG + 5
            nch = len(chunks)
            for ci, f in enumerate(chunks):
                add_bg(base + (ci * (LAG - 8)) // nch, f)
        sb_lists = {}
        fx_lists = {}

        def sb_step(sidx):
            if sidx < 0 or sidx >= HS * NH:
                return None
            h, r = divmod(sidx, HS)
            if h not in sb_lists:
                sb_lists[h] = sb_steps(h, (2 * h) % 3)
                sb_lists.pop(h - 2, None)
            return sb_lists[h][r]

        def fx_step(fidx):
            if fidx < 0 or fidx >= HS * NH:
                return None
            h, r = divmod(fidx, HS)
            if h not in fx_lists:
                fx_lists[h] = fox_steps(NH + h, (2 * h + 1) % 3)
                fx_lists.pop(h - 2, None)
            return fx_lists[h][r]

        def run_stage(stp, k):
            if stp is not None and stp[k] is not None:
                stp[k]()

        for it in sorted(k for k in sched if k <= 0):
            for f in sched.pop(it):
                f()
        total_p = HS * NH + LAG
        for p_ in range(total_p + 5):
            f_ = p_ - LAG
            run_stage(fx_step(f_ - 1), 1)
            run_stage(sb_step(p_), 0)
            run_stage(fx_step(f_), 0)
            run_stage(sb_step(p_ - 1), 1)
            run_stage(fx_step(f_ - 1), 2)
            run_stage(sb_step(p_ - 2), 2)
            run_stage(fx_step(f_ - 2), 3)
            run_stage(fx_step(f_ - 3), 4)
            for f in sched.pop(p_, []):
                f()
        for it in sorted(sched):
            for f in sched[it]:
                f()

        P.barrier()
        stk.pop().close()
        stk.append(ExitStack())
        xs = [sb(f"xsb{i}", (128, D), F32) for i in range(NXS)]
        wosb = sb("wosb", (128, 4, D), BF16)
        wofx = sb("wofx", (128, 4, D), BF16)
        wout = sb("wout", (128, 8, D), BF16)
        wg = sb("wg", (128, 8, 2 * D), BF16)
        Bw3 = Buf("w3")
        P.op("pool", lambda e: [
            e.dma_start(out=wosb[:, :, :], in_=w_o_sb_d.rearrange("(c p) n -> p c n", p=128)),
            e.dma_start(out=wofx[:, :, :], in_=w_o_fox_d.rearrange("(c p) n -> p c n", p=128)),
            e.dma_start(out=wout[:, :, :], in_=w_out_d.rearrange("(c p) n -> p c n", p=128)),
        ] + [e.dma_start(out=wg[:, c, :], in_=w_in_d[c * 128:(c + 1) * 128, C_GSB:C_GSB + 2 * D]) for c in range(8)],
            (), [Bw3], dma_key="w3", ninc=11)
        oTt = [sb(f"oTt{i}", (128, 8, 512), BF16) for i in range(2)]
        BoTt = [Buf() for _ in range(2)]
        sig = [sb(f"sig{i}", (128, 512), BF16) for i in range(4)]
        Bsig = [Buf() for _ in range(4)]
        tmp = [sb(f"tmp{i}", (128, 512), F32) for i in range(2)]
        Btmp = [Buf() for _ in range(2)]
        mixT = [sb(f"mixT{i}", (128, 8, 512), BF16) for i in range(2)]
        BmixT = [Buf() for _ in range(2)]
        x2s = [sb(f"x2s{i}", (128, D), F32) for i in range(2)]
        Bx2s = [Buf() for _ in range(2)]
        Bx2d = [Buf(f"x2d{t}") for t in range(NT)]
        Bdbg = [Buf(f"dbg{t}") for t in range(NT)]

        def phase3():
            def load_oT(T):
                s2 = T % 2
                P.op("sp", lambda e: e.dma_start(
                    out=oTt[s2][:, :, :],
                    in_=oT_d[:, :, T * 512:(T + 1) * 512].rearrange("(c two) d t -> (two d) c t", two=2)),
                    BoT_d, [BoTt[s2]], dma_key=f"oTt{s2}")
            load_oT(0)
            for T in range(NQ):
                s2 = T % 2
                if T + 1 < NQ:
                    load_oT(T + 1)
                for dc in range(8):
                    dsl = slice(dc * 128, (dc + 1) * 128)
                    b0 = 0 if dc % 2 == 0 else 4
                    for c in range(4):
                        MM(banks[b0][:, :], wosb[:, c, dsl], oTt[s2][:, c, :], c == 0, c == 3, [Bw3, BoTt[s2]], [bankB[b0]])
                    for c in range(4):
                        MM(banks[b0 + 1][:, :], wofx[:, c, dsl], oTt[s2][:, 4 + c, :], c == 0, c == 3, [Bw3, BoTt[s2]], [bankB[b0 + 1]])
                    for c in range(8):
                        MM(banks[b0 + 2][:, :], wg[:, c, dsl], hT[:, c, T * 512:(T + 1) * 512], c == 0, c == 7,
                           [Bw3, BhT[T]], [bankB[b0 + 2]])
                    for c in range(8):
                        MM(banks[b0 + 3][:, :], wg[:, c, D + dc * 128:D + (dc + 1) * 128], hT[:, c, T * 512:(T + 1) * 512],
                           c == 0, c == 7, [Bw3, BhT[T]], [bankB[b0 + 3]])
                    sa = (dc % 2) * 2
                    ACT(sig[sa][:, :], banks[b0 + 2][:, :], AF.Sigmoid, [bankB[b0 + 2]], [Bsig[sa]])
                    ACT(sig[sa + 1][:, :], banks[b0 + 3][:, :], AF.Sigmoid, [bankB[b0 + 3]], [Bsig[sa + 1]])
                    t2 = dc % 2
                    TT("dve", tmp[t2][:, :], banks[b0][:, :], sig[sa][:, :], ALU.mult, [bankB[b0], Bsig[sa]], [Btmp[t2]])
                    TT("dve", sig[sa + 1][:, :], banks[b0 + 1][:, :], sig[sa + 1][:, :], ALU.mult,
                       [bankB[b0 + 1], Bsig[sa + 1]], [Bsig[sa + 1]])
                    TT("dve", mixT[s2][:, dc, :], tmp[t2][:, :], sig[sa + 1][:, :], ALU.add,
                       [Btmp[t2], Bsig[sa + 1]], [BmixT[s2]])
                for u in range(4):
                    tt = T * 4 + u
                    s3 = tt % NXS
                    xq = tt % 2
                    DMA("sp", xs[s3][:], x_d[tt * 128:(tt + 1) * 128, :], (), [Bxs[s3]], f"xs{s3}")
                    bb = 0 if tt % 2 == 0 else 4
                    for half in range(2):
                        for c in range(8):
                            MM(banks[bb + half][:, :], mixT[s2][:, c, u * 128:(u + 1) * 128], wout[:, c, half * 512:(half + 1) * 512],
                               c == 0, c == 7, [BmixT[s2], Bw3], [bankB[bb + half]])
                    for half in range(2):
                        TT("dve", x2s[xq][:, half * 512:(half + 1) * 512], banks[bb + half][:, :],
                           xs[s3][:, half * 512:(half + 1) * 512], ALU.add, [bankB[bb + half], Bxs[s3]], [Bx2s[xq]])
                    DMA("act", x2_d[tt * 128:(tt + 1) * 128, :], x2s[xq][:], [Bx2s[xq]], [Bx2d[tt]], f"x2st{xq}")
                    if debug and upto is None:
                        DMA("sp", dbg_d[tt * 128:(tt + 1) * 128, :], x2s[xq][:], [Bx2s[xq]], [Bdbg[tt]], f"dbgst{xq}")

        phase3()

        P.barrier()
        stk.pop().close()
        stk.append(ExitStack())
        BIGR = 1.0e4
        xs = [sb(f"xsc{i}", (128, D), F32) for i in range(NXS)]
        g2 = sb("g2", (128, D), F32)
        Bg2 = Buf("g2")
        DMA("sp", g2[:], norm_ffn_d[0:1, :].partition_broadcast(128), (), [Bg2], "g2")
        Bg3 = Buf("g3")
        DMA("sp", g3[:], norm_final_d[0:1, :].partition_broadcast(128), (), [Bg3], "g3")
        wr = sb("wr", (128, 8, 36), F32)
        rbias = sb("rbias", (128, 36), F32)
        Bwr = Buf("wr")
        P.op("sp", lambda e: [
            e.dma_start(out=wr[:, :, 0:4], in_=w_rg_d.rearrange("(c p) n -> p c n", p=128)),
            e.dma_start(out=wr[:, :, 4:36], in_=w_re_d.rearrange("(c p) n -> p c n", p=128)),
            e.dma_start(out=rbias[:, 0:4], in_=b_rg_d[0:1, :].partition_broadcast(128)),
            e.dma_start(out=rbias[:, 4:36], in_=b_re_d[0:1, :].partition_broadcast(128)),
        ], (), [Bwr], dma_key="wr", ninc=4)
        h2b = hT[:].rearrange("p c t -> p (c t)")
        Bh2b = [Buf(f"h2b{t}") for t in range(NT)]
        BM = [Buf(f"M{t}") for t in range(NT)]
        BRk = [Buf(f"Rk{t}") for t in range(NT)]
        BWg = [Buf(f"Wg{t}") for t in range(NT)]
        h2f = [sb(f"h2f{i}", (128, D), F32) for i in range(2)]
        Bh2f = [Buf() for _ in range(2)]
        h2Tf = [sb(f"h2Tf{i}", (128, 8, 128), F32) for i in range(2)]
        Bh2Tf = [Buf() for _ in range(2)]
        rg = [sb(f"rg{i}", (128, 640), F32) for i in range(2)]
        Brg = [Buf() for _ in range(2)]
        msel4 = [sb(f"msel4_{i}", (128, 4, NE), BF16) for i in range(2)]
        msel = [sb(f"msel{i}", (128, NE), BF16) for i in range(2)]
        Bmsel = [Buf() for _ in range(2)]
        mcum = [sb(f"mcum{i}", (128, NE), BF16) for i in range(2)]
        Bmcum = [Buf() for _ in range(2)]

        def phase3b():
            steps = []
            for tt in range(NT):
                steps.append([lambda tt=tt: p3b_front(tt), lambda tt=tt: p3b_mid(tt),
                              (lambda tt=tt: p3b_chain(tt // 4)) if tt % 4 == 3 else None])
            run_pipeline(steps, None)

        def p3b_front(tt):
            if True:
                s3 = tt % NXS
                s2 = tt % 2
                DMA("sp", xs[s3][:], x2_d[tt * 128:(tt + 1) * 128, :], [Bx2d[tt]], [Bxs[s3]], f"xs{s3}")
                rms_rstd(xs[s3][:], junk[:], stat[s2][:, 0:1], stat[s2][:, 1:2], stat[s2][:, 2:3],
                         [Bxs[s3]], Bjunk, Bstat[s2])
                STT(h2f[s2][:], xs[s3][:], stat[s2][:, 2:3], g2[:], ALU.mult, ALU.mult,
                    [Bxs[s3], Bstat[s2], Bg2], [Bh2f[s2]])
                CP("pool", h2b[:, tt * D:(tt + 1) * D], h2f[s2][:, :], [Bh2f[s2]], [Bh2b[tt]])

        def p3b_mid(tt):
            if True:
                s2 = tt % 2
                bA = 0 if s2 == 0 else 4
                for c in range(8):
                    bk = bA + c // 4
                    TR(banks[bk][:, (c % 4) * 128:(c % 4 + 1) * 128], h2f[s2][:, c * 128:(c + 1) * 128], ident_f32[:],
                       [Bh2f[s2], Bc2], [bankB[bk]])
                for hh in range(2):
                    bk = bA + hh
                    CP("act", h2Tf[s2][:, hh * 4:(hh + 1) * 4, :], banks[bk][:, :].rearrange("p (c t) -> p c t", c=4),
                       [bankB[bk]], [Bh2Tf[s2]])
                bR = bA + 2
                for c in range(8):
                    MM(banks[bR][:, 0:36], h2Tf[s2][:, c, :], wr[:, c, :], c == 0, c == 7, [Bh2Tf[s2], Bwr], [bankB[bR]])
                gp = (tt // 4) % 2
                g_ = tt % 4
                TT("dve", rg[gp][:, g_ * 36:(g_ + 1) * 36], banks[bR][:, 0:36], rbias[:, :], ALU.add,
                   [bankB[bR], Bwr], [Brg[gp]])

        def p3b_chain(grp):
            G = 4
            gp = grp % 2
            r = rg[gp]
            B_ = [Brg[gp]]
            t0_ = grp * G
            X = mybir.AxisListType.X

            def v(lo, n, *dims):
                ap = r[:, lo:lo + n]
                if len(dims) == 2:
                    return ap.rearrange("p (a b) -> p a b", a=dims[0])
                if len(dims) == 3:
                    return ap.rearrange("p (a b c) -> p a b c", a=dims[0], b=dims[1])
                return ap
            lg = v(0, G * 36, G, 36)
            gl = lg[:, :, 0:4]
            el = lg[:, :, 4:36]
            gmax = v(144, G)
            gmask = v(148, G * 4, G, 4)
            gd = v(164, G * 4, G, 4)
            gsum = v(180, G)
            gw = v(184, G)
            pen = v(188, G * 4, G, 4)
            elm = v(204, G * NE, G, NE)
            elm4 = v(204, G * NE, G, 4, 8)
            m1 = v(332, G)
            mask1 = v(336, G * NE, G, NE)
            m2 = v(464, G)
            mask2 = v(468, G * NE, G, NE)
            dd = v(596, G)
            ee = v(600, G)
            w1 = v(604, G)
            w2 = v(608, G)

            def bc(ap, shape):
                return ap.unsqueeze(len(ap.shape)).broadcast_to(shape)
            P.op("dve", lambda e: e.tensor_reduce(out=gmax, in_=gl, axis=X, op=ALU.max), B_, B_)
            TT("dve", gmask, gl, bc(gmax, [128, G, 4]), ALU.is_equal, B_, B_)
            TT("dve", gd, gl, bc(gmax, [128, G, 4]), ALU.subtract, B_, B_)
            ACT(gd, gd, AF.Exp, B_, B_)
            P.op("dve", lambda e: e.tensor_reduce(out=gsum, in_=gd, axis=X, op=ALU.add), B_, B_)
            P.op("dve", lambda e: e.reciprocal(out=gw, in_=gsum), B_, B_)
            TS("dve", pen, gmask, BIGR, -BIGR, ALU.mult, ALU.add, B_, B_)
            TT("dve", elm4, el.rearrange("p g (a b) -> p g a b", a=4), bc(pen, [128, G, 4, 8]), ALU.add, B_, B_)
            P.op("dve", lambda e: e.tensor_reduce(out=m1, in_=elm, axis=X, op=ALU.max), B_, B_)
            TT("dve", mask1, elm, bc(m1, [128, G, NE]), ALU.is_equal, B_, B_)
            STT(elm, mask1, -3.0 * BIGR, elm, ALU.mult, ALU.add, B_, B_)
            P.op("dve", lambda e: e.tensor_reduce(out=m2, in_=elm, axis=X, op=ALU.max), B_, B_)
            TT("dve", mask2, elm, bc(m2, [128, G, NE]), ALU.is_equal, B_, B_)
            TT("dve", dd, m1, m2, ALU.subtract, B_, B_)
            ACT(ee, dd, AF.Exp, B_, B_, scale=-1.0)
            TS("dve", w1, ee, 1.0, None, ALU.add, None, B_, B_)
            P.op("dve", lambda e: e.reciprocal(out=w1, in_=w1), B_, B_)
            TT("dve", w2, ee, w1, ALU.mult, B_, B_)
            BWgs = [BWg[t0_ + g] for g in range(G)]
            BMs = [BM[t0_ + g] for g in range(G)]
            TT("dve", Wg[:, t0_:t0_ + G, 0], w1, gw, ALU.mult, B_, BWgs)
            TT("dve", Wg[:, t0_:t0_ + G, 1], w2, gw, ALU.mult, B_, BWgs)
            CP("dve", M1[:, t0_:t0_ + G, :], mask1, B_, BMs)
            CP("dve", M2[:, t0_:t0_ + G, :], mask2, B_, BMs)
            TT("dve", msel4[gp][:, :, :], mask1, mask2, ALU.add, B_, [Bmsel[gp]])
            bK = 3 if gp == 0 else 7
            for g in range(G):
                tt = t0_ + g
                s2 = tt % 2
                if tt == 0:
                    CP("dve", mcum[s2][:, :], msel4[gp][:, g, :], [Bmsel[gp]], [Bmcum[s2]])
                else:
                    TT("dve", mcum[s2][:, :], mcum[1 - s2][:, :], msel4[gp][:, g, :], ALU.add,
                       [Bmcum[1 - s2], Bmsel[gp]], [Bmcum[s2]])
                MM(banks[bK][:, g * NE:(g + 1) * NE], SL_bf[:], msel4[gp][:, g, :], True, tt == 0, [Bmsel[gp], Bc2], [bankB[bK]])
                if tt > 0:
                    MM(banks[bK][:, g * NE:(g + 1) * NE], ones_bf[:], mcum[1 - s2][:, :], False, True,
                       [Bmcum[1 - s2], Bc], [bankB[bK]])
            CP("dve", Rk[:, t0_:t0_ + G, :], banks[bK][:, 0:G * NE].rearrange("p (g e) -> p g e", g=G),
               [bankB[bK]], [BRk[t0_ + g] for g in range(G)])

        phase3b()
        if upto == "3b":
            DMA("sp", dbg_d[0:128, :], Rk[:, :, :].rearrange("p t e -> p (t e)"), BRk, [Bdbg[0]], "dbgcmb")

        cnt = sb("cnt", (128, NE), F32)
        cnti = sb("cnti", (128, NE), I32)
        pcf = sb("pcf", (128, NE), F32)
        cend = sb("cend", (128, NE), F32)
        offs = sb("offs", (128, NE), F32)
        sstart_i = sb("sstart_i", (128, NSLOT), I32)
        sstart = sb("sstart", (128, NSLOT), F32)
        eidf = sb("eidf", (128, NSLOT), F32)
        pidx_i = sb("pidx_i", (128, 1), I32)
        pidx = sb("pidx", (128, 1), F32)
        posall = sb("posall", (128, NT * NE), F32)
        pmall = sb("pmall", (128, NT * NE), F32)
        pfall = sb("pfall", (128, 2, NT), F32)
        Bpb = Buf("passB")
        Bpos = [Buf() for _ in range(2)]
        BIDX = [Buf(f"IDX{t}") for t in range(NT)]
        BWI = Buf("WI")
        BXs = Buf("Xs_d")
        BXs_t = [Buf(f"Xs_t{t}") for t in range(NT)]

        def passB():
            lastm = (NT - 1) % 2
            MM(banks[0][:, 0:NE], ones_bf[:], mcum[lastm][:, :], True, True, [Bmcum[lastm], Bc], [bankB[0]])
            B_ = [Bpb]
            TS("dve", cnti[:, :], banks[0][:, 0:NE], float(SLOTR - 1), None, ALU.add, None, [bankB[0]], B_)
            TS("dve", cnti[:, :], cnti[:, :], SHIFT, None, ALU.logical_shift_right, None, B_, B_)
            TS("dve", cnti[:, :], cnti[:, :], SHIFT, None, ALU.logical_shift_left, None, B_, B_)
            CP("dve", pcf[:, :], cnti[:, :], B_, B_)
            P.op("dve", lambda e: e.tensor_tensor_scan(out=cend[:, :], data0=pcf[:, :], data1=pcf[:, :], initial=0.0,
                                                       op0=ALU.add, op1=ALU.max), B_, B_)
            TT("dve", offs[:, :], cend[:, :], pcf[:, :], ALU.subtract, B_, B_)
            P.op("pool", lambda e: e.iota(sstart_i[:, :], pattern=[[SLOTR, NSLOT]], base=0, channel_multiplier=0), (), B_)
            P.op("pool", lambda e: e.iota(pidx_i[:, :], pattern=[[0, 1]], base=0, channel_multiplier=1), (), B_)
            CP("dve", sstart[:, :], sstart_i[:, :], B_, B_)
            CP("dve", pidx[:, :], pidx_i[:, :], B_, B_)
            for ex in range(NE):
                if ex == 0:
                    TS("dve", eidf[:, :], sstart[:, :], cend[:, 0:1], None, ALU.is_ge, None, B_, B_)
                else:
                    STT(eidf[:, :], sstart[:, :], cend[:, ex:ex + 1], eidf[:, :], ALU.is_ge, ALU.add, B_, B_)
            TS("dve", eidf[:, :], eidf[:, :], float(NE - 1), 128.0, ALU.min, ALU.mult, B_, B_)
            TS("dve", WI[:, :], eidf[:, :], pidx[:, 0:1], None, ALU.add, None, B_, [BWI])
            Bp = [Bpos[0]]
            pos3 = posall[:, :].rearrange("p (t e) -> p t e", e=NE)
            pm3 = pmall[:, :].rearrange("p (t e) -> p t e", e=NE)
            TT("dve", pos3, Rk[:, :, :], offs[:, :].unsqueeze(1).broadcast_to([128, NT, NE]), ALU.add, BRk + [Bpb], Bp)
            TT("dve", pm3, pos3, M1[:, :, :], ALU.mult, Bp + BM, Bp)
            P.op("dve", lambda e: e.tensor_reduce(out=pfall[:, 0, :], in_=pm3, axis=mybir.AxisListType.X, op=ALU.add), Bp, Bp)
            TT("dve", pm3, pos3, M2[:, :, :], ALU.mult, Bp + BM, Bp)
            P.op("dve", lambda e: e.tensor_reduce(out=pfall[:, 1, :], in_=pm3, axis=mybir.AxisListType.X, op=ALU.add), Bp, Bp)
            CP("dve", IDX[:, :, :].rearrange("p t c -> p c t"), pfall[:, :, :], Bp, BIDX)
            for tt in range(NT):
                for ch in range(2):
                    P.op("pool", lambda e, tt=tt, ch=ch: e.indirect_dma_start(
                        out=Xs_d[:, :], out_offset=bass.IndirectOffsetOnAxis(ap=IDX[:, tt, ch:ch + 1], axis=0),
                        in_=h2b[:, tt * D:(tt + 1) * D], in_offset=None),
                        [BIDX[tt], Bh2b[tt]], [BXs_t[tt]] if ch else [BXs], dma_key="scat")

        passB()
        if upto == "pb":
            DMA("sp", dbg_d[0:128, 0:64], IDX[:, :, :].rearrange("p t c -> p (t c)").bitcast(F32), BIDX, [Bdbg[0]], "dbgidx")
            DMA("sp", dbg_d[128:256, 0:NSLOT], WI[:, :].bitcast(F32), [BWI], [Bdbg[1]], "dbgwi")
            DMA("sp", dbg_d[256:384, 0:64], Wg[:, :, :].rearrange("p t c -> p (t c)"), BWg, [Bdbg[2]], "dbgwg")

        P.barrier()
        stk.pop().close()
        stk.append(ExitStack())
        NWS = 4
        wsl = [sb(f"wsl{i}", (128, WROW), BF16) for i in range(NWS)]
        Bwsl = [Buf() for _ in range(NWS)]
        xsl = [sb(f"xsl{i}", (128, NSUB, D), BF16) for i in range(2)]
        Bxsl = [Buf() for _ in range(2)]
        XsT = [sb(f"XsT{i}", (128, 8, SLOTR), BF16) for i in range(2)]
        BXsT = [[Buf(), Buf()] for _ in range(2)]
        silb = [sb(f"silb{i}", (128, SLOTR), BF16) for i in range(4)]
        Bsilb = [Buf() for _ in range(4)]
        hidT = [sb(f"hidT{i}", (128, 2, SLOTR), BF16) for i in range(2)]
        BhidT = [Buf() for _ in range(2)]
        ysb = [sb(f"ysb{i}", (128, D), F32) for i in range(3)]
        Bysb = [[Buf(), Buf()] for _ in range(3)]
        BYs = [Buf(f"Ys{i}") for i in range(NSLOT)]
        Bout = [Buf(f"out{t}") for t in range(NT)]

        def slot_loop():
            yrot = [0]
            steps = []
            for i in range(NSLOT):
                ws = i % NWS
                r2 = i % 2

                def stL(i=i, ws=ws, r2=r2):
                    P.op("pool", lambda e: e.indirect_dma_start(
                        out=wsl[ws][:, :], out_offset=None, in_=Wb_d[:, :],
                        in_offset=bass.IndirectOffsetOnAxis(ap=WI[:, i:i + 1], axis=0)),
                        [BWI] + BWb, [Bwsl[ws]], dma_key=f"wsl{ws}")
                    DMA("sp", xsl[r2][:, :, :], Xs_d[i * SLOTR:(i + 1) * SLOTR, :].rearrange("(s p) d -> p s d", p=128),
                        [BXs] + BXs_t, [Bxsl[r2]], f"xsl{r2}")

                def stT(i=i, ws=ws, r2=r2):
                    for hb_ in range(2):
                        bk = hb_
                        tv = banks[bk][:].bitcast(BF16)
                        for cc in range(4):
                            c = hb_ * 4 + cc
                            for sub in range(NSUB):
                                TR(tv[:, cc * SLOTR + sub * 128:cc * SLOTR + (sub + 1) * 128], xsl[r2][:, sub, c * 128:(c + 1) * 128],
                                   ident_bf[:], [Bxsl[r2], Bc2], [bankB[bk]])
                        CP("act" if hb_ == 0 else "dve", XsT[r2][:, hb_ * 4:hb_ * 4 + 4, :],
                           tv[:, :].rearrange("p (c t) -> p c t", c=4), [bankB[bk]], [BXsT[r2][hb_]])

                def stAB(i=i, ws=ws, r2=r2):
                    for m in range(2):
                        ba = 2 + 2 * r2 + m
                        sb_ = 2 * r2 + m
                        for c in range(8):
                            MM(banks[ba][:, 0:SLOTR], wsl[ws][:, c * DE + m * 128:c * DE + (m + 1) * 128], XsT[r2][:, c, :], c == 0, c == 7,
                               [Bwsl[ws]] + BXsT[r2], [bankB[ba]])
                        for c in range(8):
                            MM(banks[ba][:, SLOTR:2 * SLOTR], wsl[ws][:, 8 * DE + c * DE + m * 128:8 * DE + c * DE + (m + 1) * 128],
                               XsT[r2][:, c, :], c == 0, c == 7, [Bwsl[ws]] + BXsT[r2], [bankB[ba]])
                        ACT(silb[sb_][:, :], banks[ba][:, 0:SLOTR], AF.Silu, [bankB[ba]], [Bsilb[sb_]])
                        TT("dve", hidT[r2][:, m, :], banks[ba][:, SLOTR:2 * SLOTR], silb[sb_][:, :], ALU.mult,
                           [bankB[ba], Bsilb[sb_]], [BhidT[r2]])

                def stY(i=i, ws=ws, r2=r2):
                    for sub in range(NSUB):
                        yr = yrot[0] % 3
                        yrot[0] += 1
                        for half in range(2):
                            by = 6 + half
                            for m in range(2):
                                MM(banks[by][:, :], hidT[r2][:, m, sub * 128:(sub + 1) * 128],
                                   wsl[ws][:, 16 * DE + m * D + half * 512:16 * DE + m * D + (half + 1) * 512],
                                   m == 0, m == 1, [BhidT[r2], Bwsl[ws]], [bankB[by]])
                            CP("act" if half == 0 else "dve", ysb[yr][:, half * 512:(half + 1) * 512], banks[by][:, :],
                               [bankB[by]], [Bysb[yr][half]])
                        DMA("act", Ys_d[i * SLOTR + sub * 128:i * SLOTR + (sub + 1) * 128, :], ysb[yr][:, :], Bysb[yr], [BYs[i]],
                            f"yst{yr}")
                steps.append([stL, stT, stAB, stY])
            run_pipeline(steps, None)

        def combine():
            def load(tt):
                s3 = tt % NXS
                s2 = tt % 3
                DMA("sp", xs[s3][:], x2_d[tt * 128:(tt + 1) * 128, :], [Bx2d[tt]], [Bxs[s3]], f"xs{s3}")
                P.op("pool", lambda e: e.indirect_dma_start(
                    out=y1[s2][:, :], out_offset=None, in_=Ys_d[:, :],
                    in_offset=bass.IndirectOffsetOnAxis(ap=IDX[:, tt, 0:1], axis=0)),
                    [BIDX[tt]] + BYs, [By1[s2]], dma_key=f"g1_{s2}")
                P.op("pool", lambda e: e.indirect_dma_start(
                    out=y2[s2][:, :], out_offset=None, in_=Ys_d[:, :],
                    in_offset=bass.IndirectOffsetOnAxis(ap=IDX[:, tt, 1:2], axis=0)),
                    [BIDX[tt]] + BYs, [By2[s2]], dma_key=f"g2_{s2}")
            load(0)
            load(1)
            for tt in range(NT):
                s3 = tt % NXS
                s2 = tt % 3
                STT(x3[s2][:, :], y1[s2][:, :], Wg[:, tt, 0:1], xs[s3][:, :], ALU.mult, ALU.add,
                    [By1[s2], BWg[tt], Bxs[s3]], [Bx3[s2]])
                if tt + 2 < NT:
                    load(tt + 2)
                STT(x3[s2][:, :], y2[s2][:, :], Wg[:, tt, 1:2], x3[s2][:, :], ALU.mult, ALU.add,
                    [By2[s2], BWg[tt], Bx3[s2]], [Bx3[s2]])
                rms_rstd(x3[s2][:], junk[:], stat[tt % 2][:, 0:1], stat[tt % 2][:, 1:2], stat[tt % 2][:, 2:3],
                         [Bx3[s2]], Bjunk, Bstat[tt % 2])
                STT(x3[s2][:], x3[s2][:], stat[tt % 2][:, 2:3], g3[:], ALU.mult, ALU.mult,
                    [Bx3[s2], Bstat[tt % 2], Bg3], [Bx3[s2]])
                DMA("act", out_d[tt * 128:(tt + 1) * 128, :], x3[s2][:], [Bx3[s2]], [Bout[tt]], f"outst{s2}")

        if upto not in ("3b", "pb"):
            slot_loop()
        P.barrier()
        stk.pop().close()
        stk.append(ExitStack())
        xs = [sb(f"xsd{i}", (128, D), F32) for i in range(NXS)]
        y1 = [sb(f"y1_{i}", (128, D), F32) for i in range(3)]
        y2 = [sb(f"y2_{i}", (128, D), F32) for i in range(3)]
        By1 = [Buf() for _ in range(3)]
        By2 = [Buf() for _ in range(3)]
        x3 = [sb(f"x3_{i}", (128, D), F32) for i in range(3)]
        Bx3 = [Buf() for _ in range(3)]
        if upto not in ("3b", "pb"):
            combine()

        P.op("sp", None, reads=(Bout if upto is None else []) + (Bdbg if debug else []))
        P.barrier()
        stk.pop().close()
        P.emit()
    return nc


_NC_CACHE = {}


def kernel(x, norm_attn, w_in, b_forget, w_o_sb, w_o_fox, w_out, norm_ffn,
           w_router_group, b_router_group, w_router_expert, b_router_expert,
           w1, w3, w2, norm_final, _debug=False, _upto=None, _cores=8):
    f32 = lambda a: np.ascontiguousarray(np.asarray(a, dtype=np.float32))
    nc = build_program(debug=_debug, upto=_upto)
    shared = {
        "norm_attn": f32(norm_attn).reshape(1, D), "w_in": f32(w_in)[0], "b_forget": f32(b_forget).reshape(1, NH),
        "w_o_sb": f32(w_o_sb)[0], "w_o_fox": f32(w_o_fox)[0], "w_out": f32(w_out)[0],
        "norm_ffn": f32(norm_ffn).reshape(1, D), "w_router_group": f32(w_router_group)[0],
        "b_router_group": f32(b_router_group).reshape(1, 4), "w_router_expert": f32(w_router_expert)[0],
        "b_router_expert": f32(b_router_expert).reshape(1, NE), "w1": f32(w1)[0], "w3": f32(w3)[0], "w2": f32(w2)[0],
        "norm_final": f32(norm_final).reshape(1, D),
    }
    xf = f32(x)
    in_maps = [dict(shared, x=xf[b]) for b in range(_cores)]
    res = run_bass_kernel_spmd(nc, in_maps, core_ids=list(range(_cores)))
    if _debug:
        return (np.stack([res.results[b]["out"] for b in range(_cores)], axis=0),
                np.stack([res.results[b]["dbg"] for b in range(_cores)], axis=0))
    return np.stack([res.results[b]["out"] for b in range(_cores)], axis=0)
```

```python
from contextlib import ExitStack
import numpy as np
import concourse.bass as bass
import concourse.mybir as mybir
from concourse.bass_utils import run_bass_kernel_spmd

F32 = mybir.dt.float32
BF16 = mybir.dt.bfloat16
I32 = mybir.dt.int32
AF = mybir.ActivationFunctionType
ALU = mybir.AluOpType

S = 4096
D = 1024
NT = S // 128
NQ = S // 512
HD = 64
NH = 8
INC = 5128
C_QSB, C_KSB, C_VSB, C_QFX, C_KFX, C_VFX, C_F, C_GSB, C_GFX = 0, 512, 1024, 1536, 2048, 2560, 3072, 3080, 4104
NE = 32
DE = 256
EPS = 1e-6
NEGBIG = -30000.0
FOX_DUMMY = 1
FILL_SB = 0
FILL_FX = 0

ENGS = ("pe", "act", "dve", "pool", "sp")
GEN = 30000


class Buf:
    __slots__ = ("name", "writer", "readers", "excl")

    def __init__(self, name="", excl=False):
        self.name = name
        self.writer = None
        self.readers = []
        self.excl = excl


class Op:
    __slots__ = ("eng", "fn", "idx", "deps", "signal", "sig", "dma", "dkey", "dval")

    def __init__(self, eng, fn, idx, dma):
        self.eng = eng
        self.fn = fn
        self.idx = idx
        self.deps = []
        self.signal = False
        self.sig = None
        self.dma = dma
        self.dkey = None
        self.dval = None


class Prog:
    def __init__(self, nc):
        self.nc = nc
        self.ops = {e: [] for e in ENGS}
        self.known = {e: {f: -1 for f in ENGS} for e in ENGS}
        self.known_dma = {e: {} for e in ENGS}
        self.dma_cnt = {}
        self.last_dma = {}
        self.pending = {e: [] for e in ENGS}

    def barrier(self):
        lasts = []
        for e in ENGS:
            for o in reversed(self.ops[e]):
                if not o.dma and o.fn is not None:
                    lasts.append(o)
                    break
        lasts += list(self.last_dma.values())
        for e in ENGS:
            self.pending[e] = list(lasts)

    def op(self, eng, fn, reads=(), writes=(), dma_key=None, ninc=1):
        o = Op(eng, fn, len(self.ops[eng]), dma_key is not None)
        cand = []
        for b in reads:
            if b.writer is not None:
                cand.append((b.writer, "raw"))
            if b.excl:
                for r in b.readers:
                    if r.eng != eng:
                        cand.append((r, "rar"))
        for b in writes:
            if b.writer is not None:
                cand.append((b.writer, "waw"))
            for r in b.readers:
                cand.append((r, "war"))
        best = {}
        dma_deps = {}
        if self.pending[eng]:
            for p in self.pending[eng]:
                if p.dma or p.eng != eng:
                    cand.append((p, "raw"))
            self.pending[eng] = []
        for (p, kind) in cand:
            if p is o:
                continue
            if p.dma:
                if self.known_dma[eng].get(p.dkey, 0) >= p.dval:
                    continue
                if p.dkey not in dma_deps or dma_deps[p.dkey].dval < p.dval:
                    dma_deps[p.dkey] = p
                continue
            if p.eng == eng:
                if eng == "pe":
                    continue
            if self.known[eng][p.eng] >= p.idx:
                continue
            if p.eng not in best or best[p.eng].idx < p.idx:
                best[p.eng] = p
        for k, p in dma_deps.items():
            o.deps.append(p)
            self.known_dma[eng][k] = p.dval
        for f, p in best.items():
            o.deps.append(p)
            p.signal = True
            self.known[eng][f] = p.idx
        if o.dma:
            o.dkey = dma_key
            self.dma_cnt[dma_key] = self.dma_cnt.get(dma_key, 0) + 16 * ninc
            o.dval = self.dma_cnt[dma_key]
            self.last_dma[dma_key] = o
        for b in reads:
            b.readers.append(o)
        for b in writes:
            b.writer = o
            b.readers = []
        self.ops[eng].append(o)
        return o

    def emit(self):
        nc = self.nc
        with ExitStack() as st:
            sems = {}
            for e in ENGS:
                n = 0
                for o in self.ops[e]:
                    if o.signal and not o.dma:
                        o.sig = n
                        n += 1
                ngen = max(1, (n + GEN - 1) // GEN)
                sems[e] = [st.enter_context(nc.semaphore(f"s_{e}_{g}")) for g in range(ngen)]
            dsem = {}
            for k in self.dma_cnt:
                dsem[k] = st.enter_context(nc.semaphore(f"d_{len(dsem)}"))
            block = st.enter_context(nc.Block())
            handles = {"pe": block.tensor, "act": block.scalar, "dve": block.vector,
                       "pool": block.gpsimd, "sp": block.sync}

            def run(e):
                ops = self.ops[e]
                if not ops:
                    return

                def body(eng):
                    for o in ops:
                        for p in o.deps:
                            if p.dma:
                                eng.wait_ge(dsem[p.dkey], p.dval)
                            else:
                                eng.wait_ge(sems[p.eng][p.sig // GEN], p.sig % GEN + 1)
                        if o.fn is None:
                            continue
                        ins = o.fn(eng)
                        if o.dma:
                            if not isinstance(ins, (list, tuple)):
                                ins = [ins]
                            for i_ in ins:
                                i_.then_inc(dsem[o.dkey], 16)
                        elif o.signal:
                            ins.then_inc(sems[e][o.sig // GEN], 1)
                handles[e](body)
            for e in ENGS:
                run(e)


def build_program(debug=False, upto=None):
    nc = bass.Bass("TRN2", target_bir_lowering=False)
    dram_in = lambda n, s: nc.dram_tensor(n, list(s), F32, kind="ExternalInput").ap()
    x_d = dram_in("x", (S, D))
    norm_attn_d = dram_in("norm_attn", (1, D))
    w_in_d = dram_in("w_in", (D, INC))
    b_forget_d = dram_in("b_forget", (1, NH))
    w_o_sb_d = dram_in("w_o_sb", (512, D))
    w_o_fox_d = dram_in("w_o_fox", (512, D))
    w_out_d = dram_in("w_out", (D, D))
    norm_ffn_d = dram_in("norm_ffn", (1, D))
    w_rg_d = dram_in("w_router_group", (D, 4))
    b_rg_d = dram_in("b_router_group", (1, 4))
    w_re_d = dram_in("w_router_expert", (D, NE))
    b_re_d = dram_in("b_router_expert", (1, NE))
    w1_d = dram_in("w1", (NE, D, DE))
    w3_d = dram_in("w3", (NE, D, DE))
    w2_d = dram_in("w2", (NE, DE, D))
    norm_final_d = dram_in("norm_final", (1, D))
    out_d = nc.dram_tensor("out", [S, D], F32, kind="ExternalOutput").ap()
    oT_d = nc.dram_tensor("oT_scr", [16, HD, S], BF16, kind="Internal").ap()
    x2_d = nc.dram_tensor("x2_scr", [S, D], F32, kind="Internal").ap()
    NSLOT = 64
    SLOTR = 256
    NSUB = SLOTR // 128
    SHIFT = 8
    WROW = 2 * 8 * DE + 2 * D
    Wb_d = nc.dram_tensor("Wb_scr", [NE * 128, WROW], BF16, kind="Internal").ap()
    Xs_d = nc.dram_tensor("Xs_scr", [NSLOT * SLOTR, D], BF16, kind="Internal").ap()
    Ys_d = nc.dram_tensor("Ys_scr", [NSLOT * SLOTR, D], F32, kind="Internal").ap()
    cparts_d = nc.dram_tensor("cparts_scr", [NH, 3, S], BF16, kind="Internal").ap()
    ncparts_d = nc.dram_tensor("ncparts_scr", [NH, 3, S], BF16, kind="Internal").ap()
    dbg_d = None
    if debug:
        dbg_d = nc.dram_tensor("dbg", [S, D], F32, kind="ExternalOutput").ap()

    P = Prog(nc)
    with ExitStack() as st:
        stk = [st]

        def sb(name, shape, dt):
            return stk[-1].enter_context(nc.sbuf_tensor(name, list(shape), dt))

        banks = [st.enter_context(nc.psum_tensor(f"bank{i}", [128, 512], F32)) for i in range(8)]
        bankB = [Buf(f"bank{i}", excl=True) for i in range(8)]

        def MM(out, lhsT, rhs, start, stop, reads, writes, **kw):
            return P.op("pe", lambda e: e.matmul(out, lhsT=lhsT, rhs=rhs, start=start, stop=stop, **kw), reads, writes)

        def TR(out, in_, ident, reads, writes):
            return P.op("pe", lambda e: e.transpose(out=out, in_=in_, identity=ident), reads, writes)

        def ACT(out, in_, func, reads, writes, **kw):
            return P.op("act", lambda e: e.activation(out=out, in_=in_, func=func, **kw), reads, writes)

        def TT(eng, out, in0, in1, op, reads, writes):
            return P.op(eng, lambda e: e.tensor_tensor(out=out, in0=in0, in1=in1, op=op), reads, writes)

        def TS(eng, out, in0, s1, s2, op0, op1, reads, writes, **kw):
            if op1 is None:
                return P.op(eng, lambda e: e.tensor_scalar(out=out, in0=in0, scalar1=s1, scalar2=None, op0=op0, **kw), reads, writes)
            return P.op(eng, lambda e: e.tensor_scalar(out=out, in0=in0, scalar1=s1, scalar2=s2, op0=op0, op1=op1, **kw), reads, writes)

        def STT(out, in0, scalar, in1, op0, op1, reads, writes):
            return P.op("dve", lambda e: e.scalar_tensor_tensor(out=out, in0=in0, scalar=scalar, in1=in1, op0=op0, op1=op1), reads, writes)

        def CP(eng, out, in_, reads, writes):
            if eng == "act":
                return P.op("act", lambda e: e.copy(out=out, in_=in_), reads, writes)
            return P.op(eng, lambda e: e.tensor_copy(out=out, in_=in_), reads, writes)

        def MEMSET(eng, ap, val, writes):
            return P.op(eng, lambda e: e.memset(ap, val), (), writes)

        def DMA(q, out, in_, reads, writes, key, **kw):
            return P.op(q, lambda e: e.dma_start(out=out, in_=in_, **kw), reads, writes, dma_key=key)

        ones_bf = sb("ones_bf", (128, 128), BF16)
        negones_bf = sb("negones_bf", (128, 128), BF16)
        negbig_bf = sb("negbig_bf", (128, 128), BF16)
        ident_bf = sb("ident_bf", (128, 128), BF16)
        negbigI = sb("negbigI", (128, 128), BF16)
        negtri = sb("negtri", (128, 128), BF16)
        ones513 = sb("ones513", (128, 513), BF16)
        M0 = sb("M0", (128, 513), BF16)
        ones_f32 = sb("ones_f32", (128, 128), F32)
        ident_f32 = sb("ident_f32", (128, 128), F32)
        eps_t = sb("eps_t", (128, 1), F32)
        Bc = Buf("consts")
        Bg1 = Buf("g1")

        P.op("pool", lambda e: e.memset(ones_bf[:], 1.0), (), [Bc])
        P.op("pool", lambda e: e.memset(negones_bf[:], -1.0), (), [Bc])
        P.op("pool", lambda e: e.memset(negbig_bf[:], NEGBIG), (), [Bc])
        P.op("pool", lambda e: e.memset(ones513[:], 1.0), (), [Bc])
        P.op("pool", lambda e: e.memset(ones_f32[:], 1.0), (), [Bc])
        P.op("pool", lambda e: e.memset(eps_t[:], EPS), (), [Bc])
        Bc2 = Buf("consts2")
        P.op("pool", lambda e: e.affine_select(out=ident_bf[:], in_=ones_bf[:], pattern=[[-1, 128]], compare_op=ALU.is_equal,
                                               fill=0.0, base=0, channel_multiplier=1), [Bc], [Bc2])
        P.op("pool", lambda e: e.affine_select(out=ident_f32[:], in_=ones_f32[:], pattern=[[-1, 128]], compare_op=ALU.is_equal,
                                               fill=0.0, base=0, channel_multiplier=1), [Bc], [Bc2])
        P.op("pool", lambda e: e.affine_select(out=negbigI[:], in_=negbig_bf[:], pattern=[[-1, 128]], compare_op=ALU.is_equal,
                                               fill=0.0, base=0, channel_multiplier=1), [Bc], [Bc2])
        P.op("pool", lambda e: e.affine_select(out=negtri[:], in_=negones_bf[:], pattern=[[-1, 128]], compare_op=ALU.is_ge,
                                               fill=0.0, base=0, channel_multiplier=1), [Bc], [Bc2])
        P.op("pool", lambda e: e.affine_select(out=M0[:], in_=ones513[:], pattern=[[-1, 513]], compare_op=ALU.is_ge,
                                               fill=0.0, base=0, channel_multiplier=1), [Bc], [Bc2])
        P.op("pool", lambda e: e.affine_select(out=SL_bf[:], in_=ones_bf[:], pattern=[[1, 128]], compare_op=ALU.is_ge,
                                               fill=0.0, base=-1, channel_multiplier=-1), [Bc], [Bc2])

        hT = sb("hT", (128, 8, S), BF16)
        BhT = [Buf(f"hT{t}") for t in range(NQ)]
        g3 = sb("g3", (128, D), F32)
        M1 = sb("M1", (128, NT, NE), BF16)
        M2 = sb("M2", (128, NT, NE), BF16)
        Rk = sb("Rk", (128, NT, NE), F32)
        Wg = sb("Wg", (128, NT, 2), F32)
        IDX = sb("IDX", (128, NT, 2), I32)
        WI = sb("WI", (128, 64), I32)
        SL_bf = sb("SL_bf", (128, 128), BF16)

        def rms_rstd(xs_ap, junk_ap, ss_ap, ln_ap, rstd_ap, rd, wr_junk, wr_stat):
            P.op("act", lambda e: e.activation(out=junk_ap, in_=xs_ap, func=AF.Square, accum_out=ss_ap), rd, [wr_junk, wr_stat])
            ACT(ln_ap, ss_ap, AF.Ln, [wr_stat, Bc], [wr_stat], scale=1.0 / D, bias=eps_t[:, 0:1])
            ACT(rstd_ap, ln_ap, AF.Exp, [wr_stat], [wr_stat], scale=-0.5)

        NXS = 2
        Bxs = [Buf(f"xs{i}") for i in range(NXS)]
        junk = sb("junk", (128, D), BF16)
        Bjunk = Buf("junk")
        stat = [sb(f"stat{i}", (128, 4), F32) for i in range(2)]
        Bstat = [Buf(f"stat{i}") for i in range(2)]
        stk.append(ExitStack())
        g1 = sb("g1", (128, D), F32)
        DMA("sp", g1[:], norm_attn_d[0:1, :].partition_broadcast(128), (), [Bg1], "g1")
        xs = [sb(f"xsa{i}", (128, D), F32) for i in range(NXS)]
        hb = [sb(f"hb{i}", (128, D), BF16) for i in range(2)]
        Bhb = [Buf(f"hb{i}") for i in range(2)]

        def run_pipeline(stage_lists, bg, every=6):
            n = len(stage_lists)
            nst = max(len(x) for x in stage_lists)
            bgi = 0
            for s in range(n + nst - 1):
                for k in range(nst):
                    t = s - k
                    if 0 <= t < n and k < len(stage_lists[t]) and stage_lists[t][k] is not None:
                        stage_lists[t][k]()
                if bg and s % every == 3 and bgi < len(bg):
                    bg[bgi]()
                    bgi += 1
            while bg and bgi < len(bg):
                bg[bgi]()
                bgi += 1

        def phase1():
            def front(tt):
                s3 = tt % NXS
                s2 = tt % 2
                DMA("sp", xs[s3][:], x_d[tt * 128:(tt + 1) * 128, :], (), [Bxs[s3]], f"xs{s3}")
                rms_rstd(xs[s3][:], junk[:], stat[s2][:, 0:1], stat[s2][:, 1:2], stat[s2][:, 2:3],
                         [Bxs[s3]], Bjunk, Bstat[s2])
                STT(hb[s2][:], xs[s3][:], stat[s2][:, 2:3], g1[:], ALU.mult, ALU.mult,
                    [Bxs[s3], Bstat[s2], Bg1], [Bhb[s2]])

            def back(tt):
                s2 = tt % 2
                bk = 6 + (tt % 2)
                pv = banks[bk][:].bitcast(BF16)
                for c in range(8):
                    TR(pv[:, c * 128:(c + 1) * 128], hb[s2][:, c * 128:(c + 1) * 128], ident_bf[:],
                       [Bhb[s2], Bc2], [bankB[bk]])
                CP("act", hT[:, :, tt * 128:(tt + 1) * 128], pv[:, :].rearrange("p (c t) -> p c t", c=8),
                   [bankB[bk]], [BhT[tt // 4]])
            steps = [[lambda tt=tt: front(tt), lambda tt=tt: back(tt)] for tt in range(NT)]
            run_pipeline(steps, None)

        phase1()
        P.barrier()
        stk.pop().close()

        NSL = 3
        stk.append(ExitStack())
        QTa = [sb(f"QTa{i}", (128, S), BF16) for i in range(NSL)]
        KTa = [sb(f"KTa{i}", (128, S), BF16) for i in range(NSL)]
        Vt = [sb(f"Vt{i}", (128, NT, HD + 1), BF16) for i in range(NSL)]
        wqk = [sb(f"wqk{i}", (128, 8, 130), BF16) for i in range(NSL)]
        wv = [sb(f"wv{i}", (128, 8, HD), BF16) for i in range(NSL)]
        BQ = [Buf(f"Q{i}") for i in range(NSL)]
        BK = [Buf(f"K{i}") for i in range(NSL)]
        BQaug = [Buf(f"Qaug{i}") for i in range(NSL)]
        BKaug = [Buf(f"Kaug{i}") for i in range(NSL)]
        BV = [Buf(f"V{i}") for i in range(NSL)]
        BVones = [Buf(f"Vones{i}") for i in range(NSL)]
        Bwqk = [Buf(f"wqk{i}") for i in range(NSL)]
        Bwv = [Buf(f"wv{i}") for i in range(NSL)]
        ostS = sb("ostS", (64, S), BF16)
        ostF = sb("ostF", (64, S), BF16)
        BostS = Buf("ostS")
        BostF = Buf("ostF")
        wstage = [sb(f"wstage{i}", (128, 2048), BF16) for i in range(1)]
        Bwstage = [Buf(f"wstage{i}") for i in range(1)]
        BWb = [Buf(f"Wb{e}") for e in range(NE)]

        def conv_expert(ex):
            def f():
                srcs = [w1_d[ex].rearrange("(c p) n -> p c n", p=128), w3_d[ex].rearrange("(c p) n -> p c n", p=128),
                        w2_d[ex].rearrange("(k p) n -> p k n", p=128)]
                pats = ["p (c n) -> p c n", "p (c n) -> p c n", "p (k n) -> p k n"]
                for part in range(3):
                    kw = {"c": 8} if part < 2 else {"k": 2}
                    P.op("pool", lambda e, part=part, kw=kw: e.dma_start(
                        out=wstage[0][:, :].rearrange(pats[part], **kw), in_=srcs[part]),
                        (), [Bwstage[0]], dma_key="wstg0")
                    DMA("sp", Wb_d[ex * 128:(ex + 1) * 128, part * 2048:(part + 1) * 2048], wstage[0][:, :],
                        [Bwstage[0]], [BWb[ex]], "wbst0")
            return f
        BoT_d = [Buf(f"oTd{h}") for h in range(16)]

        for i in range(NSL):
            P.op("pool", lambda e, i=i: e.memset(Vt[i][:, :, HD:HD + 1], 1.0), (), [BVones[i]])
            P.op("pool", lambda e, i=i: e.memset(wqk[i][:, :, 128:130], 0.0), (), [BVones[i]])

        e_sb = [sb(f"e_sb{i}", (128, 512), F32) for i in range(2)]
        sp_bf = [sb(f"sp_bf{i}", (128, 512), BF16) for i in range(2)]
        arg_sb = [sb(f"arg_sb{i}", (128, 512), F32) for i in range(2)]
        A_bf = [sb(f"A_bf{i}", (128, 512), BF16) for i in range(3)]
        P_bf = [sb(f"P_bf{i}", (128, 512), BF16) for i in range(3)]
        BPb = [Buf() for _ in range(3)]
        oraw = [sb(f"oraw{i}", (128, 512), F32) for i in range(2)]
        Boraw = [Buf() for _ in range(2)]
        C_sb = [sb(f"C_sb{i}", (128, 512), F32) for i in range(2)]
        Be = [Buf() for _ in range(2)]
        Bsp = [Buf() for _ in range(2)]
        Barg = [Buf() for _ in range(2)]
        BA = [Buf() for _ in range(3)]
        BC = [Buf() for _ in range(2)]

        wf = sb("wf", (128, 8, NH), BF16)
        Bwf = Buf("wf")
        fexp = [sb(f"fexp{i}", (NH, 512), F32) for i in range(1)] * 2
        cpt = [sb(f"cpt{i}", (NH, 512), F32) for i in range(2)]
        r1 = fexp
        cpp = [sb(f"cpp{i}", (NH, 3, 512), BF16) for i in range(1)] * 2
        negb = sb("negb", (NH, 1), F32)
        Bnegb = Buf("negb")
        Bfexp = [Buf()] * 2
        Bcpt = [Buf() for _ in range(2)]
        Br1 = Bfexp
        Bcpp = [Buf()] * 2
        Bncpp = [Buf()] * 2
        Bcparts = [Buf(f"cparts_d{t}") for t in range(NQ)]

        def load_head_weights(hd, sl):
            typ, h = divmod(hd, NH)
            cq = (C_QSB if typ == 0 else C_QFX) + h * HD
            ck = (C_KSB if typ == 0 else C_KFX) + h * HD
            cv = (C_VSB if typ == 0 else C_VFX) + h * HD
            P.op("pool", lambda e: [
                e.dma_start(out=wqk[sl][:, :, 0:HD], in_=w_in_d[:, cq:cq + HD].rearrange("(c p) n -> p c n", p=128)),
                e.dma_start(out=wqk[sl][:, :, HD:2 * HD], in_=w_in_d[:, ck:ck + HD].rearrange("(c p) n -> p c n", p=128)),
            ], (), [Bwqk[sl]], dma_key=f"wqk{sl}", ninc=2)
            P.op("pool", lambda e: e.dma_start(out=wv[sl][:, :, :], in_=w_in_d[:, cv:cv + HD].rearrange("(c p) n -> p c n", p=128)),
                 (), [Bwv[sl]], dma_key=f"wv{sl}")

        pj_rot = [0]

        def proj_chunks(hd, sl):
            typ, h = divmod(hd, NH)
            chunks = []
            bk = 7

            def qk_chunk(T, which):
                lo = 0 if which == "q" else HD
                out = []
                for c in range(8):
                    out.append(lambda c=c: MM(banks[bk][0:HD + 1, :], wqk[sl][:, c, lo:lo + HD + 1],
                                              hT[:, c, T * 512:(T + 1) * 512], c == 0, c == 7,
                                              [Bwqk[sl], BhT[T], BVones[sl]], [bankB[bk]]))
                if which == "q":
                    out.append(lambda: TS("dve", QTa[sl][0:HD, T * 512:(T + 1) * 512], banks[bk][0:HD, :], 0.125, None,
                                          ALU.mult, None, [bankB[bk]], [BQ[sl]]))
                else:
                    out.append(lambda: CP("dve", KTa[sl][0:HD, T * 512:(T + 1) * 512], banks[bk][0:HD, :],
                                          [bankB[bk]], [BK[sl]]))
                return out

            def v_chunk(g):
                out = []
                for u in range(8):
                    def f(u=u):
                        tt = g * 8 + u
                        for c in range(8):
                            MM(banks[bk][:, u * HD:(u + 1) * HD], hT[:, c, tt * 128:(tt + 1) * 128], wv[sl][:, c, :],
                               c == 0, c == 7, [Bwv[sl], BhT[tt // 4]], [bankB[bk]])
                    out.append(f)
                out.append(lambda: CP("dve", Vt[sl][:, g * 8:(g + 1) * 8, 0:HD],
                                      banks[bk][:, :].rearrange("p (u d) -> p u d", u=8), [bankB[bk]], [BV[sl]]))
                return out

            for T in range(NQ):
                chunks += qk_chunk(T, "k")
            for g in range(4):
                chunks += v_chunk(g)
            for T in range(NQ):
                chunks += qk_chunk(T, "q")
            if typ == 0:
                def zpad():
                    P.op("pool", lambda e: e.memset(QTa[sl][64:128, :], 0.0), (), [BQaug[sl]])
                    P.op("pool", lambda e: e.memset(KTa[sl][64:128, :], 0.0), (), [BKaug[sl]])
                chunks.append(zpad)
            if typ == 1:
                def aug():
                    P.op("pool", lambda e: e.memset(QTa[sl][64:70, :], 1.0), (), [BQaug[sl]])
                    P.op("pool", lambda e: e.memset(KTa[sl][64:70, :], -1.0), (), [BKaug[sl]])
                    DMA("sp", QTa[sl][64:67, :], cparts_d[h, :, :], Bcparts, [BQaug[sl]], f"qaug{sl}")
                    DMA("sp", KTa[sl][67:70, :], cparts_d[h, :, :], Bcparts, [BKaug[sl]], f"kaug{sl}")
                chunks.append(aug)
            return chunks

        def fox_prep():
            P.op("pool", lambda e: e.dma_start(out=wf[:, :, :], in_=w_in_d[:, C_F:C_F + NH].rearrange("(c p) n -> p c n", p=128)),
                 (), [Bwf], dma_key="wf")
            DMA("sp", negb[:, 0:1], b_forget_d[0:1, :].rearrange("o h -> h o"), (), [Bnegb], "negb")
            TS("dve", negb[:, 0:1], negb[:, 0:1], -1.0, None, ALU.mult, None, [Bnegb], [Bnegb])
            for T in range(NQ):
                bk = 7
                s2 = T % 2
                for c in range(8):
                    MM(banks[bk][0:NH, :], wf[:, c, :], hT[:, c, T * 512:(T + 1) * 512], c == 0, c == 7,
                       [Bwf, BhT[T]], [bankB[bk]])
                ACT(fexp[s2][:, :], banks[bk][0:NH, :], AF.Exp, [bankB[bk], Bnegb], [Bfexp[s2]],
                    scale=-1.0, bias=negb[:, 0:1])
                ACT(fexp[s2][:, :], fexp[s2][:, :], AF.Ln, [Bfexp[s2]], [Bfexp[s2]], bias=1.0)
                init = 0.0 if T == 0 else cpt[1 - s2][:, 511:512]
                rds = [Bfexp[s2]] + ([Bcpt[1 - s2]] if T > 0 else [])
                P.op("dve", lambda e, s2=s2, init=init: e.tensor_tensor_scan(
                    out=cpt[s2][:, :], data0=fexp[s2][:, :], data1=fexp[s2][:, :], initial=init,
                    op0=ALU.add, op1=ALU.max), rds, [Bcpt[s2]])
                CP("dve", cpp[s2][:, 0, :], cpt[s2][:, :], [Bcpt[s2]], [Bcpp[s2]])
                TT("dve", r1[s2][:, :], cpt[s2][:, :], cpp[s2][:, 0, :], ALU.subtract, [Bcpt[s2], Bcpp[s2]], [Br1[s2]])
                CP("dve", cpp[s2][:, 1, :], r1[s2][:, :], [Br1[s2]], [Bcpp[s2]])
                TT("dve", r1[s2][:, :], r1[s2][:, :], cpp[s2][:, 1, :], ALU.subtract, [Br1[s2], Bcpp[s2]], [Br1[s2]])
                CP("dve", cpp[s2][:, 2, :], r1[s2][:, :], [Br1[s2]], [Bcpp[s2]])
                P.op("sp", lambda e, s2=s2, T=T: [
                    e.dma_start(out=cparts_d[:, :, T * 512:(T + 1) * 512], in_=cpp[s2][:, :, :]),
                ], [Bcpp[s2]], [Bcparts[T]], dma_key="cpst0", ninc=1)

        ZB = [0, 1]
        AB = [2, 3]
        CSB = 4
        OB = 5

        cnt = {"z": 0, "a": 0, "A": 0, "c": 0, "e": 0}

        def sb_steps(hd, sl):
            steps = []
            ZA = [0, 1]
            CSB_ = 2
            ob = 3
            for j in range(NQ):
                cslot = cnt["c"] % 2
                cnt["c"] += 1
                order = list(range(4 * j + 3, -1, -1))
                for n_, i in enumerate(order):
                    m = i - 4 * j
                    off = max(0, m) * 128
                    diag = m >= 0
                    first = n_ == 0
                    last = i == 0
                    zs = cnt["z"] % 2
                    cnt["z"] += 1
                    As = cnt["A"] % 3
                    cnt["A"] += 1
                    kT = KTa[sl][0:128, i * 128:(i + 1) * 128]
                    qT = QTa[sl][0:128, j * 512 + off:(j + 1) * 512]
                    msk = M0[:, 0:512 - off]
                    W = slice(off, 512)

                    def st0(zs=zs, kT=kT, qT=qT, msk=msk, W=W, diag=diag, first=first, cslot=cslot, off=off):
                        if first:
                            MEMSET("pool", C_sb[cslot][:], 0.0, [BC[cslot]])
                        zb = ZA[zs]
                        MM(banks[zb][:, W], kT, qT, True, not diag, [BK[sl], BQ[sl], BKaug[sl], BQaug[sl]], [bankB[zb]])
                        if diag:
                            MM(banks[zb][:, off:off + 128], negbigI[:], M0[:, 0:128], False, True, [Bc2], [bankB[zb]])
                        ACT(e_sb[zs][:, W], banks[zb][:, W], AF.Exp, [bankB[zb]], [Be[zs]])
                        ACT(sp_bf[zs][:, W], e_sb[zs][:, W], AF.Ln, [Be[zs]], [Bsp[zs]], bias=1.0)

                    def st1(zs=zs, As=As, W=W, first=first, last=last, cslot=cslot):
                        ab = ZA[zs]
                        MM(banks[ab][:, W], negtri[:], sp_bf[zs][:, W], False, True, [Bsp[zs], Bc2], [bankB[ab]],
                           skip_group_check=True)
                        if not last:
                            MM(banks[CSB_][:, W], negones_bf[:], sp_bf[zs][:, W], True, True, [Bsp[zs], Bc], [bankB[CSB_]])
                        if first:
                            ACT(A_bf[As][:, W], banks[ab][:, W], AF.Exp, [bankB[ab]], [BA[As]])
                        else:
                            TT("dve", arg_sb[zs][:, W], banks[ab][:, W], C_sb[cslot][:, W], ALU.add,
                               [bankB[ab], BC[cslot]], [Barg[zs]])
                            ACT(A_bf[As][:, W], arg_sb[zs][:, W], AF.Exp, [Barg[zs]], [BA[As]])
                        if not last:
                            TT("dve", C_sb[cslot][:, W], banks[CSB_][:, W], C_sb[cslot][:, W], ALU.add,
                               [bankB[CSB_], BC[cslot]], [BC[cslot]])
                        for _ in range(FILL_SB):
                            MM(banks[7][:, :], negtri[:], M0[:, 0:512], True, True, [Bc2], [bankB[7]])

                    def st2(As=As, i=i, j=j, W=W, first=first, last=last):
                        MM(banks[ob][0:HD + 1, W], Vt[sl][:, i, 0:HD + 1], A_bf[As][:, W], first, last,
                           [BV[sl], BVones[sl], BA[As]], [bankB[ob]], skip_group_check=True)
                        if last:
                            CP("dve", ostS[0:HD, j * 512:(j + 1) * 512], banks[ob][0:HD, :], [bankB[ob]], [BostS])
                            if j == NQ - 1:
                                DMA("sp", oT_d[hd, :, :], ostS[:, :], [BostS], [BoT_d[hd]], "ostS")
                    steps.append([st0, st1, st2])
            return steps

        def fox_steps(hd, sl):
            steps = []
            SBK = [4, 5]
            ob = 6
            MB = 2
            for j in range(NQ):
                nk = 4 * j + 4
                orot = j % 2
                for i in range(nk):
                    m = i - 4 * j
                    off = max(0, m) * 128
                    diag = m >= 0
                    first = i == 0
                    last = i == nk - 1
                    zs = cnt["fz"] % 2
                    cnt["fz"] += 1
                    As = cnt["fA"] % 3
                    cnt["fA"] += 1
                    kT = KTa[sl][0:70, i * 128:(i + 1) * 128]
                    qT = QTa[sl][0:70, j * 512 + off:(j + 1) * 512]
                    msk = M0[:, 1:1 + 512 - off]
                    W = slice(off, 512)

                    def st0(zs=zs, As=As, kT=kT, qT=qT, msk=msk, W=W, diag=diag, off=off):
                        zb = SBK[zs]
                        MM(banks[zb][:, W], kT, qT, True, not diag, [BK[sl], BQ[sl], BKaug[sl], BQaug[sl]], [bankB[zb]])
                        if diag:
                            MM(banks[zb][:, off:off + 128], negbigI[:], M0[:, 1:129], False, True, [Bc2], [bankB[zb]])

                    def stE(zs=zs, As=As, W=W):
                        zb = SBK[zs]
                        ACT(P_bf[As][:, W], banks[zb][:, W], AF.Exp, [bankB[zb]], [BPb[As]])

                    def st1(As=As, i=i, j=j, W=W, first=first, last=last, orot=orot):
                        MM(banks[ob][0:HD + 1, W], Vt[sl][:, i, 0:HD + 1], P_bf[As][:, W], first, last,
                           [BV[sl], BVones[sl], BPb[As]], [bankB[ob]])
                        if last:
                            CP("dve", oraw[orot][0:HD + 1, :], banks[ob][0:HD + 1, :], [bankB[ob]], [Boraw[orot]])

                    def stN(j=j, orot=orot, last=last):
                        if not last:
                            return
                        ACT(oraw[orot][64:65, :], oraw[orot][64:65, :], AF.Ln, [Boraw[orot]], [Boraw[orot]])
                        ACT(oraw[orot][64:65, :], oraw[orot][64:65, :], AF.Exp, [Boraw[orot]], [Boraw[orot]], scale=-1.0)

                    def stM(j=j, orot=orot, last=last):
                        if not last:
                            return
                        MM(banks[MB][0:HD, :], ones_f32[64:65, 0:HD], oraw[orot][64:65, :], True, True,
                           [Boraw[orot], Bc], [bankB[MB]])
                        TT("dve", ostF[0:HD, j * 512:(j + 1) * 512], banks[MB][0:HD, :], oraw[orot][0:HD, :], ALU.mult,
                           [bankB[MB], Boraw[orot]], [BostF])
                        if j == NQ - 1:
                            DMA("sp", oT_d[hd, :, :], ostF[:, :], [BostF], [BoT_d[hd]], "ostF")
                    steps.append([st0, stE, st1, stN, stM])
            return steps

        cnt["fz"] = 0
        cnt["fA"] = 0
        HS = 144
        LAG = HS // 2
        seq = []
        for h in range(NH):
            seq += [h, NH + h]
        sched = {}

        def add_bg(it, f):
            sched.setdefault(max(it, 0), []).append(f)

        for n, hd in enumerate(seq):
            sl = n % 3
            chunks = [lambda hd=hd, sl=sl: load_head_weights(hd, sl)]
            if n == 1:
                chunks.append(fox_prep)
            chunks += proj_chunks(hd, sl)
            k1 = len(chunks) // 3
            chunks = chunks[:k1] + [conv_expert(2 * n)] + chunks[k1:2 * k1] + [conv_expert(2 * n + 1)] + chunks[2 * k1:]
            h = n // 2
            start = HS * h if n % 2 == 0 else HS * h + LAG
            base = start - LAG + 5
            nch = len(chunks)
            for ci, f in enumerate(chunks):
                add_bg(base + (ci * (LAG - 8)) // nch, f)
        sb_lists = {}
        fx_lists = {}

        def sb_step(sidx):
            if sidx < 0 or sidx >= HS * NH:
                return None
            h, r = divmod(sidx, HS)
            if h not in sb_lists:
                sb_lists[h] = sb_steps(h, (2 * h) % 3)
                sb_lists.pop(h - 2, None)
            return sb_lists[h][r]

        def fx_step(fidx):
            if fidx < 0 or fidx >= HS * NH:
                return None
            h, r = divmod(fidx, HS)
            if h not in fx_lists:
                fx_lists[h] = fox_steps(NH + h, (2 * h + 1) % 3)
                fx_lists.pop(h - 2, None)
            return fx_lists[h][r]

        def run_stage(stp, k):
            if stp is not None and stp[k] is not None:
                stp[k]()

        for it in sorted(k for k in sched if k <= 0):
            for f in sched.pop(it):
                f()
        total_p = HS * NH + LAG
        for p_ in range(total_p + 5):
            f_ = p_ - LAG
            run_stage(fx_step(f_ - 1), 1)
            run_stage(sb_step(p_), 0)
            run_stage(fx_step(f_), 0)
            run_stage(sb_step(p_ - 1), 1)
            run_stage(fx_step(f_ - 1), 2)
            run_stage(sb_step(p_ - 2), 2)
            run_stage(fx_step(f_ - 2), 3)
            run_stage(fx_step(f_ - 3), 4)
            for f in sched.pop(p_, []):
                f()
        for it in sorted(sched):
            for f in sched[it]:
                f()

        P.barrier()
        stk.pop().close()
        stk.append(ExitStack())
        xs = [sb(f"xsb{i}", (128, D), F32) for i in range(NXS)]
        wosb = sb("wosb", (128, 4, D), BF16)
        wofx = sb("wofx", (128, 4, D), BF16)
        wout = sb("wout", (128, 8, D), BF16)
        wg = sb("wg", (128, 8, 2 * D), BF16)
        Bw3 = Buf("w3")
        P.op("pool", lambda e: [
            e.dma_start(out=wosb[:, :, :], in_=w_o_sb_d.rearrange("(c p) n -> p c n", p=128)),
            e.dma_start(out=wofx[:, :, :], in_=w_o_fox_d.rearrange("(c p) n -> p c n", p=128)),
            e.dma_start(out=wout[:, :, :], in_=w_out_d.rearrange("(c p) n -> p c n", p=128)),
        ] + [e.dma_start(out=wg[:, c, :], in_=w_in_d[c * 128:(c + 1) * 128, C_GSB:C_GSB + 2 * D]) for c in range(8)],
            (), [Bw3], dma_key="w3", ninc=11)
        oTt = [sb(f"oTt{i}", (128, 8, 512), BF16) for i in range(2)]
        BoTt = [Buf() for _ in range(2)]
        sig = [sb(f"sig{i}", (128, 512), BF16) for i in range(4)]
        Bsig = [Buf() for _ in range(4)]
        tmp = [sb(f"tmp{i}", (128, 512), F32) for i in range(2)]
        Btmp = [Buf() for _ in range(2)]
        mixT = [sb(f"mixT{i}", (128, 8, 512), BF16) for i in range(2)]
        BmixT = [Buf() for _ in range(2)]
        x2s = [sb(f"x2s{i}", (128, D), F32) for i in range(2)]
        Bx2s = [Buf() for _ in range(2)]
        Bx2d = [Buf(f"x2d{t}") for t in range(NT)]
        Bdbg = [Buf(f"dbg{t}") for t in range(NT)]

        def phase3():
            def load_oT(T):
                s2 = T % 2
                P.op("sp", lambda e: e.dma_start(
                    out=oTt[s2][:, :, :],
                    in_=oT_d[:, :, T * 512:(T + 1) * 512].rearrange("(c two) d t -> (two d) c t", two=2)),
                    BoT_d, [BoTt[s2]], dma_key=f"oTt{s2}")
            load_oT(0)
            for T in range(NQ):
                s2 = T % 2
                if T + 1 < NQ:
                    load_oT(T + 1)
                for dc in range(8):
                    dsl = slice(dc * 128, (dc + 1) * 128)
                    b0 = 0 if dc % 2 == 0 else 4
                    for c in range(4):
                        MM(banks[b0][:, :], wosb[:, c, dsl], oTt[s2][:, c, :], c == 0, c == 3, [Bw3, BoTt[s2]], [bankB[b0]])
                    for c in range(4):
                        MM(banks[b0 + 1][:, :], wofx[:, c, dsl], oTt[s2][:, 4 + c, :], c == 0, c == 3, [Bw3, BoTt[s2]], [bankB[b0 + 1]])
                    for c in range(8):
                        MM(banks[b0 + 2][:, :], wg[:, c, dsl], hT[:, c, T * 512:(T + 1) * 512], c == 0, c == 7,
                           [Bw3, BhT[T]], [bankB[b0 + 2]])
                    for c in range(8):
                        MM(banks[b0 + 3][:, :], wg[:, c, D + dc * 128:D + (dc + 1) * 128], hT[:, c, T * 512:(T + 1) * 512],
                           c == 0, c == 7, [Bw3, BhT[T]], [bankB[b0 + 3]])
                    sa = (dc % 2) * 2
                    ACT(sig[sa][:, :], banks[b0 + 2][:, :], AF.Sigmoid, [bankB[b0 + 2]], [Bsig[sa]])
                    ACT(sig[sa + 1][:, :], banks[b0 + 3][:, :], AF.Sigmoid, [bankB[b0 + 3]], [Bsig[sa + 1]])
                    t2 = dc % 2
                    TT("dve", tmp[t2][:, :], banks[b0][:, :], sig[sa][:, :], ALU.mult, [bankB[b0], Bsig[sa]], [Btmp[t2]])
                    TT("dve", sig[sa + 1][:, :], banks[b0 + 1][:, :], sig[sa + 1][:, :], ALU.mult,
                       [bankB[b0 + 1], Bsig[sa + 1]], [Bsig[sa + 1]])
                    TT("dve", mixT[s2][:, dc, :], tmp[t2][:, :], sig[sa + 1][:, :], ALU.add,
                       [Btmp[t2], Bsig[sa + 1]], [BmixT[s2]])
                for u in range(4):
                    tt = T * 4 + u
                    s3 = tt % NXS
                    xq = tt % 2
                    if tt == 0:
                        DMA("sp", xs[0][:], x_d[0:128, :], (), [Bxs[0]], "xs0")
                    if tt + 1 < NT:
                        n3 = (tt + 1) % NXS
                        DMA("sp", xs[n3][:], x_d[(tt + 1) * 128:(tt + 2) * 128, :], (), [Bxs[n3]], f"xs{n3}")
                    bb = 0 if tt % 2 == 0 else 4
                    for half in range(2):
                        for c in range(8):
                            MM(banks[bb + half][:, :], mixT[s2][:, c, u * 128:(u + 1) * 128], wout[:, c, half * 512:(half + 1) * 512],
                               c == 0, c == 7, [BmixT[s2], Bw3], [bankB[bb + half]])
                    for half in range(2):
                        TT("dve", x2s[xq][:, half * 512:(half + 1) * 512], banks[bb + half][:, :],
                           xs[s3][:, half * 512:(half + 1) * 512], ALU.add, [bankB[bb + half], Bxs[s3]], [Bx2s[xq]])
                    DMA("act", x2_d[tt * 128:(tt + 1) * 128, :], x2s[xq][:], [Bx2s[xq]], [Bx2d[tt]], f"x2st{xq}")
                    if debug and upto is None:
                        DMA("sp", dbg_d[tt * 128:(tt + 1) * 128, :], x2s[xq][:], [Bx2s[xq]], [Bdbg[tt]], f"dbgst{xq}")

        phase3()

        P.barrier()
        stk.pop().close()
        stk.append(ExitStack())
        BIGR = 1.0e4
        xs = [sb(f"xsc{i}", (128, D), F32) for i in range(NXS)]
        g2 = sb("g2", (128, D), F32)
        Bg2 = Buf("g2")
        DMA("sp", g2[:], norm_ffn_d[0:1, :].partition_broadcast(128), (), [Bg2], "g2")
        Bg3 = Buf("g3")
        DMA("sp", g3[:], norm_final_d[0:1, :].partition_broadcast(128), (), [Bg3], "g3")
        wr = sb("wr", (128, 8, 36), F32)
        rbias = sb("rbias", (128, 36), F32)
        Bwr = Buf("wr")
        P.op("sp", lambda e: [
            e.dma_start(out=wr[:, :, 0:4], in_=w_rg_d.rearrange("(c p) n -> p c n", p=128)),
            e.dma_start(out=wr[:, :, 4:36], in_=w_re_d.rearrange("(c p) n -> p c n", p=128)),
            e.dma_start(out=rbias[:, 0:4], in_=b_rg_d[0:1, :].partition_broadcast(128)),
            e.dma_start(out=rbias[:, 4:36], in_=b_re_d[0:1, :].partition_broadcast(128)),
        ], (), [Bwr], dma_key="wr", ninc=4)
        h2b = hT[:].rearrange("p c t -> p (c t)")
        Bh2b = [Buf(f"h2b{t}") for t in range(NT)]
        BM = [Buf(f"M{t}") for t in range(NT)]
        BRk = [Buf(f"Rk{t}") for t in range(NT)]
        BWg = [Buf(f"Wg{t}") for t in range(NT)]
        h2f = [sb(f"h2f{i}", (128, D), F32) for i in range(2)]
        Bh2f = [Buf() for _ in range(2)]
        h2Tf = [sb(f"h2Tf{i}", (128, 8, 128), F32) for i in range(2)]
        Bh2Tf = [Buf() for _ in range(2)]
        rg = [sb(f"rg{i}", (128, 640), F32) for i in range(2)]
        Brg = [Buf() for _ in range(2)]
        msel4 = [sb(f"msel4_{i}", (128, 4, NE), BF16) for i in range(2)]
        msel = [sb(f"msel{i}", (128, NE), BF16) for i in range(2)]
        Bmsel = [Buf() for _ in range(2)]
        mcum = [sb(f"mcum{i}", (128, NE), BF16) for i in range(2)]
        Bmcum = [Buf() for _ in range(2)]

        def phase3b():
            steps = []
            for tt in range(NT):
                steps.append([lambda tt=tt: p3b_front(tt), lambda tt=tt: p3b_mid(tt),
                              (lambda tt=tt: p3b_chain(tt // 4)) if tt % 4 == 3 else None])
            run_pipeline(steps, None)

        def p3b_front(tt):
            if True:
                s3 = tt % NXS
                s2 = tt % 2
                DMA("sp", xs[s3][:], x2_d[tt * 128:(tt + 1) * 128, :], [Bx2d[tt]], [Bxs[s3]], f"xs{s3}")
                rms_rstd(xs[s3][:], junk[:], stat[s2][:, 0:1], stat[s2][:, 1:2], stat[s2][:, 2:3],
                         [Bxs[s3]], Bjunk, Bstat[s2])
                STT(h2f[s2][:], xs[s3][:], stat[s2][:, 2:3], g2[:], ALU.mult, ALU.mult,
                    [Bxs[s3], Bstat[s2], Bg2], [Bh2f[s2]])
                CP("pool", h2b[:, tt * D:(tt + 1) * D], h2f[s2][:, :], [Bh2f[s2]], [Bh2b[tt]])

        def p3b_mid(tt):
            if True:
                s2 = tt % 2
                bA = 0 if s2 == 0 else 4
                for c in range(8):
                    bk = bA + c // 4
                    TR(banks[bk][:, (c % 4) * 128:(c % 4 + 1) * 128], h2f[s2][:, c * 128:(c + 1) * 128], ident_f32[:],
                       [Bh2f[s2], Bc2], [bankB[bk]])
                for hh in range(2):
                    bk = bA + hh
                    CP("act", h2Tf[s2][:, hh * 4:(hh + 1) * 4, :], banks[bk][:, :].rearrange("p (c t) -> p c t", c=4),
                       [bankB[bk]], [Bh2Tf[s2]])
                bR = bA + 2
                for c in range(8):
                    MM(banks[bR][:, 0:36], h2Tf[s2][:, c, :], wr[:, c, :], c == 0, c == 7, [Bh2Tf[s2], Bwr], [bankB[bR]])
                gp = (tt // 4) % 2
                g_ = tt % 4
                TT("dve", rg[gp][:, g_ * 36:(g_ + 1) * 36], banks[bR][:, 0:36], rbias[:, :], ALU.add,
                   [bankB[bR], Bwr], [Brg[gp]])

        def p3b_chain(grp):
            G = 4
            gp = grp % 2
            r = rg[gp]
            B_ = [Brg[gp]]
            t0_ = grp * G
            X = mybir.AxisListType.X

            def v(lo, n, *dims):
                ap = r[:, lo:lo + n]
                if len(dims) == 2:
                    return ap.rearrange("p (a b) -> p a b", a=dims[0])
                if len(dims) == 3:
                    return ap.rearrange("p (a b c) -> p a b c", a=dims[0], b=dims[1])
                return ap
            lg = v(0, G * 36, G, 36)
            gl = lg[:, :, 0:4]
            el = lg[:, :, 4:36]
            gmax = v(144, G)
            gmask = v(148, G * 4, G, 4)
            gd = v(164, G * 4, G, 4)
            gsum = v(180, G)
            gw = v(184, G)
            pen = v(188, G * 4, G, 4)
            elm = v(204, G * NE, G, NE)
            elm4 = v(204, G * NE, G, 4, 8)
            m1 = v(332, G)
            mask1 = v(336, G * NE, G, NE)
            m2 = v(464, G)
            mask2 = v(468, G * NE, G, NE)
            dd = v(596, G)
            ee = v(600, G)
            w1 = v(604, G)
            w2 = v(608, G)

            def bc(ap, shape):
                return ap.unsqueeze(len(ap.shape)).broadcast_to(shape)
            P.op("dve", lambda e: e.tensor_reduce(out=gmax, in_=gl, axis=X, op=ALU.max), B_, B_)
            TT("dve", gmask, gl, bc(gmax, [128, G, 4]), ALU.is_equal, B_, B_)
            TT("dve", gd, gl, bc(gmax, [128, G, 4]), ALU.subtract, B_, B_)
            ACT(gd, gd, AF.Exp, B_, B_)
            P.op("dve", lambda e: e.tensor_reduce(out=gsum, in_=gd, axis=X, op=ALU.add), B_, B_)
            P.op("dve", lambda e: e.reciprocal(out=gw, in_=gsum), B_, B_)
            TS("dve", pen, gmask, BIGR, -BIGR, ALU.mult, ALU.add, B_, B_)
            TT("dve", elm4, el.rearrange("p g (a b) -> p g a b", a=4), bc(pen, [128, G, 4, 8]), ALU.add, B_, B_)
            P.op("dve", lambda e: e.tensor_reduce(out=m1, in_=elm, axis=X, op=ALU.max), B_, B_)
            TT("dve", mask1, elm, bc(m1, [128, G, NE]), ALU.is_equal, B_, B_)
            STT(elm, mask1, -3.0 * BIGR, elm, ALU.mult, ALU.add, B_, B_)
            P.op("dve", lambda e: e.tensor_reduce(out=m2, in_=elm, axis=X, op=ALU.max), B_, B_)
            TT("dve", mask2, elm, bc(m2, [128, G, NE]), ALU.is_equal, B_, B_)
            TT("dve", dd, m1, m2, ALU.subtract, B_, B_)
            ACT(ee, dd, AF.Exp, B_, B_, scale=-1.0)
            TS("dve", w1, ee, 1.0, None, ALU.add, None, B_, B_)
            P.op("dve", lambda e: e.reciprocal(out=w1, in_=w1), B_, B_)
            TT("dve", w2, ee, w1, ALU.mult, B_, B_)
            BWgs = [BWg[t0_ + g] for g in range(G)]
            BMs = [BM[t0_ + g] for g in range(G)]
            TT("dve", Wg[:, t0_:t0_ + G, 0], w1, gw, ALU.mult, B_, BWgs)
            TT("dve", Wg[:, t0_:t0_ + G, 1], w2, gw, ALU.mult, B_, BWgs)
            CP("dve", M1[:, t0_:t0_ + G, :], mask1, B_, BMs)
            CP("dve", M2[:, t0_:t0_ + G, :], mask2, B_, BMs)
            TT("dve", msel4[gp][:, :, :], mask1, mask2, ALU.add, B_, [Bmsel[gp]])
            bK = 3 if gp == 0 else 7
            for g in range(G):
                tt = t0_ + g
                s2 = tt % 2
                if tt == 0:
                    CP("dve", mcum[s2][:, :], msel4[gp][:, g, :], [Bmsel[gp]], [Bmcum[s2]])
                else:
                    TT("dve", mcum[s2][:, :], mcum[1 - s2][:, :], msel4[gp][:, g, :], ALU.add,
                       [Bmcum[1 - s2], Bmsel[gp]], [Bmcum[s2]])
                MM(banks[bK][:, g * NE:(g + 1) * NE], SL_bf[:], msel4[gp][:, g, :], True, tt == 0, [Bmsel[gp], Bc2], [bankB[bK]])
                if tt > 0:
                    MM(banks[bK][:, g * NE:(g + 1) * NE], ones_bf[:], mcum[1 - s2][:, :], False, True,
                       [Bmcum[1 - s2], Bc], [bankB[bK]])
            CP("dve", Rk[:, t0_:t0_ + G, :], banks[bK][:, 0:G * NE].rearrange("p (g e) -> p g e", g=G),
               [bankB[bK]], [BRk[t0_ + g] for g in range(G)])

        phase3b()
        if upto == "3b":
            DMA("sp", dbg_d[0:128, :], Rk[:, :, :].rearrange("p t e -> p (t e)"), BRk, [Bdbg[0]], "dbgcmb")

        cnt = sb("cnt", (128, NE), F32)
        cnti = sb("cnti", (128, NE), I32)
        pcf = sb("pcf", (128, NE), F32)
        cend = sb("cend", (128, NE), F32)
        offs = sb("offs", (128, NE), F32)
        sstart_i = sb("sstart_i", (128, NSLOT), I32)
        sstart = sb("sstart", (128, NSLOT), F32)
        eidf = sb("eidf", (128, NSLOT), F32)
        pidx_i = sb("pidx_i", (128, 1), I32)
        pidx = sb("pidx", (128, 1), F32)
        posall = sb("posall", (128, NT * NE), F32)
        pmall = sb("pmall", (128, NT * NE), F32)
        pfall = sb("pfall", (128, 2, NT), F32)
        Bpb = Buf("passB")
        Bpos = [Buf() for _ in range(2)]
        BIDX = [Buf(f"IDX{t}") for t in range(NT)]
        BWI = Buf("WI")
        BXs = Buf("Xs_d")
        BXs_t = [Buf(f"Xs_t{t}") for t in range(NT)]

        def passB():
            lastm = (NT - 1) % 2
            MM(banks[0][:, 0:NE], ones_bf[:], mcum[lastm][:, :], True, True, [Bmcum[lastm], Bc], [bankB[0]])
            B_ = [Bpb]
            TS("dve", cnti[:, :], banks[0][:, 0:NE], float(SLOTR - 1), None, ALU.add, None, [bankB[0]], B_)
            TS("dve", cnti[:, :], cnti[:, :], SHIFT, None, ALU.logical_shift_right, None, B_, B_)
            TS("dve", cnti[:, :], cnti[:, :], SHIFT, None, ALU.logical_shift_left, None, B_, B_)
            CP("dve", pcf[:, :], cnti[:, :], B_, B_)
            P.op("dve", lambda e: e.tensor_tensor_scan(out=cend[:, :], data0=pcf[:, :], data1=pcf[:, :], initial=0.0,
                                                       op0=ALU.add, op1=ALU.max), B_, B_)
            TT("dve", offs[:, :], cend[:, :], pcf[:, :], ALU.subtract, B_, B_)
            P.op("pool", lambda e: e.iota(sstart_i[:, :], pattern=[[SLOTR, NSLOT]], base=0, channel_multiplier=0), (), B_)
            P.op("pool", lambda e: e.iota(pidx_i[:, :], pattern=[[0, 1]], base=0, channel_multiplier=1), (), B_)
            CP("dve", sstart[:, :], sstart_i[:, :], B_, B_)
            CP("dve", pidx[:, :], pidx_i[:, :], B_, B_)
            for ex in range(NE):
                if ex == 0:
                    TS("dve", eidf[:, :], sstart[:, :], cend[:, 0:1], None, ALU.is_ge, None, B_, B_)
                else:
                    STT(eidf[:, :], sstart[:, :], cend[:, ex:ex + 1], eidf[:, :], ALU.is_ge, ALU.add, B_, B_)
            TS("dve", eidf[:, :], eidf[:, :], float(NE - 1), 128.0, ALU.min, ALU.mult, B_, B_)
            TS("dve", WI[:, :], eidf[:, :], pidx[:, 0:1], None, ALU.add, None, B_, [BWI])
            Bp = [Bpos[0]]
            pos3 = posall[:, :].rearrange("p (t e) -> p t e", e=NE)
            pm3 = pmall[:, :].rearrange("p (t e) -> p t e", e=NE)
            TT("dve", pos3, Rk[:, :, :], offs[:, :].unsqueeze(1).broadcast_to([128, NT, NE]), ALU.add, BRk + [Bpb], Bp)
            TT("dve", pm3, pos3, M1[:, :, :], ALU.mult, Bp + BM, Bp)
            P.op("dve", lambda e: e.tensor_reduce(out=pfall[:, 0, :], in_=pm3, axis=mybir.AxisListType.X, op=ALU.add), Bp, Bp)
            TT("dve", pm3, pos3, M2[:, :, :], ALU.mult, Bp + BM, Bp)
            P.op("dve", lambda e: e.tensor_reduce(out=pfall[:, 1, :], in_=pm3, axis=mybir.AxisListType.X, op=ALU.add), Bp, Bp)
            CP("dve", IDX[:, :, :].rearrange("p t c -> p c t"), pfall[:, :, :], Bp, BIDX)
            for tt in range(NT):
                for ch in range(2):
                    P.op("pool", lambda e, tt=tt, ch=ch: e.indirect_dma_start(
                        out=Xs_d[:, :], out_offset=bass.IndirectOffsetOnAxis(ap=IDX[:, tt, ch:ch + 1], axis=0),
                        in_=h2b[:, tt * D:(tt + 1) * D], in_offset=None),
                        [BIDX[tt], Bh2b[tt]], [BXs_t[tt]] if ch else [BXs], dma_key="scat")

        passB()
        if upto == "pb":
            DMA("sp", dbg_d[0:128, 0:64], IDX[:, :, :].rearrange("p t c -> p (t c)").bitcast(F32), BIDX, [Bdbg[0]], "dbgidx")
            DMA("sp", dbg_d[128:256, 0:NSLOT], WI[:, :].bitcast(F32), [BWI], [Bdbg[1]], "dbgwi")
            DMA("sp", dbg_d[256:384, 0:64], Wg[:, :, :].rearrange("p t c -> p (t c)"), BWg, [Bdbg[2]], "dbgwg")

        P.barrier()
        stk.pop().close()
        stk.append(ExitStack())
        NWS = 4
        wsl = [sb(f"wsl{i}", (128, WROW), BF16) for i in range(NWS)]
        Bwsl = [Buf() for _ in range(NWS)]
        xsl = [sb(f"xsl{i}", (128, NSUB, D), BF16) for i in range(2)]
        Bxsl = [Buf() for _ in range(2)]
        XsT = [sb(f"XsT{i}", (128, 8, SLOTR), BF16) for i in range(2)]
        BXsT = [[Buf(), Buf()] for _ in range(2)]
        silb = [sb(f"silb{i}", (128, SLOTR), BF16) for i in range(4)]
        Bsilb = [Buf() for _ in range(4)]
        hidT = [sb(f"hidT{i}", (128, 2, SLOTR), BF16) for i in range(2)]
        BhidT = [Buf() for _ in range(2)]
        ysb = [sb(f"ysb{i}", (128, D), F32) for i in range(3)]
        Bysb = [[Buf(), Buf()] for _ in range(3)]
        BYs = [Buf(f"Ys{i}") for i in range(NSLOT)]
        Bout = [Buf(f"out{t}") for t in range(NT)]

        def slot_loop():
            yrot = [0]
            steps = []
            for i in range(NSLOT):
                ws = i % NWS
                r2 = i % 2

                def stL(i=i, ws=ws, r2=r2):
                    P.op("pool", lambda e: e.indirect_dma_start(
                        out=wsl[ws][:, :], out_offset=None, in_=Wb_d[:, :],
                        in_offset=bass.IndirectOffsetOnAxis(ap=WI[:, i:i + 1], axis=0)),
                        [BWI] + BWb, [Bwsl[ws]], dma_key=f"wsl{ws}")
                    DMA("sp", xsl[r2][:, :, :], Xs_d[i * SLOTR:(i + 1) * SLOTR, :].rearrange("(s p) d -> p s d", p=128),
                        [BXs] + BXs_t, [Bxsl[r2]], f"xsl{r2}")

                def stT(i=i, ws=ws, r2=r2):
                    for hb_ in range(2):
                        bk = hb_
                        tv = banks[bk][:].bitcast(BF16)
                        for cc in range(4):
                            c = hb_ * 4 + cc
                            for sub in range(NSUB):
                                TR(tv[:, cc * SLOTR + sub * 128:cc * SLOTR + (sub + 1) * 128], xsl[r2][:, sub, c * 128:(c + 1) * 128],
                                   ident_bf[:], [Bxsl[r2], Bc2], [bankB[bk]])
                        CP("act" if hb_ == 0 else "dve", XsT[r2][:, hb_ * 4:hb_ * 4 + 4, :],
                           tv[:, :].rearrange("p (c t) -> p c t", c=4), [bankB[bk]], [BXsT[r2][hb_]])

                def stAB(i=i, ws=ws, r2=r2):
                    for m in range(2):
                        ba = 2 + 2 * r2 + m
                        sb_ = 2 * r2 + m
                        for c in range(8):
                            MM(banks[ba][:, 0:SLOTR], wsl[ws][:, c * DE + m * 128:c * DE + (m + 1) * 128], XsT[r2][:, c, :], c == 0, c == 7,
                               [Bwsl[ws]] + BXsT[r2], [bankB[ba]])
                        for c in range(8):
                            MM(banks[ba][:, SLOTR:2 * SLOTR], wsl[ws][:, 8 * DE + c * DE + m * 128:8 * DE + c * DE + (m + 1) * 128],
                               XsT[r2][:, c, :], c == 0, c == 7, [Bwsl[ws]] + BXsT[r2], [bankB[ba]])
                        ACT(silb[sb_][:, :], banks[ba][:, 0:SLOTR], AF.Silu, [bankB[ba]], [Bsilb[sb_]])
                        TT("dve", hidT[r2][:, m, :], banks[ba][:, SLOTR:2 * SLOTR], silb[sb_][:, :], ALU.mult,
                           [bankB[ba], Bsilb[sb_]], [BhidT[r2]])

                def stY(i=i, ws=ws, r2=r2):
                    for sub in range(NSUB):
                        yr = yrot[0] % 3
                        yrot[0] += 1
                        for half in range(2):
                            by = 6 + half
                            for m in range(2):
                                MM(banks[by][:, :], hidT[r2][:, m, sub * 128:(sub + 1) * 128],
                                   wsl[ws][:, 16 * DE + m * D + half * 512:16 * DE + m * D + (half + 1) * 512],
                                   m == 0, m == 1, [BhidT[r2], Bwsl[ws]], [bankB[by]])
                            CP("act" if half == 0 else "dve", ysb[yr][:, half * 512:(half + 1) * 512], banks[by][:, :],
                               [bankB[by]], [Bysb[yr][half]])
                        DMA("act", Ys_d[i * SLOTR + sub * 128:i * SLOTR + (sub + 1) * 128, :], ysb[yr][:, :], Bysb[yr], [BYs[i]],
                            f"yst{yr}")
                steps.append([stL, stT, stAB, stY])
            run_pipeline(steps, None)

        def combine():
            def load(tt):
                s3 = tt % NXS
                s2 = tt % 3
                DMA("sp", xs[s3][:], x2_d[tt * 128:(tt + 1) * 128, :], [Bx2d[tt]], [Bxs[s3]], f"xs{s3}")
                P.op("pool", lambda e: e.indirect_dma_start(
                    out=y1[s2][:, :], out_offset=None, in_=Ys_d[:, :],
                    in_offset=bass.IndirectOffsetOnAxis(ap=IDX[:, tt, 0:1], axis=0)),
                    [BIDX[tt]] + BYs, [By1[s2]], dma_key=f"g1_{s2}")
                P.op("pool", lambda e: e.indirect_dma_start(
                    out=y2[s2][:, :], out_offset=None, in_=Ys_d[:, :],
                    in_offset=bass.IndirectOffsetOnAxis(ap=IDX[:, tt, 1:2], axis=0)),
                    [BIDX[tt]] + BYs, [By2[s2]], dma_key=f"g2_{s2}")
            load(0)
            load(1)
            for tt in range(NT):
                s3 = tt % NXS
                s2 = tt % 3
                STT(x3[s2][:, :], y1[s2][:, :], Wg[:, tt, 0:1], xs[s3][:, :], ALU.mult, ALU.add,
                    [By1[s2], BWg[tt], Bxs[s3]], [Bx3[s2]])
                if tt + 2 < NT:
                    load(tt + 2)
                STT(x3[s2][:, :], y2[s2][:, :], Wg[:, tt, 1:2], x3[s2][:, :], ALU.mult, ALU.add,
                    [By2[s2], BWg[tt], Bx3[s2]], [Bx3[s2]])
                rms_rstd(x3[s2][:], junk[:], stat[tt % 2][:, 0:1], stat[tt % 2][:, 1:2], stat[tt % 2][:, 2:3],
                         [Bx3[s2]], Bjunk, Bstat[tt % 2])
                STT(x3[s2][:], x3[s2][:], stat[tt % 2][:, 2:3], g3[:], ALU.mult, ALU.mult,
                    [Bx3[s2], Bstat[tt % 2], Bg3], [Bx3[s2]])
                DMA("act", out_d[tt * 128:(tt + 1) * 128, :], x3[s2][:], [Bx3[s2]], [Bout[tt]], f"outst{s2}")

        if upto not in ("3b", "pb"):
            slot_loop()
        P.barrier()
        stk.pop().close()
        stk.append(ExitStack())
        xs = [sb(f"xsd{i}", (128, D), F32) for i in range(NXS)]
        y1 = [sb(f"y1_{i}", (128, D), F32) for i in range(3)]
        y2 = [sb(f"y2_{i}", (128, D), F32) for i in range(3)]
        By1 = [Buf() for _ in range(3)]
        By2 = [Buf() for _ in range(3)]
        x3 = [sb(f"x3_{i}", (128, D), F32) for i in range(3)]
        Bx3 = [Buf() for _ in range(3)]
        if upto not in ("3b", "pb"):
            combine()

        P.op("sp", None, reads=(Bout if upto is None else []) + (Bdbg if debug else []))
        P.barrier()
        stk.pop().close()
        P.emit()
    return nc


_NC_CACHE = {}


def kernel(x, norm_attn, w_in, b_forget, w_o_sb, w_o_fox, w_out, norm_ffn,
           w_router_group, b_router_group, w_router_expert, b_router_expert,
           w1, w3, w2, norm_final, _debug=False, _upto=None, _cores=8):
    f32 = lambda a: np.ascontiguousarray(np.asarray(a, dtype=np.float32))
    nc = build_program(debug=_debug, upto=_upto)
    shared = {
        "norm_attn": f32(norm_attn).reshape(1, D), "w_in": f32(w_in)[0], "b_forget": f32(b_forget).reshape(1, NH),
        "w_o_sb": f32(w_o_sb)[0], "w_o_fox": f32(w_o_fox)[0], "w_out": f32(w_out)[0],
        "norm_ffn": f32(norm_ffn).reshape(1, D), "w_router_group": f32(w_router_group)[0],
        "b_router_group": f32(b_router_group).reshape(1, 4), "w_router_expert": f32(w_router_expert)[0],
        "b_router_expert": f32(b_router_expert).reshape(1, NE), "w1": f32(w1)[0], "w3": f32(w3)[0], "w2": f32(w2)[0],
        "norm_final": f32(norm_final).reshape(1, D),
    }
    xf = f32(x)
    in_maps = [dict(shared, x=xf[b]) for b in range(_cores)]
    res = run_bass_kernel_spmd(nc, in_maps, core_ids=list(range(_cores)))
    if _debug:
        return (np.stack([res.results[b]["out"] for b in range(_cores)], axis=0),
                np.stack([res.results[b]["dbg"] for b in range(_cores)], axis=0))
    return np.stack([res.results[b]["out"] for b in range(_cores)], axis=0)
```
